# Optimizing a Trainium2 kernel written in Bass

```python
import jax, jax.numpy as jnp
from jax import lax
import numpy as np

D_MODEL = 1024
BATCH = 16
SEQ = 4096
DEPTH = 4

GRID_W = 64
CTX_LEN = 256
N_MIXERS = 3
NORM_EPS = 1e-6
NEG_INF = -1e30

NA_HEADS = 16
NA_HEAD_DIM = D_MODEL // NA_HEADS
NA_ROWS = 8
NA_COLS = 16

GDN_DK = 128
GDN_DV = 128
GDN_HEADS = D_MODEL // GDN_DK
GDN_QK_W = GDN_HEADS * GDN_DK
GDN_V_W = GDN_HEADS * GDN_DV
GDN_CONV_CH = 2 * GDN_QK_W + GDN_V_W
GDN_IN = GDN_CONV_CH + GDN_V_W + 4 * GDN_HEADS
GDN_CONV_K = 4
GDN_CHUNK = 64

POOL_WINDOWS = (2, 4, 8, 16)
POOL_GROUP = D_MODEL // len(POOL_WINDOWS)

D_FF = 2816
N_EXPERTS = 8
TOP_K = 2
D_FF_EXPERT = 3584
MOE_BLOCK = 512

kernel_name = "hybrid_na_gdn_pool_moe_diffusion_trunk"


def rmsnorm(x, g):
    xf = x.astype(jnp.float32)
    y = xf * lax.rsqrt(jnp.mean(xf * xf, axis=-1, keepdims=True) + NORM_EPS)
    return (y * g.astype(jnp.float32)).astype(x.dtype)


def modulate(h, shift, scale):
    return h * (1 + scale) + shift


def l2norm(x):
    return x * lax.rsqrt(jnp.sum(x * x, axis=-1, keepdims=True) + NORM_EPS)


def dense_attention(q, k, v):
    s = jnp.einsum('bqhd,bkhd->bhqk', q, k, preferred_element_type=jnp.float32) * (q.shape[-1] ** -0.5)
    p = jax.nn.softmax(s, axis=-1).astype(v.dtype)
    return jnp.einsum('bhqk,bkhd->bqhd', p, v)


def neighbourhood_attention(q, k, v, k_ctx, v_ctx, rpb):
    B, N, H, Dh = q.shape
    rows = N // GRID_W
    kr = min(NA_ROWS, rows)
    scale = Dh ** -0.5
    qg = q.reshape(B, rows, GRID_W, H, Dh)
    kg = k.reshape(B, rows, GRID_W, H, Dh)
    vg = v.reshape(B, rows, GRID_W, H, Dh)
    col = jnp.arange(GRID_W)
    c0 = jnp.clip(col - NA_COLS // 2, 0, GRID_W - NA_COLS)
    in_win = (col[None, :] >= c0[:, None]) & (col[None, :] < c0[:, None] + NA_COLS)
    dc = jnp.clip(col[None, :] - col[:, None], -(NA_COLS - 1), NA_COLS - 1) + NA_COLS - 1
    rpb_c = rpb[:, :, dc]

    def one_row(r):
        r0 = jnp.clip(r - kr // 2, 0, rows - kr)
        k_blk = lax.dynamic_slice_in_dim(kg, r0, kr, axis=1)
        v_blk = lax.dynamic_slice_in_dim(vg, r0, kr, axis=1)
        q_r = lax.dynamic_index_in_dim(qg, r, axis=1, keepdims=False)
        dr = r0 + jnp.arange(kr) - r + NA_ROWS - 1
        bias = jnp.transpose(rpb_c[:, dr], (0, 2, 1, 3))
        s_loc = jnp.einsum('bqhd,brkhd->bhqrk', q_r, k_blk,
                           preferred_element_type=jnp.float32) * scale + bias
        s_loc = jnp.where(in_win[:, None, :], s_loc, NEG_INF)
        s_ctx = jnp.einsum('bqhd,blhd->bhql', q_r, k_ctx,
                           preferred_element_type=jnp.float32) * scale
        s = jnp.concatenate([s_loc.reshape(B, H, GRID_W, kr * GRID_W), s_ctx], axis=-1)
        p = jax.nn.softmax(s, axis=-1).astype(v.dtype)
        p_loc = p[..., :kr * GRID_W].reshape(B, H, GRID_W, kr, GRID_W)
        p_ctx = p[..., kr * GRID_W:]
        return (jnp.einsum('bhqrk,brkhd->bqhd', p_loc, v_blk)
                + jnp.einsum('bhql,blhd->bqhd', p_ctx, v_ctx))

    out = lax.map(one_row, jnp.arange(rows))
    return jnp.moveaxis(out, 0, 1).reshape(B, N, H, Dh)


def na_mixer(h_ctx, h_lat, w_qkv, w_o, rpb, need_ctx):
    B, N, D = h_lat.shape
    L = h_ctx.shape[1]
    qkv_c = (h_ctx @ w_qkv).reshape(B, L, 3, NA_HEADS, NA_HEAD_DIM)
    qkv_l = (h_lat @ w_qkv).reshape(B, N, 3, NA_HEADS, NA_HEAD_DIM)
    o_lat = neighbourhood_attention(qkv_l[:, :, 0], qkv_l[:, :, 1], qkv_l[:, :, 2],
                                    qkv_c[:, :, 1], qkv_c[:, :, 2], rpb)
    y_lat = o_lat.reshape(B, N, D) @ w_o
    y_ctx = None
    if need_ctx:
        o_ctx = dense_attention(qkv_c[:, :, 0], qkv_c[:, :, 1], qkv_c[:, :, 2])
        y_ctx = o_ctx.reshape(B, L, D) @ w_o
    return y_ctx, y_lat


def conv_centred(x, w):
    K = w.shape[0]
    left = K // 2
    return lax.conv_general_dilated(x, w[:, None, :], window_strides=(1,),
                                    padding=[(left, K - 1 - left)],
                                    dimension_numbers=('NWC', 'WIO', 'NWC'),
                                    feature_group_count=x.shape[-1])


def gated_delta_chunked(q, k, v, g, beta, s0):
    B, H, T, DK = q.shape
    DV = v.shape[-1]
    C = GDN_CHUNK
    n = T // C
    q = q.reshape(B, H, n, C, DK)
    k = k.reshape(B, H, n, C, DK)
    v = v.reshape(B, H, n, C, DV)
    beta = beta.reshape(B, H, n, C)
    g = jnp.cumsum(g.reshape(B, H, n, C), axis=-1)
    idx = jnp.arange(C)
    incl = idx[:, None] >= idx[None, :]
    strict = idx[:, None] > idx[None, :]
    decay = jnp.exp(jnp.where(incl, g[..., :, None] - g[..., None, :], -jnp.inf))
    kb = k * beta[..., None]
    lower = jnp.where(strict, jnp.einsum('bhnid,bhnjd->bhnij', kb, k) * decay, 0.0)
    tmat = lower + jnp.eye(C, dtype=jnp.float32)
    rhs = jnp.concatenate([v * beta[..., None], kb * jnp.exp(g)[..., None]], axis=-1)
    sol = lax.linalg.triangular_solve(tmat, rhs, left_side=True, lower=True, unit_diagonal=True)
    u, w = sol[..., :DV], sol[..., DV:]
    a_qk = jnp.einsum('bhnid,bhnjd->bhnij', q, k) * decay
    g_last = g[..., -1]
    k_tail = k * jnp.exp(g_last[..., None] - g)[..., None]
    q_dec = q * jnp.exp(g)[..., None]

    def step(s, inp):
        q_c, w_c, u_c, a_c, k_c, gl = inp
        v_new = u_c - jnp.einsum('bhck,bhkv->bhcv', w_c, s)
        o = jnp.einsum('bhck,bhkv->bhcv', q_c, s) + jnp.einsum('bhcj,bhjv->bhcv', a_c, v_new)
        s = s * jnp.exp(gl)[..., None, None] + jnp.einsum('bhck,bhcv->bhkv', k_c, v_new)
        return s, o

    xs = tuple(jnp.moveaxis(t, 2, 0) for t in (q_dec, w, u, a_qk, k_tail, g_last))
    s, o = lax.scan(step, s0, xs)
    o = jnp.moveaxis(o, 0, 2).reshape(B, H, T, DV)
    return o, s


def gdn_features(h, w_in, conv_w, a_log, dt_bias):
    B, T, _ = h.shape
    proj = h @ w_in
    qkv = jax.nn.silu(conv_centred(proj[..., :GDN_CONV_CH], conv_w))
    z = proj[..., GDN_CONV_CH:GDN_CONV_CH + GDN_V_W]
    off = GDN_CONV_CH + GDN_V_W
    a = proj[..., off:off + 2 * GDN_HEADS].astype(jnp.float32).reshape(B, T, 2, GDN_HEADS)
    b = proj[..., off + 2 * GDN_HEADS:].astype(jnp.float32).reshape(B, T, 2, GDN_HEADS)
    qkv = qkv.astype(jnp.float32)
    q = qkv[..., :GDN_QK_W].reshape(B, T, GDN_HEADS, GDN_DK)
    k = qkv[..., GDN_QK_W:2 * GDN_QK_W].reshape(B, T, GDN_HEADS, GDN_DK)
    v = qkv[..., 2 * GDN_QK_W:].reshape(B, T, GDN_HEADS, GDN_DV)
    q = jnp.transpose(l2norm(q) * (GDN_DK ** -0.5), (0, 2, 1, 3))
    k = jnp.transpose(l2norm(k), (0, 2, 1, 3))
    v = jnp.transpose(v, (0, 2, 1, 3))
    g = -jnp.exp(a_log.astype(jnp.float32)) * jax.nn.softplus(a + dt_bias.astype(jnp.float32))
    g = jnp.transpose(g, (2, 0, 3, 1))
    beta = jnp.transpose(jax.nn.sigmoid(b), (2, 0, 3, 1))
    return q, k, v, g, beta, z


def gdn_bidirectional(feats, s0_fwd, s0_bwd):
    q, k, v, g, beta, _ = feats
    flip = lambda t: jnp.flip(t, axis=2)
    o_f, s_f = gated_delta_chunked(q, k, v, g[0], beta[0], s0_fwd)
    o_b, s_b = gated_delta_chunked(flip(q), flip(k), flip(v), flip(g[1]), flip(beta[1]), s0_bwd)
    return o_f + flip(o_b), s_f, s_b


def gdn_output(o, z, norm_g, w_o):
    B, H, T, DV = o.shape
    o = rmsnorm(jnp.transpose(o, (0, 2, 1, 3)), norm_g) * jax.nn.silu(z.reshape(B, T, H, DV))
    return o.reshape(B, T, H * DV).astype(z.dtype) @ w_o


def gdn_mixer(h_ctx, h_lat, w_in, conv_w, a_log, dt_bias, norm_g, w_o, need_ctx):
    B = h_lat.shape[0]
    f_ctx = gdn_features(h_ctx, w_in, conv_w, a_log, dt_bias)
    f_lat = gdn_features(h_lat, w_in, conv_w, a_log, dt_bias)
    s0 = jnp.zeros((B, GDN_HEADS, GDN_DK, GDN_DV), jnp.float32)
    o_ctx, s_cf, s_cb = gdn_bidirectional(f_ctx, s0, s0)
    o_lat, _, _ = gdn_bidirectional(f_lat, s_cf, s_cb)
    y_lat = gdn_output(o_lat, f_lat[5], norm_g, w_o)
    y_ctx = gdn_output(o_ctx, f_ctx[5], norm_g, w_o) if need_ctx else None
    return y_ctx, y_lat


def multiscale_pool(h, w_grp, ls):
    T = h.shape[-2]
    hf = h.astype(jnp.float32)
    t = jnp.arange(T)
    parts = []
    for gi, win in enumerate(POOL_WINDOWS):
        hg = hf[..., gi * POOL_GROUP:(gi + 1) * POOL_GROUP]
        cs = jnp.cumsum(hg, axis=-2)
        cs = jnp.concatenate([jnp.zeros_like(cs[..., :1, :]), cs], axis=-2)
        lo = jnp.clip(t - win // 2, 0, T)
        hi = jnp.clip(t + win // 2, 0, T)
        mean = (jnp.take(cs, hi, axis=-2) - jnp.take(cs, lo, axis=-2)) / (hi - lo).astype(jnp.float32)[:, None]
        parts.append(mean - hg)
    pooled = jnp.stack(parts, axis=-2).astype(h.dtype)
    y = jnp.einsum('...tng,ngf->...tnf', pooled, w_grp)
    return y.reshape(h.shape) * ls


def pool_mixer(h_ctx, h_lat, w_grp, ls, need_ctx):
    B, N, D = h_lat.shape
    rows = N // GRID_W
    y_lat = multiscale_pool(h_lat.reshape(B, rows, GRID_W, D), w_grp, ls).reshape(B, N, D)
    y_ctx = multiscale_pool(h_ctx, w_grp, ls) if need_ctx else None
    return y_ctx, y_lat


def swiglu(h, w1, w3, w2):
    return (jax.nn.silu(h @ w1) * (h @ w3)) @ w2


def moe_swiglu(h, w_router, w1, w3, w2):
    M, D = h.shape
    probs = jax.nn.softmax((h @ w_router).astype(jnp.float32), axis=-1)
    top_p, top_e = lax.top_k(probs, TOP_K)
    top_p = top_p / jnp.sum(top_p, axis=-1, keepdims=True)
    e_flat = top_e.reshape(-1)
    onehot = jax.nn.one_hot(e_flat, N_EXPERTS, dtype=jnp.int32)
    rank = jnp.sum(jnp.cumsum(onehot, axis=0) * onehot, axis=-1) - 1
    counts = jnp.sum(onehot, axis=0)
    padded = ((counts + MOE_BLOCK - 1) // MOE_BLOCK) * MOE_BLOCK
    ends = jnp.cumsum(padded)
    starts = ends - padded
    pos = (starts[e_flat] + rank).reshape(M, TOP_K)
    n_blocks = -(-(M * TOP_K) // MOE_BLOCK) + N_EXPERTS
    cap = n_blocks * MOE_BLOCK
    buf = jnp.zeros((cap, D), h.dtype)
    for j in range(TOP_K):
        buf = buf.at[pos[:, j]].set(h)
    block_e = jnp.minimum(jnp.searchsorted(ends, jnp.arange(n_blocks) * MOE_BLOCK, side='right'),
                          N_EXPERTS - 1)

    def run(args):
        xb, e = args
        return (jax.nn.silu(xb @ w1[e]) * (xb @ w3[e])) @ w2[e]

    out_buf = lax.map(run, (buf.reshape(n_blocks, MOE_BLOCK, D), block_e)).reshape(cap, D)
    return jnp.einsum('mkd,mk->md', out_buf[pos], top_p.astype(h.dtype))


def moe_streams(h_ctx, h_lat, w_router, w1, w3, w2):
    B, N, D = h_lat.shape
    if h_ctx is None:
        return None, moe_swiglu(h_lat.reshape(B * N, D), w_router, w1, w3, w2).reshape(B, N, D)
    L = h_ctx.shape[1]
    flat = jnp.concatenate([h_ctx.reshape(B * L, D), h_lat.reshape(B * N, D)], axis=0)
    y = moe_swiglu(flat, w_router, w1, w3, w2)
    return y[:B * L].reshape(B, L, D), y[B * L:].reshape(B, N, D)


def setup_inputs(seed: int = 0) -> dict:
    key = jax.random.key(seed)
    ks = iter(jax.random.split(key, 40))
    D = D_MODEL
    n_a = len(range(0, DEPTH, N_MIXERS))
    n_b = len(range(1, DEPTH, N_MIXERS))
    n_c = len(range(2, DEPTH, N_MIXERS))
    n_dense = len(range(0, DEPTH, 2))
    n_moe = len(range(1, DEPTH, 2))

    def nrm(shape, scale):
        return jax.random.normal(next(ks), shape, jnp.float32) * scale

    x = nrm((BATCH, SEQ, D), 1.0)
    c = nrm((BATCH, D), 1.0)
    ctx = nrm((BATCH, CTX_LEN, D), 1.0)
    c_ctx = nrm((D,), 1.0)
    ada_w = nrm((DEPTH, D, 6 * D), 0.5 * D ** -0.5)
    ada_b = nrm((DEPTH, 6 * D), 0.01)
    norm_g = 1.0 + nrm((DEPTH, 2, D), 0.05)
    final_g = 1.0 + nrm((D,), 0.05)
    na_w_qkv = nrm((n_a, D, 3 * D), D ** -0.5)
    na_w_o = nrm((n_a, D, D), D ** -0.5)
    na_rpb = nrm((n_a, NA_HEADS, 2 * NA_ROWS - 1, 2 * NA_COLS - 1), 0.2)
    gdn_w_in = nrm((n_b, D, GDN_IN), D ** -0.5)
    gdn_conv = nrm((n_b, GDN_CONV_K, GDN_CONV_CH), GDN_CONV_K ** -0.5)
    gdn_a_log = jnp.log(jax.random.uniform(next(ks), (n_b, 2, GDN_HEADS), jnp.float32, 1.0, 16.0))
    dt = jnp.exp(jax.random.uniform(next(ks), (n_b, 2, GDN_HEADS), jnp.float32,
                                    float(np.log(1e-3)), float(np.log(1e-1))))
    gdn_dt_bias = jnp.log(jnp.expm1(dt))
    gdn_norm_g = 1.0 + nrm((n_b, GDN_DV), 0.05)
    gdn_w_o = nrm((n_b, GDN_V_W, D), GDN_V_W ** -0.5)
    pool_w = nrm((n_c, len(POOL_WINDOWS), POOL_GROUP, POOL_GROUP), POOL_GROUP ** -0.5)
    pool_scale = 1.0 + nrm((n_c, D), 0.1)
    ffn_w1 = nrm((n_dense, D, D_FF), D ** -0.5)
    ffn_w3 = nrm((n_dense, D, D_FF), D ** -0.5)
    ffn_w2 = nrm((n_dense, D_FF, D), D_FF ** -0.5)
    moe_router = nrm((n_moe, D, N_EXPERTS), D ** -0.5)
    moe_w1 = nrm((n_moe, N_EXPERTS, D, D_FF_EXPERT), D ** -0.5)
    moe_w3 = nrm((n_moe, N_EXPERTS, D, D_FF_EXPERT), D ** -0.5)
    moe_w2 = nrm((n_moe, N_EXPERTS, D_FF_EXPERT, D), D_FF_EXPERT ** -0.5)
    return {"x": x, "c": c, "ctx": ctx, "c_ctx": c_ctx,
            "ada_w": ada_w, "ada_b": ada_b, "norm_g": norm_g, "final_g": final_g,
            "na_w_qkv": na_w_qkv, "na_w_o": na_w_o, "na_rpb": na_rpb,
            "gdn_w_in": gdn_w_in, "gdn_conv": gdn_conv, "gdn_a_log": gdn_a_log,
            "gdn_dt_bias": gdn_dt_bias, "gdn_norm_g": gdn_norm_g, "gdn_w_o": gdn_w_o,
            "pool_w": pool_w, "pool_scale": pool_scale,
            "ffn_w1": ffn_w1, "ffn_w3": ffn_w3, "ffn_w2": ffn_w2,
            "moe_router": moe_router, "moe_w1": moe_w1, "moe_w3": moe_w3, "moe_w2": moe_w2}


def reference(x, c, ctx, c_ctx, ada_w, ada_b, norm_g, final_g, na_w_qkv, na_w_o, na_rpb,
              gdn_w_in, gdn_conv, gdn_a_log, gdn_dt_bias, gdn_norm_g, gdn_w_o,
              pool_w, pool_scale, ffn_w1, ffn_w3, ffn_w2, moe_router, moe_w1, moe_w3, moe_w2):
    B, N, D = x.shape
    for i in range(DEPTH):
        last = i == DEPTH - 1
        m_lat = (jax.nn.silu(c) @ ada_w[i] + ada_b[i]).reshape(B, 1, 6, D)
        m_ctx = (jax.nn.silu(c_ctx) @ ada_w[i] + ada_b[i]).reshape(6, D)

        h_lat = modulate(rmsnorm(x, norm_g[i, 0]), m_lat[..., 0, :], m_lat[..., 1, :])
        h_ctx = modulate(rmsnorm(ctx, norm_g[i, 0]), m_ctx[0], m_ctx[1])
        j = i // N_MIXERS
        kind = i % N_MIXERS
        if kind == 0:
            y_ctx, y_lat = na_mixer(h_ctx, h_lat, na_w_qkv[j], na_w_o[j], na_rpb[j], not last)
        elif kind == 1:
            y_ctx, y_lat = gdn_mixer(h_ctx, h_lat, gdn_w_in[j], gdn_conv[j], gdn_a_log[j],
                                     gdn_dt_bias[j], gdn_norm_g[j], gdn_w_o[j], not last)
        else:
            y_ctx, y_lat = pool_mixer(h_ctx, h_lat, pool_w[j], pool_scale[j], not last)
        x = x + m_lat[..., 2, :] * y_lat
        if not last:
            ctx = ctx + m_ctx[2] * y_ctx

        h_lat = modulate(rmsnorm(x, norm_g[i, 1]), m_lat[..., 3, :], m_lat[..., 4, :])
        h_ctx = None if last else modulate(rmsnorm(ctx, norm_g[i, 1]), m_ctx[3], m_ctx[4])
        f = i // 2
        if i % 2 == 0:
            y_lat = swiglu(h_lat, ffn_w1[f], ffn_w3[f], ffn_w2[f])
            y_ctx = None if last else swiglu(h_ctx, ffn_w1[f], ffn_w3[f], ffn_w2[f])
        else:
            y_ctx, y_lat = moe_streams(h_ctx, h_lat, moe_router[f], moe_w1[f], moe_w3[f], moe_w2[f])
        x = x + m_lat[..., 5, :] * y_lat
        if not last:
            ctx = ctx + m_ctx[5] * y_ctx
    return rmsnorm(x, final_g)
```

```python
import numpy as np
from contextlib import ExitStack
import concourse.bass as bass
import concourse.mybir as mybir
from concourse.bass_utils import run_bass_kernel_spmd

F32 = mybir.dt.float32
BF16 = mybir.dt.bfloat16
U32 = mybir.dt.uint32
AF = mybir.ActivationFunctionType
ALU = mybir.AluOpType

D = 1024
KC = 8
NLAT = 4096
NCTX = 256
TT = 512
NS = 2
DEPTH = 4
EPS = 1e-6
NEG = -1e30
DFF = 2816
DFE = 3584
NEXP = 8


class _Sem:
    def __init__(self, h, name):
        self.h = h
        self.val = 0
        self.name = name


class _Eng:
    def __init__(self, name, h, sem):
        self.name = name
        self.h = h
        self.sem = sem
        self.seen = {}


class Buf:
    __slots__ = ("name", "w", "r")

    def __init__(self, name=""):
        self.name = name
        self.w = None
        self.r = {}


class K:
    def __init__(self, nc, es, ndma=24):
        self.nc = nc
        self.es = es
        self.E = {}
        for name, h in (("pe", nc.tensor), ("act", nc.scalar), ("dve", nc.vector),
                        ("pool", nc.gpsimd), ("sp", nc.sync)):
            s = _Sem(es.enter_context(nc.semaphore("sem_" + name)), name)
            self.E[name] = _Eng(name, h, s)
        self.dsems = [_Sem(es.enter_context(nc.semaphore(f"dsem{i}")), f"d{i}") for i in range(ndma)]
        self.dnext = 0
        self.allsems = [e.sem for e in self.E.values()] + self.dsems

    def _need(self, e, ev):
        if ev is None:
            return
        sem, val = ev
        if sem is e.sem and e.name == "pe":
            return
        if e.seen.get(sem.name, 0) >= val:
            return
        e.h.wait_ge(sem.h, val)
        e.seen[sem.name] = val

    def _deps(self, e, reads, writes):
        for b in reads:
            self._need(e, b.w)
        for b in writes:
            self._need(e, b.w)
            for sname, ev in list(b.r.items()):
                self._need(e, ev)

    def _mark(self, ev, reads, writes):
        for b in reads:
            b.r[ev[0].name] = ev
        for b in writes:
            b.w = ev
            b.r = {}

    def op(self, ename, fn, reads=(), writes=()):
        e = self.E[ename]
        self._deps(e, reads, writes)
        ins = fn(e.h)
        e.sem.val += 1
        ins.then_inc(e.sem.h, 1)
        self._mark((e.sem, e.sem.val), reads, writes)

    def dma(self, out, in_, reads=(), writes=(), q="sp", **kw):
        e = self.E[q]
        ds = self.dsems[self.dnext]
        self.dnext = (self.dnext + 1) % len(self.dsems)
        self._need(e, (ds, ds.val))
        self._deps(e, reads, writes)
        ins = e.h.dma_start(out=out, in_=in_, **kw)
        ds.val += 16
        ins.then_inc(ds.h, 16)
        self._mark((ds, ds.val), reads, writes)

    def barrier(self):
        for e in self.E.values():
            for s in self.allsems:
                if s.val > 0:
                    self._need(e, (s, s.val))

    def mm(self, out, lhsT, rhs, start, stop, reads, writes):
        self.op("pe", lambda h: h.matmul(out, lhsT, rhs, start=start, stop=stop), reads, writes)

    def act(self, out, in_, func, reads, writes, **kw):
        self.op("act", lambda h: h.activation(out=out, in_=in_, func=func, **kw), reads, writes)

    def tt(self, eng, out, in0, in1, op, reads, writes):
        self.op(eng, lambda h: h.tensor_tensor(out=out, in0=in0, in1=in1, op=op), reads, writes)

    def ts(self, eng, out, in0, s1, s2, op0, op1, reads, writes):
        if op1 is None:
            self.op(eng, lambda h: h.tensor_scalar(out=out, in0=in0, scalar1=s1, scalar2=None, op0=op0),
                    reads, writes)
        else:
            self.op(eng, lambda h: h.tensor_scalar(out=out, in0=in0, scalar1=s1, scalar2=s2, op0=op0, op1=op1),
                    reads, writes)

    def stt(self, out, in0, scalar, in1, op0, op1, reads, writes):
        self.op("dve", lambda h: h.scalar_tensor_tensor(out=out, in0=in0, scalar=scalar, in1=in1, op0=op0, op1=op1),
                reads, writes)

    def copy(self, eng, out, in_, reads, writes):
        if eng == "act":
            self.op("act", lambda h: h.activation(out=out, in_=in_, func=AF.Copy), reads, writes)
        else:
            self.op(eng, lambda h: h.tensor_copy(out=out, in_=in_), reads, writes)

    def memset(self, eng, ap, val, writes):
        self.op(eng, lambda h: h.memset(ap, val), (), writes)


_UID = [0]


class Pool_:
    def __init__(self, k):
        self.k = k
        self.es = ExitStack()
        self.n = 0

    def __enter__(self):
        self.es.__enter__()
        return self

    def __exit__(self, *a):
        self.k.barrier()
        return self.es.__exit__(*a)

    def sb(self, shape, dt, name=None):
        _UID[0] += 1
        t = self.es.enter_context(self.k.nc.sbuf_tensor(f"{name or 't'}_{_UID[0]}", list(shape), dt))
        return t, Buf(name or "sb")

    def ps(self, name=None, shape=(128, 512), dt=F32):
        _UID[0] += 1
        t = self.es.enter_context(self.k.nc.psum_tensor(f"{name or 'p'}_{_UID[0]}", list(shape), dt))
        return t, Buf(name or "ps")


TOK = NCTX + NLAT


def tiles_for(include_ctx=True):
    out = []
    for s in range(NS):
        if include_ctx:
            out.append((s, 0, NCTX, True))
        for i in range(NLAT // TT):
            out.append((s, NCTX + i * TT, TT, False))
    return out


class Prog:
    def __init__(self, nc, k, dr, stop_after=99):
        self.nc = nc
        self.k = k
        self.dr = dr
        self.stop_after = stop_after

    def setup_consts(self, P):
        k, dr = self.k, self.dr
        self.ones_bf, self.onesB = P.sb([128, 128], BF16, "ones")
        self.id_bf, self.idbB = P.sb([128, 128], BF16, "idb")
        self.id_f, self.idfB = P.sb([128, 128], F32, "idf")
        self.ones_f, self.onesfB = P.sb([128, 128], F32, "onesf")
        self.mod, self.modB = P.sb([128, DEPTH, 48, 3], F32, "mod")
        self.ng, self.ngB = P.sb([128, DEPTH, 2, KC], F32, "ng")
        self.fg, self.fgB = P.sb([128, KC], F32, "fg")
        self.epsb, self.epsB = P.sb([128, 1], F32, "eps")
        k.dma(self.ones_bf[:], dr["ones_bf"][:, :], writes=[self.onesB])
        k.dma(self.id_bf[:], dr["id_bf"][:, :], writes=[self.idbB])
        k.dma(self.id_f[:], dr["id_f"][:, :], writes=[self.idfB])
        k.dma(self.ones_f[:], dr["ones_f"][:, :], writes=[self.onesfB])
        k.dma(self.ng[:], dr["norm_g"][:, :, :, :], writes=[self.ngB])
        k.dma(self.fg[:], dr["final_g"][:, :], writes=[self.fgB])
        k.memset("pool", self.epsb[:], EPS, [self.epsB])

    def adaln(self):
        k, dr = self.k, self.dr
        with Pool_(k) as P:
            cv, cvB = P.sb([128, KC, 3], F32, "cv")
            sc, scB = P.sb([128, KC, 3], F32, "sc")
            ab, abB = P.sb([128, DEPTH, 48], F32, "ab")
            wa = [P.sb([128, KC, 768], F32, f"wa{i}") for i in range(2)]
            ps = [P.ps(f"adaps{i}") for i in range(2)]
            k.dma(cv[:], dr["cvec"][:, :, :], writes=[cvB])
            k.dma(ab[:], dr["ada_b"][:, :, :], writes=[abB])
            k.act(sc[:], cv[:], AF.Silu, [cvB], [scB])
            gi = 0
            for l in range(DEPTH):
                src = dr["ada_w"][l].rearrange("(kc p) n -> p kc n", p=128)
                for g in range(8):
                    wt, wB = wa[gi % 2]
                    pt, pB = ps[gi % 2]
                    gi += 1
                    k.dma(wt[:], src[:, :, g * 768:(g + 1) * 768], writes=[wB])
                    for o in range(6):
                        for kc in range(KC):
                            k.mm(pt[:, o * 3:(o + 1) * 3], wt[:, kc, o * 128:(o + 1) * 128], sc[:, kc, :],
                                 kc == 0, kc == KC - 1, [wB, scB], [pB])
                    pv = pt[:, 0:18].rearrange("p (o j) -> p o j", j=3)
                    for j in range(3):
                        k.tt("dve", self.mod[:, l, g * 6:(g + 1) * 6, j], pv[:, :, j], ab[:, l, g * 6:(g + 1) * 6],
                             ALU.add, [pB, abB], [self.modB])

    def mod_vecs(self, P, l, sub):
        k = self.k
        A, AB = P.sb([128, KC, 3], F32, "modA")
        o = sub * 24
        for j in range(3):
            k.stt(A[:, :, j], self.mod[:, l, o + 8:o + 16, j], 1.0, self.ng[:, l, sub, :], ALU.add, ALU.mult,
                  [self.modB, self.ngB], [AB])
        return A, AB

    def jidx(self, s, is_ctx):
        return 2 if is_ctx else s

    def norm_pass(self, l, sub, include_ctx=True, router=None, final=False):
        k, dr = self.k, self.dr
        xs, hs = dr["xs"], dr["hs"]
        tl = tiles_for(include_ctx)
        with Pool_(k) as P:
            if not final:
                A, AB = self.mod_vecs(P, l, sub)
            xt = [P.sb([128, KC, TT], F32, f"xt{i}") for i in range(2)]
            sq, sqB = P.sb([128, KC, TT], BF16, "sq")
            rs, rsB = P.sb([128, TT], F32, "rs")
            hf, hfB = P.sb([128, KC, TT], F32, "hf")
            hb, hbB = P.sb([128, KC, TT], BF16, "hb")
            pss, pssB = P.ps("pss")
            if router is not None:
                wr, wrB = P.sb([128, KC, NEXP], F32, "wr")
                k.dma(wr[:], router, writes=[wrB])
                plg, plgB = P.ps("plg")
                pwt, pwtB = P.ps("pwt")
                lg, lgB = P.sb([128, 4, NEXP], F32, "lg")
                m8, m8B = P.sb([128, 4, 8], F32, "m8")
                nm1, nm1B = P.sb([128, 4], F32, "nm1")
                ee, eeB = P.sb([128, 4, NEXP], F32, "ee")
                mk, mkB = P.sb([128, 4, NEXP], F32, "mk")
                dn, dnB = P.sb([128, 4], F32, "dn")
                ww, wwB = P.sb([128, 4, NEXP], F32, "ww")
                wT, wTB = P.sb([NEXP, TT], F32, "wT")

            def load(i):
                s, t0, W, c = tl[i]
                t, B = xt[i % 2]
                k.dma(t[:, :, :W], xs[s].rearrange("(kc p) t -> p kc t", p=128)[:, :, t0:t0 + W], writes=[B])

            load(0)
            for i, (s, t0, W, c) in enumerate(tl):
                if i + 1 < len(tl):
                    load(i + 1)
                x, xB = xt[i % 2]
                j = self.jidx(s, c)
                k.act(sq[:, :, :W], x[:, :, :W], AF.Square, [xB], [sqB])
                for kc in range(KC):
                    k.mm(pss[:, :W], self.ones_bf[:], sq[:, kc, :W], kc == 0, kc == KC - 1, [self.onesB, sqB], [pssB])
                k.act(rs[:, :W], pss[:, :W], AF.Sqrt, [pssB, self.epsB], [rsB], scale=1.0 / D, bias=self.epsb[:, 0:1])
                k.op("dve", lambda h: h.reciprocal(out=rs[:, :W], in_=rs[:, :W]), [rsB], [rsB])
                for kc in range(KC):
                    k.tt("dve", hf[:, kc, :W], x[:, kc, :W], rs[:, :W], ALU.mult, [xB, rsB], [hfB])
                if final:
                    for kc in range(KC):
                        k.act(hf[:, kc, :W], hf[:, kc, :W], AF.Identity, [hfB, self.fgB], [hfB], scale=self.fg[:, kc:kc + 1])
                    k.dma(dr["out"][s].rearrange("(kc p) t -> p kc t", p=128)[:, :, t0 - NCTX:t0 - NCTX + W],
                          hf[:, :, :W], reads=[hfB])
                    continue
                o = sub * 24
                for kc in range(KC):
                    k.act(hf[:, kc, :W], hf[:, kc, :W], AF.Identity, [hfB, AB, self.modB], [hfB],
                          scale=A[:, kc, j:j + 1], bias=self.mod[:, l, o + kc, j:j + 1])
                k.copy("pool", hb[:, :, :W], hf[:, :, :W], [hfB], [hbB])
                k.dma(hs[s].rearrange("(kc p) t -> p kc t", p=128)[:, :, t0:t0 + W], hb[:, :, :W], reads=[hbB])
                if router is not None:
                    nst = W // 128
                    for ts_ in range(nst):
                        for kc in range(KC):
                            k.mm(plg[:, ts_ * 8:(ts_ + 1) * 8], hf[:, kc, ts_ * 128:(ts_ + 1) * 128], wr[:, kc, :],
                                 kc == 0, kc == KC - 1, [hfB, wrB], [plgB])
                    k.copy("dve", lg[:, :nst, :], plg[:, 0:nst * 8].rearrange("p (a e) -> p a e", e=8), [plgB], [lgB])
                    for ts_ in range(nst):
                        k.op("dve", lambda h: h.max(out=m8[:, ts_, :], in_=lg[:, ts_, :]), [lgB], [m8B])
                    k.ts("dve", nm1[:, :nst], m8[:, :nst, 0], -1.0, None, ALU.mult, None, [m8B], [nm1B])
                    for ts_ in range(nst):
                        k.act(ee[:, ts_, :], lg[:, ts_, :], AF.Exp, [lgB, nm1B], [eeB], bias=nm1[:, ts_:ts_ + 1])
                        k.ts("dve", mk[:, ts_, :], lg[:, ts_, :], m8[:, ts_, 1:2], None, ALU.is_ge, None,
                             [lgB, m8B], [mkB])
                    k.tt("dve", ee[:, :nst, :], ee[:, :nst, :], mk[:, :nst, :], ALU.mult, [eeB, mkB], [eeB])
                    k.op("dve", lambda h: h.reduce_sum(out=dn[:, :nst], in_=ee[:, :nst, :], axis=mybir.AxisListType.X),
                         [eeB], [dnB])
                    k.op("dve", lambda h: h.reciprocal(out=dn[:, :nst], in_=dn[:, :nst]), [dnB], [dnB])
                    for ts_ in range(nst):
                        k.ts("dve", ww[:, ts_, :], ee[:, ts_, :], dn[:, ts_:ts_ + 1], None, ALU.mult, None,
                             [eeB, dnB], [wwB])
                        k.op("pe", lambda h: h.transpose(pwt[0:NEXP, ts_ * 128:(ts_ + 1) * 128], ww[:, ts_, :], self.id_f[:]),
                             [wwB, self.idfB], [pwtB])
                    k.copy("dve", wT[:, :W], pwt[0:NEXP, :W], [pwtB], [wTB])
                    k.dma(dr["wexp"][s, :, t0:t0 + W], wT[:, :W], reads=[wTB])

    def ffn_pass(self, l, w1, w3, w2, F, include_ctx=True, expert=None):
        k, dr = self.k, self.dr
        xs, hs = dr["xs"], dr["hs"]
        FC = F // 128
        tl = tiles_for(include_ctx)
        with Pool_(k) as P:
            W1, W1B = P.sb([128, KC, F], BF16, "W1")
            W3, W3B = P.sb([128, KC, F], BF16, "W3")
            W2, W2B = P.sb([128, FC, D], BF16, "W2")
            k.dma(W1[:], w1.rearrange("(kc p) f -> p kc f", p=128), writes=[W1B], q="pool")
            k.dma(W3[:], w3.rearrange("(kc p) f -> p kc f", p=128), writes=[W3B], q="pool")
            k.dma(W2[:], w2.rearrange("(fc p) d -> p fc d", p=128), writes=[W2B], q="pool")
            ht = [P.sb([128, KC, TT], BF16, f"ht{i}") for i in range(2)]
            wb = [P.sb([128, TT], F32, f"wbc{i}") for i in range(2)] if expert is not None else None
            g, gB = P.sb([128, FC, TT], BF16, "g")
            gBs = [Buf(f"g{f}") for f in range(FC)]
            sa = [P.sb([128, TT], F32, f"sa{i}") for i in range(2)]
            yt = [P.sb([128, TT], F32, f"yt{i}") for i in range(2)]
            pa = [P.ps(f"pa{i}") for i in range(2)]
            pb = [P.ps(f"pb{i}") for i in range(2)]
            py = [P.ps(f"py{i}") for i in range(2)]

            def load(i):
                s, t0, W, c = tl[i]
                t, B = ht[i % 2]
                k.dma(t[:, :, :W], hs[s].rearrange("(kc p) t -> p kc t", p=128)[:, :, t0:t0 + W], writes=[B])
                if expert is not None:
                    wt_, wB_ = wb[i % 2]
                    k.dma(wt_[:, :W], dr["wexp"][s, expert:expert + 1, t0:t0 + W].partition_broadcast(128), writes=[wB_])

            load(0)
            cnt = 0
            ycnt = 0
            for i, (s, t0, W, c) in enumerate(tl):
                if i + 1 < len(tl):
                    load(i + 1)
                h, hB = ht[i % 2]
                j = self.jidx(s, c)
                for f in range(FC):
                    a_, aB = pa[cnt % 2]
                    b_, bB = pb[cnt % 2]
                    s_, sB = sa[cnt % 2]
                    cnt += 1
                    for kc in range(KC):
                        k.mm(a_[:, :W], W1[:, kc, f * 128:(f + 1) * 128], h[:, kc, :W], kc == 0, kc == KC - 1, [W1B, hB], [aB])
                    for kc in range(KC):
                        k.mm(b_[:, :W], W3[:, kc, f * 128:(f + 1) * 128], h[:, kc, :W], kc == 0, kc == KC - 1, [W3B, hB], [bB])
                    k.act(s_[:, :W], a_[:, :W], AF.Silu, [aB], [sB])
                    if expert is not None:
                        wt_, wB_ = wb[i % 2]
                        k.tt("pool", s_[:, :W], s_[:, :W], wt_[:, :W], ALU.mult, [sB, wB_], [sB])
                    k.tt("dve", g[:, f, :W], b_[:, :W], s_[:, :W], ALU.mult, [bB, sB], [gBs[f]])
                o = 24 + 16 + 0
                for oc in range(KC):
                    y_, yB = py[ycnt % 2]
                    yo, yoB = yt[ycnt % 2]
                    ycnt += 1
                    for f in range(FC):
                        k.mm(y_[:, :W], W2[:, f, oc * 128:(oc + 1) * 128], g[:, f, :W], f == 0, f == FC - 1, [W2B, gBs[f]], [yB])
                    k.ts("dve", yo[:, :W], y_[:, :W], self.mod[:, l, 40 + oc, j:j + 1], None, ALU.mult, None,
                         [yB, self.modB], [yoB])
                    k.dma(xs[s, oc * 128:(oc + 1) * 128, t0:t0 + W], yo[:, :W], reads=[yoB], q="pool", accum_op=ALU.add)

    def outproj_pass(self, l, src, w, include_ctx, gate_off, extra_scale=None):
        k, dr = self.k, self.dr
        xs = dr["xs"]
        tl = tiles_for(include_ctx)
        with Pool_(k) as P:
            Wo, WoB = P.sb([128, KC, D], BF16, "Wo")
            k.dma(Wo[:], w.rearrange("(kc p) d -> p kc d", p=128), writes=[WoB], q="pool")
            ot = [P.sb([128, KC, TT], BF16, f"ot{i}") for i in range(2)]
            yt = [P.sb([128, TT], F32, f"yt{i}") for i in range(2)]
            py = [P.ps(f"py{i}") for i in range(2)]

            def load(i):
                s, t0, W, c = tl[i]
                t, B = ot[i % 2]
                k.dma(t[:, :, :W], src[s].rearrange("(kc p) t -> p kc t", p=128)[:, :, t0:t0 + W], writes=[B])

            load(0)
            ycnt = 0
            for i, (s, t0, W, c) in enumerate(tl):
                if i + 1 < len(tl):
                    load(i + 1)
                o_, oB = ot[i % 2]
                j = self.jidx(s, c)
                for oc in range(KC):
                    y_, yB = py[ycnt % 2]
                    yo, yoB = yt[ycnt % 2]
                    ycnt += 1
                    for kc in range(KC):
                        k.mm(y_[:, :W], Wo[:, kc, oc * 128:(oc + 1) * 128], o_[:, kc, :W], kc == 0, kc == KC - 1, [WoB, oB], [yB])
                    if extra_scale is None:
                        k.ts("dve", yo[:, :W], y_[:, :W], self.mod[:, l, gate_off + oc, j:j + 1], None, ALU.mult, None,
                             [yB, self.modB], [yoB])
                    else:
                        est, esB = extra_scale
                        k.ts("dve", yo[:, :W], y_[:, :W], self.mod[:, l, gate_off + oc, j:j + 1], est[:, oc:oc + 1],
                             ALU.mult, ALU.mult, [yB, self.modB, esB], [yoB])
                    k.dma(xs[s, oc * 128:(oc + 1) * 128, t0:t0 + W], yo[:, :W], reads=[yoB], q="pool", accum_op=ALU.add)

    def na_qkv_pass(self, wqkv):
        k, dr = self.k, self.dr
        hs, qs, ks, vs = dr["hs"], dr["qs"], dr["ks"], dr["vs"]
        tl = tiles_for(True)
        with Pool_(k) as P:
            Wq, WqB = P.sb([128, KC, 3 * D], BF16, "Wqkv")
            src = wqkv.rearrange("(kc p) n -> p kc n", p=128)
            for i in range(3):
                k.dma(Wq[:, :, i * D:(i + 1) * D], src[:, :, i * D:(i + 1) * D], writes=[WqB], q="pool")
            ht = [P.sb([128, KC, TT], BF16, f"ht{i}") for i in range(2)]
            qk = [P.sb([128, 2 * KC, TT], BF16, f"qk{i}") for i in range(2)]
            vt = [P.sb([128, 4, D], BF16, f"vt{i}") for i in range(2)]
            pp = [P.ps(f"pp{i}") for i in range(4)]

            def load(i):
                s, t0, W, c = tl[i]
                t, B = ht[i % 2]
                k.dma(t[:, :, :W], hs[s].rearrange("(kc p) t -> p kc t", p=128)[:, :, t0:t0 + W], writes=[B])

            load(0)
            cnt = 0
            for i, (s, t0, W, c) in enumerate(tl):
                if i + 1 < len(tl):
                    load(i + 1)
                h, hB = ht[i % 2]
                q_, qB = qk[i % 2]
                v_, vB = vt[i % 2]
                for oc in range(2 * KC):
                    p_, pB = pp[cnt % 4]
                    for kc in range(KC):
                        k.mm(p_[:, :W], Wq[:, kc, oc * 128:(oc + 1) * 128], h[:, kc, :W], kc == 0, kc == KC - 1, [WqB, hB], [pB])
                    if cnt % 2 == 0:
                        k.act(q_[:, oc, :W], p_[:, :W], AF.Copy, [pB], [qB], scale=(0.125 if oc < KC else 1.0))
                    else:
                        k.ts("dve", q_[:, oc, :W], p_[:, :W], (0.125 if oc < KC else 1.0), None, ALU.mult, None, [pB], [qB])
                    cnt += 1
                for ts_ in range(W // 128):
                    for hf_ in range(2):
                        p_, pB = pp[cnt % 4]
                        for kc in range(KC):
                            k.mm(p_[:, :], h[:, kc, ts_ * 128:(ts_ + 1) * 128], Wq[:, kc, 2 * D + hf_ * 512:2 * D + (hf_ + 1) * 512],
                                 kc == 0, kc == KC - 1, [WqB, hB], [pB])
                        if cnt % 2 == 0:
                            k.act(v_[:, ts_, hf_ * 512:(hf_ + 1) * 512], p_[:, :], AF.Copy, [pB], [vB])
                        else:
                            k.copy("dve", v_[:, ts_, hf_ * 512:(hf_ + 1) * 512], p_[:, :], [pB], [vB])
                        cnt += 1
                k.dma(qs[s].rearrange("(kc p) t -> p kc t", p=128)[:, :, t0:t0 + W], q_[:, 0:KC, :W], reads=[qB])
                k.dma(ks[s].rearrange("(kc p) t -> p kc t", p=128)[:, :, t0:t0 + W], q_[:, KC:2 * KC, :W], reads=[qB])
                k.dma(vs[s, t0:t0 + W, :].rearrange("(a p) f -> p a f", p=128), v_[:, :W // 128, :], reads=[vB])

    def na_attn_pass(self, tab, need_ctx):
        k, dr = self.k, self.dr
        qs, ks, vs, os_ = dr["qs"], dr["ks"], dr["vs"], dr["os"]
        NCH = TOK // 128
        with Pool_(k) as P:
            qT, qTB = P.sb([128, TOK], BF16, "qT")
            kT, kTB = P.sb([128, TOK], BF16, "kT")
            vv, vvB = P.sb([128, NCH, 128], BF16, "vv")
            tb, tbB = P.sb([128, 2, 2, 22, 64], F32, "tb")
            negt, negB = P.sb([128, 256], F32, "negt")
            k.memset("pool", negt[:], NEG, [negB])
            sbm = [P.sb([128, TT], F32, f"sbm{i}") for i in range(2)]
            pt = [P.sb([128, TT], BF16, f"pt{i}") for i in range(3)]
            rd, rdB = P.sb([128, TT], F32, "rd")
            ot = [P.sb([128, TT], BF16, f"ot{i}") for i in range(2)]
            pS = [P.ps(f"pS{i}") for i in range(3)]
            pO = [P.ps(f"pO{i}") for i in range(2)]
            pD = [P.ps(f"pD{i}") for i in range(2)]
            scnt = 0
            tcnt = 0
            for s in range(NS):
                for hp in range(8):
                    k.dma(qT[:], qs[s, hp * 128:(hp + 1) * 128, :], writes=[qTB])
                    k.dma(kT[:], ks[s, hp * 128:(hp + 1) * 128, :], writes=[kTB])
                    k.dma(vv[:], vs[s, :, hp * 128:(hp + 1) * 128].rearrange("(a p) f -> p a f", p=128), writes=[vvB])
                    if s == 0 or True:
                        k.dma(tb[:], tab[hp], writes=[tbB])
                    qtiles = ([(0, NCTX, -1)] if need_ctx else []) + [(NCTX + ti * TT, TT, ti) for ti in range(8)]
                    for (t0, W, ti) in qtiles:
                        O_, OB = pO[tcnt % 2]
                        D_, DB = pD[tcnt % 2]
                        o_, oB = ot[tcnt % 2]
                        tcnt += 1
                        if ti < 0:
                            chunks = [(0, None), (1, None)]
                        else:
                            lo, hi = max(0, 4 * ti - 2), min(31, 4 * ti + 5)
                            chunks = [(2 + c, c) for c in range(lo, hi + 1)] + [(0, None), (1, None)]
                        for hd in range(2):
                            pb_ = 64 * hd
                            for ci, (kch, c) in enumerate(chunks):
                                S_, SB = pS[scnt % 3]
                                p_, pB = pt[scnt % 3]
                                m_, mB = sbm[scnt % 2]
                                scnt += 1
                                k.mm(S_[:, :W], kT[pb_:pb_ + 64, kch * 128:(kch + 1) * 128], qT[pb_:pb_ + 64, t0:t0 + W],
                                     True, True, [kTB, qTB], [SB])
                                if c is None:
                                    k.act(p_[:, :W], S_[:, :W], AF.Exp, [SB], [pB])
                                else:
                                    R = 8 * ti
                                    J0 = 10 - 2 * c + R
                                    if ti == 0:
                                        segs = [(0, 4, "U" if c <= 3 else "N"), (4, 8, "T")]
                                    elif ti == 7:
                                        segs = [(0, 5, "T"), (5, 8, "U" if c >= 28 else "N")]
                                    else:
                                        segs = [(0, 8, "T")]
                                    for (b0, b1, kind) in segs:
                                        c0, c1 = b0 * 64, b1 * 64
                                        if kind == "N":
                                            in1 = negt[:, 0:c1 - c0]
                                            rb = [negB]
                                        else:
                                            tix = 0 if kind == "T" else 1
                                            in1 = tb[:, tix, hd, J0 + b0:J0 + b1, :].rearrange("p a b -> p (a b)")
                                            rb = [tbB]
                                        k.tt("dve", m_[:, c0:c1], S_[:, c0:c1], in1, ALU.add, [SB] + rb, [mB])
                                    k.act(p_[:, :W], m_[:, :W], AF.Exp, [mB], [pB])
                                first, last = ci == 0, ci == len(chunks) - 1
                                k.mm(O_[pb_:pb_ + 64, :W], vv[:, kch, pb_:pb_ + 64], p_[:, :W], first, last, [vvB, pB], [OB])
                                k.mm(D_[pb_:pb_ + 64, :W], self.ones_bf[:, 0:64], p_[:, :W], first, last, [self.onesB, pB], [DB])
                        k.op("dve", lambda h: h.reciprocal(out=rd[:, :W], in_=D_[:, :W]), [DB], [rdB])
                        k.tt("dve", o_[:, :W], O_[:, :W], rd[:, :W], ALU.mult, [OB, rdB], [oB])
                        k.dma(os_[s, hp * 128:(hp + 1) * 128, t0:t0 + W], o_[:, :W], reads=[oB])

    def pool_pass(self, l, pw, include_ctx=True):
        k, dr = self.k, self.dr
        xs, hs = dr["xs"], dr["hs"]
        tl = tiles_for(include_ctx)
        PADW = 80
        with Pool_(k) as P:
            Wp, WpB = P.sb([128, 4, 2, 256], BF16, "Wp")
            k.dma(Wp[:], pw.rearrange("g (kc p) f -> p g kc f", p=128), writes=[WpB], q="pool")
            ls, lsB = P.sb([128, KC], F32, "ls")
            k.dma(ls[:], dr["pool_scale"][:, :], writes=[lsB])
            ic, icB = P.sb([128, 4, 2, 256], F32, "ic")
            k.dma(ic[:], dr["pool_ic"][:, :, :, :], writes=[icB])
            ht = [P.sb([128, KC, TT], BF16, f"ht{i}") for i in range(2)]
            lv = [P.sb([128, 8 * PADW], F32, f"lv{i}") for i in range(3)]
            lvc = [P.sb([128, 272], F32, f"lvc{i}") for i in range(3)]
            pl, plB = P.sb([128, KC, TT], BF16, "pl")
            plBs = [Buf(f"pl{i}") for i in range(KC)]
            mn, mnB = P.sb([128, TT], F32, "mn")
            yt = [P.sb([128, TT], F32, f"yt{i}") for i in range(2)]
            py = [P.ps(f"py{i}") for i in range(2)]
            for t_, B_ in lv + lvc:
                k.memset("pool", t_[:], 0.0, [B_])

            def load(i):
                s, t0, W, c = tl[i]
                t, B = ht[i % 2]
                k.dma(t[:, :, :W], hs[s].rearrange("(kc p) t -> p kc t", p=128)[:, :, t0:t0 + W], writes=[B])

            load(0)
            ycnt = 0
            for i, (s, t0, W, c) in enumerate(tl):
                if i + 1 < len(tl):
                    load(i + 1)
                h, hB = ht[i % 2]
                j = self.jidx(s, c)
                rows, rl, pw_, ri = (1, 256, 272, 1) if c else (8, 64, PADW, 0)

                def view(t, off, n=rl):
                    return t[:, 0:rows * pw_].rearrange("p (r w) -> p r w", w=pw_)[:, :, 8 + off:8 + off + n]

                for kc in range(KC):
                    gi = kc // 2
                    win = (2, 4, 8, 16)[gi]
                    a_, aB = (lvc if c else lv)[0]
                    b_, bB = (lvc if c else lv)[1]
                    c_, cB = (lvc if c else lv)[2]
                    hv = h[:, kc, :W].rearrange("p (r w) -> p r w", w=rl)
                    k.copy("pool", view(a_, 0), hv, [hB], [aB])
                    n2 = rl + 14
                    k.tt("pool", view(b_, -7, n2), view(a_, -8, n2), view(a_, -7, n2), ALU.add, [aB], [bB])
                    cur, curB = b_, bB
                    if win >= 4:
                        n4 = rl + 12
                        k.tt("pool", view(c_, -6, n4), view(b_, -7, n4), view(b_, -5, n4), ALU.add, [bB], [cB])
                        cur, curB = c_, cB
                    if win >= 8:
                        n8 = rl + 8
                        k.tt("dve", view(b_, -4, n8), view(c_, -6, n8), view(c_, -2, n8), ALU.add, [cB], [bB])
                        cur, curB = b_, bB
                    if win >= 16:
                        k.tt("dve", view(c_, 0), view(b_, -4), view(b_, 4), ALU.add, [bB], [cB])
                        cur, curB = c_, cB
                    mv = mn[:, :W].rearrange("p (r w) -> p r w", w=rl)
                    icv = ic[:, gi, ri, 0:rl]
                    for r in range(rows):
                        k.tt("dve", mv[:, r, :], view(cur, 0)[:, r, :], icv, ALU.mult, [curB, icB], [mnB])
                    k.tt("dve", pl[:, kc, :W], mn[:, :W], h[:, kc, :W], ALU.subtract, [mnB, hB], [plBs[kc]])
                for oc in range(KC):
                    gi = oc // 2
                    y_, yB = py[ycnt % 2]
                    yo, yoB = yt[ycnt % 2]
                    ycnt += 1
                    for kk in range(2):
                        k.mm(y_[:, :W], Wp[:, gi, kk, (oc % 2) * 128:(oc % 2 + 1) * 128], pl[:, 2 * gi + kk, :W], kk == 0, kk == 1,
                             [WpB, plBs[2 * gi + kk]], [yB])
                    k.ts("dve", yo[:, :W], y_[:, :W], self.mod[:, l, 16 + oc, j:j + 1], ls[:, oc:oc + 1], ALU.mult, ALU.mult,
                         [yB, self.modB, lsB], [yoB])
                    k.dma(xs[s, oc * 128:(oc + 1) * 128, t0:t0 + W], yo[:, :W], reads=[yoB], q="pool", accum_op=ALU.add)


def build(stop_after=99, debug=False):
    nc = bass.Bass("TRN2", target_bir_lowering=False)
    dr = {}

    def inp(name, shape, dt=F32):
        dr[name] = nc.dram_tensor(name, list(shape), dt, kind="ExternalInput").ap()

    def scr(name, shape, dt, out=False):
        dr[name] = nc.dram_tensor(name, list(shape), dt, kind="ExternalOutput" if out else "Internal").ap()

    inp("xin", [NS, D, TOK]); inp("cvec", [128, KC, 3]); inp("ada_w", [DEPTH, D, 6 * D]); inp("ada_b", [128, DEPTH, 48])
    inp("norm_g", [128, DEPTH, 2, KC]); inp("final_g", [128, KC])
    inp("ones_bf", [128, 128], BF16); inp("id_bf", [128, 128], BF16); inp("id_f", [128, 128]); inp("ones_f", [128, 128])
    inp("na_w_qkv", [2, D, 3 * D]); inp("na_w_o", [2, D, D]); inp("na_tab", [2, 8, 128, 2, 2, 22, 64])
    inp("ffn_w1", [2, D, DFF]); inp("ffn_w3", [2, D, DFF]); inp("ffn_w2", [2, DFF, D])
    inp("moe_router", [2, 128, KC, NEXP]); inp("moe_w1", [2, NEXP, D, DFE]); inp("moe_w3", [2, NEXP, D, DFE]); inp("moe_w2", [2, NEXP, DFE, D])
    inp("pool_w", [4, 256, 256]); inp("pool_scale", [128, KC]); inp("pool_ic", [128, 4, 2, 256])
    gdn_inputs(inp)
    scr("xs", [NS, D, TOK], F32, out=debug)
    scr("hs", [NS, D, TOK], BF16)
    scr("qs", [NS, D, TOK], BF16); scr("ks", [NS, D, TOK], BF16); scr("vs", [NS, TOK, D], BF16); scr("os", [NS, D, TOK], BF16)
    scr("wexp", [NS, NEXP, TOK], F32)
    gdn_scratch(scr)
    scr("out", [NS, D, NLAT], F32, out=True)

    with ExitStack() as es:
        k = K(nc, es)
        pg = Prog(nc, k, dr, stop_after)
        with Pool_(k) as PC:
            pg.setup_consts(PC)
            for s in range(NS):
                for kc in range(KC):
                    k.dma(dr["xs"][s, kc * 128:(kc + 1) * 128, :], dr["xin"][s, kc * 128:(kc + 1) * 128, :])
            pg.adaln()
            k.barrier()
            step = 0
            for l in range(DEPTH):
                last = l == DEPTH - 1
                if step >= stop_after:
                    break
                pg.norm_pass(l, 0, include_ctx=True)
                kind = l % 3
                jx = l // 3
                if kind == 0:
                    pg.na_qkv_pass(dr["na_w_qkv"][jx])
                    pg.na_attn_pass(dr["na_tab"][jx], need_ctx=not last)
                    pg.outproj_pass(l, dr["os"], dr["na_w_o"][jx], include_ctx=not last, gate_off=16)
                elif kind == 1:
                    gdn_mixer(pg, l, need_ctx=not last)
                else:
                    pg.pool_pass(l, dr["pool_w"], include_ctx=not last)
                step += 1
                if step >= stop_after:
                    break
                f = l // 2
                if l % 2 == 0:
                    pg.norm_pass(l, 1, include_ctx=not last)
                    H = DFF // 2
                    for hh in range(2):
                        pg.ffn_pass(l, dr["ffn_w1"][f][:, hh * H:(hh + 1) * H], dr["ffn_w3"][f][:, hh * H:(hh + 1) * H],
                                    dr["ffn_w2"][f][hh * H:(hh + 1) * H, :], H, include_ctx=not last)
                else:
                    pg.norm_pass(l, 1, include_ctx=not last, router=dr["moe_router"][f])
                    H = DFE // 2
                    for e in range(NEXP):
                        for hh in range(2):
                            pg.ffn_pass(l, dr["moe_w1"][f, e][:, hh * H:(hh + 1) * H], dr["moe_w3"][f, e][:, hh * H:(hh + 1) * H],
                                        dr["moe_w2"][f, e][hh * H:(hh + 1) * H, :], H, include_ctx=not last, expert=e)
                step += 1
            pg.norm_pass(0, 0, include_ctx=False, final=True)
            k.barrier()
    return nc


GDN_IN = 4128
NCHK = TOK // 128


def gdn_inputs(inp):
    inp("gdn_w_in", [D, GDN_IN]); inp("gdn_w_o", [D, D]); inp("gdn_cw", [128, 24, 4])
    inp("gdn_dtb", [128, 16]); inp("gdn_alog", [128, 16]); inp("gdn_ng", [128, 1])
    inp("gdn_tri", [128, 2, 128]); inp("gdn_gm", [128, 2, 3, 128]); inp("gdn_ms", [128, 7, 128], BF16)


def gdn_scratch(scr):
    scr("pj", [NS, 4 * D, TOK], BF16)
    scr("gq", [NS, D, TOK], BF16); scr("gk", [NS, D, TOK], BF16); scr("gv", [NS, D, TOK], BF16)
    scr("gt", [NS, TOK, 48], F32)
    scr("gcs", [NS, 16, TOK], F32)
    scr("gbs", [NS, 16, TOK], F32)


def gdn_proj_pass(pg):
    k, dr = pg.k, pg.dr
    hs, pj = dr["hs"], dr["pj"]
    tl = tiles_for(True)
    with Pool_(k) as P:
        Wi, WiB = P.sb([128, KC, GDN_IN], BF16, "Wi")
        src = dr["gdn_w_in"].rearrange("(kc p) n -> p kc n", p=128)
        for i in range(4):
            k.dma(Wi[:, :, i * 1032:(i + 1) * 1032], src[:, :, i * 1032:(i + 1) * 1032], writes=[WiB], q="pool")
        dtb, dtbB = P.sb([128, 16], F32, "dtb"); k.dma(dtb[:], dr["gdn_dtb"][:, :], writes=[dtbB])
        nA, nAB = P.sb([128, 16], F32, "nA"); k.dma(nA[:], dr["gdn_alog"][:, :], writes=[nAB])
        k.act(nA[:], nA[:], AF.Exp, [nAB], [nAB])
        k.ts("dve", nA[:], nA[:], -1.0, None, ALU.mult, None, [nAB], [nAB])
        tri, triB = P.sb([128, 2, 128], F32, "tri"); k.dma(tri[:], dr["gdn_tri"][:, :, :], writes=[triB])
        ht = [P.sb([128, KC, TT], BF16, f"ht{i}") for i in range(2)]
        pt = [P.sb([128, 32, TT], BF16, f"pjt{i}") for i in range(1)]
        gtt, gttB = P.sb([128, 4, 48], F32, "gtt")
        tmpa, tmpaB = P.sb([128, 16], F32, "tmpa")
        gcT, gcTB = P.sb([16, TT], F32, "gcT")
        bT, bTB = P.sb([16, TT], F32, "bT")
        pp = [P.ps(f"pp{i}") for i in range(4)]
        pab, pabB = P.ps("pab")
        pgc, pgcB = P.ps("pgc")
        ptr, ptrB = P.ps("ptr")
        pbf, pbfB = P.ps("pbf")

        def load(i):
            s, t0, W, c = tl[i]
            t, B = ht[i % 2]
            k.dma(t[:, :, :W], hs[s].rearrange("(kc p) t -> p kc t", p=128)[:, :, t0:t0 + W], writes=[B])

        load(0)
        cnt = 0
        for i, (s, t0, W, c) in enumerate(tl):
            if i + 1 < len(tl):
                load(i + 1)
            h, hB = ht[i % 2]
            p_, pjB = pt[0]
            for oc in range(32):
                q_, qB = pp[cnt % 4]
                for kc in range(KC):
                    k.mm(q_[:, :W], Wi[:, kc, oc * 128:(oc + 1) * 128], h[:, kc, :W], kc == 0, kc == KC - 1, [WiB, hB], [qB])
                if cnt % 2 == 0:
                    k.act(p_[:, oc, :W], q_[:, :W], AF.Copy, [qB], [pjB])
                else:
                    k.copy("dve", p_[:, oc, :W], q_[:, :W], [qB], [pjB])
                cnt += 1
            k.dma(pj[s].rearrange("(oc p) t -> p oc t", p=128)[:, :, t0:t0 + W], p_[:, :, :W], reads=[pjB])
            for kc in range(KC):
                k.mm(pbf[0:16, :W], Wi[:, kc, 4096 + 16:4096 + 32], h[:, kc, :W], kc == 0, kc == KC - 1, [WiB, hB], [pbfB])
            k.act(bT[:, :W], pbf[0:16, :W], AF.Sigmoid, [pbfB], [bTB])
            k.dma(dr["gbs"][s, :, t0:t0 + W], bT[:, :W], reads=[bTB])
            nst = W // 128
            for ts_ in range(nst):
                for kc in range(KC):
                    k.mm(pab[:, ts_ * 32:(ts_ + 1) * 32], h[:, kc, ts_ * 128:(ts_ + 1) * 128], Wi[:, kc, 4096:4128],
                         kc == 0, kc == KC - 1, [WiB, hB], [pabB])
            for ts_ in range(nst):
                k.tt("dve", tmpa[:], pab[:, ts_ * 32:ts_ * 32 + 16], dtb[:], ALU.add, [pabB, dtbB], [tmpaB])
                k.act(tmpa[:], tmpa[:], AF.Exp, [tmpaB], [tmpaB])
                k.act(tmpa[:], tmpa[:], AF.Ln, [tmpaB], [tmpaB], bias=1.0)
                k.tt("dve", gtt[:, ts_, 0:16], tmpa[:], nA[:], ALU.mult, [tmpaB, nAB], [gttB])
                k.act(gtt[:, ts_, 16:32], pab[:, ts_ * 32 + 16:ts_ * 32 + 32], AF.Sigmoid, [pabB], [gttB])
                for d in range(2):
                    k.mm(pgc[:, ts_ * 16 + d * 8:ts_ * 16 + d * 8 + 8], tri[:, d, :], gtt[:, ts_, d * 8:d * 8 + 8], True, True,
                         [triB, gttB], [pgcB])
                k.copy("dve", gtt[:, ts_, 32:48], pgc[:, ts_ * 16:ts_ * 16 + 16], [pgcB], [gttB])
                k.op("pe", lambda hh: hh.transpose(ptr[0:16, ts_ * 128:(ts_ + 1) * 128], gtt[:, ts_, 32:48], pg.id_f[:]),
                     [gttB, pg.idfB], [ptrB])
            k.copy("dve", gcT[:, :W], ptr[0:16, :W], [ptrB], [gcTB])
            k.dma(dr["gcs"][s, :, t0:t0 + W], gcT[:, :W], reads=[gcTB])
            k.dma(dr["gt"][s, t0:t0 + W, :].rearrange("(a p) c -> p a c", p=128), gtt[:, :nst, :], reads=[gttB])


def gdn_conv_pass(pg):
    k, dr = pg.k, pg.dr
    pj = dr["pj"]
    tl = tiles_for(True)
    with Pool_(k) as P:
        cw, cwB = P.sb([128, 24, 4], F32, "cw"); k.dma(cw[:], dr["gdn_cw"][:, :, :], writes=[cwB])
        e1, e1B = P.sb([128, 1], F32, "e1"); k.memset("pool", e1[:], EPS, [e1B])
        e2, e2B = P.sb([128, 1], F32, "e2"); k.memset("pool", e2[:], EPS * 128.0, [e2B])
        pin = [P.sb([128, 24, TT + 4], BF16, f"pin{i}") for i in range(2)]
        ot, otB = P.sb([128, 24, TT], BF16, "cot")
        acc = [P.sb([128, TT], F32, f"acc{i}") for i in range(2)]
        sl = [P.sb([128, TT], F32, f"sl{i}") for i in range(2)]
        sq = [P.sb([128, TT], BF16, f"sq{i}") for i in range(2)]
        rn = [P.sb([128, TT], F32, f"rn{i}") for i in range(2)]
        pss = [P.ps(f"pss{i}") for i in range(2)]

        def load(i):
            s, t0, W, c = tl[i]
            t, B = pin[i % 2]
            seq0, seq1 = (0, NCTX) if c else (NCTX, TOK)
            k.memset("pool", t[:], 0.0, [B])
            lo, hi = max(seq0, t0 - 2), min(seq1, t0 + W + 1)
            k.dma(t[:, :, lo - (t0 - 2):hi - (t0 - 2)], pj[s].rearrange("(oc p) t -> p oc t", p=128)[:, 0:24, lo:hi], writes=[B])

        load(0)
        cnt = 0
        for i, (s, t0, W, c) in enumerate(tl):
            if i + 1 < len(tl):
                load(i + 1)
            x, xB = pin[i % 2]
            for oc in range(24):
                a_, aB = acc[cnt % 2]; s_, sB = sl[cnt % 2]; q_, qB = sq[cnt % 2]; r_, rB = rn[cnt % 2]; p_, pB = pss[cnt % 2]
                cnt += 1
                k.ts("dve", a_[:, :W], x[:, oc, 0:W], cw[:, oc, 0:1], None, ALU.mult, None, [xB, cwB], [aB])
                for j in range(1, 4):
                    k.stt(a_[:, :W], x[:, oc, j:j + W], cw[:, oc, j:j + 1], a_[:, :W], ALU.mult, ALU.add, [xB, cwB, aB], [aB])
                if oc >= 16:
                    k.act(ot[:, oc, :W], a_[:, :W], AF.Silu, [aB], [otB])
                    continue
                k.act(s_[:, :W], a_[:, :W], AF.Silu, [aB], [sB])
                k.act(q_[:, :W], s_[:, :W], AF.Square, [sB], [qB])
                k.mm(p_[:, :W], pg.ones_bf[:], q_[:, :W], True, True, [pg.onesB, qB], [pB])
                if oc < 8:
                    k.act(r_[:, :W], p_[:, :W], AF.Sqrt, [pB, e2B], [rB], scale=128.0, bias=e2[:, 0:1])
                else:
                    k.act(r_[:, :W], p_[:, :W], AF.Sqrt, [pB, e1B], [rB], bias=e1[:, 0:1])
                k.op("dve", lambda hh: hh.reciprocal(out=r_[:, :W], in_=r_[:, :W]), [rB], [rB])
                k.tt("pool", ot[:, oc, :W], s_[:, :W], r_[:, :W], ALU.mult, [sB, rB], [otB])
            for gi, nm in enumerate(("gq", "gk", "gv")):
                k.dma(dr[nm][s].rearrange("(oc p) t -> p oc t", p=128)[:, :, t0:t0 + W], ot[:, gi * 8:(gi + 1) * 8, :W], reads=[otB])


def _rr(gens):
    gens = list(gens)
    while gens:
        nxt = []
        for g in gens:
            try:
                next(g)
                nxt.append(g)
            except StopIteration:
                pass
        gens = nxt


def gdn_core_pass(pg):
    k, dr = pg.k, pg.dr
    with Pool_(k) as P:
        gm, gmB = P.sb([128, 2, 3, 128], F32, "gm"); k.dma(gm[:], dr["gdn_gm"][:, :, :, :], writes=[gmB])
        ms, msB = P.sb([128, 7, 128], BF16, "ms"); k.dma(ms[:], dr["gdn_ms"][:, :, :], writes=[msB])
        gng, gngB = P.sb([128, 1], F32, "gng"); k.dma(gng[:], dr["gdn_ng"][:, :], writes=[gngB])
        e1, e1B = P.sb([128, 1], F32, "e1"); k.memset("pool", e1[:], EPS, [e1B])
        qT, qTB = P.sb([128, TOK], BF16, "qT"); kT, kTB = P.sb([128, TOK], BF16, "kT"); vT, vTB = P.sb([128, TOK], BF16, "vT")
        zT, zTB = P.sb([128, TOK], BF16, "zT")
        ktok, ktokB = P.sb([128, NCHK, 128], BF16, "ktok"); vtok, vtokB = P.sb([128, NCHK, 128], BF16, "vtok")
        gt, gtB = P.sb([128, NCHK, 48], F32, "gt")
        gcb, gcbB = P.sb([128, TOK], F32, "gcb"); bb, bbB = P.sb([128, TOK], F32, "bb")
        U, UB_ = P.sb([128, NCHK, 128], F32, "U"); UB = [Buf(f"U{n}") for n in range(NCHK)]
        WT, _ = P.sb([128, NCHK, 128], BF16, "WT"); WTB = [Buf(f"WT{n}") for n in range(NCHK)]
        AQ, _ = P.sb([128, NCHK, 128], BF16, "AQ"); AQB = [Buf(f"AQ{n}") for n in range(NCHK)]
        KTL, _ = P.sb([128, NCHK, 128], BF16, "KTL"); KTLB = [Buf(f"KTL{n}") for n in range(NCHK)]
        qd, qdB = P.sb([128, TOK], BF16, "qd")
        oacc, oaccB = P.sb([128, TOK], F32, "oacc"); oB = [Buf(f"o{n}") for n in range(NCHK)]
        egc, egcB = P.sb([128, NCHK], F32, "egc"); ett, ettB = P.sb([128, NCHK], F32, "ett"); egl, eglB = P.sb([128, NCHK], F32, "egl")
        S, SB = P.sb([128, 128], F32, "S"); Sb, SbB = P.sb([128, 128], BF16, "Sb")
        vn = [P.sb([128, 128], BF16, f"vn{i}") for i in range(2)]
        tmpx, tmpxB = P.sb([128, 512], F32, "tmpx")
        NI = 2
        wk = []
        for ii in range(NI):
            w = {}
            for nm in ("xa", "xb", "xc", "ea", "eb", "ec", "t1"):
                w[nm] = P.sb([128, 128], F32, f"{nm}{ii}")
            for nm in ("L", "M", "X0", "X1", "Y0", "Y1", "Wm", "Wn", "vb", "kbg"):
                w[nm] = P.sb([128, 128], BF16, f"{nm}{ii}")
            w["pA"] = P.ps(f"pA{ii}"); w["pB"] = P.ps(f"pB{ii}"); w["pC"] = P.ps(f"pC{ii}")
            wk.append(w)
        pX, pXB = P.ps("pX"); pY, pYB = P.ps("pY")

        for s in range(NS):
            k.dma(gt[:], dr["gt"][s].rearrange("(n p) c -> p n c", p=128), writes=[gtB])
            for h in range(8):
                rows = slice(h * 128, (h + 1) * 128)
                k.dma(qT[:], dr["gq"][s, rows, :], writes=[qTB]); k.dma(kT[:], dr["gk"][s, rows, :], writes=[kTB])
                k.dma(vT[:], dr["gv"][s, rows, :], writes=[vTB])
                k.dma(zT[:], dr["pj"][s, 3 * D + h * 128:3 * D + (h + 1) * 128, :], writes=[zTB])
                for n in range(NCHK):
                    cs = slice(n * 128, (n + 1) * 128)
                    pb16 = pX[:, 0:64].bitcast(BF16) if False else None
                    k.op("pe", lambda hh: hh.transpose(pX[:, 0:128].bitcast(BF16)[:, 0:128], kT[:, cs], pg.id_bf[:]), [kTB, pg.idbB], [pXB])
                    k.copy("act", ktok[:, n, :], pX[:, 0:128].bitcast(BF16)[:, 0:128], [pXB], [ktokB])
                    k.op("pe", lambda hh: hh.transpose(pY[:, 0:128].bitcast(BF16)[:, 0:128], vT[:, cs], pg.id_bf[:]), [vTB, pg.idbB], [pYB])
                    k.copy("dve", vtok[:, n, :], pY[:, 0:128].bitcast(BF16)[:, 0:128], [pYB], [vtokB])
                for d in range(2):
                    col = d * 8 + h
                    k.dma(gcb[:], dr["gcs"][s, col:col + 1, :].partition_broadcast(128), writes=[gcbB])
                    k.dma(bb[:], dr["gbs"][s, col:col + 1, :].partition_broadcast(128), writes=[bbB])
                    gcv = gt[:, :, 32 + col]
                    btv = gt[:, :, 16 + col]
                    lastoff = 127 if d == 0 else 0
                    glv = gcb[:, lastoff:TOK:128]
                    k.act(egc[:], gcv, AF.Exp, [gtB], [egcB])
                    k.tt("dve", ett[:], glv, gcv, ALU.subtract, [gcbB, gtB], [ettB])
                    k.act(ett[:], ett[:], AF.Exp, [ettB], [ettB])
                    k.act(egl[:], glv, AF.Exp, [gcbB], [eglB])
                    for c0 in range(0, TOK, 512):
                        wd = min(512, TOK - c0)
                        k.act(tmpx[:, :wd], gcb[:, c0:c0 + wd], AF.Exp, [gcbB], [tmpxB])
                        k.tt("dve", qd[:, c0:c0 + wd], qT[:, c0:c0 + wd], tmpx[:, :wd], ALU.mult, [qTB, tmpxB], [qdB])

                    def inst(n, w):
                        cs = slice(n * 128, (n + 1) * 128)
                        gcn = gt[:, n, 32 + col:33 + col]; btn = gt[:, n, 16 + col:17 + col]
                        (xa, xaB), (xb, xbB), (xc, xcB) = w["xa"], w["xb"], w["xc"]
                        (ea, eaB), (eb, ebB), (ec, ecB) = w["ea"], w["eb"], w["ec"]
                        (t1, t1B), (L, LB), (M, MB) = w["t1"], w["L"], w["M"]
                        (pA, pAB), (pB_, pBB), (pC, pCB) = w["pA"], w["pB"], w["pC"]
                        k.stt(xa[:], gcb[:, cs], gcn, gm[:, d, 0, :], ALU.subtract, ALU.add, [gcbB, gtB, gmB], [xaB])
                        k.stt(xb[:], gcb[:, cs], gcn, gm[:, d, 1, :], ALU.subtract, ALU.add, [gcbB, gtB, gmB], [xbB])
                        k.stt(xc[:], gcb[:, cs], gcn, gm[:, d, 2, :], ALU.subtract, ALU.add, [gcbB, gtB, gmB], [xcB])
                        k.mm(pA[:, 0:128], kT[:, cs], kT[:, cs], True, True, [kTB], [pAB])
                        k.mm(pB_[:, 0:128], kT[:, cs], qT[:, cs], True, True, [kTB, qTB], [pBB])
                        yield
                        k.act(ea[:], xa[:], AF.Exp, [xaB], [eaB])
                        k.act(eb[:], xb[:], AF.Exp, [xbB], [ebB])
                        k.act(ec[:], xc[:], AF.Exp, [xcB], [ecB], scale=-1.0)
                        yield
                        k.stt(L[:], pA[:, 0:128], btn, ec[:], ALU.mult, ALU.mult, [pAB, gtB, ecB], [LB])
                        k.tt("dve", t1[:], pA[:, 0:128], eb[:], ALU.mult, [pAB, ebB], [t1B])
                        k.tt("pool", M[:], t1[:], bb[:, cs], ALU.mult, [t1B, bbB], [MB])
                        k.tt("dve", AQ[:, n, :], pB_[:, 0:128], ea[:], ALU.mult, [pBB, eaB], [AQB[n]])
                        k.ts("pool", w["vb"][0][:], vtok[:, n, :], btn, None, ALU.mult, None, [vtokB, gtB], [w["vb"][1]])
                        k.ts("pool", w["kbg"][0][:], ktok[:, n, :], btn, egc[:, n:n + 1], ALU.mult, ALU.mult, [ktokB, gtB, egcB], [w["kbg"][1]])
                        k.ts("pool", KTL[:, n, :], ktok[:, n, :], ett[:, n:n + 1], None, ALU.mult, None, [ktokB, ettB], [KTLB[n]])
                        yield
                        X, XB = w["X0"]; Y, YB = w["Y0"]; Xn, XnB = w["X1"]; Yn, YnB = w["Y1"]
                        Wm, WmB = w["Wm"]; Wn, WnB = w["Wn"]
                        k.tt("pool", Wm[:], L[:], ms[:, 0, :], ALU.mult, [LB, msB], [WmB])
                        k.tt("pool", X[:], pg.id_bf[:], Wm[:], ALU.subtract, [pg.idbB, WmB], [XB])
                        k.tt("pool", Wn[:], M[:], ms[:, 0, :], ALU.mult, [MB, msB], [WnB])
                        k.tt("pool", Y[:], pg.id_bf[:], Wn[:], ALU.subtract, [pg.idbB, WnB], [YB])
                        yield
                        for lv in range(1, 7):
                            lastlv = lv == 6
                            if not lastlv:
                                k.mm(pA[:, 0:128], M[:], X[:], True, True, [MB, XB], [pAB])
                            k.mm(pB_[:, 0:128], L[:], Y[:], True, True, [LB, YB], [pBB])
                            yield
                            if not lastlv:
                                k.tt("dve", Wm[:], pA[:, 0:128], ms[:, lv, :], ALU.mult, [pAB, msB], [WmB])
                            k.tt("dve", Wn[:], pB_[:, 0:128], ms[:, lv, :], ALU.mult, [pBB, msB], [WnB])
                            yield
                            if not lastlv:
                                k.mm(pA[:, 0:128], Y[:], Wm[:], True, True, [YB, WmB], [pAB])
                            k.mm(pC[:, 0:128], X[:], Wn[:], True, True, [XB, WnB], [pCB])
                            yield
                            if not lastlv:
                                k.tt("dve", Xn[:], X[:], pA[:, 0:128], ALU.subtract, [XB, pAB], [XnB])
                            k.tt("dve", Yn[:], Y[:], pC[:, 0:128], ALU.subtract, [YB, pCB], [YnB])
                            X, XB, Xn, XnB = Xn, XnB, X, XB
                            Y, YB, Yn, YnB = Yn, YnB, Y, YB
                            yield
                        k.mm(pA[:, 0:128], Y[:], w["vb"][0][:], True, True, [YB, w["vb"][1]], [pAB])
                        k.mm(pB_[:, 0:128], w["kbg"][0][:], Y[:], True, True, [w["kbg"][1], YB], [pBB])
                        yield
                        k.copy("act", U[:, n, :], pA[:, 0:128], [pAB], [UB[n]])
                        k.copy("act", WT[:, n, :], pB_[:, 0:128], [pBB], [WTB[n]])
                        yield

                    for n0 in range(0, NCHK, NI):
                        _rr([inst(n0 + ii, wk[ii]) for ii in range(NI) if n0 + ii < NCHK])
                    k.memset("pool", S[:], 0.0, [SB]); k.memset("pool", Sb[:], 0.0, [SbB])
                    order = [0, 1] + list(range(2, NCHK)) if d == 0 else [1, 0] + list(range(NCHK - 1, 1, -1))
                    for it, n in enumerate(order):
                        cs = slice(n * 128, (n + 1) * 128)
                        v_, vB_ = vn[it % 2]
                        k.mm(pX[:, 0:128], WT[:, n, :], Sb[:], True, True, [WTB[n], SbB], [pXB])
                        k.tt("dve", v_[:], U[:, n, :], pX[:, 0:128], ALU.subtract, [UB[n], pXB], [vB_])
                        k.mm(pY[:, 0:128], Sb[:], qd[:, cs], True, False, [SbB, qdB], [pYB])
                        k.mm(pY[:, 0:128], v_[:], AQ[:, n, :], False, True, [vB_, AQB[n]], [pYB])
                        if d == 0:
                            k.copy("act", oacc[:, cs], pY[:, 0:128], [pYB], [oB[n]])
                        else:
                            k.tt("dve", oacc[:, cs], oacc[:, cs], pY[:, 0:128], ALU.add, [oB[n], pYB], [oB[n]])
                        k.mm(pX[:, 0:128], KTL[:, n, :], v_[:], True, True, [KTLB[n], vB_], [pXB])
                        k.stt(S[:], S[:], egl[:, n:n + 1], pX[:, 0:128], ALU.mult, ALU.add, [SB, eglB, pXB], [SB])
                        k.copy("act", Sb[:], S[:], [SB], [SbB])
                for c0 in range(0, TOK, 512):
                    wd = min(512, TOK - c0)
                    obs = oB[c0 // 128:(c0 + wd) // 128]
                    xa, xaB = wk[0]["xa"]; xb, xbB = wk[0]["xb"]
                    sqt, sqB = P_sq = (qd, qdB)
                    k.act(sqt[:, c0:c0 + wd], oacc[:, c0:c0 + wd], AF.Square, obs, [qdB])
                    k.mm(pX[:, :wd], pg.ones_bf[:], sqt[:, c0:c0 + wd], True, True, [pg.onesB, qdB], [pXB])
                    k.act(tmpx[:, :wd], pX[:, :wd], AF.Sqrt, [pXB, e1B], [tmpxB], scale=1.0 / 128.0, bias=e1[:, 0:1])
                    k.op("dve", lambda hh: hh.reciprocal(out=tmpx[:, :wd], in_=tmpx[:, :wd]), [tmpxB], [tmpxB])
                    k.tt("dve", oacc[:, c0:c0 + wd], oacc[:, c0:c0 + wd], tmpx[:, :wd], ALU.mult, obs + [tmpxB], obs)
                    k.act(tmpx[:, :wd], zT[:, c0:c0 + wd], AF.Silu, [zTB], [tmpxB])
                    k.stt(qd[:, c0:c0 + wd], oacc[:, c0:c0 + wd], gng[:, 0:1], tmpx[:, :wd], ALU.mult, ALU.mult, obs + [gngB, tmpxB], [qdB])
                k.dma(dr["os"][s, rows, :], qd[:], reads=[qdB])


def gdn_mixer(pg, l, need_ctx):
    gdn_proj_pass(pg)
    gdn_conv_pass(pg)
    gdn_core_pass(pg)
    pg.outproj_pass(l, pg.dr["os"], pg.dr["gdn_w_o"], include_ctx=need_ctx, gate_off=16)


def _fm(v):
    v = np.asarray(v, np.float32)
    lead = v.shape[:-1]
    return np.ascontiguousarray(np.moveaxis(v.reshape(lead + (KC, 128)), -1, 0))


def _na_tables(rpb):
    col = np.arange(64)
    c0 = np.clip(col - 8, 0, 48)
    in_win = (col[None, :] >= c0[:, None]) & (col[None, :] < c0[:, None] + 16)
    dc = np.clip(col[None, :] - col[:, None], -15, 15) + 15
    out = np.full((8, 128, 2, 2, 22, 64), NEG, np.float32)
    for jj in range(22):
        j = 17 - jj
        for a in range(2):
            drr = j + a
            if drr < 0 or drr > 14:
                continue
            base = np.where(in_win[None], rpb[:, drr][:, dc], NEG).astype(np.float32)
            base = np.transpose(base, (0, 2, 1))
            for h in range(16):
                out[h // 2, a * 64:(a + 1) * 64, 1, h % 2, jj, :] = base[h]
                if 3 <= drr <= 10:
                    out[h // 2, a * 64:(a + 1) * 64, 0, h % 2, jj, :] = base[h]
    return out


def _pool_ic():
    out = np.ones((4, 2, 256), np.float32)
    for gi, win in enumerate((2, 4, 8, 16)):
        for ri, T in enumerate((64, 256)):
            t = np.arange(T)
            lo = np.clip(t - win // 2, 0, T)
            hi = np.clip(t + win // 2, 0, T)
            out[gi, ri, :T] = 1.0 / (hi - lo).astype(np.float32)
    return np.ascontiguousarray(np.broadcast_to(out[None], (128, 4, 2, 256)))


def make_in_maps(inputs, n_cores=8):
    import ml_dtypes
    x, c, ctx, c_ctx = inputs["x"], inputs["c"], inputs["ctx"], inputs["c_ctx"]
    shared = {
        "ada_w": np.ascontiguousarray(inputs["ada_w"], np.float32),
        "ada_b": np.ascontiguousarray(np.transpose(np.asarray(inputs["ada_b"], np.float32).reshape(DEPTH, 48, 128), (2, 0, 1))),
        "norm_g": _fm(inputs["norm_g"]),
        "final_g": _fm(inputs["final_g"]),
        "ones_bf": np.ones((128, 128), ml_dtypes.bfloat16),
        "id_bf": np.eye(128, dtype=np.float32).astype(ml_dtypes.bfloat16),
        "id_f": np.eye(128, dtype=np.float32),
        "ones_f": np.ones((128, 128), np.float32),
        "na_w_qkv": np.ascontiguousarray(inputs["na_w_qkv"], np.float32),
        "na_w_o": np.ascontiguousarray(inputs["na_w_o"], np.float32),
        "na_tab": np.stack([_na_tables(np.asarray(inputs["na_rpb"][j], np.float32)) for j in range(2)]),
        "ffn_w1": np.ascontiguousarray(inputs["ffn_w1"], np.float32),
        "ffn_w3": np.ascontiguousarray(inputs["ffn_w3"], np.float32),
        "ffn_w2": np.ascontiguousarray(inputs["ffn_w2"], np.float32),
        "moe_router": np.ascontiguousarray(np.transpose(np.asarray(inputs["moe_router"], np.float32).reshape(2, KC, 128, NEXP), (0, 2, 1, 3))),
        "moe_w1": np.ascontiguousarray(inputs["moe_w1"], np.float32),
        "moe_w3": np.ascontiguousarray(inputs["moe_w3"], np.float32),
        "moe_w2": np.ascontiguousarray(inputs["moe_w2"], np.float32),
        "pool_w": np.ascontiguousarray(inputs["pool_w"][0], np.float32),
        "pool_scale": _fm(inputs["pool_scale"][0]),
        "pool_ic": _pool_ic(),
    }
    shared.update(gdn_host(inputs))
    maps = []
    for ci in range(n_cores):
        b0 = ci * NS
        xin = np.empty((NS, D, TOK), np.float32)
        for s in range(NS):
            xin[s, :, :NCTX] = np.asarray(ctx[b0 + s], np.float32).T
            xin[s, :, NCTX:] = np.asarray(x[b0 + s], np.float32).T
        cv = np.stack([c[b0], c[b0 + 1], c_ctx], axis=0).astype(np.float32)
        m = dict(shared)
        m["xin"] = xin
        m["cvec"] = np.ascontiguousarray(np.transpose(cv.reshape(3, KC, 128), (2, 1, 0)))
        maps.append(m)
    return maps


def gdn_host(inputs):
    import ml_dtypes
    cw = np.asarray(inputs["gdn_conv"][0], np.float32)
    cwl = np.ascontiguousarray(np.transpose(cw.reshape(4, 24, 128), (2, 1, 0)))
    dtb = np.asarray(inputs["gdn_dt_bias"][0], np.float32).reshape(16)
    alog = np.asarray(inputs["gdn_a_log"][0], np.float32).reshape(16)
    p = np.arange(128)
    tri = np.zeros((128, 2, 128), np.float32)
    tri[:, 0, :] = (p[:, None] <= p[None, :])
    tri[:, 1, :] = (p[:, None] >= p[None, :])
    gm = np.zeros((128, 2, 3, 128), np.float32)
    P_, F_ = p[:, None], p[None, :]
    gm[:, 0, 0, :] = np.where(F_ >= P_, 0.0, NEG); gm[:, 0, 1, :] = np.where(F_ > P_, 0.0, NEG); gm[:, 0, 2, :] = np.where(F_ < P_, 0.0, -NEG)
    gm[:, 1, 0, :] = np.where(F_ <= P_, 0.0, NEG); gm[:, 1, 1, :] = np.where(F_ < P_, 0.0, NEG); gm[:, 1, 2, :] = np.where(F_ > P_, 0.0, -NEG)
    ms = np.zeros((128, 7, 128), np.float32)
    for kk in range(7):
        ms[:, kk, :] = ((P_ >> kk) != (F_ >> kk)) & ((P_ >> (kk + 1)) == (F_ >> (kk + 1)))
    return {
        "gdn_w_in": np.ascontiguousarray(inputs["gdn_w_in"][0], np.float32),
        "gdn_w_o": np.ascontiguousarray(inputs["gdn_w_o"][0], np.float32),
        "gdn_cw": cwl,
        "gdn_dtb": np.ascontiguousarray(np.broadcast_to(dtb[None], (128, 16))),
        "gdn_alog": np.ascontiguousarray(np.broadcast_to(alog[None], (128, 16))),
        "gdn_ng": np.ascontiguousarray(np.asarray(inputs["gdn_norm_g"][0], np.float32).reshape(128, 1)),
        "gdn_tri": tri, "gdn_gm": gm, "gdn_ms": ms.astype(ml_dtypes.bfloat16),
    }


def kernel(**inputs):
    nc = build()
    maps = make_in_maps(inputs)
    res = run_bass_kernel_spmd(nc, maps, core_ids=list(range(8)))
    B = inputs["x"].shape[0]
    out = np.empty((B, NLAT, D), np.float32)
    for ci in range(8):
        o = res.results[ci]["out"]
        for s in range(NS):
            out[ci * NS + s] = o[s].T
    return out
```

```python
import numpy as np
from contextlib import ExitStack
import concourse.bass as bass
import concourse.mybir as mybir
from concourse.bass_utils import run_bass_kernel_spmd

F32 = mybir.dt.float32
BF16 = mybir.dt.bfloat16
U32 = mybir.dt.uint32
AF = mybir.ActivationFunctionType
ALU = mybir.AluOpType

D = 1024
KC = 8
NLAT = 4096
NCTX = 256
TT = 512
NS = 2
DEPTH = 4
EPS = 1e-6
NEG = -1e30
DFF = 2816
DFE = 3584
NEXP = 8


class _Sem:
    def __init__(self, h, name):
        self.h = h
        self.val = 0
        self.name = name


class _Eng:
    def __init__(self, name, h, sem):
        self.name = name
        self.h = h
        self.sem = sem
        self.seen = {}


class Buf:
    __slots__ = ("name", "w", "r")

    def __init__(self, name=""):
        self.name = name
        self.w = None
        self.r = {}


class K:
    def __init__(self, nc, es, ndma=24):
        self.nc = nc
        self.es = es
        self.E = {}
        for name, h in (("pe", nc.tensor), ("act", nc.scalar), ("dve", nc.vector),
                        ("pool", nc.gpsimd), ("sp", nc.sync)):
            s = _Sem(es.enter_context(nc.semaphore("sem_" + name)), name)
            self.E[name] = _Eng(name, h, s)
        self.dsems = [_Sem(es.enter_context(nc.semaphore(f"dsem{i}")), f"d{i}") for i in range(ndma)]
        self.dnext = 0
        self.allsems = [e.sem for e in self.E.values()] + self.dsems

    def _need(self, e, ev):
        if ev is None:
            return
        sem, val = ev
        if sem is e.sem and e.name == "pe":
            return
        if e.seen.get(sem.name, 0) >= val:
            return
        e.h.wait_ge(sem.h, val)
        e.seen[sem.name] = val

    def _deps(self, e, reads, writes):
        for b in reads:
            self._need(e, b.w)
        for b in writes:
            self._need(e, b.w)
            for sname, ev in list(b.r.items()):
                self._need(e, ev)

    def _mark(self, ev, reads, writes):
        for b in reads:
            b.r[ev[0].name] = ev
        for b in writes:
            b.w = ev
            b.r = {}

    def op(self, ename, fn, reads=(), writes=()):
        e = self.E[ename]
        self._deps(e, reads, writes)
        ins = fn(e.h)
        e.sem.val += 1
        ins.then_inc(e.sem.h, 1)
        self._mark((e.sem, e.sem.val), reads, writes)

    def dma(self, out, in_, reads=(), writes=(), q="sp", **kw):
        e = self.E[q]
        ds = self.dsems[self.dnext]
        self.dnext = (self.dnext + 1) % len(self.dsems)
        self._need(e, (ds, ds.val))
        self._deps(e, reads, writes)
        ins = e.h.dma_start(out=out, in_=in_, **kw)
        ds.val += 16
        ins.then_inc(ds.h, 16)
        self._mark((ds, ds.val), reads, writes)

    def idma(self, out, in_, idx_ap, gather, reads=(), writes=(), bounds=None):
        e = self.E["pool"]
        ds = self.dsems[self.dnext]
        self.dnext = (self.dnext + 1) % len(self.dsems)
        self._need(e, (ds, ds.val))
        self._deps(e, reads, writes)
        off = bass.IndirectOffsetOnAxis(ap=idx_ap, axis=0)
        if gather:
            ins = e.h.indirect_dma_start(out=out, out_offset=None, in_=in_, in_offset=off)
        else:
            ins = e.h.indirect_dma_start(out=out, out_offset=off, in_=in_, in_offset=None)
        ds.val += 16
        ins.then_inc(ds.h, 16)
        self._mark((ds, ds.val), reads, writes)

    def barrier(self):
        for e in self.E.values():
            for s in self.allsems:
                if s.val > 0:
                    self._need(e, (s, s.val))

    def mm(self, out, lhsT, rhs, start, stop, reads, writes):
        self.op("pe", lambda h: h.matmul(out, lhsT, rhs, start=start, stop=stop), reads, writes)

    def act(self, out, in_, func, reads, writes, **kw):
        self.op("act", lambda h: h.activation(out=out, in_=in_, func=func, **kw), reads, writes)

    def tt(self, eng, out, in0, in1, op, reads, writes):
        self.op(eng, lambda h: h.tensor_tensor(out=out, in0=in0, in1=in1, op=op), reads, writes)

    def ts(self, eng, out, in0, s1, s2, op0, op1, reads, writes):
        if op1 is None:
            self.op(eng, lambda h: h.tensor_scalar(out=out, in0=in0, scalar1=s1, scalar2=None, op0=op0),
                    reads, writes)
        else:
            self.op(eng, lambda h: h.tensor_scalar(out=out, in0=in0, scalar1=s1, scalar2=s2, op0=op0, op1=op1),
                    reads, writes)

    def stt(self, out, in0, scalar, in1, op0, op1, reads, writes):
        self.op("dve", lambda h: h.scalar_tensor_tensor(out=out, in0=in0, scalar=scalar, in1=in1, op0=op0, op1=op1),
                reads, writes)

    def copy(self, eng, out, in_, reads, writes):
        if eng == "act":
            self.op("act", lambda h: h.activation(out=out, in_=in_, func=AF.Copy), reads, writes)
        else:
            self.op(eng, lambda h: h.tensor_copy(out=out, in_=in_), reads, writes)

    def memset(self, eng, ap, val, writes):
        self.op(eng, lambda h: h.memset(ap, val), (), writes)


_UID = [0]


class Pool_:
    def __init__(self, k):
        self.k = k
        self.es = ExitStack()
        self.n = 0

    def __enter__(self):
        self.es.__enter__()
        return self

    def __exit__(self, *a):
        self.k.barrier()
        return self.es.__exit__(*a)

    def sb(self, shape, dt, name=None):
        _UID[0] += 1
        t = self.es.enter_context(self.k.nc.sbuf_tensor(f"{name or 't'}_{_UID[0]}", list(shape), dt))
        return t, Buf(name or "sb")

    def ps(self, name=None, shape=(128, 512), dt=F32):
        _UID[0] += 1
        t = self.es.enter_context(self.k.nc.psum_tensor(f"{name or 'p'}_{_UID[0]}", list(shape), dt))
        return t, Buf(name or "ps")


TOK = NCTX + NLAT


def tiles_for(include_ctx=True):
    out = []
    for s in range(NS):
        if include_ctx:
            out.append((s, 0, NCTX, True))
        for i in range(NLAT // TT):
            out.append((s, NCTX + i * TT, TT, False))
    return out


class Prog:
    def __init__(self, nc, k, dr, stop_after=99):
        self.nc = nc
        self.k = k
        self.dr = dr
        self.stop_after = stop_after

    def setup_consts(self, P):
        k, dr = self.k, self.dr
        self.ones_bf, self.onesB = P.sb([128, 128], BF16, "ones")
        self.id_bf, self.idbB = P.sb([128, 128], BF16, "idb")
        self.id_f, self.idfB = P.sb([128, 128], F32, "idf")
        self.ones_f, self.onesfB = P.sb([128, 128], F32, "onesf")
        self.mod, self.modB = P.sb([128, DEPTH, 48, 3], F32, "mod")
        self.ng, self.ngB = P.sb([128, DEPTH, 2, KC], F32, "ng")
        self.fg, self.fgB = P.sb([128, KC], F32, "fg")
        self.epsb, self.epsB = P.sb([128, 1], F32, "eps")
        k.dma(self.ones_bf[:], dr["ones_bf"][:, :], writes=[self.onesB])
        k.dma(self.id_bf[:], dr["id_bf"][:, :], writes=[self.idbB])
        k.dma(self.id_f[:], dr["id_f"][:, :], writes=[self.idfB])
        k.dma(self.ones_f[:], dr["ones_f"][:, :], writes=[self.onesfB])
        k.dma(self.ng[:], dr["norm_g"][:, :, :, :], writes=[self.ngB])
        k.dma(self.fg[:], dr["final_g"][:, :], writes=[self.fgB])
        k.memset("pool", self.epsb[:], EPS, [self.epsB])

    def adaln(self):
        k, dr = self.k, self.dr
        with Pool_(k) as P:
            cv, cvB = P.sb([128, KC, 3], F32, "cv")
            sc, scB = P.sb([128, KC, 3], F32, "sc")
            ab, abB = P.sb([128, DEPTH, 48], F32, "ab")
            wa = [P.sb([128, KC, 768], F32, f"wa{i}") for i in range(2)]
            ps = [P.ps(f"adaps{i}") for i in range(2)]
            k.dma(cv[:], dr["cvec"][:, :, :], writes=[cvB])
            k.dma(ab[:], dr["ada_b"][:, :, :], writes=[abB])
            k.act(sc[:], cv[:], AF.Silu, [cvB], [scB])
            gi = 0
            for l in range(DEPTH):
                src = dr["ada_w"][l].rearrange("(kc p) n -> p kc n", p=128)
                for g in range(8):
                    wt, wB = wa[gi % 2]
                    pt, pB = ps[gi % 2]
                    gi += 1
                    k.dma(wt[:], src[:, :, g * 768:(g + 1) * 768], writes=[wB])
                    for o in range(6):
                        for kc in range(KC):
                            k.mm(pt[:, o * 3:(o + 1) * 3], wt[:, kc, o * 128:(o + 1) * 128], sc[:, kc, :],
                                 kc == 0, kc == KC - 1, [wB, scB], [pB])
                    pv = pt[:, 0:18].rearrange("p (o j) -> p o j", j=3)
                    for j in range(3):
                        k.tt("dve", self.mod[:, l, g * 6:(g + 1) * 6, j], pv[:, :, j], ab[:, l, g * 6:(g + 1) * 6],
                             ALU.add, [pB, abB], [self.modB])

    def mod_vecs(self, P, l, sub):
        k = self.k
        A, AB = P.sb([128, KC, 3], F32, "modA")
        o = sub * 24
        for j in range(3):
            k.stt(A[:, :, j], self.mod[:, l, o + 8:o + 16, j], 1.0, self.ng[:, l, sub, :], ALU.add, ALU.mult,
                  [self.modB, self.ngB], [AB])
        return A, AB

    def jidx(self, s, is_ctx):
        return 2 if is_ctx else s

    def norm_pass(self, l, sub, include_ctx=True, router=None, final=False, moe=None):
        k, dr = self.k, self.dr
        xs, hs = dr["xs"], dr["hs"]
        tl = tiles_for(include_ctx)
        with Pool_(k) as P:
            if not final:
                A, AB = self.mod_vecs(P, l, sub)
            xt = [P.sb([128, KC, TT], F32, f"xt{i}") for i in range(2)]
            sq, sqB = P.sb([128, KC, TT], BF16, "sq")
            rs, rsB = P.sb([128, TT], F32, "rs")
            hf, hfB = P.sb([128, KC, TT], F32, "hf")
            hb, hbB = P.sb([128, KC, TT], BF16, "hb")
            pss, pssB = P.ps("pss")
            if router is not None:
                wr, wrB = P.sb([128, KC, NEXP], F32, "wr")
                k.dma(wr[:], router, writes=[wrB])
                plg, plgB = P.ps("plg")
                pwt, pwtB = P.ps("pwt")
                lg, lgB = P.sb([128, 4, NEXP], F32, "lg")
                m8, m8B = P.sb([128, 4, 8], F32, "m8")
                nm1, nm1B = P.sb([128, 4], F32, "nm1")
                ee, eeB = P.sb([128, 4, NEXP], F32, "ee")
                mk, mkB = P.sb([128, 4, NEXP], F32, "mk")
                dn, dnB = P.sb([128, 4], F32, "dn")
                ww, wwB = P.sb([128, 4, NEXP], F32, "ww")
                wT, wTB = P.sb([NEXP, TT], F32, "wT")
                if moe is not None:
                    oh, ohB = P.sb([128, 4, 24], F32, "oh")
                    rk, rkB = P.sb([128, 4, NEXP], F32, "rk")
                    t8, t8B = P.sb([128, 4, NEXP], F32, "t8")
                    htk, htkB = P.sb([128, 4, D], BF16, "htk")
                    prk, prkB = P.ps("prk")
                    ptot, ptotB = P.ps("ptot")
                    ptk, ptkB = P.ps("ptk")
                    I32_ = mybir.dt.int32
                    sub_i = 0

            def load(i):
                s, t0, W, c = tl[i]
                t, B = xt[i % 2]
                k.dma(t[:, :, :W], xs[s].rearrange("(kc p) t -> p kc t", p=128)[:, :, t0:t0 + W], writes=[B])

            load(0)
            for i, (s, t0, W, c) in enumerate(tl):
                if i + 1 < len(tl):
                    load(i + 1)
                x, xB = xt[i % 2]
                j = self.jidx(s, c)
                k.act(sq[:, :, :W], x[:, :, :W], AF.Square, [xB], [sqB])
                for kc in range(KC):
                    k.mm(pss[:, :W], self.ones_bf[:], sq[:, kc, :W], kc == 0, kc == KC - 1, [self.onesB, sqB], [pssB])
                k.act(rs[:, :W], pss[:, :W], AF.Sqrt, [pssB, self.epsB], [rsB], scale=1.0 / D, bias=self.epsb[:, 0:1])
                k.op("dve", lambda h: h.reciprocal(out=rs[:, :W], in_=rs[:, :W]), [rsB], [rsB])
                for kc in range(KC):
                    k.tt("dve", hf[:, kc, :W], x[:, kc, :W], rs[:, :W], ALU.mult, [xB, rsB], [hfB])
                if final:
                    for kc in range(KC):
                        k.act(hf[:, kc, :W], hf[:, kc, :W], AF.Identity, [hfB, self.fgB], [hfB], scale=self.fg[:, kc:kc + 1])
                    k.dma(dr["out"][s].rearrange("(kc p) t -> p kc t", p=128)[:, :, t0 - NCTX:t0 - NCTX + W],
                          hf[:, :, :W], reads=[hfB])
                    continue
                o = sub * 24
                for kc in range(KC):
                    k.act(hf[:, kc, :W], hf[:, kc, :W], AF.Identity, [hfB, AB, self.modB], [hfB],
                          scale=A[:, kc, j:j + 1], bias=self.mod[:, l, o + kc, j:j + 1])
                k.copy("pool", hb[:, :, :W], hf[:, :, :W], [hfB], [hbB])
                k.dma(hs[s].rearrange("(kc p) t -> p kc t", p=128)[:, :, t0:t0 + W], hb[:, :, :W], reads=[hbB])
                if router is not None:
                    nst = W // 128
                    for ts_ in range(nst):
                        for kc in range(KC):
                            k.mm(plg[:, ts_ * 8:(ts_ + 1) * 8], hf[:, kc, ts_ * 128:(ts_ + 1) * 128], wr[:, kc, :],
                                 kc == 0, kc == KC - 1, [hfB, wrB], [plgB])
                    k.copy("dve", lg[:, :nst, :], plg[:, 0:nst * 8].rearrange("p (a e) -> p a e", e=8), [plgB], [lgB])
                    for ts_ in range(nst):
                        k.op("dve", lambda h: h.max(out=m8[:, ts_, :], in_=lg[:, ts_, :]), [lgB], [m8B])
                    k.ts("dve", nm1[:, :nst], m8[:, :nst, 0], -1.0, None, ALU.mult, None, [m8B], [nm1B])
                    for ts_ in range(nst):
                        k.act(ee[:, ts_, :], lg[:, ts_, :], AF.Exp, [lgB, nm1B], [eeB], bias=nm1[:, ts_:ts_ + 1])
                        k.ts("dve", mk[:, ts_, :], lg[:, ts_, :], m8[:, ts_, 1:2], None, ALU.is_ge, None,
                             [lgB, m8B], [mkB])
                        if moe is not None:
                            k.ts("dve", oh[:, ts_, 0:8], lg[:, ts_, :], m8[:, ts_, 0:1], None, ALU.is_equal, None, [lgB, m8B], [ohB])
                            k.ts("dve", oh[:, ts_, 8:16], lg[:, ts_, :], m8[:, ts_, 1:2], None, ALU.is_equal, None, [lgB, m8B], [ohB])
                    k.tt("dve", ee[:, :nst, :], ee[:, :nst, :], mk[:, :nst, :], ALU.mult, [eeB, mkB], [eeB])
                    k.op("dve", lambda h: h.reduce_sum(out=dn[:, :nst], in_=ee[:, :nst, :], axis=mybir.AxisListType.X),
                         [eeB], [dnB])
                    k.op("dve", lambda h: h.reciprocal(out=dn[:, :nst], in_=dn[:, :nst]), [dnB], [dnB])
                    for ts_ in range(nst):
                        k.ts("dve", ww[:, ts_, :], ee[:, ts_, :], dn[:, ts_:ts_ + 1], None, ALU.mult, None,
                             [eeB, dnB], [wwB])
                        k.op("pe", lambda h: h.transpose(pwt[0:NEXP, ts_ * 128:(ts_ + 1) * 128], ww[:, ts_, :], self.id_f[:]),
                             [wwB, self.idfB], [pwtB])
                    k.copy("dve", wT[:, :W], pwt[0:NEXP, :W], [pwtB], [wTB])
                    k.dma(dr["wexp"][s, :, t0:t0 + W], wT[:, :W], reads=[wTB])
                    if moe is not None:
                        cum, cumB = moe["cum"]
                        for ts_ in range(nst):
                            k.mm(prk[:, 0:8], moe["tris"][0][:], mk[:, ts_, :], True, True, [moe["tris"][1], mkB], [prkB])
                            k.mm(ptot[:, 0:8], self.ones_f[:], mk[:, ts_, :], True, True, [self.onesfB, mkB], [ptotB])
                            k.tt("dve", rk[:, ts_, :], prk[:, 0:8], cum[:], ALU.add, [prkB, cumB], [rkB])
                            k.tt("dve", cum[:], cum[:], ptot[:, 0:8], ALU.add, [cumB, ptotB], [cumB])
                        for (src_, so, do) in ((rk, 0, 16), (rk, 8, 17), (ww, 0, 18), (ww, 8, 19)):
                            k.tt("dve", t8[:, :nst, :], oh[:, :nst, so:so + 8], src_[:, :nst, :], ALU.mult, [ohB, rkB, wwB], [t8B])
                            k.op("dve", lambda h: h.reduce_sum(out=oh[:, :nst, do], in_=t8[:, :nst, :], axis=mybir.AxisListType.X),
                                 [t8B], [ohB])
                        k.dma(dr["rinfo"][s, t0:t0 + W, :].rearrange("(a p) c -> p a c", p=128), oh[:, :nst, :], reads=[ohB])
                        for ts_ in range(nst):
                            for kc in range(KC):
                                k.op("pe", lambda h: h.transpose(ptk[:, :].bitcast(BF16)[:, kc * 128:(kc + 1) * 128],
                                                                 hb[:, kc, ts_ * 128:(ts_ + 1) * 128], self.id_bf[:]),
                                     [hbB, self.idbB], [ptkB])
                            if ts_ % 2 == 0:
                                k.copy("act", htk[:, ts_, :], ptk[:, :].bitcast(BF16)[:, 0:D], [ptkB], [htkB])
                            else:
                                k.copy("dve", htk[:, ts_, :], ptk[:, :].bitcast(BF16)[:, 0:D], [ptkB], [htkB])
                        k.dma(dr["htok"][s, t0:t0 + W, :].rearrange("(a p) d -> p a d", p=128), htk[:, :nst, :], reads=[htkB])

    def ffn_pass(self, l, w1, w3, w2, F, include_ctx=True, expert=None):
        k, dr = self.k, self.dr
        xs, hs = dr["xs"], dr["hs"]
        FC = F // 128
        tl = tiles_for(include_ctx)
        with Pool_(k) as P:
            W1, W1B = P.sb([128, KC, F], BF16, "W1")
            W3, W3B = P.sb([128, KC, F], BF16, "W3")
            W2, W2B = P.sb([128, FC, D], BF16, "W2")
            k.dma(W1[:], w1.rearrange("(kc p) f -> p kc f", p=128), writes=[W1B], q="pool")
            k.dma(W3[:], w3.rearrange("(kc p) f -> p kc f", p=128), writes=[W3B], q="pool")
            k.dma(W2[:], w2.rearrange("(fc p) d -> p fc d", p=128), writes=[W2B], q="pool")
            ht = [P.sb([128, KC, TT], BF16, f"ht{i}") for i in range(2)]
            wb = [P.sb([128, TT], F32, f"wbc{i}") for i in range(2)] if expert is not None else None
            g, gB = P.sb([128, FC, TT], BF16, "g")
            gBs = [Buf(f"g{f}") for f in range(FC)]
            sa = [P.sb([128, TT], F32, f"sa{i}") for i in range(2)]
            yt = [P.sb([128, TT], F32, f"yt{i}") for i in range(2)]
            pa = [P.ps(f"pa{i}") for i in range(2)]
            pb = [P.ps(f"pb{i}") for i in range(2)]
            py = [P.ps(f"py{i}") for i in range(2)]

            def load(i):
                s, t0, W, c = tl[i]
                t, B = ht[i % 2]
                k.dma(t[:, :, :W], hs[s].rearrange("(kc p) t -> p kc t", p=128)[:, :, t0:t0 + W], writes=[B])
                if expert is not None:
                    wt_, wB_ = wb[i % 2]
                    k.dma(wt_[:, :W], dr["wexp"][s, expert:expert + 1, t0:t0 + W].partition_broadcast(128), writes=[wB_])

            load(0)
            cnt = 0
            ycnt = 0
            for i, (s, t0, W, c) in enumerate(tl):
                if i + 1 < len(tl):
                    load(i + 1)
                h, hB = ht[i % 2]
                j = self.jidx(s, c)
                for f in range(FC):
                    a_, aB = pa[cnt % 2]
                    b_, bB = pb[cnt % 2]
                    s_, sB = sa[cnt % 2]
                    cnt += 1
                    for kc in range(KC):
                        k.mm(a_[:, :W], W1[:, kc, f * 128:(f + 1) * 128], h[:, kc, :W], kc == 0, kc == KC - 1, [W1B, hB], [aB])
                    for kc in range(KC):
                        k.mm(b_[:, :W], W3[:, kc, f * 128:(f + 1) * 128], h[:, kc, :W], kc == 0, kc == KC - 1, [W3B, hB], [bB])
                    k.act(s_[:, :W], a_[:, :W], AF.Silu, [aB], [sB])
                    if expert is not None:
                        wt_, wB_ = wb[i % 2]
                        k.tt("pool", s_[:, :W], s_[:, :W], wt_[:, :W], ALU.mult, [sB, wB_], [sB])
                    k.tt("dve", g[:, f, :W], b_[:, :W], s_[:, :W], ALU.mult, [bB, sB], [gBs[f]])
                o = 24 + 16 + 0
                for oc in range(KC):
                    y_, yB = py[ycnt % 2]
                    yo, yoB = yt[ycnt % 2]
                    ycnt += 1
                    for f in range(FC):
                        k.mm(y_[:, :W], W2[:, f, oc * 128:(oc + 1) * 128], g[:, f, :W], f == 0, f == FC - 1, [W2B, gBs[f]], [yB])
                    k.ts("dve", yo[:, :W], y_[:, :W], self.mod[:, l, 40 + oc, j:j + 1], None, ALU.mult, None,
                         [yB, self.modB], [yoB])
                    k.dma(xs[s, oc * 128:(oc + 1) * 128, t0:t0 + W], yo[:, :W], reads=[yoB], q="pool", accum_op=ALU.add)

    def outproj_pass(self, l, src, w, include_ctx, gate_off, extra_scale=None):
        k, dr = self.k, self.dr
        xs = dr["xs"]
        tl = tiles_for(include_ctx)
        with Pool_(k) as P:
            Wo, WoB = P.sb([128, KC, D], BF16, "Wo")
            k.dma(Wo[:], w.rearrange("(kc p) d -> p kc d", p=128), writes=[WoB], q="pool")
            ot = [P.sb([128, KC, TT], BF16, f"ot{i}") for i in range(2)]
            yt = [P.sb([128, TT], F32, f"yt{i}") for i in range(2)]
            py = [P.ps(f"py{i}") for i in range(2)]

            def load(i):
                s, t0, W, c = tl[i]
                t, B = ot[i % 2]
                k.dma(t[:, :, :W], src[s].rearrange("(kc p) t -> p kc t", p=128)[:, :, t0:t0 + W], writes=[B])

            load(0)
            ycnt = 0
            for i, (s, t0, W, c) in enumerate(tl):
                if i + 1 < len(tl):
                    load(i + 1)
                o_, oB = ot[i % 2]
                j = self.jidx(s, c)
                for oc in range(KC):
                    y_, yB = py[ycnt % 2]
                    yo, yoB = yt[ycnt % 2]
                    ycnt += 1
                    for kc in range(KC):
                        k.mm(y_[:, :W], Wo[:, kc, oc * 128:(oc + 1) * 128], o_[:, kc, :W], kc == 0, kc == KC - 1, [WoB, oB], [yB])
                    if extra_scale is None:
                        k.ts("dve", yo[:, :W], y_[:, :W], self.mod[:, l, gate_off + oc, j:j + 1], None, ALU.mult, None,
                             [yB, self.modB], [yoB])
                    else:
                        est, esB = extra_scale
                        k.ts("dve", yo[:, :W], y_[:, :W], self.mod[:, l, gate_off + oc, j:j + 1], est[:, oc:oc + 1],
                             ALU.mult, ALU.mult, [yB, self.modB, esB], [yoB])
                    k.dma(xs[s, oc * 128:(oc + 1) * 128, t0:t0 + W], yo[:, :W], reads=[yoB], q="pool", accum_op=ALU.add)

    def na_qkv_pass(self, wqkv):
        k, dr = self.k, self.dr
        hs, qs, ks, vs = dr["hs"], dr["qs"], dr["ks"], dr["vs"]
        tl = tiles_for(True)
        with Pool_(k) as P:
            Wq, WqB = P.sb([128, KC, 3 * D], BF16, "Wqkv")
            src = wqkv.rearrange("(kc p) n -> p kc n", p=128)
            for i in range(3):
                k.dma(Wq[:, :, i * D:(i + 1) * D], src[:, :, i * D:(i + 1) * D], writes=[WqB], q="pool")
            ht = [P.sb([128, KC, TT], BF16, f"ht{i}") for i in range(2)]
            qk = [P.sb([128, 2 * KC, TT], BF16, f"qk{i}") for i in range(2)]
            vt = [P.sb([128, 4, D], BF16, f"vt{i}") for i in range(2)]
            pp = [P.ps(f"pp{i}") for i in range(4)]

            def load(i):
                s, t0, W, c = tl[i]
                t, B = ht[i % 2]
                k.dma(t[:, :, :W], hs[s].rearrange("(kc p) t -> p kc t", p=128)[:, :, t0:t0 + W], writes=[B])

            load(0)
            cnt = 0
            for i, (s, t0, W, c) in enumerate(tl):
                if i + 1 < len(tl):
                    load(i + 1)
                h, hB = ht[i % 2]
                q_, qB = qk[i % 2]
                v_, vB = vt[i % 2]
                for oc in range(2 * KC):
                    p_, pB = pp[cnt % 4]
                    for kc in range(KC):
                        k.mm(p_[:, :W], Wq[:, kc, oc * 128:(oc + 1) * 128], h[:, kc, :W], kc == 0, kc == KC - 1, [WqB, hB], [pB])
                    if cnt % 2 == 0:
                        k.act(q_[:, oc, :W], p_[:, :W], AF.Copy, [pB], [qB], scale=(0.125 if oc < KC else 1.0))
                    else:
                        k.ts("dve", q_[:, oc, :W], p_[:, :W], (0.125 if oc < KC else 1.0), None, ALU.mult, None, [pB], [qB])
                    cnt += 1
                for ts_ in range(W // 128):
                    for hf_ in range(2):
                        p_, pB = pp[cnt % 4]
                        for kc in range(KC):
                            k.mm(p_[:, :], h[:, kc, ts_ * 128:(ts_ + 1) * 128], Wq[:, kc, 2 * D + hf_ * 512:2 * D + (hf_ + 1) * 512],
                                 kc == 0, kc == KC - 1, [WqB, hB], [pB])
                        if cnt % 2 == 0:
                            k.act(v_[:, ts_, hf_ * 512:(hf_ + 1) * 512], p_[:, :], AF.Copy, [pB], [vB])
                        else:
                            k.copy("dve", v_[:, ts_, hf_ * 512:(hf_ + 1) * 512], p_[:, :], [pB], [vB])
                        cnt += 1
                k.dma(qs[s].rearrange("(kc p) t -> p kc t", p=128)[:, :, t0:t0 + W], q_[:, 0:KC, :W], reads=[qB])
                k.dma(ks[s].rearrange("(kc p) t -> p kc t", p=128)[:, :, t0:t0 + W], q_[:, KC:2 * KC, :W], reads=[qB])
                k.dma(vs[s, t0:t0 + W, :].rearrange("(a p) f -> p a f", p=128), v_[:, :W // 128, :], reads=[vB])

    def na_attn_pass(self, tab, need_ctx):
        k, dr = self.k, self.dr
        qs, ks, vs, os_ = dr["qs"], dr["ks"], dr["vs"], dr["os"]
        NCH = TOK // 128
        with Pool_(k) as P:
            qT, qTB = P.sb([128, TOK], BF16, "qT")
            kT, kTB = P.sb([128, TOK], BF16, "kT")
            vv, vvB = P.sb([128, NCH, 128], BF16, "vv")
            tb, tbB = P.sb([128, 2, 2, 22, 64], F32, "tb")
            negt, negB = P.sb([128, 256], F32, "negt")
            k.memset("pool", negt[:], NEG, [negB])
            sbm = [P.sb([128, TT], F32, f"sbm{i}") for i in range(2)]
            pt = [P.sb([128, TT], BF16, f"pt{i}") for i in range(3)]
            rd, rdB = P.sb([128, TT], F32, "rd")
            ot = [P.sb([128, TT], BF16, f"ot{i}") for i in range(2)]
            pS = [P.ps(f"pS{i}") for i in range(3)]
            pO = [P.ps(f"pO{i}") for i in range(2)]
            pD = [P.ps(f"pD{i}") for i in range(2)]
            scnt = 0
            tcnt = 0
            for s in range(NS):
                for hp in range(8):
                    k.dma(qT[:], qs[s, hp * 128:(hp + 1) * 128, :], writes=[qTB])
                    k.dma(kT[:], ks[s, hp * 128:(hp + 1) * 128, :], writes=[kTB])
                    k.dma(vv[:], vs[s, :, hp * 128:(hp + 1) * 128].rearrange("(a p) f -> p a f", p=128), writes=[vvB])
                    if s == 0 or True:
                        k.dma(tb[:], tab[hp], writes=[tbB])
                    qtiles = ([(0, NCTX, -1)] if need_ctx else []) + [(NCTX + ti * TT, TT, ti) for ti in range(8)]
                    for (t0, W, ti) in qtiles:
                        O_, OB = pO[tcnt % 2]
                        D_, DB = pD[tcnt % 2]
                        o_, oB = ot[tcnt % 2]
                        tcnt += 1
                        if ti < 0:
                            chunks = [(0, None), (1, None)]
                        else:
                            lo, hi = max(0, 4 * ti - 2), min(31, 4 * ti + 5)
                            chunks = [(2 + c, c) for c in range(lo, hi + 1)] + [(0, None), (1, None)]
                        for hd in range(2):
                            pb_ = 64 * hd
                            for ci, (kch, c) in enumerate(chunks):
                                S_, SB = pS[scnt % 3]
                                p_, pB = pt[scnt % 3]
                                m_, mB = sbm[scnt % 2]
                                scnt += 1
                                k.mm(S_[:, :W], kT[pb_:pb_ + 64, kch * 128:(kch + 1) * 128], qT[pb_:pb_ + 64, t0:t0 + W],
                                     True, True, [kTB, qTB], [SB])
                                if c is None:
                                    k.act(p_[:, :W], S_[:, :W], AF.Exp, [SB], [pB])
                                else:
                                    R = 8 * ti
                                    J0 = 10 - 2 * c + R
                                    if ti == 0:
                                        segs = [(0, 4, "U" if c <= 3 else "N"), (4, 8, "T")]
                                    elif ti == 7:
                                        segs = [(0, 5, "T"), (5, 8, "U" if c >= 28 else "N")]
                                    else:
                                        segs = [(0, 8, "T")]
                                    for (b0, b1, kind) in segs:
                                        c0, c1 = b0 * 64, b1 * 64
                                        if kind == "N":
                                            in1 = negt[:, 0:c1 - c0]
                                            rb = [negB]
                                        else:
                                            tix = 0 if kind == "T" else 1
                                            in1 = tb[:, tix, hd, J0 + b0:J0 + b1, :].rearrange("p a b -> p (a b)")
                                            rb = [tbB]
                                        k.tt("dve", m_[:, c0:c1], S_[:, c0:c1], in1, ALU.add, [SB] + rb, [mB])
                                    k.act(p_[:, :W], m_[:, :W], AF.Exp, [mB], [pB])
                                first, last = ci == 0, ci == len(chunks) - 1
                                k.mm(O_[pb_:pb_ + 64, :W], vv[:, kch, pb_:pb_ + 64], p_[:, :W], first, last, [vvB, pB], [OB])
                                k.mm(D_[pb_:pb_ + 64, :W], self.ones_bf[:, 0:64], p_[:, :W], first, last, [self.onesB, pB], [DB])
                        k.op("dve", lambda h: h.reciprocal(out=rd[:, :W], in_=D_[:, :W]), [DB], [rdB])
                        k.tt("dve", o_[:, :W], O_[:, :W], rd[:, :W], ALU.mult, [OB, rdB], [oB])
                        k.dma(os_[s, hp * 128:(hp + 1) * 128, t0:t0 + W], o_[:, :W], reads=[oB])

    def pool_pass(self, l, pw, include_ctx=True):
        k, dr = self.k, self.dr
        xs, hs = dr["xs"], dr["hs"]
        tl = tiles_for(include_ctx)
        PADW = 80
        with Pool_(k) as P:
            Wp, WpB = P.sb([128, 4, 2, 256], BF16, "Wp")
            k.dma(Wp[:], pw.rearrange("g (kc p) f -> p g kc f", p=128), writes=[WpB], q="pool")
            ls, lsB = P.sb([128, KC], F32, "ls")
            k.dma(ls[:], dr["pool_scale"][:, :], writes=[lsB])
            ic, icB = P.sb([128, 4, 2, 256], F32, "ic")
            k.dma(ic[:], dr["pool_ic"][:, :, :, :], writes=[icB])
            ht = [P.sb([128, KC, TT], BF16, f"ht{i}") for i in range(2)]
            lv = [P.sb([128, 8 * PADW], F32, f"lv{i}") for i in range(3)]
            lvc = [P.sb([128, 272], F32, f"lvc{i}") for i in range(3)]
            pl, plB = P.sb([128, KC, TT], BF16, "pl")
            plBs = [Buf(f"pl{i}") for i in range(KC)]
            mn, mnB = P.sb([128, TT], F32, "mn")
            yt = [P.sb([128, TT], F32, f"yt{i}") for i in range(2)]
            py = [P.ps(f"py{i}") for i in range(2)]
            for t_, B_ in lv + lvc:
                k.memset("pool", t_[:], 0.0, [B_])

            def load(i):
                s, t0, W, c = tl[i]
                t, B = ht[i % 2]
                k.dma(t[:, :, :W], hs[s].rearrange("(kc p) t -> p kc t", p=128)[:, :, t0:t0 + W], writes=[B])

            load(0)
            ycnt = 0
            for i, (s, t0, W, c) in enumerate(tl):
                if i + 1 < len(tl):
                    load(i + 1)
                h, hB = ht[i % 2]
                j = self.jidx(s, c)
                rows, rl, pw_, ri = (1, 256, 272, 1) if c else (8, 64, PADW, 0)

                def view(t, off, n=rl):
                    return t[:, 0:rows * pw_].rearrange("p (r w) -> p r w", w=pw_)[:, :, 8 + off:8 + off + n]

                for kc in range(KC):
                    gi = kc // 2
                    win = (2, 4, 8, 16)[gi]
                    a_, aB = (lvc if c else lv)[0]
                    b_, bB = (lvc if c else lv)[1]
                    c_, cB = (lvc if c else lv)[2]
                    hv = h[:, kc, :W].rearrange("p (r w) -> p r w", w=rl)
                    k.copy("pool", view(a_, 0), hv, [hB], [aB])
                    n2 = rl + 14
                    k.tt("pool", view(b_, -7, n2), view(a_, -8, n2), view(a_, -7, n2), ALU.add, [aB], [bB])
                    cur, curB = b_, bB
                    if win >= 4:
                        n4 = rl + 12
                        k.tt("pool", view(c_, -6, n4), view(b_, -7, n4), view(b_, -5, n4), ALU.add, [bB], [cB])
                        cur, curB = c_, cB
                    if win >= 8:
                        n8 = rl + 8
                        k.tt("dve", view(b_, -4, n8), view(c_, -6, n8), view(c_, -2, n8), ALU.add, [cB], [bB])
                        cur, curB = b_, bB
                    if win >= 16:
                        k.tt("dve", view(c_, 0), view(b_, -4), view(b_, 4), ALU.add, [bB], [cB])
                        cur, curB = c_, cB
                    mv = mn[:, :W].rearrange("p (r w) -> p r w", w=rl)
                    icv = ic[:, gi, ri, 0:rl]
                    for r in range(rows):
                        k.tt("dve", mv[:, r, :], view(cur, 0)[:, r, :], icv, ALU.mult, [curB, icB], [mnB])
                    k.tt("dve", pl[:, kc, :W], mn[:, :W], h[:, kc, :W], ALU.subtract, [mnB, hB], [plBs[kc]])
                for oc in range(KC):
                    gi = oc // 2
                    y_, yB = py[ycnt % 2]
                    yo, yoB = yt[ycnt % 2]
                    ycnt += 1
                    for kk in range(2):
                        k.mm(y_[:, :W], Wp[:, gi, kk, (oc % 2) * 128:(oc % 2 + 1) * 128], pl[:, 2 * gi + kk, :W], kk == 0, kk == 1,
                             [WpB, plBs[2 * gi + kk]], [yB])
                    k.ts("dve", yo[:, :W], y_[:, :W], self.mod[:, l, 16 + oc, j:j + 1], ls[:, oc:oc + 1], ALU.mult, ALU.mult,
                         [yB, self.modB, lsB], [yoB])
                    k.dma(xs[s, oc * 128:(oc + 1) * 128, t0:t0 + W], yo[:, :W], reads=[yoB], q="pool", accum_op=ALU.add)


BLK = 512
GF = 896
NG = DFE // GF
I32 = mybir.dt.int32


def moe_layer(pg, l, f, include_ctx):
    k, dr = pg.k, pg.dr
    tl = tiles_for(include_ctx)
    subt = [(s, t0 + a * 128, c) for (s, t0, W, c) in tl for a in range(W // 128)]
    NSUB = len(subt)
    NB = NSUB * 2 * 128 // BLK + NEXP
    w1f = dr[f"moe_w1_{f}"].rearrange("e k (g c) -> (e k g) c", c=GF)
    w3f = dr[f"moe_w3_{f}"].rearrange("e k (g c) -> (e k g) c", c=GF)
    w2f = dr[f"moe_w2_{f}"].rearrange("e f d -> (e f) d")
    hbuf, obuf = dr["hbuf"], dr["obuf"]
    with Pool_(k) as PM:
        cum = PM.sb([128, NEXP], F32, "cum")
        tris = PM.sb([128, 128], F32, "tris")
        starts = PM.sb([128, NEXP], F32, "starts")
        posall = PM.sb([128, NSUB, 2], I32, "posall")
        pwall = PM.sb([128, NSUB, 2], F32, "pwall")
        widx1 = PM.sb([128, NB, NG, KC], I32, "widx1")
        widx2 = PM.sb([128, NB, 28], I32, "widx2")
        k.memset("pool", cum[0][:], 0.0, [cum[1]])
        k.dma(tris[0][:], dr["tris"][:, :], writes=[tris[1]])
        pg.norm_pass(l, 1, include_ctx=include_ctx, router=dr["moe_router"][f], moe={"cum": cum, "tris": tris})
        with Pool_(k) as P:
            thr, thrB = P.sb([128, 48], F32, "thr"); k.dma(thr[:], dr["thr48"][:, :], writes=[thrB])
            io, ioB = P.sb([128, 48], F32, "io"); k.dma(io[:], dr["iota48"][:, :], writes=[ioB])
            pk1, pk1B = P.sb([128, KC], F32, "pk1"); k.dma(pk1[:], dr["pk1"][:, :], writes=[pk1B])
            pk2, pk2B = P.sb([128, 28], F32, "pk2"); k.dma(pk2[:], dr["pk2"][:, :], writes=[pk2B])
            tmp, tmpB = P.sb([128, 48], F32, "tmp")
            nblk, nblkB = P.sb([128, NEXP], F32, "nblk")
            sblk, sblkB = P.sb([128, NEXP], F32, "sblk")
            eb, ebB = P.sb([128, 48], F32, "eb")
            w1f_, w1fB = P.sb([128, NB, NG, KC], F32, "w1f")
            w2f_, w2fB = P.sb([128, NB, 28], F32, "w2f")
            for e in range(NEXP):
                k.ts("dve", tmp[:], thr[:], cum[0][:, e:e + 1], None, ALU.is_lt, None, [thrB, cum[1]], [tmpB])
                k.op("dve", lambda h: h.reduce_sum(out=nblk[:, e:e + 1], in_=tmp[:], axis=mybir.AxisListType.X), [tmpB], [nblkB])
            k.memset("pool", sblk[:], 0.0, [sblkB])
            for e in range(1, NEXP):
                k.tt("dve", sblk[:, e:e + 1], sblk[:, e - 1:e], nblk[:, e - 1:e], ALU.add, [sblkB, nblkB], [sblkB])
            k.ts("dve", starts[0][:], sblk[:], float(BLK), None, ALU.mult, None, [sblkB], [starts[1]])
            k.memset("pool", eb[:], -1.0, [ebB])
            for e in range(NEXP):
                k.ts("dve", tmp[:], io[:], sblk[:, e:e + 1], None, ALU.is_ge, None, [ioB, sblkB], [tmpB])
                k.tt("dve", eb[:], eb[:], tmp[:], ALU.add, [ebB, tmpB], [ebB])
            e4, e4B = P.sb([128, 48], F32, "e4")
            e35, e35B = P.sb([128, 48], F32, "e35")
            k.ts("dve", e4[:], eb[:], 4096.0, None, ALU.mult, None, [ebB], [e4B])
            k.ts("dve", e35[:], eb[:], float(DFE), None, ALU.mult, None, [ebB], [e35B])
            for g in range(NG):
                for kc in range(KC):
                    k.ts("dve", w1f_[:, :, g, kc], e4[:, :NB], pk1[:, kc:kc + 1], None, ALU.add, None, [e4B, pk1B], [w1fB])
            for g in range(1, NG):
                k.ts("dve", w1f_[:, :, g, :], w1f_[:, :, g, :], float(g), None, ALU.add, None, [w1fB], [w1fB])
            for fc in range(28):
                k.ts("dve", w2f_[:, :, fc], e35[:, :NB], pk2[:, fc:fc + 1], None, ALU.add, None, [e35B, pk2B], [w2fB])
            k.copy("dve", widx1[0][:], w1f_[:], [w1fB], [widx1[1]])
            k.copy("dve", widx2[0][:], w2f_[:], [w2fB], [widx2[1]])
            if "widx_dbg" in dr:
                k.dma(dr["widx_dbg"][:, 0:NB * NG * KC], widx1[0][:].rearrange("p a b c -> p (a b c)"), reads=[widx1[1]])
        with Pool_(k) as P:
            z, zB = P.sb([128, 4, D], BF16, "z")
            k.memset("pool", z[:], 0.0, [zB])
            for b in range(NB):
                k.dma(hbuf[b * BLK:(b + 1) * BLK, :].rearrange("(a p) d -> p a d", p=128), z[:], reads=[zB])
            k.barrier()
            ht = [P.sb([128, D], BF16, f"sht{i}") for i in range(3)]
            inf = [P.sb([128, 24], F32, f"inf{i}") for i in range(3)]
            t8, t8B = P.sb([128, 2, NEXP], F32, "t8")
            pf, pfB = P.sb([128, 2], F32, "pf")

            def loadi(i):
                s, tk, c = subt[i]
                k.dma(inf[i % 3][0][:], dr["rinfo"][s, tk:tk + 128, :], writes=[inf[i % 3][1]])

            def loadh(i):
                s, tk, c = subt[i]
                k.dma(ht[i % 3][0][:], dr["htok"][s, tk:tk + 128, :], writes=[ht[i % 3][1]])

            loadi(0)
            for i in range(NSUB):
                if i + 1 < NSUB:
                    loadi(i + 1)
                n_, nB_ = inf[i % 3]
                k.tt("dve", t8[:, 0, :], n_[:, 0:8], starts[0][:], ALU.mult, [nB_, starts[1]], [t8B])
                k.tt("dve", t8[:, 1, :], n_[:, 8:16], starts[0][:], ALU.mult, [nB_, starts[1]], [t8B])
                k.op("dve", lambda h: h.reduce_sum(out=pf[:, 0:2], in_=t8[:, :, :], axis=mybir.AxisListType.X), [t8B], [pfB])
                k.tt("dve", pf[:], pf[:], n_[:, 16:18], ALU.add, [pfB, nB_], [pfB])
                k.copy("dve", posall[0][:, i, :], pf[:], [pfB], [posall[1]])
                k.copy("dve", pwall[0][:, i, :], n_[:, 18:20], [nB_], [pwall[1]])
            k.barrier()
            loadh(0)
            for i in range(NSUB):
                if i + 1 < NSUB:
                    loadh(i + 1)
                h_, hB_ = ht[i % 3]
                for j in range(2):
                    k.idma(hbuf[:, :], h_[:], posall[0][:, i, j:j + 1], gather=False, reads=[hB_, posall[1]])
        with Pool_(k) as P:
            Wg = []
            for i in range(2):
                Wg.append(((P.sb([128, KC, GF], BF16, f"W1g{i}")[0], [Buf() for _ in range(KC)]),
                           (P.sb([128, KC, GF], BF16, f"W3g{i}")[0], [Buf() for _ in range(KC)]),
                           (P.sb([128, 7, D], BF16, f"W2g{i}")[0], [Buf() for _ in range(7)])))
            hblk = [P.sb([128, 4, D], BF16, f"hblk{i}") for i in range(1)] * 2
            hT = [P.sb([128, KC, BLK], BF16, f"hT{i}") for i in range(2)]
            gT = [P.sb([128, 7, BLK], BF16, f"gT{i}") for i in range(2)]
            yacc = [P.sb([128, 4, D], F32, f"yacc{i}") for i in range(1)] * 2
            sa = [P.sb([128, BLK], F32, f"sa{i}") for i in range(2)]
            pa = [P.ps(f"pa{i}") for i in range(2)]
            pb = [P.ps(f"pb{i}") for i in range(2)]
            py = [P.ps(f"py{i}") for i in range(2)]
            pth, pthB = P.ps("pth")
            groups = [(b, g) for b in range(NB) for g in range(NG)]

            def wload(gi):
                b, g = groups[gi]
                (W1, W1B), (W3, W3B), (W2, W2B) = Wg[gi % 2]
                for kc in range(KC):
                    k.idma(W1[:, kc, :], w1f[:, :], widx1[0][:, b, g, kc:kc + 1], gather=True, reads=[widx1[1]], writes=[W1B[kc]])
                    k.idma(W3[:, kc, :], w3f[:, :], widx1[0][:, b, g, kc:kc + 1], gather=True, reads=[widx1[1]], writes=[W3B[kc]])
                for j in range(7):
                    k.idma(W2[:, j, :], w2f[:, :], widx2[0][:, b, g * 7 + j:g * 7 + j + 1], gather=True, reads=[widx2[1]], writes=[W2B[j]])

            def hload(b):
                k.dma(hblk[b % 2][0][:], hbuf[b * BLK:(b + 1) * BLK, :].rearrange("(a p) d -> p a d", p=128), writes=[hblk[b % 2][1]])

            wload(0)
            hload(0)
            cnt = 0
            ycnt = 0
            for gi, (b, g) in enumerate(groups):
                if gi + 1 < len(groups):
                    wload(gi + 1)
                (W1, W1B), (W3, W3B), (W2, W2B) = Wg[gi % 2]
                h_, hB_ = hT[b % 2]
                ya, yaB = yacc[b % 2]
                if g == 0:
                    hb_, hbB_ = hblk[b % 2]
                    for kc in range(KC):
                        for a in range(4):
                            k.op("pe", lambda h: h.transpose(pth[:, 0:256].bitcast(BF16)[:, a * 128:(a + 1) * 128],
                                                             hb_[:, a, kc * 128:(kc + 1) * 128], pg.id_bf[:]), [hbB_, pg.idbB], [pthB])
                        if kc % 2 == 0:
                            k.copy("act", h_[:, kc, :], pth[:, 0:256].bitcast(BF16)[:, 0:BLK], [pthB], [hB_])
                        else:
                            k.copy("dve", h_[:, kc, :], pth[:, 0:256].bitcast(BF16)[:, 0:BLK], [pthB], [hB_])
                    if b + 1 < NB:
                        hload(b + 1)
                g_, gB_ = gT[gi % 2]
                for j in range(7):
                    a_, aB = pa[cnt % 2]; b_, bB = pb[cnt % 2]; s_, sB = sa[cnt % 2]
                    cnt += 1
                    for kc in range(KC):
                        k.mm(a_[:, :], W1[:, kc, j * 128:(j + 1) * 128], h_[:, kc, :], kc == 0, kc == KC - 1, [W1B[kc], hB_], [aB])
                    for kc in range(KC):
                        k.mm(b_[:, :], W3[:, kc, j * 128:(j + 1) * 128], h_[:, kc, :], kc == 0, kc == KC - 1, [W3B[kc], hB_], [bB])
                    k.act(s_[:], a_[:, :], AF.Silu, [aB], [sB])
                    k.tt("dve", g_[:, j, :], b_[:, :], s_[:], ALU.mult, [bB, sB], [gB_])
                for a in range(4):
                    for hf_ in range(2):
                        y_, yB = py[ycnt % 2]
                        ycnt += 1
                        for j in range(7):
                            k.mm(y_[:, :], g_[:, j, a * 128:(a + 1) * 128], W2[:, j, hf_ * 512:(hf_ + 1) * 512], j == 0, j == 6, [gB_, W2B[j]], [yB])
                        dst = ya[:, a, hf_ * 512:(hf_ + 1) * 512]
                        if g == 0:
                            k.copy("act", dst, y_[:, :], [yB], [yaB])
                        else:
                            k.tt("dve", dst, dst, y_[:, :], ALU.add, [yaB, yB], [yaB])
                if g == NG - 1:
                    k.dma(obuf[b * BLK:(b + 1) * BLK, :].rearrange("(a p) d -> p a d", p=128), ya[:], reads=[yaB])
        with Pool_(k) as P:
            r1 = [P.sb([128, D], F32, f"r1_{i}") for i in range(2)]
            r2 = [P.sb([128, D], F32, f"r2_{i}") for i in range(2)]
            yo = [P.sb([128, KC, 128], F32, f"yo{i}") for i in range(2)]
            pt = [P.ps(f"pt{i}") for i in range(2)]

            def gload(i):
                k.idma(r1[i % 2][0][:], obuf[:, :], posall[0][:, i, 0:1], gather=True, reads=[posall[1]], writes=[r1[i % 2][1]])
                k.idma(r2[i % 2][0][:], obuf[:, :], posall[0][:, i, 1:2], gather=True, reads=[posall[1]], writes=[r2[i % 2][1]])

            gload(0)
            for i, (s, tk, c) in enumerate(subt):
                if i + 1 < NSUB:
                    gload(i + 1)
                a_, aB = r1[i % 2]; b_, bB = r2[i % 2]; o_, oB = yo[i % 2]
                j = pg.jidx(s, c)
                k.ts("dve", a_[:], a_[:], pwall[0][:, i, 0:1], None, ALU.mult, None, [aB, pwall[1]], [aB])
                k.stt(a_[:], b_[:], pwall[0][:, i, 1:2], a_[:], ALU.mult, ALU.add, [bB, pwall[1], aB], [aB])
                for hf_ in range(2):
                    p_, pB = pt[hf_]
                    for q in range(4):
                        oc = hf_ * 4 + q
                        k.op("pe", lambda h: h.transpose(p_[:, q * 128:(q + 1) * 128], a_[:, oc * 128:(oc + 1) * 128], pg.id_f[:]),
                             [aB, pg.idfB], [pB])
                    for q in range(4):
                        oc = hf_ * 4 + q
                        if q % 2 == 0:
                            k.act(o_[:, oc, :], p_[:, q * 128:(q + 1) * 128], AF.Identity, [pB, pg.modB], [oB], scale=pg.mod[:, l, 40 + oc, j:j + 1])
                        else:
                            k.ts("dve", o_[:, oc, :], p_[:, q * 128:(q + 1) * 128], pg.mod[:, l, 40 + oc, j:j + 1], None, ALU.mult, None,
                                 [pB, pg.modB], [oB])
                k.dma(dr["xs"][s].rearrange("(kc p) t -> p kc t", p=128)[:, :, tk:tk + 128], o_[:], reads=[oB], q="pool", accum_op=ALU.add)


def build(stop_after=99, debug=False):
    nc = bass.Bass("TRN2", target_bir_lowering=False)
    dr = {}

    def inp(name, shape, dt=F32):
        dr[name] = nc.dram_tensor(name, list(shape), dt, kind="ExternalInput").ap()

    def scr(name, shape, dt, out=False):
        dr[name] = nc.dram_tensor(name, list(shape), dt, kind="ExternalOutput" if out else "Internal").ap()

    inp("xin", [NS, D, TOK]); inp("cvec", [128, KC, 3]); inp("ada_w", [DEPTH, D, 6 * D]); inp("ada_b", [128, DEPTH, 48])
    inp("norm_g", [128, DEPTH, 2, KC]); inp("final_g", [128, KC])
    inp("ones_bf", [128, 128], BF16); inp("id_bf", [128, 128], BF16); inp("id_f", [128, 128]); inp("ones_f", [128, 128])
    inp("na_w_qkv", [2, D, 3 * D]); inp("na_w_o", [2, D, D]); inp("na_tab", [2, 8, 128, 2, 2, 22, 64])
    inp("ffn_w1", [2, D, DFF]); inp("ffn_w3", [2, D, DFF]); inp("ffn_w2", [2, DFF, D])
    inp("moe_router", [2, 128, KC, NEXP])
    for f_ in range(2):
        inp(f"moe_w1_{f_}", [NEXP, D, DFE]); inp(f"moe_w3_{f_}", [NEXP, D, DFE]); inp(f"moe_w2_{f_}", [NEXP, DFE, D])
    inp("tris", [128, 128]); inp("thr48", [128, 48]); inp("iota48", [128, 48]); inp("pk1", [128, KC]); inp("pk2", [128, 28])
    inp("pool_w", [4, 256, 256]); inp("pool_scale", [128, KC]); inp("pool_ic", [128, 4, 2, 256])
    gdn_inputs(inp)
    scr("xs", [NS, D, TOK], F32, out=debug)
    scr("hs", [NS, D, TOK], BF16)
    scr("qs", [NS, D, TOK], BF16); scr("ks", [NS, D, TOK], BF16); scr("vs", [NS, TOK, D], BF16); scr("os", [NS, D, TOK], BF16)
    scr("wexp", [NS, NEXP, TOK], F32)
    scr("htok", [NS, TOK, D], BF16, out=debug); scr("rinfo", [NS, TOK, 24], F32, out=debug)
    scr("hbuf", [42 * BLK, D], BF16, out=debug); scr("obuf", [42 * BLK, D], F32, out=debug)

    gdn_scratch(scr)
    scr("out", [NS, D, NLAT], F32, out=True)

    with ExitStack() as es:
        k = K(nc, es)
        pg = Prog(nc, k, dr, stop_after)
        with Pool_(k) as PC:
            pg.setup_consts(PC)
            for s in range(NS):
                for kc in range(KC):
                    k.dma(dr["xs"][s, kc * 128:(kc + 1) * 128, :], dr["xin"][s, kc * 128:(kc + 1) * 128, :])
            pg.adaln()
            k.barrier()
            step = 0
            for l in range(DEPTH):
                last = l == DEPTH - 1
                if step >= stop_after:
                    break
                pg.norm_pass(l, 0, include_ctx=True)
                kind = l % 3
                jx = l // 3
                if kind == 0:
                    pg.na_qkv_pass(dr["na_w_qkv"][jx])
                    pg.na_attn_pass(dr["na_tab"][jx], need_ctx=not last)
                    pg.outproj_pass(l, dr["os"], dr["na_w_o"][jx], include_ctx=not last, gate_off=16)
                elif kind == 1:
                    gdn_mixer(pg, l, need_ctx=not last)
                else:
                    pg.pool_pass(l, dr["pool_w"], include_ctx=not last)
                step += 1
                if step >= stop_after:
                    break
                f = l // 2
                if l % 2 == 0:
                    pg.norm_pass(l, 1, include_ctx=not last)
                    H = DFF // 2
                    for hh in range(2):
                        pg.ffn_pass(l, dr["ffn_w1"][f][:, hh * H:(hh + 1) * H], dr["ffn_w3"][f][:, hh * H:(hh + 1) * H],
                                    dr["ffn_w2"][f][hh * H:(hh + 1) * H, :], H, include_ctx=not last)
                else:
                    moe_layer(pg, l, f, include_ctx=not last)
                step += 1
            pg.norm_pass(0, 0, include_ctx=False, final=True)
            k.barrier()
    return nc


GDN_IN = 4128
NCHK = TOK // 128


def gdn_inputs(inp):
    inp("gdn_w_in", [D, GDN_IN]); inp("gdn_w_o", [D, D]); inp("gdn_cw", [128, 24, 4])
    inp("gdn_dtb", [128, 16]); inp("gdn_alog", [128, 16]); inp("gdn_ng", [128, 1])
    inp("gdn_tri", [128, 2, 128]); inp("gdn_gm", [128, 2, 3, 128]); inp("gdn_ms", [128, 7, 128], BF16)


def gdn_scratch(scr):
    scr("pj", [NS, 4 * D, TOK], BF16)
    scr("gq", [NS, D, TOK], BF16); scr("gk", [NS, D, TOK], BF16); scr("gv", [NS, D, TOK], BF16)
    scr("gt", [NS, TOK, 48], F32)
    scr("gcs", [NS, 16, TOK], F32)
    scr("gbs", [NS, 16, TOK], F32)


def gdn_proj_pass(pg):
    k, dr = pg.k, pg.dr
    hs, pj = dr["hs"], dr["pj"]
    tl = tiles_for(True)
    with Pool_(k) as P:
        Wi, WiB = P.sb([128, KC, GDN_IN], BF16, "Wi")
        src = dr["gdn_w_in"].rearrange("(kc p) n -> p kc n", p=128)
        for i in range(4):
            k.dma(Wi[:, :, i * 1032:(i + 1) * 1032], src[:, :, i * 1032:(i + 1) * 1032], writes=[WiB], q="pool")
        dtb, dtbB = P.sb([128, 16], F32, "dtb"); k.dma(dtb[:], dr["gdn_dtb"][:, :], writes=[dtbB])
        nA, nAB = P.sb([128, 16], F32, "nA"); k.dma(nA[:], dr["gdn_alog"][:, :], writes=[nAB])
        k.act(nA[:], nA[:], AF.Exp, [nAB], [nAB])
        k.ts("dve", nA[:], nA[:], -1.0, None, ALU.mult, None, [nAB], [nAB])
        tri, triB = P.sb([128, 2, 128], F32, "tri"); k.dma(tri[:], dr["gdn_tri"][:, :, :], writes=[triB])
        ht = [P.sb([128, KC, TT], BF16, f"ht{i}") for i in range(2)]
        pt = [P.sb([128, 32, TT], BF16, f"pjt{i}") for i in range(1)]
        gtt, gttB = P.sb([128, 4, 48], F32, "gtt")
        tmpa, tmpaB = P.sb([128, 16], F32, "tmpa")
        gcT, gcTB = P.sb([16, TT], F32, "gcT")
        bT, bTB = P.sb([16, TT], F32, "bT")
        pp = [P.ps(f"pp{i}") for i in range(4)]
        pab, pabB = P.ps("pab")
        pgc, pgcB = P.ps("pgc")
        ptr, ptrB = P.ps("ptr")
        pbf, pbfB = P.ps("pbf")

        def load(i):
            s, t0, W, c = tl[i]
            t, B = ht[i % 2]
            k.dma(t[:, :, :W], hs[s].rearrange("(kc p) t -> p kc t", p=128)[:, :, t0:t0 + W], writes=[B])

        load(0)
        cnt = 0
        for i, (s, t0, W, c) in enumerate(tl):
            if i + 1 < len(tl):
                load(i + 1)
            h, hB = ht[i % 2]
            p_, pjB = pt[0]
            for oc in range(32):
                q_, qB = pp[cnt % 4]
                for kc in range(KC):
                    k.mm(q_[:, :W], Wi[:, kc, oc * 128:(oc + 1) * 128], h[:, kc, :W], kc == 0, kc == KC - 1, [WiB, hB], [qB])
                if cnt % 2 == 0:
                    k.act(p_[:, oc, :W], q_[:, :W], AF.Copy, [qB], [pjB])
                else:
                    k.copy("dve", p_[:, oc, :W], q_[:, :W], [qB], [pjB])
                cnt += 1
            k.dma(pj[s].rearrange("(oc p) t -> p oc t", p=128)[:, :, t0:t0 + W], p_[:, :, :W], reads=[pjB])
            for kc in range(KC):
                k.mm(pbf[0:16, :W], Wi[:, kc, 4096 + 16:4096 + 32], h[:, kc, :W], kc == 0, kc == KC - 1, [WiB, hB], [pbfB])
            k.act(bT[:, :W], pbf[0:16, :W], AF.Sigmoid, [pbfB], [bTB])
            k.dma(dr["gbs"][s, :, t0:t0 + W], bT[:, :W], reads=[bTB])
            nst = W // 128
            for ts_ in range(nst):
                for kc in range(KC):
                    k.mm(pab[:, ts_ * 32:(ts_ + 1) * 32], h[:, kc, ts_ * 128:(ts_ + 1) * 128], Wi[:, kc, 4096:4128],
                         kc == 0, kc == KC - 1, [WiB, hB], [pabB])
            for ts_ in range(nst):
                k.tt("dve", tmpa[:], pab[:, ts_ * 32:ts_ * 32 + 16], dtb[:], ALU.add, [pabB, dtbB], [tmpaB])
                k.act(tmpa[:], tmpa[:], AF.Exp, [tmpaB], [tmpaB])
                k.act(tmpa[:], tmpa[:], AF.Ln, [tmpaB], [tmpaB], bias=1.0)
                k.tt("dve", gtt[:, ts_, 0:16], tmpa[:], nA[:], ALU.mult, [tmpaB, nAB], [gttB])
                k.act(gtt[:, ts_, 16:32], pab[:, ts_ * 32 + 16:ts_ * 32 + 32], AF.Sigmoid, [pabB], [gttB])
                for d in range(2):
                    k.mm(pgc[:, ts_ * 16 + d * 8:ts_ * 16 + d * 8 + 8], tri[:, d, :], gtt[:, ts_, d * 8:d * 8 + 8], True, True,
                         [triB, gttB], [pgcB])
                k.copy("dve", gtt[:, ts_, 32:48], pgc[:, ts_ * 16:ts_ * 16 + 16], [pgcB], [gttB])
                k.op("pe", lambda hh: hh.transpose(ptr[0:16, ts_ * 128:(ts_ + 1) * 128], gtt[:, ts_, 32:48], pg.id_f[:]),
                     [gttB, pg.idfB], [ptrB])
            k.copy("dve", gcT[:, :W], ptr[0:16, :W], [ptrB], [gcTB])
            k.dma(dr["gcs"][s, :, t0:t0 + W], gcT[:, :W], reads=[gcTB])
            k.dma(dr["gt"][s, t0:t0 + W, :].rearrange("(a p) c -> p a c", p=128), gtt[:, :nst, :], reads=[gttB])


def gdn_conv_pass(pg):
    k, dr = pg.k, pg.dr
    pj = dr["pj"]
    tl = tiles_for(True)
    with Pool_(k) as P:
        cw, cwB = P.sb([128, 24, 4], F32, "cw"); k.dma(cw[:], dr["gdn_cw"][:, :, :], writes=[cwB])
        e1, e1B = P.sb([128, 1], F32, "e1"); k.memset("pool", e1[:], EPS, [e1B])
        e2, e2B = P.sb([128, 1], F32, "e2"); k.memset("pool", e2[:], EPS * 128.0, [e2B])
        pin = [P.sb([128, 24, TT + 4], BF16, f"pin{i}") for i in range(2)]
        ot, otB = P.sb([128, 24, TT], BF16, "cot")
        acc = [P.sb([128, TT], F32, f"acc{i}") for i in range(2)]
        sl = [P.sb([128, TT], F32, f"sl{i}") for i in range(2)]
        sq = [P.sb([128, TT], BF16, f"sq{i}") for i in range(2)]
        rn = [P.sb([128, TT], F32, f"rn{i}") for i in range(2)]
        pss = [P.ps(f"pss{i}") for i in range(2)]

        def load(i):
            s, t0, W, c = tl[i]
            t, B = pin[i % 2]
            seq0, seq1 = (0, NCTX) if c else (NCTX, TOK)
            k.memset("pool", t[:], 0.0, [B])
            lo, hi = max(seq0, t0 - 2), min(seq1, t0 + W + 1)
            k.dma(t[:, :, lo - (t0 - 2):hi - (t0 - 2)], pj[s].rearrange("(oc p) t -> p oc t", p=128)[:, 0:24, lo:hi], writes=[B])

        load(0)
        cnt = 0
        for i, (s, t0, W, c) in enumerate(tl):
            if i + 1 < len(tl):
                load(i + 1)
            x, xB = pin[i % 2]
            for oc in range(24):
                a_, aB = acc[cnt % 2]; s_, sB = sl[cnt % 2]; q_, qB = sq[cnt % 2]; r_, rB = rn[cnt % 2]; p_, pB = pss[cnt % 2]
                cnt += 1
                k.ts("dve", a_[:, :W], x[:, oc, 0:W], cw[:, oc, 0:1], None, ALU.mult, None, [xB, cwB], [aB])
                for j in range(1, 4):
                    k.stt(a_[:, :W], x[:, oc, j:j + W], cw[:, oc, j:j + 1], a_[:, :W], ALU.mult, ALU.add, [xB, cwB, aB], [aB])
                if oc >= 16:
                    k.act(ot[:, oc, :W], a_[:, :W], AF.Silu, [aB], [otB])
                    continue
                k.act(s_[:, :W], a_[:, :W], AF.Silu, [aB], [sB])
                k.act(q_[:, :W], s_[:, :W], AF.Square, [sB], [qB])
                k.mm(p_[:, :W], pg.ones_bf[:], q_[:, :W], True, True, [pg.onesB, qB], [pB])
                if oc < 8:
                    k.act(r_[:, :W], p_[:, :W], AF.Sqrt, [pB, e2B], [rB], scale=128.0, bias=e2[:, 0:1])
                else:
                    k.act(r_[:, :W], p_[:, :W], AF.Sqrt, [pB, e1B], [rB], bias=e1[:, 0:1])
                k.op("dve", lambda hh: hh.reciprocal(out=r_[:, :W], in_=r_[:, :W]), [rB], [rB])
                k.tt("pool", ot[:, oc, :W], s_[:, :W], r_[:, :W], ALU.mult, [sB, rB], [otB])
            for gi, nm in enumerate(("gq", "gk", "gv")):
                k.dma(dr[nm][s].rearrange("(oc p) t -> p oc t", p=128)[:, :, t0:t0 + W], ot[:, gi * 8:(gi + 1) * 8, :W], reads=[otB])


def _rr(gens):
    gens = list(gens)
    while gens:
        nxt = []
        for g in gens:
            try:
                next(g)
                nxt.append(g)
            except StopIteration:
                pass
        gens = nxt


def gdn_core_pass(pg):
    k, dr = pg.k, pg.dr
    with Pool_(k) as P:
        gm, gmB = P.sb([128, 2, 3, 128], F32, "gm"); k.dma(gm[:], dr["gdn_gm"][:, :, :, :], writes=[gmB])
        ms, msB = P.sb([128, 7, 128], BF16, "ms"); k.dma(ms[:], dr["gdn_ms"][:, :, :], writes=[msB])
        gng, gngB = P.sb([128, 1], F32, "gng"); k.dma(gng[:], dr["gdn_ng"][:, :], writes=[gngB])
        e1, e1B = P.sb([128, 1], F32, "e1"); k.memset("pool", e1[:], EPS, [e1B])
        qT, qTB = P.sb([128, TOK], BF16, "qT"); kT, kTB = P.sb([128, TOK], BF16, "kT"); vT, vTB = P.sb([128, TOK], BF16, "vT")
        zT, zTB = P.sb([128, TOK], BF16, "zT")
        ktok, ktokB = P.sb([128, NCHK, 128], BF16, "ktok"); vtok, vtokB = P.sb([128, NCHK, 128], BF16, "vtok")
        gt, gtB = P.sb([128, NCHK, 48], F32, "gt")
        gcb, gcbB = P.sb([128, TOK], F32, "gcb"); bb, bbB = P.sb([128, TOK], F32, "bb")
        U, UB_ = P.sb([128, NCHK, 128], F32, "U"); UB = [Buf(f"U{n}") for n in range(NCHK)]
        WT, _ = P.sb([128, NCHK, 128], BF16, "WT"); WTB = [Buf(f"WT{n}") for n in range(NCHK)]
        AQ, _ = P.sb([128, NCHK, 128], BF16, "AQ"); AQB = [Buf(f"AQ{n}") for n in range(NCHK)]
        KTL, _ = P.sb([128, NCHK, 128], BF16, "KTL"); KTLB = [Buf(f"KTL{n}") for n in range(NCHK)]
        qd, qdB = P.sb([128, TOK], BF16, "qd")
        oacc, oaccB = P.sb([128, TOK], F32, "oacc"); oB = [Buf(f"o{n}") for n in range(NCHK)]
        egc, egcB = P.sb([128, NCHK], F32, "egc"); ett, ettB = P.sb([128, NCHK], F32, "ett"); egl, eglB = P.sb([128, NCHK], F32, "egl")
        S, SB = P.sb([128, 128], F32, "S"); Sb, SbB = P.sb([128, 128], BF16, "Sb")
        vn = [P.sb([128, 128], BF16, f"vn{i}") for i in range(2)]
        tmpx, tmpxB = P.sb([128, 512], F32, "tmpx")
        NI = 2
        wk = []
        for ii in range(NI):
            w = {}
            for nm in ("xa", "xb", "xc", "ea", "eb", "ec", "t1"):
                w[nm] = P.sb([128, 128], F32, f"{nm}{ii}")
            for nm in ("L", "M", "X0", "X1", "Y0", "Y1", "Wm", "Wn", "vb", "kbg"):
                w[nm] = P.sb([128, 128], BF16, f"{nm}{ii}")
            w["pA"] = P.ps(f"pA{ii}"); w["pB"] = P.ps(f"pB{ii}"); w["pC"] = P.ps(f"pC{ii}")
            wk.append(w)
        pX, pXB = P.ps("pX"); pY, pYB = P.ps("pY")

        for s in range(NS):
            k.dma(gt[:], dr["gt"][s].rearrange("(n p) c -> p n c", p=128), writes=[gtB])
            for h in range(8):
                rows = slice(h * 128, (h + 1) * 128)
                k.dma(qT[:], dr["gq"][s, rows, :], writes=[qTB]); k.dma(kT[:], dr["gk"][s, rows, :], writes=[kTB])
                k.dma(vT[:], dr["gv"][s, rows, :], writes=[vTB])
                k.dma(zT[:], dr["pj"][s, 3 * D + h * 128:3 * D + (h + 1) * 128, :], writes=[zTB])
                for n in range(NCHK):
                    cs = slice(n * 128, (n + 1) * 128)
                    pb16 = pX[:, 0:64].bitcast(BF16) if False else None
                    k.op("pe", lambda hh: hh.transpose(pX[:, 0:128].bitcast(BF16)[:, 0:128], kT[:, cs], pg.id_bf[:]), [kTB, pg.idbB], [pXB])
                    k.copy("act", ktok[:, n, :], pX[:, 0:128].bitcast(BF16)[:, 0:128], [pXB], [ktokB])
                    k.op("pe", lambda hh: hh.transpose(pY[:, 0:128].bitcast(BF16)[:, 0:128], vT[:, cs], pg.id_bf[:]), [vTB, pg.idbB], [pYB])
                    k.copy("dve", vtok[:, n, :], pY[:, 0:128].bitcast(BF16)[:, 0:128], [pYB], [vtokB])
                for d in range(2):
                    col = d * 8 + h
                    k.dma(gcb[:], dr["gcs"][s, col:col + 1, :].partition_broadcast(128), writes=[gcbB])
                    k.dma(bb[:], dr["gbs"][s, col:col + 1, :].partition_broadcast(128), writes=[bbB])
                    gcv = gt[:, :, 32 + col]
                    btv = gt[:, :, 16 + col]
                    lastoff = 127 if d == 0 else 0
                    glv = gcb[:, lastoff:TOK:128]
                    k.act(egc[:], gcv, AF.Exp, [gtB], [egcB])
                    k.tt("dve", ett[:], glv, gcv, ALU.subtract, [gcbB, gtB], [ettB])
                    k.act(ett[:], ett[:], AF.Exp, [ettB], [ettB])
                    k.act(egl[:], glv, AF.Exp, [gcbB], [eglB])
                    for c0 in range(0, TOK, 512):
                        wd = min(512, TOK - c0)
                        k.act(tmpx[:, :wd], gcb[:, c0:c0 + wd], AF.Exp, [gcbB], [tmpxB])
                        k.tt("dve", qd[:, c0:c0 + wd], qT[:, c0:c0 + wd], tmpx[:, :wd], ALU.mult, [qTB, tmpxB], [qdB])

                    def inst(n, w):
                        cs = slice(n * 128, (n + 1) * 128)
                        gcn = gt[:, n, 32 + col:33 + col]; btn = gt[:, n, 16 + col:17 + col]
                        (xa, xaB), (xb, xbB), (xc, xcB) = w["xa"], w["xb"], w["xc"]
                        (ea, eaB), (eb, ebB), (ec, ecB) = w["ea"], w["eb"], w["ec"]
                        (t1, t1B), (L, LB), (M, MB) = w["t1"], w["L"], w["M"]
                        (pA, pAB), (pB_, pBB), (pC, pCB) = w["pA"], w["pB"], w["pC"]
                        k.stt(xa[:], gcb[:, cs], gcn, gm[:, d, 0, :], ALU.subtract, ALU.add, [gcbB, gtB, gmB], [xaB])
                        k.stt(xb[:], gcb[:, cs], gcn, gm[:, d, 1, :], ALU.subtract, ALU.add, [gcbB, gtB, gmB], [xbB])
                        k.stt(xc[:], gcb[:, cs], gcn, gm[:, d, 2, :], ALU.subtract, ALU.add, [gcbB, gtB, gmB], [xcB])
                        k.mm(pA[:, 0:128], kT[:, cs], kT[:, cs], True, True, [kTB], [pAB])
                        k.mm(pB_[:, 0:128], kT[:, cs], qT[:, cs], True, True, [kTB, qTB], [pBB])
                        yield
                        k.act(ea[:], xa[:], AF.Exp, [xaB], [eaB])
                        k.act(eb[:], xb[:], AF.Exp, [xbB], [ebB])
                        k.act(ec[:], xc[:], AF.Exp, [xcB], [ecB], scale=-1.0)
                        yield
                        k.stt(L[:], pA[:, 0:128], btn, ec[:], ALU.mult, ALU.mult, [pAB, gtB, ecB], [LB])
                        k.tt("dve", t1[:], pA[:, 0:128], eb[:], ALU.mult, [pAB, ebB], [t1B])
                        k.tt("pool", M[:], t1[:], bb[:, cs], ALU.mult, [t1B, bbB], [MB])
                        k.tt("dve", AQ[:, n, :], pB_[:, 0:128], ea[:], ALU.mult, [pBB, eaB], [AQB[n]])
                        k.ts("pool", w["vb"][0][:], vtok[:, n, :], btn, None, ALU.mult, None, [vtokB, gtB], [w["vb"][1]])
                        k.ts("pool", w["kbg"][0][:], ktok[:, n, :], btn, egc[:, n:n + 1], ALU.mult, ALU.mult, [ktokB, gtB, egcB], [w["kbg"][1]])
                        k.ts("pool", KTL[:, n, :], ktok[:, n, :], ett[:, n:n + 1], None, ALU.mult, None, [ktokB, ettB], [KTLB[n]])
                        yield
                        X, XB = w["X0"]; Y, YB = w["Y0"]; Xn, XnB = w["X1"]; Yn, YnB = w["Y1"]
                        Wm, WmB = w["Wm"]; Wn, WnB = w["Wn"]
                        k.tt("pool", Wm[:], L[:], ms[:, 0, :], ALU.mult, [LB, msB], [WmB])
                        k.tt("pool", X[:], pg.id_bf[:], Wm[:], ALU.subtract, [pg.idbB, WmB], [XB])
                        k.tt("pool", Wn[:], M[:], ms[:, 0, :], ALU.mult, [MB, msB], [WnB])
                        k.tt("pool", Y[:], pg.id_bf[:], Wn[:], ALU.subtract, [pg.idbB, WnB], [YB])
                        yield
                        for lv in range(1, 7):
                            lastlv = lv == 6
                            if not lastlv:
                                k.mm(pA[:, 0:128], M[:], X[:], True, True, [MB, XB], [pAB])
                            k.mm(pB_[:, 0:128], L[:], Y[:], True, True, [LB, YB], [pBB])
                            yield
                            if not lastlv:
                                k.tt("dve", Wm[:], pA[:, 0:128], ms[:, lv, :], ALU.mult, [pAB, msB], [WmB])
                            k.tt("dve", Wn[:], pB_[:, 0:128], ms[:, lv, :], ALU.mult, [pBB, msB], [WnB])
                            yield
                            if not lastlv:
                                k.mm(pA[:, 0:128], Y[:], Wm[:], True, True, [YB, WmB], [pAB])
                            k.mm(pC[:, 0:128], X[:], Wn[:], True, True, [XB, WnB], [pCB])
                            yield
                            if not lastlv:
                                k.tt("dve", Xn[:], X[:], pA[:, 0:128], ALU.subtract, [XB, pAB], [XnB])
                            k.tt("dve", Yn[:], Y[:], pC[:, 0:128], ALU.subtract, [YB, pCB], [YnB])
                            X, XB, Xn, XnB = Xn, XnB, X, XB
                            Y, YB, Yn, YnB = Yn, YnB, Y, YB
                            yield
                        k.mm(pA[:, 0:128], Y[:], w["vb"][0][:], True, True, [YB, w["vb"][1]], [pAB])
                        k.mm(pB_[:, 0:128], w["kbg"][0][:], Y[:], True, True, [w["kbg"][1], YB], [pBB])
                        yield
                        k.copy("act", U[:, n, :], pA[:, 0:128], [pAB], [UB[n]])
                        k.copy("act", WT[:, n, :], pB_[:, 0:128], [pBB], [WTB[n]])
                        yield

                    for n0 in range(0, NCHK, NI):
                        _rr([inst(n0 + ii, wk[ii]) for ii in range(NI) if n0 + ii < NCHK])
                    k.memset("pool", S[:], 0.0, [SB]); k.memset("pool", Sb[:], 0.0, [SbB])
                    order = [0, 1] + list(range(2, NCHK)) if d == 0 else [1, 0] + list(range(NCHK - 1, 1, -1))
                    for it, n in enumerate(order):
                        cs = slice(n * 128, (n + 1) * 128)
                        v_, vB_ = vn[it % 2]
                        k.mm(pX[:, 0:128], WT[:, n, :], Sb[:], True, True, [WTB[n], SbB], [pXB])
                        k.tt("dve", v_[:], U[:, n, :], pX[:, 0:128], ALU.subtract, [UB[n], pXB], [vB_])
                        k.mm(pY[:, 0:128], Sb[:], qd[:, cs], True, False, [SbB, qdB], [pYB])
                        k.mm(pY[:, 0:128], v_[:], AQ[:, n, :], False, True, [vB_, AQB[n]], [pYB])
                        if d == 0:
                            k.copy("act", oacc[:, cs], pY[:, 0:128], [pYB], [oB[n]])
                        else:
                            k.tt("dve", oacc[:, cs], oacc[:, cs], pY[:, 0:128], ALU.add, [oB[n], pYB], [oB[n]])
                        k.mm(pX[:, 0:128], KTL[:, n, :], v_[:], True, True, [KTLB[n], vB_], [pXB])
                        k.stt(S[:], S[:], egl[:, n:n + 1], pX[:, 0:128], ALU.mult, ALU.add, [SB, eglB, pXB], [SB])
                        k.copy("act", Sb[:], S[:], [SB], [SbB])
                for c0 in range(0, TOK, 512):
                    wd = min(512, TOK - c0)
                    obs = oB[c0 // 128:(c0 + wd) // 128]
                    xa, xaB = wk[0]["xa"]; xb, xbB = wk[0]["xb"]
                    sqt, sqB = P_sq = (qd, qdB)
                    k.act(sqt[:, c0:c0 + wd], oacc[:, c0:c0 + wd], AF.Square, obs, [qdB])
                    k.mm(pX[:, :wd], pg.ones_bf[:], sqt[:, c0:c0 + wd], True, True, [pg.onesB, qdB], [pXB])
                    k.act(tmpx[:, :wd], pX[:, :wd], AF.Sqrt, [pXB, e1B], [tmpxB], scale=1.0 / 128.0, bias=e1[:, 0:1])
                    k.op("dve", lambda hh: hh.reciprocal(out=tmpx[:, :wd], in_=tmpx[:, :wd]), [tmpxB], [tmpxB])
                    k.tt("dve", oacc[:, c0:c0 + wd], oacc[:, c0:c0 + wd], tmpx[:, :wd], ALU.mult, obs + [tmpxB], obs)
                    k.act(tmpx[:, :wd], zT[:, c0:c0 + wd], AF.Silu, [zTB], [tmpxB])
                    k.stt(qd[:, c0:c0 + wd], oacc[:, c0:c0 + wd], gng[:, 0:1], tmpx[:, :wd], ALU.mult, ALU.mult, obs + [gngB, tmpxB], [qdB])
                k.dma(dr["os"][s, rows, :], qd[:], reads=[qdB])


def gdn_mixer(pg, l, need_ctx):
    gdn_proj_pass(pg)
    gdn_conv_pass(pg)
    gdn_core_pass(pg)
    pg.outproj_pass(l, pg.dr["os"], pg.dr["gdn_w_o"], include_ctx=need_ctx, gate_off=16)


def _fm(v):
    v = np.asarray(v, np.float32)
    lead = v.shape[:-1]
    return np.ascontiguousarray(np.moveaxis(v.reshape(lead + (KC, 128)), -1, 0))


def _na_tables(rpb):
    col = np.arange(64)
    c0 = np.clip(col - 8, 0, 48)
    in_win = (col[None, :] >= c0[:, None]) & (col[None, :] < c0[:, None] + 16)
    dc = np.clip(col[None, :] - col[:, None], -15, 15) + 15
    out = np.full((8, 128, 2, 2, 22, 64), NEG, np.float32)
    for jj in range(22):
        j = 17 - jj
        for a in range(2):
            drr = j + a
            if drr < 0 or drr > 14:
                continue
            base = np.where(in_win[None], rpb[:, drr][:, dc], NEG).astype(np.float32)
            base = np.transpose(base, (0, 2, 1))
            for h in range(16):
                out[h // 2, a * 64:(a + 1) * 64, 1, h % 2, jj, :] = base[h]
                if 3 <= drr <= 10:
                    out[h // 2, a * 64:(a + 1) * 64, 0, h % 2, jj, :] = base[h]
    return out


def _pool_ic():
    out = np.ones((4, 2, 256), np.float32)
    for gi, win in enumerate((2, 4, 8, 16)):
        for ri, T in enumerate((64, 256)):
            t = np.arange(T)
            lo = np.clip(t - win // 2, 0, T)
            hi = np.clip(t + win // 2, 0, T)
            out[gi, ri, :T] = 1.0 / (hi - lo).astype(np.float32)
    return np.ascontiguousarray(np.broadcast_to(out[None], (128, 4, 2, 256)))


def make_in_maps(inputs, n_cores=8):
    import ml_dtypes
    x, c, ctx, c_ctx = inputs["x"], inputs["c"], inputs["ctx"], inputs["c_ctx"]
    shared = {
        "ada_w": np.ascontiguousarray(inputs["ada_w"], np.float32),
        "ada_b": np.ascontiguousarray(np.transpose(np.asarray(inputs["ada_b"], np.float32).reshape(DEPTH, 48, 128), (2, 0, 1))),
        "norm_g": _fm(inputs["norm_g"]),
        "final_g": _fm(inputs["final_g"]),
        "ones_bf": np.ones((128, 128), ml_dtypes.bfloat16),
        "id_bf": np.eye(128, dtype=np.float32).astype(ml_dtypes.bfloat16),
        "id_f": np.eye(128, dtype=np.float32),
        "ones_f": np.ones((128, 128), np.float32),
        "na_w_qkv": np.ascontiguousarray(inputs["na_w_qkv"], np.float32),
        "na_w_o": np.ascontiguousarray(inputs["na_w_o"], np.float32),
        "na_tab": np.stack([_na_tables(np.asarray(inputs["na_rpb"][j], np.float32)) for j in range(2)]),
        "ffn_w1": np.ascontiguousarray(inputs["ffn_w1"], np.float32),
        "ffn_w3": np.ascontiguousarray(inputs["ffn_w3"], np.float32),
        "ffn_w2": np.ascontiguousarray(inputs["ffn_w2"], np.float32),
        "moe_router": np.ascontiguousarray(np.transpose(np.asarray(inputs["moe_router"], np.float32).reshape(2, KC, 128, NEXP), (0, 2, 1, 3))),
        "moe_w1_0": np.ascontiguousarray(inputs["moe_w1"][0], np.float32), "moe_w1_1": np.ascontiguousarray(inputs["moe_w1"][1], np.float32),
        "moe_w3_0": np.ascontiguousarray(inputs["moe_w3"][0], np.float32), "moe_w3_1": np.ascontiguousarray(inputs["moe_w3"][1], np.float32),
        "moe_w2_0": np.ascontiguousarray(inputs["moe_w2"][0], np.float32), "moe_w2_1": np.ascontiguousarray(inputs["moe_w2"][1], np.float32),
        "tris": np.ascontiguousarray((np.arange(128)[:, None] < np.arange(128)[None, :]).astype(np.float32)),
        "thr48": np.ascontiguousarray(np.broadcast_to((np.arange(48) * 512.0).astype(np.float32)[None], (128, 48))),
        "iota48": np.ascontiguousarray(np.broadcast_to(np.arange(48).astype(np.float32)[None], (128, 48))),
        "pk1": np.ascontiguousarray(((np.arange(KC)[None, :] * 128 + np.arange(128)[:, None]) * 4).astype(np.float32)),
        "pk2": np.ascontiguousarray((np.arange(28)[None, :] * 128 + np.arange(128)[:, None]).astype(np.float32)),
        "pool_w": np.ascontiguousarray(inputs["pool_w"][0], np.float32),
        "pool_scale": _fm(inputs["pool_scale"][0]),
        "pool_ic": _pool_ic(),
    }
    shared.update(gdn_host(inputs))
    maps = []
    for ci in range(n_cores):
        b0 = ci * NS
        xin = np.empty((NS, D, TOK), np.float32)
        for s in range(NS):
            xin[s, :, :NCTX] = np.asarray(ctx[b0 + s], np.float32).T
            xin[s, :, NCTX:] = np.asarray(x[b0 + s], np.float32).T
        cv = np.stack([c[b0], c[b0 + 1], c_ctx], axis=0).astype(np.float32)
        m = dict(shared)
        m["xin"] = xin
        m["cvec"] = np.ascontiguousarray(np.transpose(cv.reshape(3, KC, 128), (2, 1, 0)))
        maps.append(m)
    return maps


def gdn_host(inputs):
    import ml_dtypes
    cw = np.asarray(inputs["gdn_conv"][0], np.float32)
    cwl = np.ascontiguousarray(np.transpose(cw.reshape(4, 24, 128), (2, 1, 0)))
    dtb = np.asarray(inputs["gdn_dt_bias"][0], np.float32).reshape(16)
    alog = np.asarray(inputs["gdn_a_log"][0], np.float32).reshape(16)
    p = np.arange(128)
    tri = np.zeros((128, 2, 128), np.float32)
    tri[:, 0, :] = (p[:, None] <= p[None, :])
    tri[:, 1, :] = (p[:, None] >= p[None, :])
    gm = np.zeros((128, 2, 3, 128), np.float32)
    P_, F_ = p[:, None], p[None, :]
    gm[:, 0, 0, :] = np.where(F_ >= P_, 0.0, NEG); gm[:, 0, 1, :] = np.where(F_ > P_, 0.0, NEG); gm[:, 0, 2, :] = np.where(F_ < P_, 0.0, -NEG)
    gm[:, 1, 0, :] = np.where(F_ <= P_, 0.0, NEG); gm[:, 1, 1, :] = np.where(F_ < P_, 0.0, NEG); gm[:, 1, 2, :] = np.where(F_ > P_, 0.0, -NEG)
    ms = np.zeros((128, 7, 128), np.float32)
    for kk in range(7):
        ms[:, kk, :] = ((P_ >> kk) != (F_ >> kk)) & ((P_ >> (kk + 1)) == (F_ >> (kk + 1)))
    return {
        "gdn_w_in": np.ascontiguousarray(inputs["gdn_w_in"][0], np.float32),
        "gdn_w_o": np.ascontiguousarray(inputs["gdn_w_o"][0], np.float32),
        "gdn_cw": cwl,
        "gdn_dtb": np.ascontiguousarray(np.broadcast_to(dtb[None], (128, 16))),
        "gdn_alog": np.ascontiguousarray(np.broadcast_to(alog[None], (128, 16))),
        "gdn_ng": np.ascontiguousarray(np.asarray(inputs["gdn_norm_g"][0], np.float32).reshape(128, 1)),
        "gdn_tri": tri, "gdn_gm": gm, "gdn_ms": ms.astype(ml_dtypes.bfloat16),
    }


def kernel(**inputs):
    nc = build()
    maps = make_in_maps(inputs)
    res = run_bass_kernel_spmd(nc, maps, core_ids=list(range(8)))
    B = inputs["x"].shape[0]
    out = np.empty((B, NLAT, D), np.float32)
    for ci in range(8):
        o = res.results[ci]["out"]
        for s in range(NS):
            out[ci * NS + s] = o[s].T
    return out
```

```python
import numpy as np
from contextlib import ExitStack
import concourse.bass as bass
import concourse.mybir as mybir
from concourse.bass_utils import run_bass_kernel_spmd

F32 = mybir.dt.float32
BF16 = mybir.dt.bfloat16
U32 = mybir.dt.uint32
AF = mybir.ActivationFunctionType
ALU = mybir.AluOpType

D = 1024
KC = 8
NLAT = 4096
NCTX = 256
TT = 512
NS = 2
DEPTH = 4
EPS = 1e-6
NEG = -1e30
DFF = 2816
DFE = 3584
NEXP = 8


class _Sem:
    def __init__(self, h, name):
        self.h = h
        self.val = 0
        self.name = name


class _Eng:
    def __init__(self, name, h, sem):
        self.name = name
        self.h = h
        self.sem = sem
        self.seen = {}


class Buf:
    __slots__ = ("name", "w", "r")

    def __init__(self, name=""):
        self.name = name
        self.w = None
        self.r = {}


class K:
    def __init__(self, nc, es, ndma=24):
        self.nc = nc
        self.es = es
        self.E = {}
        for name, h in (("pe", nc.tensor), ("act", nc.scalar), ("dve", nc.vector),
                        ("pool", nc.gpsimd), ("sp", nc.sync)):
            s = _Sem(es.enter_context(nc.semaphore("sem_" + name)), name)
            self.E[name] = _Eng(name, h, s)
        self.dsems = [_Sem(es.enter_context(nc.semaphore(f"dsem{i}")), f"d{i}") for i in range(ndma)]
        self.dnext = 0
        self.allsems = [e.sem for e in self.E.values()] + self.dsems

    def _need(self, e, ev):
        if ev is None:
            return
        sem, val = ev
        if sem is e.sem and e.name == "pe":
            return
        if e.seen.get(sem.name, 0) >= val:
            return
        e.h.wait_ge(sem.h, val)
        e.seen[sem.name] = val

    def _deps(self, e, reads, writes):
        for b in reads:
            self._need(e, b.w)
        for b in writes:
            self._need(e, b.w)
            for sname, ev in list(b.r.items()):
                self._need(e, ev)

    def _mark(self, ev, reads, writes):
        for b in reads:
            b.r[ev[0].name] = ev
        for b in writes:
            b.w = ev
            b.r = {}

    def op(self, ename, fn, reads=(), writes=()):
        e = self.E[ename]
        self._deps(e, reads, writes)
        ins = fn(e.h)
        e.sem.val += 1
        ins.then_inc(e.sem.h, 1)
        self._mark((e.sem, e.sem.val), reads, writes)

    def dma(self, out, in_, reads=(), writes=(), q="sp", **kw):
        e = self.E[q]
        ds = self.dsems[self.dnext]
        self.dnext = (self.dnext + 1) % len(self.dsems)
        self._need(e, (ds, ds.val))
        self._deps(e, reads, writes)
        ins = e.h.dma_start(out=out, in_=in_, **kw)
        ds.val += 16
        ins.then_inc(ds.h, 16)
        self._mark((ds, ds.val), reads, writes)

    def idma(self, out, in_, idx_ap, gather, reads=(), writes=(), bounds=None):
        e = self.E["pool"]
        ds = self.dsems[self.dnext]
        self.dnext = (self.dnext + 1) % len(self.dsems)
        self._need(e, (ds, ds.val))
        self._deps(e, reads, writes)
        off = bass.IndirectOffsetOnAxis(ap=idx_ap, axis=0)
        if gather:
            ins = e.h.indirect_dma_start(out=out, out_offset=None, in_=in_, in_offset=off)
        else:
            ins = e.h.indirect_dma_start(out=out, out_offset=off, in_=in_, in_offset=None)
        ds.val += 16
        ins.then_inc(ds.h, 16)
        self._mark((ds, ds.val), reads, writes)

    def barrier(self):
        for e in self.E.values():
            for s in self.allsems:
                if s.val > 0:
                    self._need(e, (s, s.val))

    def mm(self, out, lhsT, rhs, start, stop, reads, writes):
        self.op("pe", lambda h: h.matmul(out, lhsT, rhs, start=start, stop=stop), reads, writes)

    def act(self, out, in_, func, reads, writes, **kw):
        self.op("act", lambda h: h.activation(out=out, in_=in_, func=func, **kw), reads, writes)

    def tt(self, eng, out, in0, in1, op, reads, writes):
        self.op(eng, lambda h: h.tensor_tensor(out=out, in0=in0, in1=in1, op=op), reads, writes)

    def ts(self, eng, out, in0, s1, s2, op0, op1, reads, writes):
        if op1 is None:
            self.op(eng, lambda h: h.tensor_scalar(out=out, in0=in0, scalar1=s1, scalar2=None, op0=op0),
                    reads, writes)
        else:
            self.op(eng, lambda h: h.tensor_scalar(out=out, in0=in0, scalar1=s1, scalar2=s2, op0=op0, op1=op1),
                    reads, writes)

    def stt(self, out, in0, scalar, in1, op0, op1, reads, writes):
        self.op("dve", lambda h: h.scalar_tensor_tensor(out=out, in0=in0, scalar=scalar, in1=in1, op0=op0, op1=op1),
                reads, writes)

    def copy(self, eng, out, in_, reads, writes):
        if eng == "act":
            self.op("act", lambda h: h.activation(out=out, in_=in_, func=AF.Copy), reads, writes)
        else:
            self.op(eng, lambda h: h.tensor_copy(out=out, in_=in_), reads, writes)

    def memset(self, eng, ap, val, writes):
        self.op(eng, lambda h: h.memset(ap, val), (), writes)


_UID = [0]


class Pool_:
    def __init__(self, k):
        self.k = k
        self.es = ExitStack()
        self.n = 0

    def __enter__(self):
        self.es.__enter__()
        return self

    def __exit__(self, *a):
        self.k.barrier()
        return self.es.__exit__(*a)

    def sb(self, shape, dt, name=None):
        _UID[0] += 1
        t = self.es.enter_context(self.k.nc.sbuf_tensor(f"{name or 't'}_{_UID[0]}", list(shape), dt))
        return t, Buf(name or "sb")

    def ps(self, name=None, shape=(128, 512), dt=F32):
        _UID[0] += 1
        t = self.es.enter_context(self.k.nc.psum_tensor(f"{name or 'p'}_{_UID[0]}", list(shape), dt))
        return t, Buf(name or "ps")


TOK = NCTX + NLAT


def tiles_for(include_ctx=True):
    out = []
    for s in range(NS):
        if include_ctx:
            out.append((s, 0, NCTX, True))
        for i in range(NLAT // TT):
            out.append((s, NCTX + i * TT, TT, False))
    return out


class Prog:
    def __init__(self, nc, k, dr, stop_after=99):
        self.nc = nc
        self.k = k
        self.dr = dr
        self.stop_after = stop_after

    def setup_consts(self, P):
        k, dr = self.k, self.dr
        self.ones_bf, self.onesB = P.sb([128, 128], BF16, "ones")
        self.id_bf, self.idbB = P.sb([128, 128], BF16, "idb")
        self.id_f, self.idfB = P.sb([128, 128], F32, "idf")
        self.ones_f, self.onesfB = P.sb([128, 128], F32, "onesf")
        self.mod, self.modB = P.sb([128, DEPTH, 48, 3], F32, "mod")
        self.ng, self.ngB = P.sb([128, DEPTH, 2, KC], F32, "ng")
        self.fg, self.fgB = P.sb([128, KC], F32, "fg")
        self.epsb, self.epsB = P.sb([128, 1], F32, "eps")
        k.dma(self.ones_bf[:], dr["ones_bf"][:, :], writes=[self.onesB])
        k.dma(self.id_bf[:], dr["id_bf"][:, :], writes=[self.idbB])
        k.dma(self.id_f[:], dr["id_f"][:, :], writes=[self.idfB])
        k.dma(self.ones_f[:], dr["ones_f"][:, :], writes=[self.onesfB])
        k.dma(self.ng[:], dr["norm_g"][:, :, :, :], writes=[self.ngB])
        k.dma(self.fg[:], dr["final_g"][:, :], writes=[self.fgB])
        k.memset("pool", self.epsb[:], EPS, [self.epsB])

    def adaln(self):
        k, dr = self.k, self.dr
        with Pool_(k) as P:
            cv, cvB = P.sb([128, KC, 3], F32, "cv")
            sc, scB = P.sb([128, KC, 3], F32, "sc")
            ab, abB = P.sb([128, DEPTH, 48], F32, "ab")
            wa = [P.sb([128, KC, 768], F32, f"wa{i}") for i in range(2)]
            ps = [P.ps(f"adaps{i}") for i in range(2)]
            k.dma(cv[:], dr["cvec"][:, :, :], writes=[cvB])
            k.dma(ab[:], dr["ada_b"][:, :, :], writes=[abB])
            k.act(sc[:], cv[:], AF.Silu, [cvB], [scB])
            gi = 0
            for l in range(DEPTH):
                src = dr["ada_w"][l].rearrange("(kc p) n -> p kc n", p=128)
                for g in range(8):
                    wt, wB = wa[gi % 2]
                    pt, pB = ps[gi % 2]
                    gi += 1
                    k.dma(wt[:], src[:, :, g * 768:(g + 1) * 768], writes=[wB])
                    for o in range(6):
                        for kc in range(KC):
                            k.mm(pt[:, o * 3:(o + 1) * 3], wt[:, kc, o * 128:(o + 1) * 128], sc[:, kc, :],
                                 kc == 0, kc == KC - 1, [wB, scB], [pB])
                    pv = pt[:, 0:18].rearrange("p (o j) -> p o j", j=3)
                    for j in range(3):
                        k.tt("dve", self.mod[:, l, g * 6:(g + 1) * 6, j], pv[:, :, j], ab[:, l, g * 6:(g + 1) * 6],
                             ALU.add, [pB, abB], [self.modB])

    def mod_vecs(self, P, l, sub):
        k = self.k
        A, AB = P.sb([128, KC, 3], F32, "modA")
        o = sub * 24
        for j in range(3):
            k.stt(A[:, :, j], self.mod[:, l, o + 8:o + 16, j], 1.0, self.ng[:, l, sub, :], ALU.add, ALU.mult,
                  [self.modB, self.ngB], [AB])
        return A, AB

    def jidx(self, s, is_ctx):
        return 2 if is_ctx else s

    def norm_pass(self, l, sub, include_ctx=True, router=None, final=False, moe=None):
        k, dr = self.k, self.dr
        xs, hs = dr["xs"], dr["hs"]
        tl = tiles_for(include_ctx)
        with Pool_(k) as P:
            if not final:
                A, AB = self.mod_vecs(P, l, sub)
            xt = [P.sb([128, KC, TT], F32, f"xt{i}") for i in range(2)]
            sq, sqB = P.sb([128, KC, TT], BF16, "sq")
            rs, rsB = P.sb([128, TT], F32, "rs")
            hf, hfB = P.sb([128, KC, TT], F32, "hf")
            hb, hbB = P.sb([128, KC, TT], BF16, "hb")
            pss, pssB = P.ps("pss")
            if router is not None:
                wr, wrB = P.sb([128, KC, NEXP], F32, "wr")
                k.dma(wr[:], router, writes=[wrB])
                plg, plgB = P.ps("plg")
                pwt, pwtB = P.ps("pwt")
                lg, lgB = P.sb([128, 4, NEXP], F32, "lg")
                m8, m8B = P.sb([128, 4, 8], F32, "m8")
                nm1, nm1B = P.sb([128, 4], F32, "nm1")
                ee, eeB = P.sb([128, 4, NEXP], F32, "ee")
                mk, mkB = P.sb([128, 4, NEXP], F32, "mk")
                dn, dnB = P.sb([128, 4], F32, "dn")
                ww, wwB = P.sb([128, 4, NEXP], F32, "ww")
                wT, wTB = P.sb([NEXP, TT], F32, "wT")
                if moe is not None:
                    oh, ohB = P.sb([128, 4, 24], F32, "oh")
                    rk, rkB = P.sb([128, 4, NEXP], F32, "rk")
                    t8, t8B = P.sb([128, 4, NEXP], F32, "t8")
                    htk, htkB = P.sb([128, 4, D], BF16, "htk")
                    prk, prkB = P.ps("prk")
                    ptot, ptotB = P.ps("ptot")
                    ptk, ptkB = P.ps("ptk")
                    I32_ = mybir.dt.int32
                    sub_i = 0

            def load(i):
                s, t0, W, c = tl[i]
                t, B = xt[i % 2]
                k.dma(t[:, :, :W], xs[s].rearrange("(kc p) t -> p kc t", p=128)[:, :, t0:t0 + W], writes=[B])

            load(0)
            for i, (s, t0, W, c) in enumerate(tl):
                if i + 1 < len(tl):
                    load(i + 1)
                x, xB = xt[i % 2]
                j = self.jidx(s, c)
                k.act(sq[:, :, :W], x[:, :, :W], AF.Square, [xB], [sqB])
                for kc in range(KC):
                    k.mm(pss[:, :W], self.ones_bf[:], sq[:, kc, :W], kc == 0, kc == KC - 1, [self.onesB, sqB], [pssB])
                k.act(rs[:, :W], pss[:, :W], AF.Sqrt, [pssB, self.epsB], [rsB], scale=1.0 / D, bias=self.epsb[:, 0:1])
                k.op("dve", lambda h: h.reciprocal(out=rs[:, :W], in_=rs[:, :W]), [rsB], [rsB])
                for kc in range(KC):
                    k.tt("dve", hf[:, kc, :W], x[:, kc, :W], rs[:, :W], ALU.mult, [xB, rsB], [hfB])
                if final:
                    for kc in range(KC):
                        k.act(hf[:, kc, :W], hf[:, kc, :W], AF.Identity, [hfB, self.fgB], [hfB], scale=self.fg[:, kc:kc + 1])
                    k.dma(dr["out"][s].rearrange("(kc p) t -> p kc t", p=128)[:, :, t0 - NCTX:t0 - NCTX + W],
                          hf[:, :, :W], reads=[hfB])
                    continue
                o = sub * 24
                for kc in range(KC):
                    k.act(hf[:, kc, :W], hf[:, kc, :W], AF.Identity, [hfB, AB, self.modB], [hfB],
                          scale=A[:, kc, j:j + 1], bias=self.mod[:, l, o + kc, j:j + 1])
                k.copy("pool", hb[:, :, :W], hf[:, :, :W], [hfB], [hbB])
                k.dma(hs[s].rearrange("(kc p) t -> p kc t", p=128)[:, :, t0:t0 + W], hb[:, :, :W], reads=[hbB])
                if router is not None:
                    nst = W // 128
                    for ts_ in range(nst):
                        for kc in range(KC):
                            k.mm(plg[:, ts_ * 8:(ts_ + 1) * 8], hf[:, kc, ts_ * 128:(ts_ + 1) * 128], wr[:, kc, :],
                                 kc == 0, kc == KC - 1, [hfB, wrB], [plgB])
                    k.copy("dve", lg[:, :nst, :], plg[:, 0:nst * 8].rearrange("p (a e) -> p a e", e=8), [plgB], [lgB])
                    for ts_ in range(nst):
                        k.op("dve", lambda h: h.max(out=m8[:, ts_, :], in_=lg[:, ts_, :]), [lgB], [m8B])
                    k.ts("dve", nm1[:, :nst], m8[:, :nst, 0], -1.0, None, ALU.mult, None, [m8B], [nm1B])
                    for ts_ in range(nst):
                        k.act(ee[:, ts_, :], lg[:, ts_, :], AF.Exp, [lgB, nm1B], [eeB], bias=nm1[:, ts_:ts_ + 1])
                        k.ts("dve", mk[:, ts_, :], lg[:, ts_, :], m8[:, ts_, 1:2], None, ALU.is_ge, None,
                             [lgB, m8B], [mkB])
                        if moe is not None:
                            k.ts("dve", oh[:, ts_, 0:8], lg[:, ts_, :], m8[:, ts_, 0:1], None, ALU.is_equal, None, [lgB, m8B], [ohB])
                            k.ts("dve", oh[:, ts_, 8:16], lg[:, ts_, :], m8[:, ts_, 1:2], None, ALU.is_equal, None, [lgB, m8B], [ohB])
                    k.tt("dve", ee[:, :nst, :], ee[:, :nst, :], mk[:, :nst, :], ALU.mult, [eeB, mkB], [eeB])
                    k.op("dve", lambda h: h.reduce_sum(out=dn[:, :nst], in_=ee[:, :nst, :], axis=mybir.AxisListType.X),
                         [eeB], [dnB])
                    k.op("dve", lambda h: h.reciprocal(out=dn[:, :nst], in_=dn[:, :nst]), [dnB], [dnB])
                    for ts_ in range(nst):
                        k.ts("dve", ww[:, ts_, :], ee[:, ts_, :], dn[:, ts_:ts_ + 1], None, ALU.mult, None,
                             [eeB, dnB], [wwB])
                        k.op("pe", lambda h: h.transpose(pwt[0:NEXP, ts_ * 128:(ts_ + 1) * 128], ww[:, ts_, :], self.id_f[:]),
                             [wwB, self.idfB], [pwtB])
                    k.copy("dve", wT[:, :W], pwt[0:NEXP, :W], [pwtB], [wTB])
                    k.dma(dr["wexp"][s, :, t0:t0 + W], wT[:, :W], reads=[wTB])
                    if moe is not None:
                        cum, cumB = moe["cum"]
                        for ts_ in range(nst):
                            k.mm(prk[:, 0:8], moe["tris"][0][:], mk[:, ts_, :], True, True, [moe["tris"][1], mkB], [prkB])
                            k.mm(ptot[:, 0:8], self.ones_f[:], mk[:, ts_, :], True, True, [self.onesfB, mkB], [ptotB])
                            k.tt("dve", rk[:, ts_, :], prk[:, 0:8], cum[:], ALU.add, [prkB, cumB], [rkB])
                            k.tt("dve", cum[:], cum[:], ptot[:, 0:8], ALU.add, [cumB, ptotB], [cumB])
                        for (src_, so, do) in ((rk, 0, 16), (rk, 8, 17), (ww, 0, 18), (ww, 8, 19)):
                            k.tt("dve", t8[:, :nst, :], oh[:, :nst, so:so + 8], src_[:, :nst, :], ALU.mult, [ohB, rkB, wwB], [t8B])
                            k.op("dve", lambda h: h.reduce_sum(out=oh[:, :nst, do], in_=t8[:, :nst, :], axis=mybir.AxisListType.X),
                                 [t8B], [ohB])
                        k.dma(dr["rinfo"][s, t0:t0 + W, :].rearrange("(a p) c -> p a c", p=128), oh[:, :nst, :], reads=[ohB])
                        for ts_ in range(nst):
                            for kc in range(KC):
                                k.op("pe", lambda h: h.transpose(ptk[:, :].bitcast(BF16)[:, kc * 128:(kc + 1) * 128],
                                                                 hb[:, kc, ts_ * 128:(ts_ + 1) * 128], self.id_bf[:]),
                                     [hbB, self.idbB], [ptkB])
                            if ts_ % 2 == 0:
                                k.copy("act", htk[:, ts_, :], ptk[:, :].bitcast(BF16)[:, 0:D], [ptkB], [htkB])
                            else:
                                k.copy("dve", htk[:, ts_, :], ptk[:, :].bitcast(BF16)[:, 0:D], [ptkB], [htkB])
                        k.dma(dr["htok"][s, t0:t0 + W, :].rearrange("(a p) d -> p a d", p=128), htk[:, :nst, :], reads=[htkB])

    def ffn_pass(self, l, w1, w3, w2, F, include_ctx=True, expert=None):
        k, dr = self.k, self.dr
        xs, hs = dr["xs"], dr["hs"]
        FC = F // 128
        tl = tiles_for(include_ctx)
        with Pool_(k) as P:
            W1, W1B = P.sb([128, KC, F], BF16, "W1")
            W3, W3B = P.sb([128, KC, F], BF16, "W3")
            W2, W2B = P.sb([128, FC, D], BF16, "W2")
            k.dma(W1[:], w1.rearrange("(kc p) f -> p kc f", p=128), writes=[W1B], q="pool")
            k.dma(W3[:], w3.rearrange("(kc p) f -> p kc f", p=128), writes=[W3B], q="pool")
            k.dma(W2[:], w2.rearrange("(fc p) d -> p fc d", p=128), writes=[W2B], q="pool")
            ht = [P.sb([128, KC, TT], BF16, f"ht{i}") for i in range(2)]
            wb = [P.sb([128, TT], F32, f"wbc{i}") for i in range(2)] if expert is not None else None
            g, gB = P.sb([128, FC, TT], BF16, "g")
            gBs = [Buf(f"g{f}") for f in range(FC)]
            sa = [P.sb([128, TT], F32, f"sa{i}") for i in range(2)]
            yt = [P.sb([128, TT], F32, f"yt{i}") for i in range(2)]
            pa = [P.ps(f"pa{i}") for i in range(2)]
            pb = [P.ps(f"pb{i}") for i in range(2)]
            py = [P.ps(f"py{i}") for i in range(2)]

            def load(i):
                s, t0, W, c = tl[i]
                t, B = ht[i % 2]
                k.dma(t[:, :, :W], hs[s].rearrange("(kc p) t -> p kc t", p=128)[:, :, t0:t0 + W], writes=[B])
                if expert is not None:
                    wt_, wB_ = wb[i % 2]
                    k.dma(wt_[:, :W], dr["wexp"][s, expert:expert + 1, t0:t0 + W].partition_broadcast(128), writes=[wB_])

            load(0)
            cnt = 0
            ycnt = 0
            for i, (s, t0, W, c) in enumerate(tl):
                if i + 1 < len(tl):
                    load(i + 1)
                h, hB = ht[i % 2]
                j = self.jidx(s, c)
                for f in range(FC):
                    a_, aB = pa[cnt % 2]
                    b_, bB = pb[cnt % 2]
                    s_, sB = sa[cnt % 2]
                    cnt += 1
                    for kc in range(KC):
                        k.mm(a_[:, :W], W1[:, kc, f * 128:(f + 1) * 128], h[:, kc, :W], kc == 0, kc == KC - 1, [W1B, hB], [aB])
                    for kc in range(KC):
                        k.mm(b_[:, :W], W3[:, kc, f * 128:(f + 1) * 128], h[:, kc, :W], kc == 0, kc == KC - 1, [W3B, hB], [bB])
                    k.act(s_[:, :W], a_[:, :W], AF.Silu, [aB], [sB])
                    if expert is not None:
                        wt_, wB_ = wb[i % 2]
                        k.tt("pool", s_[:, :W], s_[:, :W], wt_[:, :W], ALU.mult, [sB, wB_], [sB])
                    k.tt("dve", g[:, f, :W], b_[:, :W], s_[:, :W], ALU.mult, [bB, sB], [gBs[f]])
                o = 24 + 16 + 0
                for oc in range(KC):
                    y_, yB = py[ycnt % 2]
                    yo, yoB = yt[ycnt % 2]
                    ycnt += 1
                    for f in range(FC):
                        k.mm(y_[:, :W], W2[:, f, oc * 128:(oc + 1) * 128], g[:, f, :W], f == 0, f == FC - 1, [W2B, gBs[f]], [yB])
                    k.ts("dve", yo[:, :W], y_[:, :W], self.mod[:, l, 40 + oc, j:j + 1], None, ALU.mult, None,
                         [yB, self.modB], [yoB])
                    k.dma(xs[s, oc * 128:(oc + 1) * 128, t0:t0 + W], yo[:, :W], reads=[yoB], q="pool", accum_op=ALU.add)

    def outproj_pass(self, l, src, w, include_ctx, gate_off, extra_scale=None):
        k, dr = self.k, self.dr
        xs = dr["xs"]
        tl = tiles_for(include_ctx)
        with Pool_(k) as P:
            Wo, WoB = P.sb([128, KC, D], BF16, "Wo")
            k.dma(Wo[:], w.rearrange("(kc p) d -> p kc d", p=128), writes=[WoB], q="pool")
            ot = [P.sb([128, KC, TT], BF16, f"ot{i}") for i in range(2)]
            yt = [P.sb([128, TT], F32, f"yt{i}") for i in range(2)]
            py = [P.ps(f"py{i}") for i in range(2)]

            def load(i):
                s, t0, W, c = tl[i]
                t, B = ot[i % 2]
                k.dma(t[:, :, :W], src[s].rearrange("(kc p) t -> p kc t", p=128)[:, :, t0:t0 + W], writes=[B])

            load(0)
            ycnt = 0
            for i, (s, t0, W, c) in enumerate(tl):
                if i + 1 < len(tl):
                    load(i + 1)
                o_, oB = ot[i % 2]
                j = self.jidx(s, c)
                for oc in range(KC):
                    y_, yB = py[ycnt % 2]
                    yo, yoB = yt[ycnt % 2]
                    ycnt += 1
                    for kc in range(KC):
                        k.mm(y_[:, :W], Wo[:, kc, oc * 128:(oc + 1) * 128], o_[:, kc, :W], kc == 0, kc == KC - 1, [WoB, oB], [yB])
                    if extra_scale is None:
                        k.ts("dve", yo[:, :W], y_[:, :W], self.mod[:, l, gate_off + oc, j:j + 1], None, ALU.mult, None,
                             [yB, self.modB], [yoB])
                    else:
                        est, esB = extra_scale
                        k.ts("dve", yo[:, :W], y_[:, :W], self.mod[:, l, gate_off + oc, j:j + 1], est[:, oc:oc + 1],
                             ALU.mult, ALU.mult, [yB, self.modB, esB], [yoB])
                    k.dma(xs[s, oc * 128:(oc + 1) * 128, t0:t0 + W], yo[:, :W], reads=[yoB], q="pool", accum_op=ALU.add)

    def na_qkv_pass(self, wqkv):
        k, dr = self.k, self.dr
        hs, qs, ks, vs = dr["hs"], dr["qs"], dr["ks"], dr["vs"]
        tl = tiles_for(True)
        with Pool_(k) as P:
            Wq, WqB = P.sb([128, KC, 3 * D], BF16, "Wqkv")
            src = wqkv.rearrange("(kc p) n -> p kc n", p=128)
            for i in range(3):
                k.dma(Wq[:, :, i * D:(i + 1) * D], src[:, :, i * D:(i + 1) * D], writes=[WqB], q="pool")
            ht = [P.sb([128, KC, TT], BF16, f"ht{i}") for i in range(2)]
            qk = [P.sb([128, 2 * KC, TT], BF16, f"qk{i}") for i in range(2)]
            vt = [P.sb([128, 4, D], BF16, f"vt{i}") for i in range(2)]
            pp = [P.ps(f"pp{i}") for i in range(4)]

            def load(i):
                s, t0, W, c = tl[i]
                t, B = ht[i % 2]
                k.dma(t[:, :, :W], hs[s].rearrange("(kc p) t -> p kc t", p=128)[:, :, t0:t0 + W], writes=[B])

            load(0)
            cnt = 0
            for i, (s, t0, W, c) in enumerate(tl):
                if i + 1 < len(tl):
                    load(i + 1)
                h, hB = ht[i % 2]
                q_, qB = qk[i % 2]
                v_, vB = vt[i % 2]
                for oc in range(2 * KC):
                    p_, pB = pp[cnt % 4]
                    for kc in range(KC):
                        k.mm(p_[:, :W], Wq[:, kc, oc * 128:(oc + 1) * 128], h[:, kc, :W], kc == 0, kc == KC - 1, [WqB, hB], [pB])
                    if cnt % 2 == 0:
                        k.act(q_[:, oc, :W], p_[:, :W], AF.Copy, [pB], [qB], scale=(0.125 if oc < KC else 1.0))
                    else:
                        k.ts("dve", q_[:, oc, :W], p_[:, :W], (0.125 if oc < KC else 1.0), None, ALU.mult, None, [pB], [qB])
                    cnt += 1
                for ts_ in range(W // 128):
                    for hf_ in range(2):
                        p_, pB = pp[cnt % 4]
                        for kc in range(KC):
                            k.mm(p_[:, :], h[:, kc, ts_ * 128:(ts_ + 1) * 128], Wq[:, kc, 2 * D + hf_ * 512:2 * D + (hf_ + 1) * 512],
                                 kc == 0, kc == KC - 1, [WqB, hB], [pB])
                        if cnt % 2 == 0:
                            k.act(v_[:, ts_, hf_ * 512:(hf_ + 1) * 512], p_[:, :], AF.Copy, [pB], [vB])
                        else:
                            k.copy("dve", v_[:, ts_, hf_ * 512:(hf_ + 1) * 512], p_[:, :], [pB], [vB])
                        cnt += 1
                k.dma(qs[s].rearrange("(kc p) t -> p kc t", p=128)[:, :, t0:t0 + W], q_[:, 0:KC, :W], reads=[qB])
                k.dma(ks[s].rearrange("(kc p) t -> p kc t", p=128)[:, :, t0:t0 + W], q_[:, KC:2 * KC, :W], reads=[qB])
                k.dma(vs[s, t0:t0 + W, :].rearrange("(a p) f -> p a f", p=128), v_[:, :W // 128, :], reads=[vB])

    def na_attn_pass(self, tab, need_ctx):
        k, dr = self.k, self.dr
        qs, ks, vs, os_ = dr["qs"], dr["ks"], dr["vs"], dr["os"]
        NCH = TOK // 128
        with Pool_(k) as P:
            qT, qTB = P.sb([128, TOK], BF16, "qT")
            kT, kTB = P.sb([128, TOK], BF16, "kT")
            vv, vvB = P.sb([128, NCH, 128], BF16, "vv")
            tb, tbB = P.sb([128, 2, 2, 22, 64], F32, "tb")
            negt, negB = P.sb([128, 256], F32, "negt")
            k.memset("pool", negt[:], NEG, [negB])
            sbm = [P.sb([128, TT], F32, f"sbm{i}") for i in range(2)]
            pt = [P.sb([128, TT], BF16, f"pt{i}") for i in range(3)]
            rd, rdB = P.sb([128, TT], F32, "rd")
            ot = [P.sb([128, TT], BF16, f"ot{i}") for i in range(2)]
            pS = [P.ps(f"pS{i}") for i in range(3)]
            pO = [P.ps(f"pO{i}") for i in range(2)]
            pD = [P.ps(f"pD{i}") for i in range(2)]
            scnt = 0
            tcnt = 0
            for s in range(NS):
                for hp in range(8):
                    k.dma(qT[:], qs[s, hp * 128:(hp + 1) * 128, :], writes=[qTB])
                    k.dma(kT[:], ks[s, hp * 128:(hp + 1) * 128, :], writes=[kTB])
                    k.dma(vv[:], vs[s, :, hp * 128:(hp + 1) * 128].rearrange("(a p) f -> p a f", p=128), writes=[vvB])
                    if s == 0 or True:
                        k.dma(tb[:], tab[hp], writes=[tbB])
                    qtiles = ([(0, NCTX, -1)] if need_ctx else []) + [(NCTX + ti * TT, TT, ti) for ti in range(8)]
                    for (t0, W, ti) in qtiles:
                        O_, OB = pO[tcnt % 2]
                        D_, DB = pD[tcnt % 2]
                        o_, oB = ot[tcnt % 2]
                        tcnt += 1
                        if ti < 0:
                            chunks = [(0, None), (1, None)]
                        else:
                            lo, hi = max(0, 4 * ti - 2), min(31, 4 * ti + 5)
                            chunks = [(2 + c, c) for c in range(lo, hi + 1)] + [(0, None), (1, None)]
                        steps = [(hd, ci, kch, c) for hd in range(2) for ci, (kch, c) in enumerate(chunks)]
                        slots = {}

                        def emitS(st):
                            nonlocal scnt
                            hd, ci, kch, c = st
                            pb_ = 64 * hd
                            S_, SB = pS[scnt % 3]
                            p_, pB = pt[scnt % 3]
                            m_, mB = sbm[scnt % 2]
                            scnt += 1
                            slots[st] = (p_, pB)
                            k.mm(S_[:, :W], kT[pb_:pb_ + 64, kch * 128:(kch + 1) * 128], qT[pb_:pb_ + 64, t0:t0 + W],
                                 True, True, [kTB, qTB], [SB])
                            if c is None:
                                k.act(p_[:, :W], S_[:, :W], AF.Exp, [SB], [pB])
                                return
                            R = 8 * ti
                            J0 = 10 - 2 * c + R
                            if ti == 0:
                                segs = [(0, 4, "U" if c <= 3 else "N"), (4, 8, "T")]
                            elif ti == 7:
                                segs = [(0, 5, "T"), (5, 8, "U" if c >= 28 else "N")]
                            else:
                                segs = [(0, 8, "T")]
                            for (b0, b1, kind) in segs:
                                c0, c1 = b0 * 64, b1 * 64
                                if kind == "N":
                                    in1 = negt[:, 0:c1 - c0]
                                    rb = [negB]
                                else:
                                    tix = 0 if kind == "T" else 1
                                    in1 = tb[:, tix, hd, J0 + b0:J0 + b1, :].rearrange("p a b -> p (a b)")
                                    rb = [tbB]
                                k.tt("dve", m_[:, c0:c1], S_[:, c0:c1], in1, ALU.add, [SB] + rb, [mB])
                            k.act(p_[:, :W], m_[:, :W], AF.Exp, [mB], [pB])

                        def emitO(st):
                            hd, ci, kch, c = st
                            pb_ = 64 * hd
                            p_, pB = slots.pop(st)
                            first, last = ci == 0, ci == len(chunks) - 1
                            k.mm(O_[pb_:pb_ + 64, :W], vv[:, kch, pb_:pb_ + 64], p_[:, :W], first, last, [vvB, pB], [OB])
                            k.mm(D_[pb_:pb_ + 64, :W], self.ones_bf[:, 0:64], p_[:, :W], first, last, [self.onesB, pB], [DB])

                        LA = 2
                        for i_ in range(len(steps) + LA):
                            if i_ < len(steps):
                                emitS(steps[i_])
                            if i_ - LA >= 0:
                                emitO(steps[i_ - LA])
                        k.op("dve", lambda h: h.reciprocal(out=rd[:, :W], in_=D_[:, :W]), [DB], [rdB])
                        k.tt("dve", o_[:, :W], O_[:, :W], rd[:, :W], ALU.mult, [OB, rdB], [oB])
                        k.dma(os_[s, hp * 128:(hp + 1) * 128, t0:t0 + W], o_[:, :W], reads=[oB])

    def pool_pass(self, l, pw, include_ctx=True):
        k, dr = self.k, self.dr
        xs, hs = dr["xs"], dr["hs"]
        tl = tiles_for(include_ctx)
        PADW = 80
        with Pool_(k) as P:
            Wp, WpB = P.sb([128, 4, 2, 256], BF16, "Wp")
            k.dma(Wp[:], pw.rearrange("g (kc p) f -> p g kc f", p=128), writes=[WpB], q="pool")
            ls, lsB = P.sb([128, KC], F32, "ls")
            k.dma(ls[:], dr["pool_scale"][:, :], writes=[lsB])
            ic, icB = P.sb([128, 4, 2, 256], F32, "ic")
            k.dma(ic[:], dr["pool_ic"][:, :, :, :], writes=[icB])
            ht = [P.sb([128, KC, TT], BF16, f"ht{i}") for i in range(2)]
            lv = [P.sb([128, 8 * PADW], F32, f"lv{i}") for i in range(3)]
            lvc = [P.sb([128, 272], F32, f"lvc{i}") for i in range(3)]
            pl, plB = P.sb([128, KC, TT], BF16, "pl")
            plBs = [Buf(f"pl{i}") for i in range(KC)]
            mn, mnB = P.sb([128, TT], F32, "mn")
            yt = [P.sb([128, TT], F32, f"yt{i}") for i in range(2)]
            py = [P.ps(f"py{i}") for i in range(2)]
            for t_, B_ in lv + lvc:
                k.memset("pool", t_[:], 0.0, [B_])

            def load(i):
                s, t0, W, c = tl[i]
                t, B = ht[i % 2]
                k.dma(t[:, :, :W], hs[s].rearrange("(kc p) t -> p kc t", p=128)[:, :, t0:t0 + W], writes=[B])

            load(0)
            ycnt = 0
            for i, (s, t0, W, c) in enumerate(tl):
                if i + 1 < len(tl):
                    load(i + 1)
                h, hB = ht[i % 2]
                j = self.jidx(s, c)
                rows, rl, pw_, ri = (1, 256, 272, 1) if c else (8, 64, PADW, 0)

                def view(t, off, n=rl):
                    return t[:, 0:rows * pw_].rearrange("p (r w) -> p r w", w=pw_)[:, :, 8 + off:8 + off + n]

                for kc in range(KC):
                    gi = kc // 2
                    win = (2, 4, 8, 16)[gi]
                    a_, aB = (lvc if c else lv)[0]
                    b_, bB = (lvc if c else lv)[1]
                    c_, cB = (lvc if c else lv)[2]
                    hv = h[:, kc, :W].rearrange("p (r w) -> p r w", w=rl)
                    k.copy("pool", view(a_, 0), hv, [hB], [aB])
                    n2 = rl + 14
                    k.tt("pool", view(b_, -7, n2), view(a_, -8, n2), view(a_, -7, n2), ALU.add, [aB], [bB])
                    cur, curB = b_, bB
                    if win >= 4:
                        n4 = rl + 12
                        k.tt("pool", view(c_, -6, n4), view(b_, -7, n4), view(b_, -5, n4), ALU.add, [bB], [cB])
                        cur, curB = c_, cB
                    if win >= 8:
                        n8 = rl + 8
                        k.tt("dve", view(b_, -4, n8), view(c_, -6, n8), view(c_, -2, n8), ALU.add, [cB], [bB])
                        cur, curB = b_, bB
                    if win >= 16:
                        k.tt("dve", view(c_, 0), view(b_, -4), view(b_, 4), ALU.add, [bB], [cB])
                        cur, curB = c_, cB
                    mv = mn[:, :W].rearrange("p (r w) -> p r w", w=rl)
                    icv = ic[:, gi, ri, 0:rl]
                    for r in range(rows):
                        k.tt("dve", mv[:, r, :], view(cur, 0)[:, r, :], icv, ALU.mult, [curB, icB], [mnB])
                    k.tt("dve", pl[:, kc, :W], mn[:, :W], h[:, kc, :W], ALU.subtract, [mnB, hB], [plBs[kc]])
                for oc in range(KC):
                    gi = oc // 2
                    y_, yB = py[ycnt % 2]
                    yo, yoB = yt[ycnt % 2]
                    ycnt += 1
                    for kk in range(2):
                        k.mm(y_[:, :W], Wp[:, gi, kk, (oc % 2) * 128:(oc % 2 + 1) * 128], pl[:, 2 * gi + kk, :W], kk == 0, kk == 1,
                             [WpB, plBs[2 * gi + kk]], [yB])
                    k.ts("dve", yo[:, :W], y_[:, :W], self.mod[:, l, 16 + oc, j:j + 1], ls[:, oc:oc + 1], ALU.mult, ALU.mult,
                         [yB, self.modB, lsB], [yoB])
                    k.dma(xs[s, oc * 128:(oc + 1) * 128, t0:t0 + W], yo[:, :W], reads=[yoB], q="pool", accum_op=ALU.add)


BLK = 512
GF = 896
NG = DFE // GF
I32 = mybir.dt.int32


def moe_layer(pg, l, f, include_ctx):
    k, dr = pg.k, pg.dr
    tl = tiles_for(include_ctx)
    subt = [(s, t0 + a * 128, c) for (s, t0, W, c) in tl for a in range(W // 128)]
    NSUB = len(subt)
    NB = NSUB * 2 * 128 // BLK + NEXP
    w1f = dr[f"moe_w1_{f}"].rearrange("e k (g c) -> (e k g) c", c=GF)
    w3f = dr[f"moe_w3_{f}"].rearrange("e k (g c) -> (e k g) c", c=GF)
    w2f = dr[f"moe_w2_{f}"].rearrange("e f d -> (e f) d")
    hbuf, obuf = dr["hbuf"], dr["obuf"]
    with Pool_(k) as PM:
        cum = PM.sb([128, NEXP], F32, "cum")
        tris = PM.sb([128, 128], F32, "tris")
        starts = PM.sb([128, NEXP], F32, "starts")
        posall = PM.sb([128, NSUB, 2], I32, "posall")
        pwall = PM.sb([128, NSUB, 2], F32, "pwall")
        widx1 = PM.sb([128, NB, NG, KC], I32, "widx1")
        widx2 = PM.sb([128, NB, 28], I32, "widx2")
        k.memset("pool", cum[0][:], 0.0, [cum[1]])
        k.dma(tris[0][:], dr["tris"][:, :], writes=[tris[1]])
        pg.norm_pass(l, 1, include_ctx=include_ctx, router=dr["moe_router"][f], moe={"cum": cum, "tris": tris})
        with Pool_(k) as P:
            thr, thrB = P.sb([128, 48], F32, "thr"); k.dma(thr[:], dr["thr48"][:, :], writes=[thrB])
            io, ioB = P.sb([128, 48], F32, "io"); k.dma(io[:], dr["iota48"][:, :], writes=[ioB])
            pk1, pk1B = P.sb([128, KC], F32, "pk1"); k.dma(pk1[:], dr["pk1"][:, :], writes=[pk1B])
            pk2, pk2B = P.sb([128, 28], F32, "pk2"); k.dma(pk2[:], dr["pk2"][:, :], writes=[pk2B])
            tmp, tmpB = P.sb([128, 48], F32, "tmp")
            nblk, nblkB = P.sb([128, NEXP], F32, "nblk")
            sblk, sblkB = P.sb([128, NEXP], F32, "sblk")
            eb, ebB = P.sb([128, 48], F32, "eb")
            w1f_, w1fB = P.sb([128, NB, NG, KC], F32, "w1f")
            w2f_, w2fB = P.sb([128, NB, 28], F32, "w2f")
            for e in range(NEXP):
                k.ts("dve", tmp[:], thr[:], cum[0][:, e:e + 1], None, ALU.is_lt, None, [thrB, cum[1]], [tmpB])
                k.op("dve", lambda h: h.reduce_sum(out=nblk[:, e:e + 1], in_=tmp[:], axis=mybir.AxisListType.X), [tmpB], [nblkB])
            k.memset("pool", sblk[:], 0.0, [sblkB])
            for e in range(1, NEXP):
                k.tt("dve", sblk[:, e:e + 1], sblk[:, e - 1:e], nblk[:, e - 1:e], ALU.add, [sblkB, nblkB], [sblkB])
            k.ts("dve", starts[0][:], sblk[:], float(BLK), None, ALU.mult, None, [sblkB], [starts[1]])
            k.memset("pool", eb[:], -1.0, [ebB])
            for e in range(NEXP):
                k.ts("dve", tmp[:], io[:], sblk[:, e:e + 1], None, ALU.is_ge, None, [ioB, sblkB], [tmpB])
                k.tt("dve", eb[:], eb[:], tmp[:], ALU.add, [ebB, tmpB], [ebB])
            e4, e4B = P.sb([128, 48], F32, "e4")
            e35, e35B = P.sb([128, 48], F32, "e35")
            k.ts("dve", e4[:], eb[:], 4096.0, None, ALU.mult, None, [ebB], [e4B])
            k.ts("dve", e35[:], eb[:], float(DFE), None, ALU.mult, None, [ebB], [e35B])
            for g in range(NG):
                for kc in range(KC):
                    k.ts("dve", w1f_[:, :, g, kc], e4[:, :NB], pk1[:, kc:kc + 1], None, ALU.add, None, [e4B, pk1B], [w1fB])
            for g in range(1, NG):
                k.ts("dve", w1f_[:, :, g, :], w1f_[:, :, g, :], float(g), None, ALU.add, None, [w1fB], [w1fB])
            for fc in range(28):
                k.ts("dve", w2f_[:, :, fc], e35[:, :NB], pk2[:, fc:fc + 1], None, ALU.add, None, [e35B, pk2B], [w2fB])
            k.copy("dve", widx1[0][:], w1f_[:], [w1fB], [widx1[1]])
            k.copy("dve", widx2[0][:], w2f_[:], [w2fB], [widx2[1]])
            if "widx_dbg" in dr:
                k.dma(dr["widx_dbg"][:, 0:NB * NG * KC], widx1[0][:].rearrange("p a b c -> p (a b c)"), reads=[widx1[1]])
        with Pool_(k) as P:
            z, zB = P.sb([128, 4, D], BF16, "z")
            k.memset("pool", z[:], 0.0, [zB])
            for b in range(NB):
                k.dma(hbuf[b * BLK:(b + 1) * BLK, :].rearrange("(a p) d -> p a d", p=128), z[:], reads=[zB])
            k.barrier()
            ht = [P.sb([128, D], BF16, f"sht{i}") for i in range(3)]
            inf = [P.sb([128, 24], F32, f"inf{i}") for i in range(3)]
            t8, t8B = P.sb([128, 2, NEXP], F32, "t8")
            pf, pfB = P.sb([128, 2], F32, "pf")

            def loadi(i):
                s, tk, c = subt[i]
                k.dma(inf[i % 3][0][:], dr["rinfo"][s, tk:tk + 128, :], writes=[inf[i % 3][1]])

            def loadh(i):
                s, tk, c = subt[i]
                k.dma(ht[i % 3][0][:], dr["htok"][s, tk:tk + 128, :], writes=[ht[i % 3][1]])

            loadi(0)
            for i in range(NSUB):
                if i + 1 < NSUB:
                    loadi(i + 1)
                n_, nB_ = inf[i % 3]
                k.tt("dve", t8[:, 0, :], n_[:, 0:8], starts[0][:], ALU.mult, [nB_, starts[1]], [t8B])
                k.tt("dve", t8[:, 1, :], n_[:, 8:16], starts[0][:], ALU.mult, [nB_, starts[1]], [t8B])
                k.op("dve", lambda h: h.reduce_sum(out=pf[:, 0:2], in_=t8[:, :, :], axis=mybir.AxisListType.X), [t8B], [pfB])
                k.tt("dve", pf[:], pf[:], n_[:, 16:18], ALU.add, [pfB, nB_], [pfB])
                k.copy("dve", posall[0][:, i, :], pf[:], [pfB], [posall[1]])
                k.copy("dve", pwall[0][:, i, :], n_[:, 18:20], [nB_], [pwall[1]])
            k.barrier()
            loadh(0)
            for i in range(NSUB):
                if i + 1 < NSUB:
                    loadh(i + 1)
                h_, hB_ = ht[i % 3]
                for j in range(2):
                    k.idma(hbuf[:, :], h_[:], posall[0][:, i, j:j + 1], gather=False, reads=[hB_, posall[1]])
        with Pool_(k) as P:
            Wg = []
            for i in range(2):
                Wg.append(((P.sb([128, KC, GF], BF16, f"W1g{i}")[0], [Buf() for _ in range(KC)]),
                           (P.sb([128, KC, GF], BF16, f"W3g{i}")[0], [Buf() for _ in range(KC)]),
                           (P.sb([128, 7, D], BF16, f"W2g{i}")[0], [Buf() for _ in range(7)])))
            hblk = [P.sb([128, 4, D], BF16, f"hblk{i}") for i in range(1)] * 2
            hT = [P.sb([128, KC, BLK], BF16, f"hT{i}") for i in range(2)]
            gT = [P.sb([128, 7, BLK], BF16, f"gT{i}") for i in range(2)]
            yacc = [P.sb([128, 4, D], F32, f"yacc{i}") for i in range(1)] * 2
            sa = [P.sb([128, BLK], F32, f"sa{i}") for i in range(2)]
            pa = [P.ps(f"pa{i}") for i in range(2)]
            pb = [P.ps(f"pb{i}") for i in range(2)]
            py = [P.ps(f"py{i}") for i in range(2)]
            pth, pthB = P.ps("pth")
            groups = [(b, g) for b in range(NB) for g in range(NG)]

            def wload(gi):
                b, g = groups[gi]
                (W1, W1B), (W3, W3B), (W2, W2B) = Wg[gi % 2]
                for kc in range(KC):
                    k.idma(W1[:, kc, :], w1f[:, :], widx1[0][:, b, g, kc:kc + 1], gather=True, reads=[widx1[1]], writes=[W1B[kc]])
                    k.idma(W3[:, kc, :], w3f[:, :], widx1[0][:, b, g, kc:kc + 1], gather=True, reads=[widx1[1]], writes=[W3B[kc]])
                for j in range(7):
                    k.idma(W2[:, j, :], w2f[:, :], widx2[0][:, b, g * 7 + j:g * 7 + j + 1], gather=True, reads=[widx2[1]], writes=[W2B[j]])

            def hload(b):
                k.dma(hblk[b % 2][0][:], hbuf[b * BLK:(b + 1) * BLK, :].rearrange("(a p) d -> p a d", p=128), writes=[hblk[b % 2][1]])

            wload(0)
            hload(0)
            cnt = 0
            ycnt = 0
            for gi, (b, g) in enumerate(groups):
                if gi + 1 < len(groups):
                    wload(gi + 1)
                (W1, W1B), (W3, W3B), (W2, W2B) = Wg[gi % 2]
                h_, hB_ = hT[b % 2]
                ya, yaB = yacc[b % 2]
                if g == 0:
                    hb_, hbB_ = hblk[b % 2]
                    for kc in range(KC):
                        for a in range(4):
                            k.op("pe", lambda h: h.transpose(pth[:, 0:256].bitcast(BF16)[:, a * 128:(a + 1) * 128],
                                                             hb_[:, a, kc * 128:(kc + 1) * 128], pg.id_bf[:]), [hbB_, pg.idbB], [pthB])
                        if kc % 2 == 0:
                            k.copy("act", h_[:, kc, :], pth[:, 0:256].bitcast(BF16)[:, 0:BLK], [pthB], [hB_])
                        else:
                            k.copy("dve", h_[:, kc, :], pth[:, 0:256].bitcast(BF16)[:, 0:BLK], [pthB], [hB_])
                    if b + 1 < NB:
                        hload(b + 1)
                g_, gB_ = gT[gi % 2]
                for j in range(7):
                    a_, aB = pa[cnt % 2]; b_, bB = pb[cnt % 2]; s_, sB = sa[cnt % 2]
                    cnt += 1
                    for kc in range(KC):
                        k.mm(a_[:, :], W1[:, kc, j * 128:(j + 1) * 128], h_[:, kc, :], kc == 0, kc == KC - 1, [W1B[kc], hB_], [aB])
                    for kc in range(KC):
                        k.mm(b_[:, :], W3[:, kc, j * 128:(j + 1) * 128], h_[:, kc, :], kc == 0, kc == KC - 1, [W3B[kc], hB_], [bB])
                    k.act(s_[:], a_[:, :], AF.Silu, [aB], [sB])
                    k.tt("dve", g_[:, j, :], b_[:, :], s_[:], ALU.mult, [bB, sB], [gB_])
                for a in range(4):
                    for hf_ in range(2):
                        y_, yB = py[ycnt % 2]
                        ycnt += 1
                        for j in range(7):
                            k.mm(y_[:, :], g_[:, j, a * 128:(a + 1) * 128], W2[:, j, hf_ * 512:(hf_ + 1) * 512], j == 0, j == 6, [gB_, W2B[j]], [yB])
                        dst = ya[:, a, hf_ * 512:(hf_ + 1) * 512]
                        if g == 0:
                            k.copy("act", dst, y_[:, :], [yB], [yaB])
                        else:
                            k.tt("dve", dst, dst, y_[:, :], ALU.add, [yaB, yB], [yaB])
                if g == NG - 1:
                    k.dma(obuf[b * BLK:(b + 1) * BLK, :].rearrange("(a p) d -> p a d", p=128), ya[:], reads=[yaB])
        with Pool_(k) as P:
            r1 = [P.sb([128, D], F32, f"r1_{i}") for i in range(2)]
            r2 = [P.sb([128, D], F32, f"r2_{i}") for i in range(2)]
            yo = [P.sb([128, KC, 128], F32, f"yo{i}") for i in range(2)]
            pt = [P.ps(f"pt{i}") for i in range(2)]

            def gload(i):
                k.idma(r1[i % 2][0][:], obuf[:, :], posall[0][:, i, 0:1], gather=True, reads=[posall[1]], writes=[r1[i % 2][1]])
                k.idma(r2[i % 2][0][:], obuf[:, :], posall[0][:, i, 1:2], gather=True, reads=[posall[1]], writes=[r2[i % 2][1]])

            gload(0)
            for i, (s, tk, c) in enumerate(subt):
                if i + 1 < NSUB:
                    gload(i + 1)
                a_, aB = r1[i % 2]; b_, bB = r2[i % 2]; o_, oB = yo[i % 2]
                j = pg.jidx(s, c)
                k.ts("dve", a_[:], a_[:], pwall[0][:, i, 0:1], None, ALU.mult, None, [aB, pwall[1]], [aB])
                k.stt(a_[:], b_[:], pwall[0][:, i, 1:2], a_[:], ALU.mult, ALU.add, [bB, pwall[1], aB], [aB])
                for hf_ in range(2):
                    p_, pB = pt[hf_]
                    for q in range(4):
                        oc = hf_ * 4 + q
                        k.op("pe", lambda h: h.transpose(p_[:, q * 128:(q + 1) * 128], a_[:, oc * 128:(oc + 1) * 128], pg.id_f[:]),
                             [aB, pg.idfB], [pB])
                    for q in range(4):
                        oc = hf_ * 4 + q
                        if q % 2 == 0:
                            k.act(o_[:, oc, :], p_[:, q * 128:(q + 1) * 128], AF.Identity, [pB, pg.modB], [oB], scale=pg.mod[:, l, 40 + oc, j:j + 1])
                        else:
                            k.ts("dve", o_[:, oc, :], p_[:, q * 128:(q + 1) * 128], pg.mod[:, l, 40 + oc, j:j + 1], None, ALU.mult, None,
                                 [pB, pg.modB], [oB])
                k.dma(dr["xs"][s].rearrange("(kc p) t -> p kc t", p=128)[:, :, tk:tk + 128], o_[:], reads=[oB], q="pool", accum_op=ALU.add)


def build(stop_after=99, debug=False):
    nc = bass.Bass("TRN2", target_bir_lowering=False)
    dr = {}

    def inp(name, shape, dt=F32):
        dr[name] = nc.dram_tensor(name, list(shape), dt, kind="ExternalInput").ap()

    def scr(name, shape, dt, out=False):
        dr[name] = nc.dram_tensor(name, list(shape), dt, kind="ExternalOutput" if out else "Internal").ap()

    inp("xin", [NS, D, TOK]); inp("cvec", [128, KC, 3]); inp("ada_w", [DEPTH, D, 6 * D]); inp("ada_b", [128, DEPTH, 48])
    inp("norm_g", [128, DEPTH, 2, KC]); inp("final_g", [128, KC])
    inp("ones_bf", [128, 128], BF16); inp("id_bf", [128, 128], BF16); inp("id_f", [128, 128]); inp("ones_f", [128, 128])
    inp("na_w_qkv", [2, D, 3 * D]); inp("na_w_o", [2, D, D]); inp("na_tab", [2, 8, 128, 2, 2, 22, 64])
    inp("ffn_w1", [2, D, DFF]); inp("ffn_w3", [2, D, DFF]); inp("ffn_w2", [2, DFF, D])
    inp("moe_router", [2, 128, KC, NEXP])
    for f_ in range(2):
        inp(f"moe_w1_{f_}", [NEXP, D, DFE]); inp(f"moe_w3_{f_}", [NEXP, D, DFE]); inp(f"moe_w2_{f_}", [NEXP, DFE, D])
    inp("tris", [128, 128]); inp("thr48", [128, 48]); inp("iota48", [128, 48]); inp("pk1", [128, KC]); inp("pk2", [128, 28])
    inp("pool_w", [4, 256, 256]); inp("pool_scale", [128, KC]); inp("pool_ic", [128, 4, 2, 256])
    gdn_inputs(inp)
    scr("xs", [NS, D, TOK], F32, out=debug)
    scr("hs", [NS, D, TOK], BF16)
    scr("qs", [NS, D, TOK], BF16); scr("ks", [NS, D, TOK], BF16); scr("vs", [NS, TOK, D], BF16); scr("os", [NS, D, TOK], BF16)
    scr("wexp", [NS, NEXP, TOK], F32)
    scr("htok", [NS, TOK, D], BF16, out=debug); scr("rinfo", [NS, TOK, 24], F32, out=debug)
    scr("hbuf", [42 * BLK, D], BF16, out=debug); scr("obuf", [42 * BLK, D], F32, out=debug)

    gdn_scratch(scr)
    scr("out", [NS, D, NLAT], F32, out=True)

    with ExitStack() as es:
        k = K(nc, es)
        pg = Prog(nc, k, dr, stop_after)
        with Pool_(k) as PC:
            pg.setup_consts(PC)
            for s in range(NS):
                for kc in range(KC):
                    k.dma(dr["xs"][s, kc * 128:(kc + 1) * 128, :], dr["xin"][s, kc * 128:(kc + 1) * 128, :])
            pg.adaln()
            k.barrier()
            step = 0
            for l in range(DEPTH):
                last = l == DEPTH - 1
                if step >= stop_after:
                    break
                pg.norm_pass(l, 0, include_ctx=True)
                kind = l % 3
                jx = l // 3
                if kind == 0:
                    pg.na_qkv_pass(dr["na_w_qkv"][jx])
                    pg.na_attn_pass(dr["na_tab"][jx], need_ctx=not last)
                    pg.outproj_pass(l, dr["os"], dr["na_w_o"][jx], include_ctx=not last, gate_off=16)
                elif kind == 1:
                    gdn_mixer(pg, l, need_ctx=not last)
                else:
                    pg.pool_pass(l, dr["pool_w"], include_ctx=not last)
                step += 1
                if step >= stop_after:
                    break
                f = l // 2
                if l % 2 == 0:
                    pg.norm_pass(l, 1, include_ctx=not last)
                    H = DFF // 2
                    for hh in range(2):
                        pg.ffn_pass(l, dr["ffn_w1"][f][:, hh * H:(hh + 1) * H], dr["ffn_w3"][f][:, hh * H:(hh + 1) * H],
                                    dr["ffn_w2"][f][hh * H:(hh + 1) * H, :], H, include_ctx=not last)
                else:
                    moe_layer(pg, l, f, include_ctx=not last)
                step += 1
            pg.norm_pass(0, 0, include_ctx=False, final=True)
            k.barrier()
    return nc


GDN_IN = 4128
NCHK = TOK // 128


def gdn_inputs(inp):
    inp("gdn_w_in", [D, GDN_IN]); inp("gdn_w_o", [D, D]); inp("gdn_cw", [128, 24, 4])
    inp("gdn_dtb", [128, 16]); inp("gdn_alog", [128, 16]); inp("gdn_ng", [128, 1])
    inp("gdn_tri", [128, 2, 128]); inp("gdn_gm", [128, 2, 3, 128]); inp("gdn_ms", [128, 7, 128], BF16)


def gdn_scratch(scr):
    scr("pj", [NS, 4 * D, TOK], BF16)
    scr("gq", [NS, D, TOK], BF16); scr("gk", [NS, D, TOK], BF16); scr("gv", [NS, D, TOK], BF16)
    scr("gt", [NS, TOK, 48], F32)
    scr("gcs", [NS, 16, TOK], F32)
    scr("gbs", [NS, 16, TOK], F32)


def gdn_proj_pass(pg):
    k, dr = pg.k, pg.dr
    hs, pj = dr["hs"], dr["pj"]
    tl = tiles_for(True)
    with Pool_(k) as P:
        Wi, WiB = P.sb([128, KC, GDN_IN], BF16, "Wi")
        src = dr["gdn_w_in"].rearrange("(kc p) n -> p kc n", p=128)
        for i in range(4):
            k.dma(Wi[:, :, i * 1032:(i + 1) * 1032], src[:, :, i * 1032:(i + 1) * 1032], writes=[WiB], q="pool")
        dtb, dtbB = P.sb([128, 16], F32, "dtb"); k.dma(dtb[:], dr["gdn_dtb"][:, :], writes=[dtbB])
        nA, nAB = P.sb([128, 16], F32, "nA"); k.dma(nA[:], dr["gdn_alog"][:, :], writes=[nAB])
        k.act(nA[:], nA[:], AF.Exp, [nAB], [nAB])
        k.ts("dve", nA[:], nA[:], -1.0, None, ALU.mult, None, [nAB], [nAB])
        tri, triB = P.sb([128, 2, 128], F32, "tri"); k.dma(tri[:], dr["gdn_tri"][:, :, :], writes=[triB])
        ht = [P.sb([128, KC, TT], BF16, f"ht{i}") for i in range(2)]
        pt = [P.sb([128, 32, TT], BF16, f"pjt{i}") for i in range(1)]
        gtt, gttB = P.sb([128, 4, 48], F32, "gtt")
        tmpa, tmpaB = P.sb([128, 16], F32, "tmpa")
        gcT, gcTB = P.sb([16, TT], F32, "gcT")
        bT, bTB = P.sb([16, TT], F32, "bT")
        pp = [P.ps(f"pp{i}") for i in range(4)]
        pab, pabB = P.ps("pab")
        pgc, pgcB = P.ps("pgc")
        ptr, ptrB = P.ps("ptr")
        pbf, pbfB = P.ps("pbf")

        def load(i):
            s, t0, W, c = tl[i]
            t, B = ht[i % 2]
            k.dma(t[:, :, :W], hs[s].rearrange("(kc p) t -> p kc t", p=128)[:, :, t0:t0 + W], writes=[B])

        load(0)
        cnt = 0
        for i, (s, t0, W, c) in enumerate(tl):
            if i + 1 < len(tl):
                load(i + 1)
            h, hB = ht[i % 2]
            p_, pjB = pt[0]
            for oc in range(32):
                q_, qB = pp[cnt % 4]
                for kc in range(KC):
                    k.mm(q_[:, :W], Wi[:, kc, oc * 128:(oc + 1) * 128], h[:, kc, :W], kc == 0, kc == KC - 1, [WiB, hB], [qB])
                if cnt % 2 == 0:
                    k.act(p_[:, oc, :W], q_[:, :W], AF.Copy, [qB], [pjB])
                else:
                    k.copy("dve", p_[:, oc, :W], q_[:, :W], [qB], [pjB])
                cnt += 1
            k.dma(pj[s].rearrange("(oc p) t -> p oc t", p=128)[:, :, t0:t0 + W], p_[:, :, :W], reads=[pjB])
            for kc in range(KC):
                k.mm(pbf[0:16, :W], Wi[:, kc, 4096 + 16:4096 + 32], h[:, kc, :W], kc == 0, kc == KC - 1, [WiB, hB], [pbfB])
            k.act(bT[:, :W], pbf[0:16, :W], AF.Sigmoid, [pbfB], [bTB])
            k.dma(dr["gbs"][s, :, t0:t0 + W], bT[:, :W], reads=[bTB])
            nst = W // 128
            for ts_ in range(nst):
                for kc in range(KC):
                    k.mm(pab[:, ts_ * 32:(ts_ + 1) * 32], h[:, kc, ts_ * 128:(ts_ + 1) * 128], Wi[:, kc, 4096:4128],
                         kc == 0, kc == KC - 1, [WiB, hB], [pabB])
            for ts_ in range(nst):
                k.tt("dve", tmpa[:], pab[:, ts_ * 32:ts_ * 32 + 16], dtb[:], ALU.add, [pabB, dtbB], [tmpaB])
                k.act(tmpa[:], tmpa[:], AF.Exp, [tmpaB], [tmpaB])
                k.act(tmpa[:], tmpa[:], AF.Ln, [tmpaB], [tmpaB], bias=1.0)
                k.tt("dve", gtt[:, ts_, 0:16], tmpa[:], nA[:], ALU.mult, [tmpaB, nAB], [gttB])
                k.act(gtt[:, ts_, 16:32], pab[:, ts_ * 32 + 16:ts_ * 32 + 32], AF.Sigmoid, [pabB], [gttB])
                for d in range(2):
                    k.mm(pgc[:, ts_ * 16 + d * 8:ts_ * 16 + d * 8 + 8], tri[:, d, :], gtt[:, ts_, d * 8:d * 8 + 8], True, True,
                         [triB, gttB], [pgcB])
                k.copy("dve", gtt[:, ts_, 32:48], pgc[:, ts_ * 16:ts_ * 16 + 16], [pgcB], [gttB])
                k.op("pe", lambda hh: hh.transpose(ptr[0:16, ts_ * 128:(ts_ + 1) * 128], gtt[:, ts_, 32:48], pg.id_f[:]),
                     [gttB, pg.idfB], [ptrB])
            k.copy("dve", gcT[:, :W], ptr[0:16, :W], [ptrB], [gcTB])
            k.dma(dr["gcs"][s, :, t0:t0 + W], gcT[:, :W], reads=[gcTB])
            k.dma(dr["gt"][s, t0:t0 + W, :].rearrange("(a p) c -> p a c", p=128), gtt[:, :nst, :], reads=[gttB])


def gdn_conv_pass(pg):
    k, dr = pg.k, pg.dr
    pj = dr["pj"]
    tl = tiles_for(True)
    with Pool_(k) as P:
        cw, cwB = P.sb([128, 24, 4], F32, "cw"); k.dma(cw[:], dr["gdn_cw"][:, :, :], writes=[cwB])
        e1, e1B = P.sb([128, 1], F32, "e1"); k.memset("pool", e1[:], EPS, [e1B])
        e2, e2B = P.sb([128, 1], F32, "e2"); k.memset("pool", e2[:], EPS * 128.0, [e2B])
        pin = [P.sb([128, 24, TT + 4], BF16, f"pin{i}") for i in range(2)]
        ot, otB = P.sb([128, 24, TT], BF16, "cot")
        acc = [P.sb([128, TT], F32, f"acc{i}") for i in range(2)]
        sl = [P.sb([128, TT], F32, f"sl{i}") for i in range(2)]
        sq = [P.sb([128, TT], BF16, f"sq{i}") for i in range(2)]
        rn = [P.sb([128, TT], F32, f"rn{i}") for i in range(2)]
        pss = [P.ps(f"pss{i}") for i in range(2)]

        def load(i):
            s, t0, W, c = tl[i]
            t, B = pin[i % 2]
            seq0, seq1 = (0, NCTX) if c else (NCTX, TOK)
            k.memset("pool", t[:], 0.0, [B])
            lo, hi = max(seq0, t0 - 2), min(seq1, t0 + W + 1)
            k.dma(t[:, :, lo - (t0 - 2):hi - (t0 - 2)], pj[s].rearrange("(oc p) t -> p oc t", p=128)[:, 0:24, lo:hi], writes=[B])

        load(0)
        cnt = 0
        for i, (s, t0, W, c) in enumerate(tl):
            if i + 1 < len(tl):
                load(i + 1)
            x, xB = pin[i % 2]
            for oc in range(24):
                a_, aB = acc[cnt % 2]; s_, sB = sl[cnt % 2]; q_, qB = sq[cnt % 2]; r_, rB = rn[cnt % 2]; p_, pB = pss[cnt % 2]
                cnt += 1
                k.ts("dve", a_[:, :W], x[:, oc, 0:W], cw[:, oc, 0:1], None, ALU.mult, None, [xB, cwB], [aB])
                for j in range(1, 4):
                    k.stt(a_[:, :W], x[:, oc, j:j + W], cw[:, oc, j:j + 1], a_[:, :W], ALU.mult, ALU.add, [xB, cwB, aB], [aB])
                if oc >= 16:
                    k.act(ot[:, oc, :W], a_[:, :W], AF.Silu, [aB], [otB])
                    continue
                k.act(s_[:, :W], a_[:, :W], AF.Silu, [aB], [sB])
                k.act(q_[:, :W], s_[:, :W], AF.Square, [sB], [qB])
                k.mm(p_[:, :W], pg.ones_bf[:], q_[:, :W], True, True, [pg.onesB, qB], [pB])
                if oc < 8:
                    k.act(r_[:, :W], p_[:, :W], AF.Sqrt, [pB, e2B], [rB], scale=128.0, bias=e2[:, 0:1])
                else:
                    k.act(r_[:, :W], p_[:, :W], AF.Sqrt, [pB, e1B], [rB], bias=e1[:, 0:1])
                k.op("dve", lambda hh: hh.reciprocal(out=r_[:, :W], in_=r_[:, :W]), [rB], [rB])
                k.tt("pool", ot[:, oc, :W], s_[:, :W], r_[:, :W], ALU.mult, [sB, rB], [otB])
            for gi, nm in enumerate(("gq", "gk", "gv")):
                k.dma(dr[nm][s].rearrange("(oc p) t -> p oc t", p=128)[:, :, t0:t0 + W], ot[:, gi * 8:(gi + 1) * 8, :W], reads=[otB])


def _rr(gens):
    gens = list(gens)
    while gens:
        nxt = []
        for g in gens:
            try:
                next(g)
                nxt.append(g)
            except StopIteration:
                pass
        gens = nxt


def gdn_core_pass(pg):
    k, dr = pg.k, pg.dr
    with Pool_(k) as P:
        gm, gmB = P.sb([128, 2, 3, 128], F32, "gm"); k.dma(gm[:], dr["gdn_gm"][:, :, :, :], writes=[gmB])
        ms, msB = P.sb([128, 7, 128], BF16, "ms"); k.dma(ms[:], dr["gdn_ms"][:, :, :], writes=[msB])
        gng, gngB = P.sb([128, 1], F32, "gng"); k.dma(gng[:], dr["gdn_ng"][:, :], writes=[gngB])
        e1, e1B = P.sb([128, 1], F32, "e1"); k.memset("pool", e1[:], EPS, [e1B])
        qT, qTB = P.sb([128, TOK], BF16, "qT"); kT, kTB = P.sb([128, TOK], BF16, "kT"); vT, vTB = P.sb([128, TOK], BF16, "vT")
        zT, zTB = P.sb([128, TOK], BF16, "zT")
        ktok, ktokB = P.sb([128, NCHK, 128], BF16, "ktok"); vtok, vtokB = P.sb([128, NCHK, 128], BF16, "vtok")
        gt, gtB = P.sb([128, NCHK, 48], F32, "gt")
        gcb, gcbB = P.sb([128, TOK], F32, "gcb"); bb, bbB = P.sb([128, TOK], F32, "bb")
        U, UB_ = P.sb([128, NCHK, 128], F32, "U"); UB = [Buf(f"U{n}") for n in range(NCHK)]
        WT, _ = P.sb([128, NCHK, 128], BF16, "WT"); WTB = [Buf(f"WT{n}") for n in range(NCHK)]
        AQ, _ = P.sb([128, NCHK, 128], BF16, "AQ"); AQB = [Buf(f"AQ{n}") for n in range(NCHK)]
        KTL, _ = P.sb([128, NCHK, 128], BF16, "KTL"); KTLB = [Buf(f"KTL{n}") for n in range(NCHK)]
        qd, qdB = P.sb([128, TOK], BF16, "qd")
        oacc, oaccB = P.sb([128, TOK], F32, "oacc"); oB = [Buf(f"o{n}") for n in range(NCHK)]
        egc, egcB = P.sb([128, NCHK], F32, "egc"); ett, ettB = P.sb([128, NCHK], F32, "ett"); egl, eglB = P.sb([128, NCHK], F32, "egl")
        S, SB = P.sb([128, 128], F32, "S"); Sb, SbB = P.sb([128, 128], BF16, "Sb")
        vn = [P.sb([128, 128], BF16, f"vn{i}") for i in range(2)]
        tmpx, tmpxB = P.sb([128, 512], F32, "tmpx")
        NI = 4
        wk = []
        for ii in range(NI):
            w = {}
            for nm in ("xa", "xb", "xc", "t1"):
                w[nm] = P.sb([128, 128], F32, f"{nm}{ii}")
            w["ea"], w["eb"], w["ec"] = w["xa"], w["xb"], w["xc"]
            for nm in ("L", "M", "X0", "X1", "Y0", "Y1", "Wm", "Wn", "vb", "kbg"):
                w[nm] = P.sb([128, 128], BF16, f"{nm}{ii}")
            w["pA"] = P.ps(f"pA{ii}"); w["pB"] = P.ps(f"pB{ii}"); w["pC"] = w["pB"]
            wk.append(w)
        pX, pXB = wk[0]["pA"]; pY, pYB = wk[1]["pA"]

        for s in range(NS):
            k.dma(gt[:], dr["gt"][s].rearrange("(n p) c -> p n c", p=128), writes=[gtB])
            for h in range(8):
                rows = slice(h * 128, (h + 1) * 128)
                k.dma(qT[:], dr["gq"][s, rows, :], writes=[qTB]); k.dma(kT[:], dr["gk"][s, rows, :], writes=[kTB])
                k.dma(vT[:], dr["gv"][s, rows, :], writes=[vTB])
                k.dma(zT[:], dr["pj"][s, 3 * D + h * 128:3 * D + (h + 1) * 128, :], writes=[zTB])
                for n in range(NCHK):
                    cs = slice(n * 128, (n + 1) * 128)
                    pb16 = pX[:, 0:64].bitcast(BF16) if False else None
                    k.op("pe", lambda hh: hh.transpose(pX[:, 0:128].bitcast(BF16)[:, 0:128], kT[:, cs], pg.id_bf[:]), [kTB, pg.idbB], [pXB])
                    k.copy("act", ktok[:, n, :], pX[:, 0:128].bitcast(BF16)[:, 0:128], [pXB], [ktokB])
                    k.op("pe", lambda hh: hh.transpose(pY[:, 0:128].bitcast(BF16)[:, 0:128], vT[:, cs], pg.id_bf[:]), [vTB, pg.idbB], [pYB])
                    k.copy("dve", vtok[:, n, :], pY[:, 0:128].bitcast(BF16)[:, 0:128], [pYB], [vtokB])
                for d in range(2):
                    col = d * 8 + h
                    k.dma(gcb[:], dr["gcs"][s, col:col + 1, :].partition_broadcast(128), writes=[gcbB])
                    k.dma(bb[:], dr["gbs"][s, col:col + 1, :].partition_broadcast(128), writes=[bbB])
                    gcv = gt[:, :, 32 + col]
                    btv = gt[:, :, 16 + col]
                    lastoff = 127 if d == 0 else 0
                    glv = gcb[:, lastoff:TOK:128]
                    k.act(egc[:], gcv, AF.Exp, [gtB], [egcB])
                    k.tt("dve", ett[:], glv, gcv, ALU.subtract, [gcbB, gtB], [ettB])
                    k.act(ett[:], ett[:], AF.Exp, [ettB], [ettB])
                    k.act(egl[:], glv, AF.Exp, [gcbB], [eglB])
                    for c0 in range(0, TOK, 512):
                        wd = min(512, TOK - c0)
                        k.act(tmpx[:, :wd], gcb[:, c0:c0 + wd], AF.Exp, [gcbB], [tmpxB])
                        k.tt("dve", qd[:, c0:c0 + wd], qT[:, c0:c0 + wd], tmpx[:, :wd], ALU.mult, [qTB, tmpxB], [qdB])

                    def inst(n, w):
                        cs = slice(n * 128, (n + 1) * 128)
                        gcn = gt[:, n, 32 + col:33 + col]; btn = gt[:, n, 16 + col:17 + col]
                        (xa, xaB), (xb, xbB), (xc, xcB) = w["xa"], w["xb"], w["xc"]
                        (ea, eaB), (eb, ebB), (ec, ecB) = w["ea"], w["eb"], w["ec"]
                        (t1, t1B), (L, LB), (M, MB) = w["t1"], w["L"], w["M"]
                        (pA, pAB), (pB_, pBB), (pC, pCB) = w["pA"], w["pB"], w["pC"]
                        k.stt(xa[:], gcb[:, cs], gcn, gm[:, d, 0, :], ALU.subtract, ALU.add, [gcbB, gtB, gmB], [xaB])
                        k.stt(xb[:], gcb[:, cs], gcn, gm[:, d, 1, :], ALU.subtract, ALU.add, [gcbB, gtB, gmB], [xbB])
                        k.stt(xc[:], gcb[:, cs], gcn, gm[:, d, 2, :], ALU.subtract, ALU.add, [gcbB, gtB, gmB], [xcB])
                        k.mm(pA[:, 0:128], kT[:, cs], kT[:, cs], True, True, [kTB], [pAB])
                        k.mm(pB_[:, 0:128], kT[:, cs], qT[:, cs], True, True, [kTB, qTB], [pBB])
                        yield
                        k.act(ea[:], xa[:], AF.Exp, [xaB], [eaB])
                        k.act(eb[:], xb[:], AF.Exp, [xbB], [ebB])
                        k.act(ec[:], xc[:], AF.Exp, [xcB], [ecB], scale=-1.0)
                        yield
                        k.stt(L[:], pA[:, 0:128], btn, ec[:], ALU.mult, ALU.mult, [pAB, gtB, ecB], [LB])
                        k.tt("dve", t1[:], pA[:, 0:128], eb[:], ALU.mult, [pAB, ebB], [t1B])
                        k.tt("pool", M[:], t1[:], bb[:, cs], ALU.mult, [t1B, bbB], [MB])
                        k.tt("dve", AQ[:, n, :], pB_[:, 0:128], ea[:], ALU.mult, [pBB, eaB], [AQB[n]])
                        k.ts("pool", w["vb"][0][:], vtok[:, n, :], btn, None, ALU.mult, None, [vtokB, gtB], [w["vb"][1]])
                        k.ts("pool", w["kbg"][0][:], ktok[:, n, :], btn, egc[:, n:n + 1], ALU.mult, ALU.mult, [ktokB, gtB, egcB], [w["kbg"][1]])
                        k.ts("pool", KTL[:, n, :], ktok[:, n, :], ett[:, n:n + 1], None, ALU.mult, None, [ktokB, ettB], [KTLB[n]])
                        yield
                        X, XB = w["X0"]; Y, YB = w["Y0"]; Xn, XnB = w["X1"]; Yn, YnB = w["Y1"]
                        Wm, WmB = w["Wm"]; Wn, WnB = w["Wn"]
                        k.tt("pool", Wm[:], L[:], ms[:, 0, :], ALU.mult, [LB, msB], [WmB])
                        k.tt("pool", X[:], pg.id_bf[:], Wm[:], ALU.subtract, [pg.idbB, WmB], [XB])
                        k.tt("pool", Wn[:], M[:], ms[:, 0, :], ALU.mult, [MB, msB], [WnB])
                        k.tt("pool", Y[:], pg.id_bf[:], Wn[:], ALU.subtract, [pg.idbB, WnB], [YB])
                        yield
                        for lv in range(1, 7):
                            lastlv = lv == 6
                            if not lastlv:
                                k.mm(pA[:, 0:128], M[:], X[:], True, True, [MB, XB], [pAB])
                            k.mm(pB_[:, 0:128], L[:], Y[:], True, True, [LB, YB], [pBB])
                            yield
                            if not lastlv:
                                k.tt("dve", Wm[:], pA[:, 0:128], ms[:, lv, :], ALU.mult, [pAB, msB], [WmB])
                            k.tt("dve", Wn[:], pB_[:, 0:128], ms[:, lv, :], ALU.mult, [pBB, msB], [WnB])
                            yield
                            if not lastlv:
                                k.mm(pA[:, 0:128], Y[:], Wm[:], True, True, [YB, WmB], [pAB])
                            k.mm(pC[:, 0:128], X[:], Wn[:], True, True, [XB, WnB], [pCB])
                            yield
                            if not lastlv:
                                k.tt("dve", Xn[:], X[:], pA[:, 0:128], ALU.subtract, [XB, pAB], [XnB])
                            k.tt("dve", Yn[:], Y[:], pC[:, 0:128], ALU.subtract, [YB, pCB], [YnB])
                            X, XB, Xn, XnB = Xn, XnB, X, XB
                            Y, YB, Yn, YnB = Yn, YnB, Y, YB
                            yield
                        k.mm(pA[:, 0:128], Y[:], w["vb"][0][:], True, True, [YB, w["vb"][1]], [pAB])
                        k.mm(pB_[:, 0:128], w["kbg"][0][:], Y[:], True, True, [w["kbg"][1], YB], [pBB])
                        yield
                        k.copy("act", U[:, n, :], pA[:, 0:128], [pAB], [UB[n]])
                        k.copy("act", WT[:, n, :], pB_[:, 0:128], [pBB], [WTB[n]])
                        yield

                    for n0 in range(0, NCHK, NI):
                        _rr([inst(n0 + ii, wk[ii]) for ii in range(NI) if n0 + ii < NCHK])
                    k.memset("pool", S[:], 0.0, [SB]); k.memset("pool", Sb[:], 0.0, [SbB])
                    order = [0, 1] + list(range(2, NCHK)) if d == 0 else [1, 0] + list(range(NCHK - 1, 1, -1))
                    for it, n in enumerate(order):
                        cs = slice(n * 128, (n + 1) * 128)
                        v_, vB_ = vn[it % 2]
                        k.mm(pX[:, 0:128], WT[:, n, :], Sb[:], True, True, [WTB[n], SbB], [pXB])
                        k.tt("dve", v_[:], U[:, n, :], pX[:, 0:128], ALU.subtract, [UB[n], pXB], [vB_])
                        k.mm(pY[:, 0:128], Sb[:], qd[:, cs], True, False, [SbB, qdB], [pYB])
                        k.mm(pY[:, 0:128], v_[:], AQ[:, n, :], False, True, [vB_, AQB[n]], [pYB])
                        if d == 0:
                            k.copy("act", oacc[:, cs], pY[:, 0:128], [pYB], [oB[n]])
                        else:
                            k.tt("dve", oacc[:, cs], oacc[:, cs], pY[:, 0:128], ALU.add, [oB[n], pYB], [oB[n]])
                        k.mm(pX[:, 0:128], KTL[:, n, :], v_[:], True, True, [KTLB[n], vB_], [pXB])
                        k.stt(S[:], S[:], egl[:, n:n + 1], pX[:, 0:128], ALU.mult, ALU.add, [SB, eglB, pXB], [SB])
                        k.copy("act", Sb[:], S[:], [SB], [SbB])
                for c0 in range(0, TOK, 512):
                    wd = min(512, TOK - c0)
                    obs = oB[c0 // 128:(c0 + wd) // 128]
                    xa, xaB = wk[0]["xa"]; xb, xbB = wk[0]["xb"]
                    sqt, sqB = P_sq = (qd, qdB)
                    k.act(sqt[:, c0:c0 + wd], oacc[:, c0:c0 + wd], AF.Square, obs, [qdB])
                    k.mm(pX[:, :wd], pg.ones_bf[:], sqt[:, c0:c0 + wd], True, True, [pg.onesB, qdB], [pXB])
                    k.act(tmpx[:, :wd], pX[:, :wd], AF.Sqrt, [pXB, e1B], [tmpxB], scale=1.0 / 128.0, bias=e1[:, 0:1])
                    k.op("dve", lambda hh: hh.reciprocal(out=tmpx[:, :wd], in_=tmpx[:, :wd]), [tmpxB], [tmpxB])
                    k.tt("dve", oacc[:, c0:c0 + wd], oacc[:, c0:c0 + wd], tmpx[:, :wd], ALU.mult, obs + [tmpxB], obs)
                    k.act(tmpx[:, :wd], zT[:, c0:c0 + wd], AF.Silu, [zTB], [tmpxB])
                    k.stt(qd[:, c0:c0 + wd], oacc[:, c0:c0 + wd], gng[:, 0:1], tmpx[:, :wd], ALU.mult, ALU.mult, obs + [gngB, tmpxB], [qdB])
                k.dma(dr["os"][s, rows, :], qd[:], reads=[qdB])


def gdn_mixer(pg, l, need_ctx):
    gdn_proj_pass(pg)
    gdn_conv_pass(pg)
    gdn_core_pass(pg)
    pg.outproj_pass(l, pg.dr["os"], pg.dr["gdn_w_o"], include_ctx=need_ctx, gate_off=16)


def _fm(v):
    v = np.asarray(v, np.float32)
    lead = v.shape[:-1]
    return np.ascontiguousarray(np.moveaxis(v.reshape(lead + (KC, 128)), -1, 0))


def _na_tables(rpb):
    col = np.arange(64)
    c0 = np.clip(col - 8, 0, 48)
    in_win = (col[None, :] >= c0[:, None]) & (col[None, :] < c0[:, None] + 16)
    dc = np.clip(col[None, :] - col[:, None], -15, 15) + 15
    out = np.full((8, 128, 2, 2, 22, 64), NEG, np.float32)
    for jj in range(22):
        j = 17 - jj
        for a in range(2):
            drr = j + a
            if drr < 0 or drr > 14:
                continue
            base = np.where(in_win[None], rpb[:, drr][:, dc], NEG).astype(np.float32)
            base = np.transpose(base, (0, 2, 1))
            for h in range(16):
                out[h // 2, a * 64:(a + 1) * 64, 1, h % 2, jj, :] = base[h]
                if 3 <= drr <= 10:
                    out[h // 2, a * 64:(a + 1) * 64, 0, h % 2, jj, :] = base[h]
    return out


def _pool_ic():
    out = np.ones((4, 2, 256), np.float32)
    for gi, win in enumerate((2, 4, 8, 16)):
        for ri, T in enumerate((64, 256)):
            t = np.arange(T)
            lo = np.clip(t - win // 2, 0, T)
            hi = np.clip(t + win // 2, 0, T)
            out[gi, ri, :T] = 1.0 / (hi - lo).astype(np.float32)
    return np.ascontiguousarray(np.broadcast_to(out[None], (128, 4, 2, 256)))


def make_in_maps(inputs, n_cores=8):
    import ml_dtypes
    x, c, ctx, c_ctx = inputs["x"], inputs["c"], inputs["ctx"], inputs["c_ctx"]
    shared = {
        "ada_w": np.ascontiguousarray(inputs["ada_w"], np.float32),
        "ada_b": np.ascontiguousarray(np.transpose(np.asarray(inputs["ada_b"], np.float32).reshape(DEPTH, 48, 128), (2, 0, 1))),
        "norm_g": _fm(inputs["norm_g"]),
        "final_g": _fm(inputs["final_g"]),
        "ones_bf": np.ones((128, 128), ml_dtypes.bfloat16),
        "id_bf": np.eye(128, dtype=np.float32).astype(ml_dtypes.bfloat16),
        "id_f": np.eye(128, dtype=np.float32),
        "ones_f": np.ones((128, 128), np.float32),
        "na_w_qkv": np.ascontiguousarray(inputs["na_w_qkv"], np.float32),
        "na_w_o": np.ascontiguousarray(inputs["na_w_o"], np.float32),
        "na_tab": np.stack([_na_tables(np.asarray(inputs["na_rpb"][j], np.float32)) for j in range(2)]),
        "ffn_w1": np.ascontiguousarray(inputs["ffn_w1"], np.float32),
        "ffn_w3": np.ascontiguousarray(inputs["ffn_w3"], np.float32),
        "ffn_w2": np.ascontiguousarray(inputs["ffn_w2"], np.float32),
        "moe_router": np.ascontiguousarray(np.transpose(np.asarray(inputs["moe_router"], np.float32).reshape(2, KC, 128, NEXP), (0, 2, 1, 3))),
        "moe_w1_0": np.ascontiguousarray(inputs["moe_w1"][0], np.float32), "moe_w1_1": np.ascontiguousarray(inputs["moe_w1"][1], np.float32),
        "moe_w3_0": np.ascontiguousarray(inputs["moe_w3"][0], np.float32), "moe_w3_1": np.ascontiguousarray(inputs["moe_w3"][1], np.float32),
        "moe_w2_0": np.ascontiguousarray(inputs["moe_w2"][0], np.float32), "moe_w2_1": np.ascontiguousarray(inputs["moe_w2"][1], np.float32),
        "tris": np.ascontiguousarray((np.arange(128)[:, None] < np.arange(128)[None, :]).astype(np.float32)),
        "thr48": np.ascontiguousarray(np.broadcast_to((np.arange(48) * 512.0).astype(np.float32)[None], (128, 48))),
        "iota48": np.ascontiguousarray(np.broadcast_to(np.arange(48).astype(np.float32)[None], (128, 48))),
        "pk1": np.ascontiguousarray(((np.arange(KC)[None, :] * 128 + np.arange(128)[:, None]) * 4).astype(np.float32)),
        "pk2": np.ascontiguousarray((np.arange(28)[None, :] * 128 + np.arange(128)[:, None]).astype(np.float32)),
        "pool_w": np.ascontiguousarray(inputs["pool_w"][0], np.float32),
        "pool_scale": _fm(inputs["pool_scale"][0]),
        "pool_ic": _pool_ic(),
    }
    shared.update(gdn_host(inputs))
    maps = []
    for ci in range(n_cores):
        b0 = ci * NS
        xin = np.empty((NS, D, TOK), np.float32)
        for s in range(NS):
            xin[s, :, :NCTX] = np.asarray(ctx[b0 + s], np.float32).T
            xin[s, :, NCTX:] = np.asarray(x[b0 + s], np.float32).T
        cv = np.stack([c[b0], c[b0 + 1], c_ctx], axis=0).astype(np.float32)
        m = dict(shared)
        m["xin"] = xin
        m["cvec"] = np.ascontiguousarray(np.transpose(cv.reshape(3, KC, 128), (2, 1, 0)))
        maps.append(m)
    return maps


def gdn_host(inputs):
    import ml_dtypes
    cw = np.asarray(inputs["gdn_conv"][0], np.float32)
    cwl = np.ascontiguousarray(np.transpose(cw.reshape(4, 24, 128), (2, 1, 0)))
    dtb = np.asarray(inputs["gdn_dt_bias"][0], np.float32).reshape(16)
    alog = np.asarray(inputs["gdn_a_log"][0], np.float32).reshape(16)
    p = np.arange(128)
    tri = np.zeros((128, 2, 128), np.float32)
    tri[:, 0, :] = (p[:, None] <= p[None, :])
    tri[:, 1, :] = (p[:, None] >= p[None, :])
    gm = np.zeros((128, 2, 3, 128), np.float32)
    P_, F_ = p[:, None], p[None, :]
    gm[:, 0, 0, :] = np.where(F_ >= P_, 0.0, NEG); gm[:, 0, 1, :] = np.where(F_ > P_, 0.0, NEG); gm[:, 0, 2, :] = np.where(F_ < P_, 0.0, -NEG)
    gm[:, 1, 0, :] = np.where(F_ <= P_, 0.0, NEG); gm[:, 1, 1, :] = np.where(F_ < P_, 0.0, NEG); gm[:, 1, 2, :] = np.where(F_ > P_, 0.0, -NEG)
    ms = np.zeros((128, 7, 128), np.float32)
    for kk in range(7):
        ms[:, kk, :] = ((P_ >> kk) != (F_ >> kk)) & ((P_ >> (kk + 1)) == (F_ >> (kk + 1)))
    return {
        "gdn_w_in": np.ascontiguousarray(inputs["gdn_w_in"][0], np.float32),
        "gdn_w_o": np.ascontiguousarray(inputs["gdn_w_o"][0], np.float32),
        "gdn_cw": cwl,
        "gdn_dtb": np.ascontiguousarray(np.broadcast_to(dtb[None], (128, 16))),
        "gdn_alog": np.ascontiguousarray(np.broadcast_to(alog[None], (128, 16))),
        "gdn_ng": np.ascontiguousarray(np.asarray(inputs["gdn_norm_g"][0], np.float32).reshape(128, 1)),
        "gdn_tri": tri, "gdn_gm": gm, "gdn_ms": ms.astype(ml_dtypes.bfloat16),
    }


def kernel(**inputs):
    nc = build()
    maps = make_in_maps(inputs)
    res = run_bass_kernel_spmd(nc, maps, core_ids=list(range(8)))
    B = inputs["x"].shape[0]
    out = np.empty((B, NLAT, D), np.float32)
    for ci in range(8):
        o = res.results[ci]["out"]
        for s in range(NS):
            out[ci * NS + s] = o[s].T
    return out
```

```python
import numpy as np
from contextlib import ExitStack
import concourse.bass as bass
import concourse.mybir as mybir
from concourse.bass_utils import run_bass_kernel_spmd

F32 = mybir.dt.float32
BF16 = mybir.dt.bfloat16
U32 = mybir.dt.uint32
AF = mybir.ActivationFunctionType
ALU = mybir.AluOpType

D = 1024
KC = 8
NLAT = 4096
NCTX = 256
TT = 512
NS = 2
DEPTH = 4
EPS = 1e-6
NEG = -1e30
DFF = 2816
DFE = 3584
NEXP = 8


class _Sem:
    def __init__(self, h, name):
        self.h = h
        self.val = 0
        self.name = name


class _Eng:
    def __init__(self, name, h, sem):
        self.name = name
        self.h = h
        self.sem = sem
        self.seen = {}


class Buf:
    __slots__ = ("name", "w", "r")

    def __init__(self, name=""):
        self.name = name
        self.w = None
        self.r = {}


class K:
    def __init__(self, nc, es, ndma=24):
        self.nc = nc
        self.es = es
        self.E = {}
        for name, h in (("pe", nc.tensor), ("act", nc.scalar), ("dve", nc.vector),
                        ("pool", nc.gpsimd), ("sp", nc.sync)):
            s = _Sem(es.enter_context(nc.semaphore("sem_" + name)), name)
            self.E[name] = _Eng(name, h, s)
        self.dsems = [_Sem(es.enter_context(nc.semaphore(f"dsem{i}")), f"d{i}") for i in range(ndma)]
        self.dnext = 0
        self.allsems = [e.sem for e in self.E.values()] + self.dsems

    def _need(self, e, ev):
        if ev is None:
            return
        sem, val = ev
        if sem is e.sem and e.name == "pe":
            return
        if e.seen.get(sem.name, 0) >= val:
            return
        e.h.wait_ge(sem.h, val)
        e.seen[sem.name] = val

    def _deps(self, e, reads, writes):
        for b in reads:
            self._need(e, b.w)
        for b in writes:
            self._need(e, b.w)
            for sname, ev in list(b.r.items()):
                self._need(e, ev)

    def _mark(self, ev, reads, writes):
        for b in reads:
            b.r[ev[0].name] = ev
        for b in writes:
            b.w = ev
            b.r = {}

    def op(self, ename, fn, reads=(), writes=()):
        e = self.E[ename]
        self._deps(e, reads, writes)
        ins = fn(e.h)
        e.sem.val += 1
        ins.then_inc(e.sem.h, 1)
        self._mark((e.sem, e.sem.val), reads, writes)

    def dma(self, out, in_, reads=(), writes=(), q="sp", **kw):
        e = self.E[q]
        ds = self.dsems[self.dnext]
        self.dnext = (self.dnext + 1) % len(self.dsems)
        self._need(e, (ds, ds.val))
        self._deps(e, reads, writes)
        ins = e.h.dma_start(out=out, in_=in_, **kw)
        ds.val += 16
        ins.then_inc(ds.h, 16)
        self._mark((ds, ds.val), reads, writes)

    def idma(self, out, in_, idx_ap, gather, reads=(), writes=(), bounds=None):
        e = self.E["pool"]
        ds = self.dsems[self.dnext]
        self.dnext = (self.dnext + 1) % len(self.dsems)
        self._need(e, (ds, ds.val))
        self._deps(e, reads, writes)
        off = bass.IndirectOffsetOnAxis(ap=idx_ap, axis=0)
        if gather:
            ins = e.h.indirect_dma_start(out=out, out_offset=None, in_=in_, in_offset=off)
        else:
            ins = e.h.indirect_dma_start(out=out, out_offset=off, in_=in_, in_offset=None)
        ds.val += 16
        ins.then_inc(ds.h, 16)
        self._mark((ds, ds.val), reads, writes)

    def barrier(self):
        for e in self.E.values():
            for s in self.allsems:
                if s.val > 0:
                    self._need(e, (s, s.val))

    def mm(self, out, lhsT, rhs, start, stop, reads, writes):
        self.op("pe", lambda h: h.matmul(out, lhsT, rhs, start=start, stop=stop), reads, writes)

    def act(self, out, in_, func, reads, writes, **kw):
        self.op("act", lambda h: h.activation(out=out, in_=in_, func=func, **kw), reads, writes)

    def tt(self, eng, out, in0, in1, op, reads, writes):
        self.op(eng, lambda h: h.tensor_tensor(out=out, in0=in0, in1=in1, op=op), reads, writes)

    def ts(self, eng, out, in0, s1, s2, op0, op1, reads, writes):
        if op1 is None:
            self.op(eng, lambda h: h.tensor_scalar(out=out, in0=in0, scalar1=s1, scalar2=None, op0=op0),
                    reads, writes)
        else:
            self.op(eng, lambda h: h.tensor_scalar(out=out, in0=in0, scalar1=s1, scalar2=s2, op0=op0, op1=op1),
                    reads, writes)

    def stt(self, out, in0, scalar, in1, op0, op1, reads, writes):
        self.op("dve", lambda h: h.scalar_tensor_tensor(out=out, in0=in0, scalar=scalar, in1=in1, op0=op0, op1=op1),
                reads, writes)

    def copy(self, eng, out, in_, reads, writes):
        if eng == "act":
            self.op("act", lambda h: h.activation(out=out, in_=in_, func=AF.Copy), reads, writes)
        else:
            self.op(eng, lambda h: h.tensor_copy(out=out, in_=in_), reads, writes)

    def memset(self, eng, ap, val, writes):
        self.op(eng, lambda h: h.memset(ap, val), (), writes)


_UID = [0]


class Pool_:
    def __init__(self, k):
        self.k = k
        self.es = ExitStack()
        self.n = 0

    def __enter__(self):
        self.es.__enter__()
        return self

    def __exit__(self, *a):
        self.k.barrier()
        return self.es.__exit__(*a)

    def sb(self, shape, dt, name=None):
        _UID[0] += 1
        t = self.es.enter_context(self.k.nc.sbuf_tensor(f"{name or 't'}_{_UID[0]}", list(shape), dt))
        return t, Buf(name or "sb")

    def ps(self, name=None, shape=(128, 512), dt=F32):
        _UID[0] += 1
        t = self.es.enter_context(self.k.nc.psum_tensor(f"{name or 'p'}_{_UID[0]}", list(shape), dt))
        return t, Buf(name or "ps")


TOK = NCTX + NLAT


def tiles_for(include_ctx=True):
    out = []
    for s in range(NS):
        if include_ctx:
            out.append((s, 0, NCTX, True))
        for i in range(NLAT // TT):
            out.append((s, NCTX + i * TT, TT, False))
    return out


class Prog:
    def __init__(self, nc, k, dr, stop_after=99):
        self.nc = nc
        self.k = k
        self.dr = dr
        self.stop_after = stop_after

    def setup_consts(self, P):
        k, dr = self.k, self.dr
        self.ones_bf, self.onesB = P.sb([128, 128], BF16, "ones")
        self.id_bf, self.idbB = P.sb([128, 128], BF16, "idb")
        self.id_f, self.idfB = P.sb([128, 128], F32, "idf")
        self.ones_f, self.onesfB = P.sb([128, 128], F32, "onesf")
        self.mod, self.modB = P.sb([128, DEPTH, 48, 3], F32, "mod")
        self.ng, self.ngB = P.sb([128, DEPTH, 2, KC], F32, "ng")
        self.fg, self.fgB = P.sb([128, KC], F32, "fg")
        self.epsb, self.epsB = P.sb([128, 1], F32, "eps")
        k.dma(self.ones_bf[:], dr["ones_bf"][:, :], writes=[self.onesB])
        k.dma(self.id_bf[:], dr["id_bf"][:, :], writes=[self.idbB])
        k.dma(self.id_f[:], dr["id_f"][:, :], writes=[self.idfB])
        k.dma(self.ones_f[:], dr["ones_f"][:, :], writes=[self.onesfB])
        k.dma(self.ng[:], dr["norm_g"][:, :, :, :], writes=[self.ngB])
        k.dma(self.fg[:], dr["final_g"][:, :], writes=[self.fgB])
        k.memset("pool", self.epsb[:], EPS, [self.epsB])

    def adaln(self):
        k, dr = self.k, self.dr
        with Pool_(k) as P:
            cv, cvB = P.sb([128, KC, 3], F32, "cv")
            sc, scB = P.sb([128, KC, 3], F32, "sc")
            ab, abB = P.sb([128, DEPTH, 48], F32, "ab")
            wa = [P.sb([128, KC, 768], F32, f"wa{i}") for i in range(2)]
            ps = [P.ps(f"adaps{i}") for i in range(2)]
            k.dma(cv[:], dr["cvec"][:, :, :], writes=[cvB])
            k.dma(ab[:], dr["ada_b"][:, :, :], writes=[abB])
            k.act(sc[:], cv[:], AF.Silu, [cvB], [scB])
            gi = 0
            for l in range(DEPTH):
                src = dr["ada_w"][l].rearrange("(kc p) n -> p kc n", p=128)
                for g in range(8):
                    wt, wB = wa[gi % 2]
                    pt, pB = ps[gi % 2]
                    gi += 1
                    k.dma(wt[:], src[:, :, g * 768:(g + 1) * 768], writes=[wB])
                    for o in range(6):
                        for kc in range(KC):
                            k.mm(pt[:, o * 3:(o + 1) * 3], wt[:, kc, o * 128:(o + 1) * 128], sc[:, kc, :],
                                 kc == 0, kc == KC - 1, [wB, scB], [pB])
                    pv = pt[:, 0:18].rearrange("p (o j) -> p o j", j=3)
                    for j in range(3):
                        k.tt("dve", self.mod[:, l, g * 6:(g + 1) * 6, j], pv[:, :, j], ab[:, l, g * 6:(g + 1) * 6],
                             ALU.add, [pB, abB], [self.modB])

    def mod_vecs(self, P, l, sub):
        k = self.k
        A, AB = P.sb([128, KC, 3], F32, "modA")
        o = sub * 24
        for j in range(3):
            k.stt(A[:, :, j], self.mod[:, l, o + 8:o + 16, j], 1.0, self.ng[:, l, sub, :], ALU.add, ALU.mult,
                  [self.modB, self.ngB], [AB])
        return A, AB

    def jidx(self, s, is_ctx):
        return 2 if is_ctx else s

    def norm_pass(self, l, sub, include_ctx=True, router=None, final=False, moe=None):
        k, dr = self.k, self.dr
        xs, hs = dr["xs"], dr["hs"]
        tl = tiles_for(include_ctx)
        with Pool_(k) as P:
            if not final:
                A, AB = self.mod_vecs(P, l, sub)
            xt = [P.sb([128, KC, TT], F32, f"xt{i}") for i in range(2)]
            sq, sqB = P.sb([128, KC, TT], BF16, "sq")
            rs, rsB = P.sb([128, TT], F32, "rs")
            hf, hfB = P.sb([128, KC, TT], F32, "hf")
            hb, hbB = P.sb([128, KC, TT], BF16, "hb")
            pss, pssB = P.ps("pss")
            if router is not None:
                wr, wrB = P.sb([128, KC, NEXP], F32, "wr")
                k.dma(wr[:], router, writes=[wrB])
                plg, plgB = P.ps("plg")
                pwt, pwtB = P.ps("pwt")
                lg, lgB = P.sb([128, 4, NEXP], F32, "lg")
                m8, m8B = P.sb([128, 4, 8], F32, "m8")
                nm1, nm1B = P.sb([128, 4], F32, "nm1")
                ee, eeB = P.sb([128, 4, NEXP], F32, "ee")
                mk, mkB = P.sb([128, 4, NEXP], F32, "mk")
                dn, dnB = P.sb([128, 4], F32, "dn")
                ww, wwB = P.sb([128, 4, NEXP], F32, "ww")
                wT, wTB = P.sb([NEXP, TT], F32, "wT")
                if moe is not None:
                    oh, ohB = P.sb([128, 4, 24], F32, "oh")
                    rk, rkB = P.sb([128, 4, NEXP], F32, "rk")
                    t8, t8B = P.sb([128, 4, NEXP], F32, "t8")
                    htk, htkB = P.sb([128, 4, D], BF16, "htk")
                    prk, prkB = P.ps("prk")
                    ptot, ptotB = P.ps("ptot")
                    ptk, ptkB = P.ps("ptk")
                    I32_ = mybir.dt.int32
                    sub_i = 0

            def load(i):
                s, t0, W, c = tl[i]
                t, B = xt[i % 2]
                k.dma(t[:, :, :W], xs[s].rearrange("(kc p) t -> p kc t", p=128)[:, :, t0:t0 + W], writes=[B])

            load(0)
            for i, (s, t0, W, c) in enumerate(tl):
                if i + 1 < len(tl):
                    load(i + 1)
                x, xB = xt[i % 2]
                j = self.jidx(s, c)
                k.act(sq[:, :, :W], x[:, :, :W], AF.Square, [xB], [sqB])
                for kc in range(KC):
                    k.mm(pss[:, :W], self.ones_bf[:], sq[:, kc, :W], kc == 0, kc == KC - 1, [self.onesB, sqB], [pssB])
                k.act(rs[:, :W], pss[:, :W], AF.Sqrt, [pssB, self.epsB], [rsB], scale=1.0 / D, bias=self.epsb[:, 0:1])
                k.op("dve", lambda h: h.reciprocal(out=rs[:, :W], in_=rs[:, :W]), [rsB], [rsB])
                for kc in range(KC):
                    k.tt("dve", hf[:, kc, :W], x[:, kc, :W], rs[:, :W], ALU.mult, [xB, rsB], [hfB])
                if final:
                    for kc in range(KC):
                        k.act(hf[:, kc, :W], hf[:, kc, :W], AF.Identity, [hfB, self.fgB], [hfB], scale=self.fg[:, kc:kc + 1])
                    k.dma(dr["out"][s].rearrange("(kc p) t -> p kc t", p=128)[:, :, t0 - NCTX:t0 - NCTX + W],
                          hf[:, :, :W], reads=[hfB])
                    continue
                o = sub * 24
                for kc in range(KC):
                    k.act(hf[:, kc, :W], hf[:, kc, :W], AF.Identity, [hfB, AB, self.modB], [hfB],
                          scale=A[:, kc, j:j + 1], bias=self.mod[:, l, o + kc, j:j + 1])
                k.copy("pool", hb[:, :, :W], hf[:, :, :W], [hfB], [hbB])
                k.dma(hs[s].rearrange("(kc p) t -> p kc t", p=128)[:, :, t0:t0 + W], hb[:, :, :W], reads=[hbB])
                if router is not None:
                    nst = W // 128
                    for ts_ in range(nst):
                        for kc in range(KC):
                            k.mm(plg[:, ts_ * 8:(ts_ + 1) * 8], hf[:, kc, ts_ * 128:(ts_ + 1) * 128], wr[:, kc, :],
                                 kc == 0, kc == KC - 1, [hfB, wrB], [plgB])
                    k.copy("dve", lg[:, :nst, :], plg[:, 0:nst * 8].rearrange("p (a e) -> p a e", e=8), [plgB], [lgB])
                    for ts_ in range(nst):
                        k.op("dve", lambda h: h.max(out=m8[:, ts_, :], in_=lg[:, ts_, :]), [lgB], [m8B])
                    k.ts("dve", nm1[:, :nst], m8[:, :nst, 0], -1.0, None, ALU.mult, None, [m8B], [nm1B])
                    for ts_ in range(nst):
                        k.act(ee[:, ts_, :], lg[:, ts_, :], AF.Exp, [lgB, nm1B], [eeB], bias=nm1[:, ts_:ts_ + 1])
                        k.ts("dve", mk[:, ts_, :], lg[:, ts_, :], m8[:, ts_, 1:2], None, ALU.is_ge, None,
                             [lgB, m8B], [mkB])
                        if moe is not None:
                            k.ts("dve", oh[:, ts_, 0:8], lg[:, ts_, :], m8[:, ts_, 0:1], None, ALU.is_equal, None, [lgB, m8B], [ohB])
                            k.ts("dve", oh[:, ts_, 8:16], lg[:, ts_, :], m8[:, ts_, 1:2], None, ALU.is_equal, None, [lgB, m8B], [ohB])
                    k.tt("dve", ee[:, :nst, :], ee[:, :nst, :], mk[:, :nst, :], ALU.mult, [eeB, mkB], [eeB])
                    k.op("dve", lambda h: h.reduce_sum(out=dn[:, :nst], in_=ee[:, :nst, :], axis=mybir.AxisListType.X),
                         [eeB], [dnB])
                    k.op("dve", lambda h: h.reciprocal(out=dn[:, :nst], in_=dn[:, :nst]), [dnB], [dnB])
                    for ts_ in range(nst):
                        k.ts("dve", ww[:, ts_, :], ee[:, ts_, :], dn[:, ts_:ts_ + 1], None, ALU.mult, None,
                             [eeB, dnB], [wwB])
                        k.op("pe", lambda h: h.transpose(pwt[0:NEXP, ts_ * 128:(ts_ + 1) * 128], ww[:, ts_, :], self.id_f[:]),
                             [wwB, self.idfB], [pwtB])
                    k.copy("dve", wT[:, :W], pwt[0:NEXP, :W], [pwtB], [wTB])
                    k.dma(dr["wexp"][s, :, t0:t0 + W], wT[:, :W], reads=[wTB])
                    if moe is not None:
                        cum, cumB = moe["cum"]
                        for ts_ in range(nst):
                            k.mm(prk[:, 0:8], moe["tris"][0][:], mk[:, ts_, :], True, True, [moe["tris"][1], mkB], [prkB])
                            k.mm(ptot[:, 0:8], self.ones_f[:], mk[:, ts_, :], True, True, [self.onesfB, mkB], [ptotB])
                            k.tt("dve", rk[:, ts_, :], prk[:, 0:8], cum[:], ALU.add, [prkB, cumB], [rkB])
                            k.tt("dve", cum[:], cum[:], ptot[:, 0:8], ALU.add, [cumB, ptotB], [cumB])
                        for (src_, so, do) in ((rk, 0, 16), (rk, 8, 17), (ww, 0, 18), (ww, 8, 19)):
                            k.tt("dve", t8[:, :nst, :], oh[:, :nst, so:so + 8], src_[:, :nst, :], ALU.mult, [ohB, rkB, wwB], [t8B])
                            k.op("dve", lambda h: h.reduce_sum(out=oh[:, :nst, do], in_=t8[:, :nst, :], axis=mybir.AxisListType.X),
                                 [t8B], [ohB])
                        k.dma(dr["rinfo"][s, t0:t0 + W, :].rearrange("(a p) c -> p a c", p=128), oh[:, :nst, :], reads=[ohB])
                        for ts_ in range(nst):
                            for kc in range(KC):
                                k.op("pe", lambda h: h.transpose(ptk[:, :].bitcast(BF16)[:, kc * 128:(kc + 1) * 128],
                                                                 hb[:, kc, ts_ * 128:(ts_ + 1) * 128], self.id_bf[:]),
                                     [hbB, self.idbB], [ptkB])
                            if ts_ % 2 == 0:
                                k.copy("act", htk[:, ts_, :], ptk[:, :].bitcast(BF16)[:, 0:D], [ptkB], [htkB])
                            else:
                                k.copy("dve", htk[:, ts_, :], ptk[:, :].bitcast(BF16)[:, 0:D], [ptkB], [htkB])
                        k.dma(dr["htok"][s, t0:t0 + W, :].rearrange("(a p) d -> p a d", p=128), htk[:, :nst, :], reads=[htkB])

    def ffn_pass(self, l, w1, w3, w2, F, include_ctx=True, expert=None):
        k, dr = self.k, self.dr
        xs, hs = dr["xs"], dr["hs"]
        FC = F // 128
        tl = tiles_for(include_ctx)
        with Pool_(k) as P:
            W1, W1B = P.sb([128, KC, F], BF16, "W1")
            W3, W3B = P.sb([128, KC, F], BF16, "W3")
            W2, W2B = P.sb([128, FC, D], BF16, "W2")
            k.dma(W1[:], w1.rearrange("(kc p) f -> p kc f", p=128), writes=[W1B], q="pool")
            k.dma(W3[:], w3.rearrange("(kc p) f -> p kc f", p=128), writes=[W3B], q="pool")
            k.dma(W2[:], w2.rearrange("(fc p) d -> p fc d", p=128), writes=[W2B], q="pool")
            ht = [P.sb([128, KC, TT], BF16, f"ht{i}") for i in range(2)]
            wb = [P.sb([128, TT], F32, f"wbc{i}") for i in range(2)] if expert is not None else None
            g, gB = P.sb([128, FC, TT], BF16, "g")
            gBs = [Buf(f"g{f}") for f in range(FC)]
            sa = [P.sb([128, TT], F32, f"sa{i}") for i in range(2)]
            yt = [P.sb([128, TT], F32, f"yt{i}") for i in range(2)]
            pa = [P.ps(f"pa{i}") for i in range(2)]
            pb = [P.ps(f"pb{i}") for i in range(2)]
            py = [P.ps(f"py{i}") for i in range(2)]

            def load(i):
                s, t0, W, c = tl[i]
                t, B = ht[i % 2]
                k.dma(t[:, :, :W], hs[s].rearrange("(kc p) t -> p kc t", p=128)[:, :, t0:t0 + W], writes=[B])
                if expert is not None:
                    wt_, wB_ = wb[i % 2]
                    k.dma(wt_[:, :W], dr["wexp"][s, expert:expert + 1, t0:t0 + W].partition_broadcast(128), writes=[wB_])

            load(0)
            cnt = 0
            ycnt = 0
            for i, (s, t0, W, c) in enumerate(tl):
                if i + 1 < len(tl):
                    load(i + 1)
                h, hB = ht[i % 2]
                j = self.jidx(s, c)
                for f in range(FC):
                    a_, aB = pa[cnt % 2]
                    b_, bB = pb[cnt % 2]
                    s_, sB = sa[cnt % 2]
                    cnt += 1
                    for kc in range(KC):
                        k.mm(a_[:, :W], W1[:, kc, f * 128:(f + 1) * 128], h[:, kc, :W], kc == 0, kc == KC - 1, [W1B, hB], [aB])
                    for kc in range(KC):
                        k.mm(b_[:, :W], W3[:, kc, f * 128:(f + 1) * 128], h[:, kc, :W], kc == 0, kc == KC - 1, [W3B, hB], [bB])
                    k.act(s_[:, :W], a_[:, :W], AF.Silu, [aB], [sB])
                    if expert is not None:
                        wt_, wB_ = wb[i % 2]
                        k.tt("pool", s_[:, :W], s_[:, :W], wt_[:, :W], ALU.mult, [sB, wB_], [sB])
                    k.tt("dve", g[:, f, :W], b_[:, :W], s_[:, :W], ALU.mult, [bB, sB], [gBs[f]])
                o = 24 + 16 + 0
                for oc in range(KC):
                    y_, yB = py[ycnt % 2]
                    yo, yoB = yt[ycnt % 2]
                    ycnt += 1
                    for f in range(FC):
                        k.mm(y_[:, :W], W2[:, f, oc * 128:(oc + 1) * 128], g[:, f, :W], f == 0, f == FC - 1, [W2B, gBs[f]], [yB])
                    k.ts("dve", yo[:, :W], y_[:, :W], self.mod[:, l, 40 + oc, j:j + 1], None, ALU.mult, None,
                         [yB, self.modB], [yoB])
                    k.dma(xs[s, oc * 128:(oc + 1) * 128, t0:t0 + W], yo[:, :W], reads=[yoB], q="pool", accum_op=ALU.add)

    def outproj_pass(self, l, src, w, include_ctx, gate_off, extra_scale=None):
        k, dr = self.k, self.dr
        xs = dr["xs"]
        tl = tiles_for(include_ctx)
        with Pool_(k) as P:
            Wo, WoB = P.sb([128, KC, D], BF16, "Wo")
            k.dma(Wo[:], w.rearrange("(kc p) d -> p kc d", p=128), writes=[WoB], q="pool")
            ot = [P.sb([128, KC, TT], BF16, f"ot{i}") for i in range(2)]
            yt = [P.sb([128, TT], F32, f"yt{i}") for i in range(2)]
            py = [P.ps(f"py{i}") for i in range(2)]

            def load(i):
                s, t0, W, c = tl[i]
                t, B = ot[i % 2]
                k.dma(t[:, :, :W], src[s].rearrange("(kc p) t -> p kc t", p=128)[:, :, t0:t0 + W], writes=[B])

            load(0)
            ycnt = 0
            for i, (s, t0, W, c) in enumerate(tl):
                if i + 1 < len(tl):
                    load(i + 1)
                o_, oB = ot[i % 2]
                j = self.jidx(s, c)
                for oc in range(KC):
                    y_, yB = py[ycnt % 2]
                    yo, yoB = yt[ycnt % 2]
                    ycnt += 1
                    for kc in range(KC):
                        k.mm(y_[:, :W], Wo[:, kc, oc * 128:(oc + 1) * 128], o_[:, kc, :W], kc == 0, kc == KC - 1, [WoB, oB], [yB])
                    if extra_scale is None:
                        k.ts("dve", yo[:, :W], y_[:, :W], self.mod[:, l, gate_off + oc, j:j + 1], None, ALU.mult, None,
                             [yB, self.modB], [yoB])
                    else:
                        est, esB = extra_scale
                        k.ts("dve", yo[:, :W], y_[:, :W], self.mod[:, l, gate_off + oc, j:j + 1], est[:, oc:oc + 1],
                             ALU.mult, ALU.mult, [yB, self.modB, esB], [yoB])
                    k.dma(xs[s, oc * 128:(oc + 1) * 128, t0:t0 + W], yo[:, :W], reads=[yoB], q="pool", accum_op=ALU.add)

    def na_qkv_pass(self, wqkv):
        k, dr = self.k, self.dr
        hs, qs, ks, vs = dr["hs"], dr["qs"], dr["ks"], dr["vs"]
        tl = tiles_for(True)
        with Pool_(k) as P:
            Wq, WqB = P.sb([128, KC, 3 * D], BF16, "Wqkv")
            src = wqkv.rearrange("(kc p) n -> p kc n", p=128)
            for i in range(3):
                k.dma(Wq[:, :, i * D:(i + 1) * D], src[:, :, i * D:(i + 1) * D], writes=[WqB], q="pool")
            ht = [P.sb([128, KC, TT], BF16, f"ht{i}") for i in range(2)]
            qk = [P.sb([128, 2 * KC, TT], BF16, f"qk{i}") for i in range(2)]
            vt = [P.sb([128, 4, D], BF16, f"vt{i}") for i in range(2)]
            pp = [P.ps(f"pp{i}") for i in range(4)]

            def load(i):
                s, t0, W, c = tl[i]
                t, B = ht[i % 2]
                k.dma(t[:, :, :W], hs[s].rearrange("(kc p) t -> p kc t", p=128)[:, :, t0:t0 + W], writes=[B])

            load(0)
            cnt = 0
            for i, (s, t0, W, c) in enumerate(tl):
                if i + 1 < len(tl):
                    load(i + 1)
                h, hB = ht[i % 2]
                q_, qB = qk[i % 2]
                v_, vB = vt[i % 2]
                for oc in range(2 * KC):
                    p_, pB = pp[cnt % 4]
                    for kc in range(KC):
                        k.mm(p_[:, :W], Wq[:, kc, oc * 128:(oc + 1) * 128], h[:, kc, :W], kc == 0, kc == KC - 1, [WqB, hB], [pB])
                    if cnt % 2 == 0:
                        k.act(q_[:, oc, :W], p_[:, :W], AF.Copy, [pB], [qB], scale=(0.125 if oc < KC else 1.0))
                    else:
                        k.ts("dve", q_[:, oc, :W], p_[:, :W], (0.125 if oc < KC else 1.0), None, ALU.mult, None, [pB], [qB])
                    cnt += 1
                for ts_ in range(W // 128):
                    for hf_ in range(2):
                        p_, pB = pp[cnt % 4]
                        for kc in range(KC):
                            k.mm(p_[:, :], h[:, kc, ts_ * 128:(ts_ + 1) * 128], Wq[:, kc, 2 * D + hf_ * 512:2 * D + (hf_ + 1) * 512],
                                 kc == 0, kc == KC - 1, [WqB, hB], [pB])
                        if cnt % 2 == 0:
                            k.act(v_[:, ts_, hf_ * 512:(hf_ + 1) * 512], p_[:, :], AF.Copy, [pB], [vB])
                        else:
                            k.copy("dve", v_[:, ts_, hf_ * 512:(hf_ + 1) * 512], p_[:, :], [pB], [vB])
                        cnt += 1
                k.dma(qs[s].rearrange("(kc p) t -> p kc t", p=128)[:, :, t0:t0 + W], q_[:, 0:KC, :W], reads=[qB])
                k.dma(ks[s].rearrange("(kc p) t -> p kc t", p=128)[:, :, t0:t0 + W], q_[:, KC:2 * KC, :W], reads=[qB])
                k.dma(vs[s, t0:t0 + W, :].rearrange("(a p) f -> p a f", p=128), v_[:, :W // 128, :], reads=[vB])

    def na_attn_pass(self, tab, need_ctx):
        k, dr = self.k, self.dr
        qs, ks, vs, os_ = dr["qs"], dr["ks"], dr["vs"], dr["os"]
        NCH = TOK // 128
        with Pool_(k) as P:
            qT, qTB = P.sb([128, TOK], BF16, "qT")
            kT, kTB = P.sb([128, TOK], BF16, "kT")
            vv, vvB = P.sb([128, NCH, 128], BF16, "vv")
            tb, tbB = P.sb([128, 2, 2, 22, 64], F32, "tb")
            negt, negB = P.sb([128, 256], F32, "negt")
            k.memset("pool", negt[:], NEG, [negB])
            sbm = [P.sb([128, TT], F32, f"sbm{i}") for i in range(2)]
            pt = [P.sb([128, TT], BF16, f"pt{i}") for i in range(3)]
            rd, rdB = P.sb([128, TT], F32, "rd")
            ot = [P.sb([128, TT], BF16, f"ot{i}") for i in range(2)]
            pS = [P.ps(f"pS{i}") for i in range(3)]
            pO = [P.ps(f"pO{i}") for i in range(2)]
            pD = [P.ps(f"pD{i}") for i in range(2)]
            scnt = 0
            tcnt = 0
            for s in range(NS):
                for hp in range(8):
                    k.dma(qT[:], qs[s, hp * 128:(hp + 1) * 128, :], writes=[qTB])
                    k.dma(kT[:], ks[s, hp * 128:(hp + 1) * 128, :], writes=[kTB])
                    k.dma(vv[:], vs[s, :, hp * 128:(hp + 1) * 128].rearrange("(a p) f -> p a f", p=128), writes=[vvB])
                    if s == 0 or True:
                        k.dma(tb[:], tab[hp], writes=[tbB])
                    qtiles = ([(0, NCTX, -1)] if need_ctx else []) + [(NCTX + ti * TT, TT, ti) for ti in range(8)]
                    for (t0, W, ti) in qtiles:
                        O_, OB = pO[tcnt % 2]
                        D_, DB = pD[tcnt % 2]
                        o_, oB = ot[tcnt % 2]
                        tcnt += 1
                        if ti < 0:
                            chunks = [(0, None), (1, None)]
                        else:
                            lo, hi = max(0, 4 * ti - 2), min(31, 4 * ti + 5)
                            chunks = [(2 + c, c) for c in range(lo, hi + 1)] + [(0, None), (1, None)]
                        steps = [(hd, ci, kch, c) for hd in range(2) for ci, (kch, c) in enumerate(chunks)]
                        slots = {}

                        def emitS(st):
                            nonlocal scnt
                            hd, ci, kch, c = st
                            pb_ = 64 * hd
                            S_, SB = pS[scnt % 3]
                            p_, pB = pt[scnt % 3]
                            m_, mB = sbm[scnt % 2]
                            scnt += 1
                            slots[st] = (p_, pB)
                            k.mm(S_[:, :W], kT[pb_:pb_ + 64, kch * 128:(kch + 1) * 128], qT[pb_:pb_ + 64, t0:t0 + W],
                                 True, True, [kTB, qTB], [SB])
                            if c is None:
                                k.act(p_[:, :W], S_[:, :W], AF.Exp, [SB], [pB])
                                return
                            R = 8 * ti
                            J0 = 10 - 2 * c + R
                            if ti == 0:
                                segs = [(0, 4, "U" if c <= 3 else "N"), (4, 8, "T")]
                            elif ti == 7:
                                segs = [(0, 5, "T"), (5, 8, "U" if c >= 28 else "N")]
                            else:
                                segs = [(0, 8, "T")]
                            for (b0, b1, kind) in segs:
                                c0, c1 = b0 * 64, b1 * 64
                                if kind == "N":
                                    in1 = negt[:, 0:c1 - c0]
                                    rb = [negB]
                                else:
                                    tix = 0 if kind == "T" else 1
                                    in1 = tb[:, tix, hd, J0 + b0:J0 + b1, :].rearrange("p a b -> p (a b)")
                                    rb = [tbB]
                                k.tt("dve", m_[:, c0:c1], S_[:, c0:c1], in1, ALU.add, [SB] + rb, [mB])
                            k.act(p_[:, :W], m_[:, :W], AF.Exp, [mB], [pB])

                        def emitO(st):
                            hd, ci, kch, c = st
                            pb_ = 64 * hd
                            p_, pB = slots.pop(st)
                            first, last = ci == 0, ci == len(chunks) - 1
                            k.mm(O_[pb_:pb_ + 64, :W], vv[:, kch, pb_:pb_ + 64], p_[:, :W], first, last, [vvB, pB], [OB])
                            k.mm(D_[pb_:pb_ + 64, :W], self.ones_bf[:, 0:64], p_[:, :W], first, last, [self.onesB, pB], [DB])

                        LA = 2
                        for i_ in range(len(steps) + LA):
                            if i_ < len(steps):
                                emitS(steps[i_])
                            if i_ - LA >= 0:
                                emitO(steps[i_ - LA])
                        k.op("dve", lambda h: h.reciprocal(out=rd[:, :W], in_=D_[:, :W]), [DB], [rdB])
                        k.tt("dve", o_[:, :W], O_[:, :W], rd[:, :W], ALU.mult, [OB, rdB], [oB])
                        k.dma(os_[s, hp * 128:(hp + 1) * 128, t0:t0 + W], o_[:, :W], reads=[oB])

    def pool_pass(self, l, pw, include_ctx=True):
        k, dr = self.k, self.dr
        xs, hs = dr["xs"], dr["hs"]
        tl = tiles_for(include_ctx)
        PADW = 80
        with Pool_(k) as P:
            Wp, WpB = P.sb([128, 4, 2, 256], BF16, "Wp")
            k.dma(Wp[:], pw.rearrange("g (kc p) f -> p g kc f", p=128), writes=[WpB], q="pool")
            ls, lsB = P.sb([128, KC], F32, "ls")
            k.dma(ls[:], dr["pool_scale"][:, :], writes=[lsB])
            ic, icB = P.sb([128, 4, 2, 256], F32, "ic")
            k.dma(ic[:], dr["pool_ic"][:, :, :, :], writes=[icB])
            ht = [P.sb([128, KC, TT], BF16, f"ht{i}") for i in range(2)]
            lv = [P.sb([128, 8 * PADW], F32, f"lv{i}") for i in range(3)]
            lvc = [P.sb([128, 272], F32, f"lvc{i}") for i in range(3)]
            pl, plB = P.sb([128, KC, TT], BF16, "pl")
            plBs = [Buf(f"pl{i}") for i in range(KC)]
            mn, mnB = P.sb([128, TT], F32, "mn")
            yt = [P.sb([128, TT], F32, f"yt{i}") for i in range(2)]
            py = [P.ps(f"py{i}") for i in range(2)]
            for t_, B_ in lv + lvc:
                k.memset("pool", t_[:], 0.0, [B_])

            def load(i):
                s, t0, W, c = tl[i]
                t, B = ht[i % 2]
                k.dma(t[:, :, :W], hs[s].rearrange("(kc p) t -> p kc t", p=128)[:, :, t0:t0 + W], writes=[B])

            load(0)
            ycnt = 0
            for i, (s, t0, W, c) in enumerate(tl):
                if i + 1 < len(tl):
                    load(i + 1)
                h, hB = ht[i % 2]
                j = self.jidx(s, c)
                rows, rl, pw_, ri = (1, 256, 272, 1) if c else (8, 64, PADW, 0)

                def view(t, off, n=rl):
                    return t[:, 0:rows * pw_].rearrange("p (r w) -> p r w", w=pw_)[:, :, 8 + off:8 + off + n]

                for kc in range(KC):
                    gi = kc // 2
                    win = (2, 4, 8, 16)[gi]
                    a_, aB = (lvc if c else lv)[0]
                    b_, bB = (lvc if c else lv)[1]
                    c_, cB = (lvc if c else lv)[2]
                    hv = h[:, kc, :W].rearrange("p (r w) -> p r w", w=rl)
                    k.copy("pool", view(a_, 0), hv, [hB], [aB])
                    n2 = rl + 14
                    k.tt("pool", view(b_, -7, n2), view(a_, -8, n2), view(a_, -7, n2), ALU.add, [aB], [bB])
                    cur, curB = b_, bB
                    if win >= 4:
                        n4 = rl + 12
                        k.tt("pool", view(c_, -6, n4), view(b_, -7, n4), view(b_, -5, n4), ALU.add, [bB], [cB])
                        cur, curB = c_, cB
                    if win >= 8:
                        n8 = rl + 8
                        k.tt("dve", view(b_, -4, n8), view(c_, -6, n8), view(c_, -2, n8), ALU.add, [cB], [bB])
                        cur, curB = b_, bB
                    if win >= 16:
                        k.tt("dve", view(c_, 0), view(b_, -4), view(b_, 4), ALU.add, [bB], [cB])
                        cur, curB = c_, cB
                    mv = mn[:, :W].rearrange("p (r w) -> p r w", w=rl)
                    icv = ic[:, gi, ri, 0:rl]
                    for r in range(rows):
                        k.tt("dve", mv[:, r, :], view(cur, 0)[:, r, :], icv, ALU.mult, [curB, icB], [mnB])
                    k.tt("dve", pl[:, kc, :W], mn[:, :W], h[:, kc, :W], ALU.subtract, [mnB, hB], [plBs[kc]])
                for oc in range(KC):
                    gi = oc // 2
                    y_, yB = py[ycnt % 2]
                    yo, yoB = yt[ycnt % 2]
                    ycnt += 1
                    for kk in range(2):
                        k.mm(y_[:, :W], Wp[:, gi, kk, (oc % 2) * 128:(oc % 2 + 1) * 128], pl[:, 2 * gi + kk, :W], kk == 0, kk == 1,
                             [WpB, plBs[2 * gi + kk]], [yB])
                    k.ts("dve", yo[:, :W], y_[:, :W], self.mod[:, l, 16 + oc, j:j + 1], ls[:, oc:oc + 1], ALU.mult, ALU.mult,
                         [yB, self.modB, lsB], [yoB])
                    k.dma(xs[s, oc * 128:(oc + 1) * 128, t0:t0 + W], yo[:, :W], reads=[yoB], q="pool", accum_op=ALU.add)


BLK = 512
GF = 896
NG = DFE // GF
I32 = mybir.dt.int32


def moe_layer(pg, l, f, include_ctx):
    k, dr = pg.k, pg.dr
    tl = tiles_for(include_ctx)
    subt = [(s, t0 + a * 128, c) for (s, t0, W, c) in tl for a in range(W // 128)]
    NSUB = len(subt)
    NB = NSUB * 2 * 128 // BLK + NEXP
    w1f = dr[f"moe_w1_{f}"].rearrange("e k (g c) -> (e k g) c", c=GF)
    w3f = dr[f"moe_w3_{f}"].rearrange("e k (g c) -> (e k g) c", c=GF)
    w2f = dr[f"moe_w2_{f}"].rearrange("e f d -> (e f) d")
    hbuf, obuf = dr["hbuf"], dr["obuf"]
    with Pool_(k) as PM:
        cum = PM.sb([128, NEXP], F32, "cum")
        tris = PM.sb([128, 128], F32, "tris")
        starts = PM.sb([128, NEXP], F32, "starts")
        posall = PM.sb([128, NSUB, 2], I32, "posall")
        pwall = PM.sb([128, NSUB, 2], F32, "pwall")
        widx1 = PM.sb([128, NB, NG, KC], I32, "widx1")
        widx2 = PM.sb([128, NB, 28], I32, "widx2")
        k.memset("pool", cum[0][:], 0.0, [cum[1]])
        k.dma(tris[0][:], dr["tris"][:, :], writes=[tris[1]])
        pg.norm_pass(l, 1, include_ctx=include_ctx, router=dr["moe_router"][f], moe={"cum": cum, "tris": tris})
        with Pool_(k) as P:
            thr, thrB = P.sb([128, 48], F32, "thr"); k.dma(thr[:], dr["thr48"][:, :], writes=[thrB])
            io, ioB = P.sb([128, 48], F32, "io"); k.dma(io[:], dr["iota48"][:, :], writes=[ioB])
            pk1, pk1B = P.sb([128, KC], F32, "pk1"); k.dma(pk1[:], dr["pk1"][:, :], writes=[pk1B])
            pk2, pk2B = P.sb([128, 28], F32, "pk2"); k.dma(pk2[:], dr["pk2"][:, :], writes=[pk2B])
            tmp, tmpB = P.sb([128, 48], F32, "tmp")
            nblk, nblkB = P.sb([128, NEXP], F32, "nblk")
            sblk, sblkB = P.sb([128, NEXP], F32, "sblk")
            eb, ebB = P.sb([128, 48], F32, "eb")
            w1f_, w1fB = P.sb([128, NB, NG, KC], F32, "w1f")
            w2f_, w2fB = P.sb([128, NB, 28], F32, "w2f")
            for e in range(NEXP):
                k.ts("dve", tmp[:], thr[:], cum[0][:, e:e + 1], None, ALU.is_lt, None, [thrB, cum[1]], [tmpB])
                k.op("dve", lambda h: h.reduce_sum(out=nblk[:, e:e + 1], in_=tmp[:], axis=mybir.AxisListType.X), [tmpB], [nblkB])
            k.memset("pool", sblk[:], 0.0, [sblkB])
            for e in range(1, NEXP):
                k.tt("dve", sblk[:, e:e + 1], sblk[:, e - 1:e], nblk[:, e - 1:e], ALU.add, [sblkB, nblkB], [sblkB])
            k.ts("dve", starts[0][:], sblk[:], float(BLK), None, ALU.mult, None, [sblkB], [starts[1]])
            k.memset("pool", eb[:], -1.0, [ebB])
            for e in range(NEXP):
                k.ts("dve", tmp[:], io[:], sblk[:, e:e + 1], None, ALU.is_ge, None, [ioB, sblkB], [tmpB])
                k.tt("dve", eb[:], eb[:], tmp[:], ALU.add, [ebB, tmpB], [ebB])
            e4, e4B = P.sb([128, 48], F32, "e4")
            e35, e35B = P.sb([128, 48], F32, "e35")
            k.ts("dve", e4[:], eb[:], 4096.0, None, ALU.mult, None, [ebB], [e4B])
            k.ts("dve", e35[:], eb[:], float(DFE), None, ALU.mult, None, [ebB], [e35B])
            for g in range(NG):
                for kc in range(KC):
                    k.ts("dve", w1f_[:, :, g, kc], e4[:, :NB], pk1[:, kc:kc + 1], None, ALU.add, None, [e4B, pk1B], [w1fB])
            for g in range(1, NG):
                k.ts("dve", w1f_[:, :, g, :], w1f_[:, :, g, :], float(g), None, ALU.add, None, [w1fB], [w1fB])
            for fc in range(28):
                k.ts("dve", w2f_[:, :, fc], e35[:, :NB], pk2[:, fc:fc + 1], None, ALU.add, None, [e35B, pk2B], [w2fB])
            k.copy("dve", widx1[0][:], w1f_[:], [w1fB], [widx1[1]])
            k.copy("dve", widx2[0][:], w2f_[:], [w2fB], [widx2[1]])
            if "widx_dbg" in dr:
                k.dma(dr["widx_dbg"][:, 0:NB * NG * KC], widx1[0][:].rearrange("p a b c -> p (a b c)"), reads=[widx1[1]])
        with Pool_(k) as P:
            z, zB = P.sb([128, 4, D], BF16, "z")
            k.memset("pool", z[:], 0.0, [zB])
            for b in range(NB):
                k.dma(hbuf[b * BLK:(b + 1) * BLK, :].rearrange("(a p) d -> p a d", p=128), z[:], reads=[zB])
            k.barrier()
            ht = [P.sb([128, D], BF16, f"sht{i}") for i in range(3)]
            inf = [P.sb([128, 24], F32, f"inf{i}") for i in range(3)]
            t8, t8B = P.sb([128, 2, NEXP], F32, "t8")
            pf, pfB = P.sb([128, 2], F32, "pf")

            def loadi(i):
                s, tk, c = subt[i]
                k.dma(inf[i % 3][0][:], dr["rinfo"][s, tk:tk + 128, :], writes=[inf[i % 3][1]])

            def loadh(i):
                s, tk, c = subt[i]
                k.dma(ht[i % 3][0][:], dr["htok"][s, tk:tk + 128, :], writes=[ht[i % 3][1]])

            loadi(0)
            for i in range(NSUB):
                if i + 1 < NSUB:
                    loadi(i + 1)
                n_, nB_ = inf[i % 3]
                k.tt("dve", t8[:, 0, :], n_[:, 0:8], starts[0][:], ALU.mult, [nB_, starts[1]], [t8B])
                k.tt("dve", t8[:, 1, :], n_[:, 8:16], starts[0][:], ALU.mult, [nB_, starts[1]], [t8B])
                k.op("dve", lambda h: h.reduce_sum(out=pf[:, 0:2], in_=t8[:, :, :], axis=mybir.AxisListType.X), [t8B], [pfB])
                k.tt("dve", pf[:], pf[:], n_[:, 16:18], ALU.add, [pfB, nB_], [pfB])
                k.copy("dve", posall[0][:, i, :], pf[:], [pfB], [posall[1]])
                k.copy("dve", pwall[0][:, i, :], n_[:, 18:20], [nB_], [pwall[1]])
            k.barrier()
            loadh(0)
            for i in range(NSUB):
                if i + 1 < NSUB:
                    loadh(i + 1)
                h_, hB_ = ht[i % 3]
                for j in range(2):
                    k.idma(hbuf[:, :], h_[:], posall[0][:, i, j:j + 1], gather=False, reads=[hB_, posall[1]])
        with Pool_(k) as P:
            Wg = []
            for i in range(2):
                Wg.append(((P.sb([128, KC, GF], BF16, f"W1g{i}")[0], [Buf() for _ in range(KC)]),
                           (P.sb([128, KC, GF], BF16, f"W3g{i}")[0], [Buf() for _ in range(KC)]),
                           (P.sb([128, 7, D], BF16, f"W2g{i}")[0], [Buf() for _ in range(7)])))
            hblk = [P.sb([128, 4, D], BF16, f"hblk{i}") for i in range(1)] * 2
            hT = [P.sb([128, KC, BLK], BF16, f"hT{i}") for i in range(2)]
            gT = [P.sb([128, 7, BLK], BF16, f"gT{i}") for i in range(2)]
            yacc = [P.sb([128, 4, D], F32, f"yacc{i}") for i in range(1)] * 2
            sa = [P.sb([128, BLK], F32, f"sa{i}") for i in range(2)]
            pa = [P.ps(f"pa{i}") for i in range(2)]
            pb = [P.ps(f"pb{i}") for i in range(2)]
            py = [P.ps(f"py{i}") for i in range(2)]
            pth, pthB = P.ps("pth")
            groups = [(b, g) for b in range(NB) for g in range(NG)]

            def wload(gi):
                b, g = groups[gi]
                (W1, W1B), (W3, W3B), (W2, W2B) = Wg[gi % 2]
                for kc in range(KC):
                    k.idma(W1[:, kc, :], w1f[:, :], widx1[0][:, b, g, kc:kc + 1], gather=True, reads=[widx1[1]], writes=[W1B[kc]])
                    k.idma(W3[:, kc, :], w3f[:, :], widx1[0][:, b, g, kc:kc + 1], gather=True, reads=[widx1[1]], writes=[W3B[kc]])
                for j in range(7):
                    k.idma(W2[:, j, :], w2f[:, :], widx2[0][:, b, g * 7 + j:g * 7 + j + 1], gather=True, reads=[widx2[1]], writes=[W2B[j]])

            def hload(b):
                k.dma(hblk[b % 2][0][:], hbuf[b * BLK:(b + 1) * BLK, :].rearrange("(a p) d -> p a d", p=128), writes=[hblk[b % 2][1]])

            wload(0)
            hload(0)
            cnt = 0
            ycnt = 0
            for gi, (b, g) in enumerate(groups):
                if gi + 1 < len(groups):
                    wload(gi + 1)
                (W1, W1B), (W3, W3B), (W2, W2B) = Wg[gi % 2]
                h_, hB_ = hT[b % 2]
                ya, yaB = yacc[b % 2]
                if g == 0:
                    hb_, hbB_ = hblk[b % 2]
                    for kc in range(KC):
                        for a in range(4):
                            k.op("pe", lambda h: h.transpose(pth[:, 0:256].bitcast(BF16)[:, a * 128:(a + 1) * 128],
                                                             hb_[:, a, kc * 128:(kc + 1) * 128], pg.id_bf[:]), [hbB_, pg.idbB], [pthB])
                        if kc % 2 == 0:
                            k.copy("act", h_[:, kc, :], pth[:, 0:256].bitcast(BF16)[:, 0:BLK], [pthB], [hB_])
                        else:
                            k.copy("dve", h_[:, kc, :], pth[:, 0:256].bitcast(BF16)[:, 0:BLK], [pthB], [hB_])
                    if b + 1 < NB:
                        hload(b + 1)
                g_, gB_ = gT[gi % 2]
                for j in range(7):
                    a_, aB = pa[cnt % 2]; b_, bB = pb[cnt % 2]; s_, sB = sa[cnt % 2]
                    cnt += 1
                    for kc in range(KC):
                        k.mm(a_[:, :], W1[:, kc, j * 128:(j + 1) * 128], h_[:, kc, :], kc == 0, kc == KC - 1, [W1B[kc], hB_], [aB])
                    for kc in range(KC):
                        k.mm(b_[:, :], W3[:, kc, j * 128:(j + 1) * 128], h_[:, kc, :], kc == 0, kc == KC - 1, [W3B[kc], hB_], [bB])
                    k.act(s_[:], a_[:, :], AF.Silu, [aB], [sB])
                    k.tt("dve", g_[:, j, :], b_[:, :], s_[:], ALU.mult, [bB, sB], [gB_])
                for a in range(4):
                    for hf_ in range(2):
                        y_, yB = py[ycnt % 2]
                        ycnt += 1
                        for j in range(7):
                            k.mm(y_[:, :], g_[:, j, a * 128:(a + 1) * 128], W2[:, j, hf_ * 512:(hf_ + 1) * 512], j == 0, j == 6, [gB_, W2B[j]], [yB])
                        dst = ya[:, a, hf_ * 512:(hf_ + 1) * 512]
                        if g == 0:
                            k.copy("act", dst, y_[:, :], [yB], [yaB])
                        else:
                            k.tt("dve", dst, dst, y_[:, :], ALU.add, [yaB, yB], [yaB])
                if g == NG - 1:
                    k.dma(obuf[b * BLK:(b + 1) * BLK, :].rearrange("(a p) d -> p a d", p=128), ya[:], reads=[yaB])
        with Pool_(k) as P:
            r1 = [P.sb([128, D], F32, f"r1_{i}") for i in range(2)]
            r2 = [P.sb([128, D], F32, f"r2_{i}") for i in range(2)]
            yo = [P.sb([128, KC, 128], F32, f"yo{i}") for i in range(2)]
            pt = [P.ps(f"pt{i}") for i in range(2)]

            def gload(i):
                k.idma(r1[i % 2][0][:], obuf[:, :], posall[0][:, i, 0:1], gather=True, reads=[posall[1]], writes=[r1[i % 2][1]])
                k.idma(r2[i % 2][0][:], obuf[:, :], posall[0][:, i, 1:2], gather=True, reads=[posall[1]], writes=[r2[i % 2][1]])

            gload(0)
            for i, (s, tk, c) in enumerate(subt):
                if i + 1 < NSUB:
                    gload(i + 1)
                a_, aB = r1[i % 2]; b_, bB = r2[i % 2]; o_, oB = yo[i % 2]
                j = pg.jidx(s, c)
                k.ts("dve", a_[:], a_[:], pwall[0][:, i, 0:1], None, ALU.mult, None, [aB, pwall[1]], [aB])
                k.stt(a_[:], b_[:], pwall[0][:, i, 1:2], a_[:], ALU.mult, ALU.add, [bB, pwall[1], aB], [aB])
                for hf_ in range(2):
                    p_, pB = pt[hf_]
                    for q in range(4):
                        oc = hf_ * 4 + q
                        k.op("pe", lambda h: h.transpose(p_[:, q * 128:(q + 1) * 128], a_[:, oc * 128:(oc + 1) * 128], pg.id_f[:]),
                             [aB, pg.idfB], [pB])
                    for q in range(4):
                        oc = hf_ * 4 + q
                        if q % 2 == 0:
                            k.act(o_[:, oc, :], p_[:, q * 128:(q + 1) * 128], AF.Identity, [pB, pg.modB], [oB], scale=pg.mod[:, l, 40 + oc, j:j + 1])
                        else:
                            k.ts("dve", o_[:, oc, :], p_[:, q * 128:(q + 1) * 128], pg.mod[:, l, 40 + oc, j:j + 1], None, ALU.mult, None,
                                 [pB, pg.modB], [oB])
                k.dma(dr["xs"][s].rearrange("(kc p) t -> p kc t", p=128)[:, :, tk:tk + 128], o_[:], reads=[oB], q="pool", accum_op=ALU.add)


def build(stop_after=99, debug=False):
    nc = bass.Bass("TRN2", target_bir_lowering=False)
    dr = {}

    def inp(name, shape, dt=F32):
        dr[name] = nc.dram_tensor(name, list(shape), dt, kind="ExternalInput").ap()

    def scr(name, shape, dt, out=False):
        dr[name] = nc.dram_tensor(name, list(shape), dt, kind="ExternalOutput" if out else "Internal").ap()

    inp("xin", [NS, D, TOK]); inp("cvec", [128, KC, 3]); inp("ada_w", [DEPTH, D, 6 * D]); inp("ada_b", [128, DEPTH, 48])
    inp("norm_g", [128, DEPTH, 2, KC]); inp("final_g", [128, KC])
    inp("ones_bf", [128, 128], BF16); inp("id_bf", [128, 128], BF16); inp("id_f", [128, 128]); inp("ones_f", [128, 128])
    inp("na_w_qkv", [2, D, 3 * D]); inp("na_w_o", [2, D, D]); inp("na_tab", [2, 8, 128, 2, 2, 22, 64])
    inp("ffn_w1", [2, D, DFF]); inp("ffn_w3", [2, D, DFF]); inp("ffn_w2", [2, DFF, D])
    inp("moe_router", [2, 128, KC, NEXP])
    for f_ in range(2):
        inp(f"moe_w1_{f_}", [NEXP, D, DFE]); inp(f"moe_w3_{f_}", [NEXP, D, DFE]); inp(f"moe_w2_{f_}", [NEXP, DFE, D])
    inp("tris", [128, 128]); inp("thr48", [128, 48]); inp("iota48", [128, 48]); inp("pk1", [128, KC]); inp("pk2", [128, 28])
    inp("pool_w", [4, 256, 256]); inp("pool_scale", [128, KC]); inp("pool_ic", [128, 4, 2, 256])
    gdn_inputs(inp)
    scr("xs", [NS, D, TOK], F32, out=debug)
    scr("hs", [NS, D, TOK], BF16)
    scr("qs", [NS, D, TOK], BF16); scr("ks", [NS, D, TOK], BF16); scr("vs", [NS, TOK, D], BF16); scr("os", [NS, D, TOK], BF16)
    scr("wexp", [NS, NEXP, TOK], F32)
    scr("htok", [NS, TOK, D], BF16, out=debug); scr("rinfo", [NS, TOK, 24], F32, out=debug)
    scr("hbuf", [42 * BLK, D], BF16, out=debug); scr("obuf", [42 * BLK, D], F32, out=debug)

    gdn_scratch(scr)
    scr("out", [NS, D, NLAT], F32, out=True)

    with ExitStack() as es:
        k = K(nc, es)
        pg = Prog(nc, k, dr, stop_after)
        with Pool_(k) as PC:
            pg.setup_consts(PC)
            for s in range(NS):
                for kc in range(KC):
                    k.dma(dr["xs"][s, kc * 128:(kc + 1) * 128, :], dr["xin"][s, kc * 128:(kc + 1) * 128, :])
            pg.adaln()
            k.barrier()
            step = 0
            for l in range(DEPTH):
                last = l == DEPTH - 1
                if step >= stop_after:
                    break
                pg.norm_pass(l, 0, include_ctx=True)
                kind = l % 3
                jx = l // 3
                if kind == 0:
                    pg.na_qkv_pass(dr["na_w_qkv"][jx])
                    pg.na_attn_pass(dr["na_tab"][jx], need_ctx=not last)
                    pg.outproj_pass(l, dr["os"], dr["na_w_o"][jx], include_ctx=not last, gate_off=16)
                elif kind == 1:
                    gdn_mixer(pg, l, need_ctx=not last)
                else:
                    pg.pool_pass(l, dr["pool_w"], include_ctx=not last)
                step += 1
                if step >= stop_after:
                    break
                f = l // 2
                if l % 2 == 0:
                    pg.norm_pass(l, 1, include_ctx=not last)
                    H = DFF // 2
                    for hh in range(2):
                        pg.ffn_pass(l, dr["ffn_w1"][f][:, hh * H:(hh + 1) * H], dr["ffn_w3"][f][:, hh * H:(hh + 1) * H],
                                    dr["ffn_w2"][f][hh * H:(hh + 1) * H, :], H, include_ctx=not last)
                else:
                    moe_layer(pg, l, f, include_ctx=not last)
                step += 1
            pg.norm_pass(0, 0, include_ctx=False, final=True)
            k.barrier()
    return nc


GDN_IN = 4128
NCHK = TOK // 128


def gdn_inputs(inp):
    inp("gdn_w_in", [D, GDN_IN]); inp("gdn_w_o", [D, D]); inp("gdn_cw", [128, 24, 4])
    inp("gdn_dtb", [128, 16]); inp("gdn_alog", [128, 16]); inp("gdn_ng", [128, 1])
    inp("gdn_tri", [128, 2, 128]); inp("gdn_gm", [128, 2, 3, 128]); inp("gdn_ms", [128, 7, 128], BF16)


def gdn_scratch(scr):
    scr("pj", [NS, 4 * D, TOK], BF16)
    scr("gq", [NS, D, TOK], BF16); scr("gk", [NS, D, TOK], BF16); scr("gv", [NS, D, TOK], BF16)
    scr("gt", [NS, TOK, 48], F32)
    scr("gcs", [NS, 16, TOK], F32)
    scr("gbs", [NS, 16, TOK], F32)


def gdn_proj_pass(pg):
    k, dr = pg.k, pg.dr
    hs, pj = dr["hs"], dr["pj"]
    tl = tiles_for(True)
    with Pool_(k) as P:
        Wi, WiB = P.sb([128, KC, GDN_IN], BF16, "Wi")
        src = dr["gdn_w_in"].rearrange("(kc p) n -> p kc n", p=128)
        for i in range(4):
            k.dma(Wi[:, :, i * 1032:(i + 1) * 1032], src[:, :, i * 1032:(i + 1) * 1032], writes=[WiB], q="pool")
        dtb, dtbB = P.sb([128, 16], F32, "dtb"); k.dma(dtb[:], dr["gdn_dtb"][:, :], writes=[dtbB])
        nA, nAB = P.sb([128, 16], F32, "nA"); k.dma(nA[:], dr["gdn_alog"][:, :], writes=[nAB])
        k.act(nA[:], nA[:], AF.Exp, [nAB], [nAB])
        k.ts("dve", nA[:], nA[:], -1.0, None, ALU.mult, None, [nAB], [nAB])
        tri, triB = P.sb([128, 2, 128], F32, "tri"); k.dma(tri[:], dr["gdn_tri"][:, :, :], writes=[triB])
        ht = [P.sb([128, KC, TT], BF16, f"ht{i}") for i in range(2)]
        pt = [P.sb([128, 32, TT], BF16, f"pjt{i}") for i in range(1)]
        gtt, gttB = P.sb([128, 4, 48], F32, "gtt")
        tmpa, tmpaB = P.sb([128, 16], F32, "tmpa")
        gcT, gcTB = P.sb([16, TT], F32, "gcT")
        bT, bTB = P.sb([16, TT], F32, "bT")
        pp = [P.ps(f"pp{i}") for i in range(4)]
        pab, pabB = P.ps("pab")
        pgc, pgcB = P.ps("pgc")
        ptr, ptrB = P.ps("ptr")
        pbf, pbfB = P.ps("pbf")

        def load(i):
            s, t0, W, c = tl[i]
            t, B = ht[i % 2]
            k.dma(t[:, :, :W], hs[s].rearrange("(kc p) t -> p kc t", p=128)[:, :, t0:t0 + W], writes=[B])

        load(0)
        cnt = 0
        for i, (s, t0, W, c) in enumerate(tl):
            if i + 1 < len(tl):
                load(i + 1)
            h, hB = ht[i % 2]
            p_, pjB = pt[0]
            for oc in range(32):
                q_, qB = pp[cnt % 4]
                for kc in range(KC):
                    k.mm(q_[:, :W], Wi[:, kc, oc * 128:(oc + 1) * 128], h[:, kc, :W], kc == 0, kc == KC - 1, [WiB, hB], [qB])
                if cnt % 2 == 0:
                    k.act(p_[:, oc, :W], q_[:, :W], AF.Copy, [qB], [pjB])
                else:
                    k.copy("dve", p_[:, oc, :W], q_[:, :W], [qB], [pjB])
                cnt += 1
            k.dma(pj[s].rearrange("(oc p) t -> p oc t", p=128)[:, :, t0:t0 + W], p_[:, :, :W], reads=[pjB])
            for kc in range(KC):
                k.mm(pbf[0:16, :W], Wi[:, kc, 4096 + 16:4096 + 32], h[:, kc, :W], kc == 0, kc == KC - 1, [WiB, hB], [pbfB])
            k.act(bT[:, :W], pbf[0:16, :W], AF.Sigmoid, [pbfB], [bTB])
            k.dma(dr["gbs"][s, :, t0:t0 + W], bT[:, :W], reads=[bTB])
            nst = W // 128
            for ts_ in range(nst):
                for kc in range(KC):
                    k.mm(pab[:, ts_ * 32:(ts_ + 1) * 32], h[:, kc, ts_ * 128:(ts_ + 1) * 128], Wi[:, kc, 4096:4128],
                         kc == 0, kc == KC - 1, [WiB, hB], [pabB])
            for ts_ in range(nst):
                k.tt("dve", tmpa[:], pab[:, ts_ * 32:ts_ * 32 + 16], dtb[:], ALU.add, [pabB, dtbB], [tmpaB])
                k.act(tmpa[:], tmpa[:], AF.Exp, [tmpaB], [tmpaB])
                k.act(tmpa[:], tmpa[:], AF.Ln, [tmpaB], [tmpaB], bias=1.0)
                k.tt("dve", gtt[:, ts_, 0:16], tmpa[:], nA[:], ALU.mult, [tmpaB, nAB], [gttB])
                k.act(gtt[:, ts_, 16:32], pab[:, ts_ * 32 + 16:ts_ * 32 + 32], AF.Sigmoid, [pabB], [gttB])
                for d in range(2):
                    k.mm(pgc[:, ts_ * 16 + d * 8:ts_ * 16 + d * 8 + 8], tri[:, d, :], gtt[:, ts_, d * 8:d * 8 + 8], True, True,
                         [triB, gttB], [pgcB])
                k.copy("dve", gtt[:, ts_, 32:48], pgc[:, ts_ * 16:ts_ * 16 + 16], [pgcB], [gttB])
                k.op("pe", lambda hh: hh.transpose(ptr[0:16, ts_ * 128:(ts_ + 1) * 128], gtt[:, ts_, 32:48], pg.id_f[:]),
                     [gttB, pg.idfB], [ptrB])
            k.copy("dve", gcT[:, :W], ptr[0:16, :W], [ptrB], [gcTB])
            k.dma(dr["gcs"][s, :, t0:t0 + W], gcT[:, :W], reads=[gcTB])
            k.dma(dr["gt"][s, t0:t0 + W, :].rearrange("(a p) c -> p a c", p=128), gtt[:, :nst, :], reads=[gttB])


def gdn_conv_pass(pg):
    k, dr = pg.k, pg.dr
    pj = dr["pj"]
    tl = tiles_for(True)
    with Pool_(k) as P:
        cw, cwB = P.sb([128, 24, 4], F32, "cw"); k.dma(cw[:], dr["gdn_cw"][:, :, :], writes=[cwB])
        e1, e1B = P.sb([128, 1], F32, "e1"); k.memset("pool", e1[:], EPS, [e1B])
        e2, e2B = P.sb([128, 1], F32, "e2"); k.memset("pool", e2[:], EPS * 128.0, [e2B])
        pin = [P.sb([128, 24, TT + 4], BF16, f"pin{i}") for i in range(2)]
        ot, otB = P.sb([128, 24, TT], BF16, "cot")
        acc = [P.sb([128, TT], F32, f"acc{i}") for i in range(2)]
        sl = [P.sb([128, TT], F32, f"sl{i}") for i in range(2)]
        sq = [P.sb([128, TT], BF16, f"sq{i}") for i in range(2)]
        rn = [P.sb([128, TT], F32, f"rn{i}") for i in range(2)]
        pss = [P.ps(f"pss{i}") for i in range(2)]

        def load(i):
            s, t0, W, c = tl[i]
            t, B = pin[i % 2]
            seq0, seq1 = (0, NCTX) if c else (NCTX, TOK)
            k.memset("pool", t[:], 0.0, [B])
            lo, hi = max(seq0, t0 - 2), min(seq1, t0 + W + 1)
            k.dma(t[:, :, lo - (t0 - 2):hi - (t0 - 2)], pj[s].rearrange("(oc p) t -> p oc t", p=128)[:, 0:24, lo:hi], writes=[B])

        load(0)
        cnt = 0
        for i, (s, t0, W, c) in enumerate(tl):
            if i + 1 < len(tl):
                load(i + 1)
            x, xB = pin[i % 2]
            for oc in range(24):
                a_, aB = acc[cnt % 2]; s_, sB = sl[cnt % 2]; q_, qB = sq[cnt % 2]; r_, rB = rn[cnt % 2]; p_, pB = pss[cnt % 2]
                cnt += 1
                k.ts("dve", a_[:, :W], x[:, oc, 0:W], cw[:, oc, 0:1], None, ALU.mult, None, [xB, cwB], [aB])
                for j in range(1, 4):
                    k.stt(a_[:, :W], x[:, oc, j:j + W], cw[:, oc, j:j + 1], a_[:, :W], ALU.mult, ALU.add, [xB, cwB, aB], [aB])
                if oc >= 16:
                    k.act(ot[:, oc, :W], a_[:, :W], AF.Silu, [aB], [otB])
                    continue
                k.act(s_[:, :W], a_[:, :W], AF.Silu, [aB], [sB])
                k.act(q_[:, :W], s_[:, :W], AF.Square, [sB], [qB])
                k.mm(p_[:, :W], pg.ones_bf[:], q_[:, :W], True, True, [pg.onesB, qB], [pB])
                if oc < 8:
                    k.act(r_[:, :W], p_[:, :W], AF.Sqrt, [pB, e2B], [rB], scale=128.0, bias=e2[:, 0:1])
                else:
                    k.act(r_[:, :W], p_[:, :W], AF.Sqrt, [pB, e1B], [rB], bias=e1[:, 0:1])
                k.op("dve", lambda hh: hh.reciprocal(out=r_[:, :W], in_=r_[:, :W]), [rB], [rB])
                k.tt("pool", ot[:, oc, :W], s_[:, :W], r_[:, :W], ALU.mult, [sB, rB], [otB])
            for gi, nm in enumerate(("gq", "gk", "gv")):
                k.dma(dr[nm][s].rearrange("(oc p) t -> p oc t", p=128)[:, :, t0:t0 + W], ot[:, gi * 8:(gi + 1) * 8, :W], reads=[otB])


def _rr(gens):
    gens = list(gens)
    while gens:
        nxt = []
        for g in gens:
            try:
                next(g)
                nxt.append(g)
            except StopIteration:
                pass
        gens = nxt


def gdn_core_pass(pg):
    k, dr = pg.k, pg.dr
    with Pool_(k) as P:
        gm, gmB = P.sb([128, 2, 3, 128], F32, "gm"); k.dma(gm[:], dr["gdn_gm"][:, :, :, :], writes=[gmB])
        ms, msB = P.sb([128, 7, 128], BF16, "ms"); k.dma(ms[:], dr["gdn_ms"][:, :, :], writes=[msB])
        gng, gngB = P.sb([128, 1], F32, "gng"); k.dma(gng[:], dr["gdn_ng"][:, :], writes=[gngB])
        e1, e1B = P.sb([128, 1], F32, "e1"); k.memset("pool", e1[:], EPS, [e1B])
        qT, qTB = P.sb([128, TOK], BF16, "qT"); kT, kTB = P.sb([128, TOK], BF16, "kT"); vT, vTB = P.sb([128, TOK], BF16, "vT")
        zT, zTB = P.sb([128, TOK], BF16, "zT")
        ktok, ktokB = P.sb([128, NCHK, 128], BF16, "ktok"); vtok, vtokB = P.sb([128, NCHK, 128], BF16, "vtok")
        gt, gtB = P.sb([128, NCHK, 48], F32, "gt")
        gcb, gcbB = P.sb([128, TOK], F32, "gcb"); bb, bbB = P.sb([128, TOK], F32, "bb")
        U, UB_ = P.sb([128, NCHK, 128], F32, "U"); UB = [Buf(f"U{n}") for n in range(NCHK)]
        WT, _ = P.sb([128, NCHK, 128], BF16, "WT"); WTB = [Buf(f"WT{n}") for n in range(NCHK)]
        AQ, _ = P.sb([128, NCHK, 128], BF16, "AQ"); AQB = [Buf(f"AQ{n}") for n in range(NCHK)]
        KTL, _ = P.sb([128, NCHK, 128], BF16, "KTL"); KTLB = [Buf(f"KTL{n}") for n in range(NCHK)]
        qd, qdB = P.sb([128, TOK], BF16, "qd")
        oacc, oaccB = P.sb([128, TOK], F32, "oacc"); oB = [Buf(f"o{n}") for n in range(NCHK)]
        egc, egcB = P.sb([128, NCHK], F32, "egc"); ett, ettB = P.sb([128, NCHK], F32, "ett"); egl, eglB = P.sb([128, NCHK], F32, "egl")
        S, SB = P.sb([128, 128], F32, "S"); Sb, SbB = P.sb([128, 128], BF16, "Sb")
        vn = [P.sb([128, 128], BF16, f"vn{i}") for i in range(2)]
        tmpx, tmpxB = P.sb([128, 512], F32, "tmpx")
        NI = 4
        wk = []
        for ii in range(NI):
            w = {}
            for nm in ("xa", "xb", "xc", "t1"):
                w[nm] = P.sb([128, 128], F32, f"{nm}{ii}")
            w["ea"], w["eb"], w["ec"] = w["xa"], w["xb"], w["xc"]
            for nm in ("L", "M", "X0", "X1", "Y0", "Y1", "Wm", "Wn", "vb", "kbg"):
                w[nm] = P.sb([128, 128], BF16, f"{nm}{ii}")
            w["pA"] = P.ps(f"pA{ii}"); w["pB"] = P.ps(f"pB{ii}"); w["pC"] = w["pB"]
            wk.append(w)
        pX, pXB = wk[0]["pA"]; pY, pYB = wk[1]["pA"]

        for s in range(NS):
            k.dma(gt[:], dr["gt"][s].rearrange("(n p) c -> p n c", p=128), writes=[gtB])
            for h in range(8):
                rows = slice(h * 128, (h + 1) * 128)
                k.dma(qT[:], dr["gq"][s, rows, :], writes=[qTB]); k.dma(kT[:], dr["gk"][s, rows, :], writes=[kTB])
                k.dma(vT[:], dr["gv"][s, rows, :], writes=[vTB])
                k.dma(zT[:], dr["pj"][s, 3 * D + h * 128:3 * D + (h + 1) * 128, :], writes=[zTB])
                ktBs = [Buf() for _ in range(NCHK)]
                vtBs = [Buf() for _ in range(NCHK)]
                for n in range(NCHK):
                    cs = slice(n * 128, (n + 1) * 128)
                    ka, kaB = wk[(2 * n) % NI]["pA"]
                    va, vaB = wk[(2 * n + 1) % NI]["pA"]
                    k.op("pe", lambda hh: hh.transpose(ka[:, 0:128].bitcast(BF16)[:, 0:128], kT[:, cs], pg.id_bf[:]), [kTB, pg.idbB], [kaB])
                    k.copy("act", ktok[:, n, :], ka[:, 0:128].bitcast(BF16)[:, 0:128], [kaB], [ktBs[n]] + ([ktokB] if n == 0 else []))
                    k.op("pe", lambda hh: hh.transpose(va[:, 0:128].bitcast(BF16)[:, 0:128], vT[:, cs], pg.id_bf[:]), [vTB, pg.idbB], [vaB])
                    k.copy("dve", vtok[:, n, :], va[:, 0:128].bitcast(BF16)[:, 0:128], [vaB], [vtBs[n]] + ([vtokB] if n == 0 else []))
                k.op("act", lambda hh: hh.activation(out=egc[:, 0:1], in_=egc[:, 0:1], func=AF.Copy), ktBs, [ktokB, egcB])
                k.op("dve", lambda hh: hh.tensor_copy(out=ett[:, 0:1], in_=ett[:, 0:1]), vtBs, [vtokB, ettB])
                for d in range(2):
                    col = d * 8 + h
                    k.dma(gcb[:], dr["gcs"][s, col:col + 1, :].partition_broadcast(128), writes=[gcbB])
                    k.dma(bb[:], dr["gbs"][s, col:col + 1, :].partition_broadcast(128), writes=[bbB])
                    gcv = gt[:, :, 32 + col]
                    btv = gt[:, :, 16 + col]
                    lastoff = 127 if d == 0 else 0
                    glv = gcb[:, lastoff:TOK:128]
                    k.act(egc[:], gcv, AF.Exp, [gtB], [egcB])
                    k.tt("dve", ett[:], glv, gcv, ALU.subtract, [gcbB, gtB], [ettB])
                    k.act(ett[:], ett[:], AF.Exp, [ettB], [ettB])
                    k.act(egl[:], glv, AF.Exp, [gcbB], [eglB])
                    for c0 in range(0, TOK, 512):
                        wd = min(512, TOK - c0)
                        k.act(tmpx[:, :wd], gcb[:, c0:c0 + wd], AF.Exp, [gcbB], [tmpxB])
                        k.tt("dve", qd[:, c0:c0 + wd], qT[:, c0:c0 + wd], tmpx[:, :wd], ALU.mult, [qTB, tmpxB], [qdB])

                    def inst(n, w):
                        cs = slice(n * 128, (n + 1) * 128)
                        gcn = gt[:, n, 32 + col:33 + col]; btn = gt[:, n, 16 + col:17 + col]
                        (xa, xaB), (xb, xbB), (xc, xcB) = w["xa"], w["xb"], w["xc"]
                        (ea, eaB), (eb, ebB), (ec, ecB) = w["ea"], w["eb"], w["ec"]
                        (t1, t1B), (L, LB), (M, MB) = w["t1"], w["L"], w["M"]
                        (pA, pAB), (pB_, pBB), (pC, pCB) = w["pA"], w["pB"], w["pC"]
                        k.stt(xa[:], gcb[:, cs], gcn, gm[:, d, 0, :], ALU.subtract, ALU.add, [gcbB, gtB, gmB], [xaB])
                        k.stt(xb[:], gcb[:, cs], gcn, gm[:, d, 1, :], ALU.subtract, ALU.add, [gcbB, gtB, gmB], [xbB])
                        k.stt(xc[:], gcb[:, cs], gcn, gm[:, d, 2, :], ALU.subtract, ALU.add, [gcbB, gtB, gmB], [xcB])
                        k.mm(pA[:, 0:128], kT[:, cs], kT[:, cs], True, True, [kTB], [pAB])
                        k.mm(pB_[:, 0:128], kT[:, cs], qT[:, cs], True, True, [kTB, qTB], [pBB])
                        yield
                        k.act(ea[:], xa[:], AF.Exp, [xaB], [eaB])
                        k.act(eb[:], xb[:], AF.Exp, [xbB], [ebB])
                        k.act(ec[:], xc[:], AF.Exp, [xcB], [ecB], scale=-1.0)
                        yield
                        k.stt(L[:], pA[:, 0:128], btn, ec[:], ALU.mult, ALU.mult, [pAB, gtB, ecB], [LB])
                        k.tt("dve", t1[:], pA[:, 0:128], eb[:], ALU.mult, [pAB, ebB], [t1B])
                        k.tt("pool", M[:], t1[:], bb[:, cs], ALU.mult, [t1B, bbB], [MB])
                        k.tt("dve", AQ[:, n, :], pB_[:, 0:128], ea[:], ALU.mult, [pBB, eaB], [AQB[n]])
                        k.ts("pool", w["vb"][0][:], vtok[:, n, :], btn, None, ALU.mult, None, [vtokB, gtB], [w["vb"][1]])
                        k.ts("pool", w["kbg"][0][:], ktok[:, n, :], btn, egc[:, n:n + 1], ALU.mult, ALU.mult, [ktokB, gtB, egcB], [w["kbg"][1]])
                        k.ts("pool", KTL[:, n, :], ktok[:, n, :], ett[:, n:n + 1], None, ALU.mult, None, [ktokB, ettB], [KTLB[n]])
                        yield
                        X, XB = w["X0"]; Y, YB = w["Y0"]; Xn, XnB = w["X1"]; Yn, YnB = w["Y1"]
                        Wm, WmB = w["Wm"]; Wn, WnB = w["Wn"]
                        k.tt("pool", Wm[:], L[:], ms[:, 0, :], ALU.mult, [LB, msB], [WmB])
                        k.tt("pool", X[:], pg.id_bf[:], Wm[:], ALU.subtract, [pg.idbB, WmB], [XB])
                        k.tt("pool", Wn[:], M[:], ms[:, 0, :], ALU.mult, [MB, msB], [WnB])
                        k.tt("pool", Y[:], pg.id_bf[:], Wn[:], ALU.subtract, [pg.idbB, WnB], [YB])
                        yield
                        for lv in range(1, 7):
                            lastlv = lv == 6
                            if not lastlv:
                                k.mm(pA[:, 0:128], M[:], X[:], True, True, [MB, XB], [pAB])
                            k.mm(pB_[:, 0:128], L[:], Y[:], True, True, [LB, YB], [pBB])
                            yield
                            if not lastlv:
                                k.tt("dve", Wm[:], pA[:, 0:128], ms[:, lv, :], ALU.mult, [pAB, msB], [WmB])
                            k.tt("dve", Wn[:], pB_[:, 0:128], ms[:, lv, :], ALU.mult, [pBB, msB], [WnB])
                            yield
                            if not lastlv:
                                k.mm(pA[:, 0:128], Y[:], Wm[:], True, True, [YB, WmB], [pAB])
                            k.mm(pC[:, 0:128], X[:], Wn[:], True, True, [XB, WnB], [pCB])
                            yield
                            if not lastlv:
                                k.tt("dve", Xn[:], X[:], pA[:, 0:128], ALU.subtract, [XB, pAB], [XnB])
                            k.tt("dve", Yn[:], Y[:], pC[:, 0:128], ALU.subtract, [YB, pCB], [YnB])
                            X, XB, Xn, XnB = Xn, XnB, X, XB
                            Y, YB, Yn, YnB = Yn, YnB, Y, YB
                            yield
                        k.mm(pA[:, 0:128], Y[:], w["vb"][0][:], True, True, [YB, w["vb"][1]], [pAB])
                        k.mm(pB_[:, 0:128], w["kbg"][0][:], Y[:], True, True, [w["kbg"][1], YB], [pBB])
                        yield
                        k.copy("act", U[:, n, :], pA[:, 0:128], [pAB], [UB[n]])
                        k.copy("act", WT[:, n, :], pB_[:, 0:128], [pBB], [WTB[n]])
                        yield

                    import contextlib
                    sc_ = (lambda nm: pg.nc.named_scope(nm)) if (s == 0 and h == 1) else (lambda nm: contextlib.nullcontext())
                    with sc_(f"gi_inst{d}"):
                        for n0 in range(0, NCHK, NI):
                            _rr([inst(n0 + ii, wk[ii]) for ii in range(NI) if n0 + ii < NCHK])
                    k.memset("pool", S[:], 0.0, [SB]); k.memset("pool", Sb[:], 0.0, [SbB])
                    order = [0, 1] + list(range(2, NCHK)) if d == 0 else [1, 0] + list(range(NCHK - 1, 1, -1))
                    for it, n in enumerate(order):
                        cs = slice(n * 128, (n + 1) * 128)
                        v_, vB_ = vn[it % 2]
                        k.mm(pX[:, 0:128], WT[:, n, :], Sb[:], True, True, [WTB[n], SbB], [pXB])
                        k.tt("dve", v_[:], U[:, n, :], pX[:, 0:128], ALU.subtract, [UB[n], pXB], [vB_])
                        k.mm(pY[:, 0:128], Sb[:], qd[:, cs], True, False, [SbB, qdB], [pYB])
                        k.mm(pY[:, 0:128], v_[:], AQ[:, n, :], False, True, [vB_, AQB[n]], [pYB])
                        if d == 0:
                            k.copy("act", oacc[:, cs], pY[:, 0:128], [pYB], [oB[n]])
                        else:
                            k.tt("dve", oacc[:, cs], oacc[:, cs], pY[:, 0:128], ALU.add, [oB[n], pYB], [oB[n]])
                        k.mm(pX[:, 0:128], KTL[:, n, :], v_[:], True, True, [KTLB[n], vB_], [pXB])
                        k.stt(Sb[:], S[:], egl[:, n:n + 1], pX[:, 0:128], ALU.mult, ALU.add, [SB, eglB, pXB], [SbB])
                        k.stt(S[:], S[:], egl[:, n:n + 1], pX[:, 0:128], ALU.mult, ALU.add, [SB, eglB, pXB], [SB])
                for c0 in range(0, TOK, 512):
                    wd = min(512, TOK - c0)
                    obs = oB[c0 // 128:(c0 + wd) // 128]
                    xa, xaB = wk[0]["xa"]; xb, xbB = wk[0]["xb"]
                    sqt, sqB = P_sq = (qd, qdB)
                    k.act(sqt[:, c0:c0 + wd], oacc[:, c0:c0 + wd], AF.Square, obs, [qdB])
                    k.mm(pX[:, :wd], pg.ones_bf[:], sqt[:, c0:c0 + wd], True, True, [pg.onesB, qdB], [pXB])
                    k.act(tmpx[:, :wd], pX[:, :wd], AF.Sqrt, [pXB, e1B], [tmpxB], scale=1.0 / 128.0, bias=e1[:, 0:1])
                    k.op("dve", lambda hh: hh.reciprocal(out=tmpx[:, :wd], in_=tmpx[:, :wd]), [tmpxB], [tmpxB])
                    k.tt("dve", oacc[:, c0:c0 + wd], oacc[:, c0:c0 + wd], tmpx[:, :wd], ALU.mult, obs + [tmpxB], obs)
                    k.act(tmpx[:, :wd], zT[:, c0:c0 + wd], AF.Silu, [zTB], [tmpxB])
                    k.stt(qd[:, c0:c0 + wd], oacc[:, c0:c0 + wd], gng[:, 0:1], tmpx[:, :wd], ALU.mult, ALU.mult, obs + [gngB, tmpxB], [qdB])
                k.dma(dr["os"][s, rows, :], qd[:], reads=[qdB])


def gdn_mixer(pg, l, need_ctx):
    with pg.nc.named_scope("g_proj"):
        gdn_proj_pass(pg)
    with pg.nc.named_scope("g_conv"):
        gdn_conv_pass(pg)
    with pg.nc.named_scope("g_core"):
        gdn_core_pass(pg)
    with pg.nc.named_scope("g_out"):
        pg.outproj_pass(l, pg.dr["os"], pg.dr["gdn_w_o"], include_ctx=need_ctx, gate_off=16)


def _fm(v):
    v = np.asarray(v, np.float32)
    lead = v.shape[:-1]
    return np.ascontiguousarray(np.moveaxis(v.reshape(lead + (KC, 128)), -1, 0))


def _na_tables(rpb):
    col = np.arange(64)
    c0 = np.clip(col - 8, 0, 48)
    in_win = (col[None, :] >= c0[:, None]) & (col[None, :] < c0[:, None] + 16)
    dc = np.clip(col[None, :] - col[:, None], -15, 15) + 15
    out = np.full((8, 128, 2, 2, 22, 64), NEG, np.float32)
    for jj in range(22):
        j = 17 - jj
        for a in range(2):
            drr = j + a
            if drr < 0 or drr > 14:
                continue
            base = np.where(in_win[None], rpb[:, drr][:, dc], NEG).astype(np.float32)
            base = np.transpose(base, (0, 2, 1))
            for h in range(16):
                out[h // 2, a * 64:(a + 1) * 64, 1, h % 2, jj, :] = base[h]
                if 3 <= drr <= 10:
                    out[h // 2, a * 64:(a + 1) * 64, 0, h % 2, jj, :] = base[h]
    return out


def _pool_ic():
    out = np.ones((4, 2, 256), np.float32)
    for gi, win in enumerate((2, 4, 8, 16)):
        for ri, T in enumerate((64, 256)):
            t = np.arange(T)
            lo = np.clip(t - win // 2, 0, T)
            hi = np.clip(t + win // 2, 0, T)
            out[gi, ri, :T] = 1.0 / (hi - lo).astype(np.float32)
    return np.ascontiguousarray(np.broadcast_to(out[None], (128, 4, 2, 256)))


def make_in_maps(inputs, n_cores=8):
    import ml_dtypes
    x, c, ctx, c_ctx = inputs["x"], inputs["c"], inputs["ctx"], inputs["c_ctx"]
    shared = {
        "ada_w": np.ascontiguousarray(inputs["ada_w"], np.float32),
        "ada_b": np.ascontiguousarray(np.transpose(np.asarray(inputs["ada_b"], np.float32).reshape(DEPTH, 48, 128), (2, 0, 1))),
        "norm_g": _fm(inputs["norm_g"]),
        "final_g": _fm(inputs["final_g"]),
        "ones_bf": np.ones((128, 128), ml_dtypes.bfloat16),
        "id_bf": np.eye(128, dtype=np.float32).astype(ml_dtypes.bfloat16),
        "id_f": np.eye(128, dtype=np.float32),
        "ones_f": np.ones((128, 128), np.float32),
        "na_w_qkv": np.ascontiguousarray(inputs["na_w_qkv"], np.float32),
        "na_w_o": np.ascontiguousarray(inputs["na_w_o"], np.float32),
        "na_tab": np.stack([_na_tables(np.asarray(inputs["na_rpb"][j], np.float32)) for j in range(2)]),
        "ffn_w1": np.ascontiguousarray(inputs["ffn_w1"], np.float32),
        "ffn_w3": np.ascontiguousarray(inputs["ffn_w3"], np.float32),
        "ffn_w2": np.ascontiguousarray(inputs["ffn_w2"], np.float32),
        "moe_router": np.ascontiguousarray(np.transpose(np.asarray(inputs["moe_router"], np.float32).reshape(2, KC, 128, NEXP), (0, 2, 1, 3))),
        "moe_w1_0": np.ascontiguousarray(inputs["moe_w1"][0], np.float32), "moe_w1_1": np.ascontiguousarray(inputs["moe_w1"][1], np.float32),
        "moe_w3_0": np.ascontiguousarray(inputs["moe_w3"][0], np.float32), "moe_w3_1": np.ascontiguousarray(inputs["moe_w3"][1], np.float32),
        "moe_w2_0": np.ascontiguousarray(inputs["moe_w2"][0], np.float32), "moe_w2_1": np.ascontiguousarray(inputs["moe_w2"][1], np.float32),
        "tris": np.ascontiguousarray((np.arange(128)[:, None] < np.arange(128)[None, :]).astype(np.float32)),
        "thr48": np.ascontiguousarray(np.broadcast_to((np.arange(48) * 512.0).astype(np.float32)[None], (128, 48))),
        "iota48": np.ascontiguousarray(np.broadcast_to(np.arange(48).astype(np.float32)[None], (128, 48))),
        "pk1": np.ascontiguousarray(((np.arange(KC)[None, :] * 128 + np.arange(128)[:, None]) * 4).astype(np.float32)),
        "pk2": np.ascontiguousarray((np.arange(28)[None, :] * 128 + np.arange(128)[:, None]).astype(np.float32)),
        "pool_w": np.ascontiguousarray(inputs["pool_w"][0], np.float32),
        "pool_scale": _fm(inputs["pool_scale"][0]),
        "pool_ic": _pool_ic(),
    }
    shared.update(gdn_host(inputs))
    maps = []
    for ci in range(n_cores):
        b0 = ci * NS
        xin = np.empty((NS, D, TOK), np.float32)
        for s in range(NS):
            xin[s, :, :NCTX] = np.asarray(ctx[b0 + s], np.float32).T
            xin[s, :, NCTX:] = np.asarray(x[b0 + s], np.float32).T
        cv = np.stack([c[b0], c[b0 + 1], c_ctx], axis=0).astype(np.float32)
        m = dict(shared)
        m["xin"] = xin
        m["cvec"] = np.ascontiguousarray(np.transpose(cv.reshape(3, KC, 128), (2, 1, 0)))
        maps.append(m)
    return maps


def gdn_host(inputs):
    import ml_dtypes
    cw = np.asarray(inputs["gdn_conv"][0], np.float32)
    cwl = np.ascontiguousarray(np.transpose(cw.reshape(4, 24, 128), (2, 1, 0)))
    dtb = np.asarray(inputs["gdn_dt_bias"][0], np.float32).reshape(16)
    alog = np.asarray(inputs["gdn_a_log"][0], np.float32).reshape(16)
    p = np.arange(128)
    tri = np.zeros((128, 2, 128), np.float32)
    tri[:, 0, :] = (p[:, None] <= p[None, :])
    tri[:, 1, :] = (p[:, None] >= p[None, :])
    gm = np.zeros((128, 2, 3, 128), np.float32)
    P_, F_ = p[:, None], p[None, :]
    gm[:, 0, 0, :] = np.where(F_ >= P_, 0.0, NEG); gm[:, 0, 1, :] = np.where(F_ > P_, 0.0, NEG); gm[:, 0, 2, :] = np.where(F_ < P_, 0.0, -NEG)
    gm[:, 1, 0, :] = np.where(F_ <= P_, 0.0, NEG); gm[:, 1, 1, :] = np.where(F_ < P_, 0.0, NEG); gm[:, 1, 2, :] = np.where(F_ > P_, 0.0, -NEG)
    ms = np.zeros((128, 7, 128), np.float32)
    for kk in range(7):
        ms[:, kk, :] = ((P_ >> kk) != (F_ >> kk)) & ((P_ >> (kk + 1)) == (F_ >> (kk + 1)))
    return {
        "gdn_w_in": np.ascontiguousarray(inputs["gdn_w_in"][0], np.float32),
        "gdn_w_o": np.ascontiguousarray(inputs["gdn_w_o"][0], np.float32),
        "gdn_cw": cwl,
        "gdn_dtb": np.ascontiguousarray(np.broadcast_to(dtb[None], (128, 16))),
        "gdn_alog": np.ascontiguousarray(np.broadcast_to(alog[None], (128, 16))),
        "gdn_ng": np.ascontiguousarray(np.asarray(inputs["gdn_norm_g"][0], np.float32).reshape(128, 1)),
        "gdn_tri": tri, "gdn_gm": gm, "gdn_ms": ms.astype(ml_dtypes.bfloat16),
    }


def kernel(**inputs):
    nc = build()
    maps = make_in_maps(inputs)
    res = run_bass_kernel_spmd(nc, maps, core_ids=list(range(8)))
    B = inputs["x"].shape[0]
    out = np.empty((B, NLAT, D), np.float32)
    for ci in range(8):
        o = res.results[ci]["out"]
        for s in range(NS):
            out[ci * NS + s] = o[s].T
    return out
```

```python
import numpy as np
from contextlib import ExitStack
import concourse.bass as bass
import concourse.mybir as mybir
from concourse.bass_utils import run_bass_kernel_spmd

F32 = mybir.dt.float32
BF16 = mybir.dt.bfloat16
U32 = mybir.dt.uint32
AF = mybir.ActivationFunctionType
ALU = mybir.AluOpType

D = 1024
KC = 8
NLAT = 4096
NCTX = 256
TT = 512
NS = 2
DEPTH = 4
EPS = 1e-6
NEG = -1e30
DFF = 2816
DFE = 3584
NEXP = 8


class _Sem:
    def __init__(self, h, name):
        self.h = h
        self.val = 0
        self.name = name


class _Eng:
    def __init__(self, name, h, sem):
        self.name = name
        self.h = h
        self.sem = sem
        self.seen = {}


class Buf:
    __slots__ = ("name", "w", "r")

    def __init__(self, name=""):
        self.name = name
        self.w = None
        self.r = {}


class K:
    def __init__(self, nc, es, ndma=24):
        self.nc = nc
        self.es = es
        self.E = {}
        for name, h in (("pe", nc.tensor), ("act", nc.scalar), ("dve", nc.vector),
                        ("pool", nc.gpsimd), ("sp", nc.sync)):
            s = _Sem(es.enter_context(nc.semaphore("sem_" + name)), name)
            self.E[name] = _Eng(name, h, s)
        self.dsems = [_Sem(es.enter_context(nc.semaphore(f"dsem{i}")), f"d{i}") for i in range(ndma)]
        self.dnext = 0
        self.allsems = [e.sem for e in self.E.values()] + self.dsems

    def _need(self, e, ev):
        if ev is None:
            return
        sem, val = ev
        if sem is e.sem and e.name == "pe":
            return
        if e.seen.get(sem.name, 0) >= val:
            return
        e.h.wait_ge(sem.h, val)
        e.seen[sem.name] = val

    def _deps(self, e, reads, writes):
        for b in reads:
            self._need(e, b.w)
        for b in writes:
            self._need(e, b.w)
            for sname, ev in list(b.r.items()):
                self._need(e, ev)

    def _mark(self, ev, reads, writes):
        for b in reads:
            b.r[ev[0].name] = ev
        for b in writes:
            b.w = ev
            b.r = {}

    def op(self, ename, fn, reads=(), writes=()):
        e = self.E[ename]
        self._deps(e, reads, writes)
        ins = fn(e.h)
        e.sem.val += 1
        ins.then_inc(e.sem.h, 1)
        self._mark((e.sem, e.sem.val), reads, writes)

    def dma(self, out, in_, reads=(), writes=(), q="sp", **kw):
        e = self.E[q]
        ds = self.dsems[self.dnext]
        self.dnext = (self.dnext + 1) % len(self.dsems)
        self._need(e, (ds, ds.val))
        self._deps(e, reads, writes)
        ins = e.h.dma_start(out=out, in_=in_, **kw)
        ds.val += 16
        ins.then_inc(ds.h, 16)
        self._mark((ds, ds.val), reads, writes)

    def idma(self, out, in_, idx_ap, gather, reads=(), writes=(), bounds=None):
        e = self.E["pool"]
        ds = self.dsems[self.dnext]
        self.dnext = (self.dnext + 1) % len(self.dsems)
        self._need(e, (ds, ds.val))
        self._deps(e, reads, writes)
        off = bass.IndirectOffsetOnAxis(ap=idx_ap, axis=0)
        if gather:
            ins = e.h.indirect_dma_start(out=out, out_offset=None, in_=in_, in_offset=off)
        else:
            ins = e.h.indirect_dma_start(out=out, out_offset=off, in_=in_, in_offset=None)
        ds.val += 16
        ins.then_inc(ds.h, 16)
        self._mark((ds, ds.val), reads, writes)

    def barrier(self):
        for e in self.E.values():
            for s in self.allsems:
                if s.val > 0:
                    self._need(e, (s, s.val))

    def mm(self, out, lhsT, rhs, start, stop, reads, writes):
        self.op("pe", lambda h: h.matmul(out, lhsT, rhs, start=start, stop=stop), reads, writes)

    def act(self, out, in_, func, reads, writes, **kw):
        self.op("act", lambda h: h.activation(out=out, in_=in_, func=func, **kw), reads, writes)

    def tt(self, eng, out, in0, in1, op, reads, writes):
        self.op(eng, lambda h: h.tensor_tensor(out=out, in0=in0, in1=in1, op=op), reads, writes)

    def ts(self, eng, out, in0, s1, s2, op0, op1, reads, writes):
        if op1 is None:
            self.op(eng, lambda h: h.tensor_scalar(out=out, in0=in0, scalar1=s1, scalar2=None, op0=op0),
                    reads, writes)
        else:
            self.op(eng, lambda h: h.tensor_scalar(out=out, in0=in0, scalar1=s1, scalar2=s2, op0=op0, op1=op1),
                    reads, writes)

    def stt(self, out, in0, scalar, in1, op0, op1, reads, writes):
        self.op("dve", lambda h: h.scalar_tensor_tensor(out=out, in0=in0, scalar=scalar, in1=in1, op0=op0, op1=op1),
                reads, writes)

    def copy(self, eng, out, in_, reads, writes):
        if eng == "act":
            self.op("act", lambda h: h.activation(out=out, in_=in_, func=AF.Copy), reads, writes)
        else:
            self.op(eng, lambda h: h.tensor_copy(out=out, in_=in_), reads, writes)

    def memset(self, eng, ap, val, writes):
        self.op(eng, lambda h: h.memset(ap, val), (), writes)


_UID = [0]


class Pool_:
    def __init__(self, k):
        self.k = k
        self.es = ExitStack()
        self.n = 0

    def __enter__(self):
        self.es.__enter__()
        return self

    def __exit__(self, *a):
        self.k.barrier()
        return self.es.__exit__(*a)

    def sb(self, shape, dt, name=None):
        _UID[0] += 1
        t = self.es.enter_context(self.k.nc.sbuf_tensor(f"{name or 't'}_{_UID[0]}", list(shape), dt))
        return t, Buf(name or "sb")

    def ps(self, name=None, shape=(128, 512), dt=F32):
        _UID[0] += 1
        t = self.es.enter_context(self.k.nc.psum_tensor(f"{name or 'p'}_{_UID[0]}", list(shape), dt))
        return t, Buf(name or "ps")


TOK = NCTX + NLAT


def tiles_for(include_ctx=True):
    out = []
    for s in range(NS):
        if include_ctx:
            out.append((s, 0, NCTX, True))
        for i in range(NLAT // TT):
            out.append((s, NCTX + i * TT, TT, False))
    return out


class Prog:
    def __init__(self, nc, k, dr, stop_after=99):
        self.nc = nc
        self.k = k
        self.dr = dr
        self.stop_after = stop_after

    def setup_consts(self, P):
        k, dr = self.k, self.dr
        self.ones_bf, self.onesB = P.sb([128, 128], BF16, "ones")
        self.id_bf, self.idbB = P.sb([128, 128], BF16, "idb")
        self.id_f, self.idfB = P.sb([128, 128], F32, "idf")
        self.ones_f, self.onesfB = P.sb([128, 128], F32, "onesf")
        self.mod, self.modB = P.sb([128, DEPTH, 48, 3], F32, "mod")
        self.ng, self.ngB = P.sb([128, DEPTH, 2, KC], F32, "ng")
        self.fg, self.fgB = P.sb([128, KC], F32, "fg")
        self.epsb, self.epsB = P.sb([128, 1], F32, "eps")
        k.dma(self.ones_bf[:], dr["ones_bf"][:, :], writes=[self.onesB])
        k.dma(self.id_bf[:], dr["id_bf"][:, :], writes=[self.idbB])
        k.dma(self.id_f[:], dr["id_f"][:, :], writes=[self.idfB])
        k.dma(self.ones_f[:], dr["ones_f"][:, :], writes=[self.onesfB])
        k.dma(self.ng[:], dr["norm_g"][:, :, :, :], writes=[self.ngB])
        k.dma(self.fg[:], dr["final_g"][:, :], writes=[self.fgB])
        k.memset("pool", self.epsb[:], EPS, [self.epsB])

    def adaln(self):
        k, dr = self.k, self.dr
        with Pool_(k) as P:
            cv, cvB = P.sb([128, KC, 3], F32, "cv")
            sc, scB = P.sb([128, KC, 3], F32, "sc")
            ab, abB = P.sb([128, DEPTH, 48], F32, "ab")
            wa = [P.sb([128, KC, 768], F32, f"wa{i}") for i in range(2)]
            ps = [P.ps(f"adaps{i}") for i in range(2)]
            k.dma(cv[:], dr["cvec"][:, :, :], writes=[cvB])
            k.dma(ab[:], dr["ada_b"][:, :, :], writes=[abB])
            k.act(sc[:], cv[:], AF.Silu, [cvB], [scB])
            gi = 0
            for l in range(DEPTH):
                src = dr["ada_w"][l].rearrange("(kc p) n -> p kc n", p=128)
                for g in range(8):
                    wt, wB = wa[gi % 2]
                    pt, pB = ps[gi % 2]
                    gi += 1
                    k.dma(wt[:], src[:, :, g * 768:(g + 1) * 768], writes=[wB])
                    for o in range(6):
                        for kc in range(KC):
                            k.mm(pt[:, o * 3:(o + 1) * 3], wt[:, kc, o * 128:(o + 1) * 128], sc[:, kc, :],
                                 kc == 0, kc == KC - 1, [wB, scB], [pB])
                    pv = pt[:, 0:18].rearrange("p (o j) -> p o j", j=3)
                    for j in range(3):
                        k.tt("dve", self.mod[:, l, g * 6:(g + 1) * 6, j], pv[:, :, j], ab[:, l, g * 6:(g + 1) * 6],
                             ALU.add, [pB, abB], [self.modB])

    def mod_vecs(self, P, l, sub):
        k = self.k
        A, AB = P.sb([128, KC, 3], F32, "modA")
        o = sub * 24
        for j in range(3):
            k.stt(A[:, :, j], self.mod[:, l, o + 8:o + 16, j], 1.0, self.ng[:, l, sub, :], ALU.add, ALU.mult,
                  [self.modB, self.ngB], [AB])
        return A, AB

    def jidx(self, s, is_ctx):
        return 2 if is_ctx else s

    def norm_pass(self, l, sub, include_ctx=True, router=None, final=False, moe=None):
        k, dr = self.k, self.dr
        xs, hs = dr["xs"], dr["hs"]
        tl = tiles_for(include_ctx)
        with Pool_(k) as P:
            if not final:
                A, AB = self.mod_vecs(P, l, sub)
            xt = [P.sb([128, KC, TT], F32, f"xt{i}") for i in range(2)]
            sq, sqB = P.sb([128, KC, TT], BF16, "sq")
            rs, rsB = P.sb([128, TT], F32, "rs")
            hf, hfB = P.sb([128, KC, TT], F32, "hf")
            hb, hbB = P.sb([128, KC, TT], BF16, "hb")
            pss, pssB = P.ps("pss")
            if router is not None:
                wr, wrB = P.sb([128, KC, NEXP], F32, "wr")
                k.dma(wr[:], router, writes=[wrB])
                plg, plgB = P.ps("plg")
                pwt, pwtB = P.ps("pwt")
                lg, lgB = P.sb([128, 4, NEXP], F32, "lg")
                m8, m8B = P.sb([128, 4, 8], F32, "m8")
                nm1, nm1B = P.sb([128, 4], F32, "nm1")
                ee, eeB = P.sb([128, 4, NEXP], F32, "ee")
                mk, mkB = P.sb([128, 4, NEXP], F32, "mk")
                dn, dnB = P.sb([128, 4], F32, "dn")
                ww, wwB = P.sb([128, 4, NEXP], F32, "ww")
                wT, wTB = P.sb([NEXP, TT], F32, "wT")
                if moe is not None:
                    oh, ohB = P.sb([128, 4, 24], F32, "oh")
                    rk, rkB = P.sb([128, 4, NEXP], F32, "rk")
                    t8, t8B = P.sb([128, 4, NEXP], F32, "t8")
                    htk, htkB = P.sb([128, 4, D], BF16, "htk")
                    prk, prkB = P.ps("prk")
                    ptot, ptotB = P.ps("ptot")
                    ptk, ptkB = P.ps("ptk")
                    I32_ = mybir.dt.int32
                    sub_i = 0

            def load(i):
                s, t0, W, c = tl[i]
                t, B = xt[i % 2]
                k.dma(t[:, :, :W], xs[s].rearrange("(kc p) t -> p kc t", p=128)[:, :, t0:t0 + W], writes=[B])

            load(0)
            for i, (s, t0, W, c) in enumerate(tl):
                if i + 1 < len(tl):
                    load(i + 1)
                x, xB = xt[i % 2]
                j = self.jidx(s, c)
                k.act(sq[:, :, :W], x[:, :, :W], AF.Square, [xB], [sqB])
                for kc in range(KC):
                    k.mm(pss[:, :W], self.ones_bf[:], sq[:, kc, :W], kc == 0, kc == KC - 1, [self.onesB, sqB], [pssB])
                k.act(rs[:, :W], pss[:, :W], AF.Sqrt, [pssB, self.epsB], [rsB], scale=1.0 / D, bias=self.epsb[:, 0:1])
                k.op("dve", lambda h: h.reciprocal(out=rs[:, :W], in_=rs[:, :W]), [rsB], [rsB])
                for kc in range(KC):
                    k.tt("dve", hf[:, kc, :W], x[:, kc, :W], rs[:, :W], ALU.mult, [xB, rsB], [hfB])
                if final:
                    for kc in range(KC):
                        k.act(hf[:, kc, :W], hf[:, kc, :W], AF.Identity, [hfB, self.fgB], [hfB], scale=self.fg[:, kc:kc + 1])
                    k.dma(dr["out"][s].rearrange("(kc p) t -> p kc t", p=128)[:, :, t0 - NCTX:t0 - NCTX + W],
                          hf[:, :, :W], reads=[hfB])
                    continue
                o = sub * 24
                for kc in range(KC):
                    k.act(hf[:, kc, :W], hf[:, kc, :W], AF.Identity, [hfB, AB, self.modB], [hfB],
                          scale=A[:, kc, j:j + 1], bias=self.mod[:, l, o + kc, j:j + 1])
                k.copy("pool", hb[:, :, :W], hf[:, :, :W], [hfB], [hbB])
                k.dma(hs[s].rearrange("(kc p) t -> p kc t", p=128)[:, :, t0:t0 + W], hb[:, :, :W], reads=[hbB])
                if router is not None:
                    nst = W // 128
                    for ts_ in range(nst):
                        for kc in range(KC):
                            k.mm(plg[:, ts_ * 8:(ts_ + 1) * 8], hf[:, kc, ts_ * 128:(ts_ + 1) * 128], wr[:, kc, :],
                                 kc == 0, kc == KC - 1, [hfB, wrB], [plgB])
                    k.copy("dve", lg[:, :nst, :], plg[:, 0:nst * 8].rearrange("p (a e) -> p a e", e=8), [plgB], [lgB])
                    for ts_ in range(nst):
                        k.op("dve", lambda h: h.max(out=m8[:, ts_, :], in_=lg[:, ts_, :]), [lgB], [m8B])
                    k.ts("dve", nm1[:, :nst], m8[:, :nst, 0], -1.0, None, ALU.mult, None, [m8B], [nm1B])
                    for ts_ in range(nst):
                        k.act(ee[:, ts_, :], lg[:, ts_, :], AF.Exp, [lgB, nm1B], [eeB], bias=nm1[:, ts_:ts_ + 1])
                        k.ts("dve", mk[:, ts_, :], lg[:, ts_, :], m8[:, ts_, 1:2], None, ALU.is_ge, None,
                             [lgB, m8B], [mkB])
                        if moe is not None:
                            k.ts("dve", oh[:, ts_, 0:8], lg[:, ts_, :], m8[:, ts_, 0:1], None, ALU.is_equal, None, [lgB, m8B], [ohB])
                            k.ts("dve", oh[:, ts_, 8:16], lg[:, ts_, :], m8[:, ts_, 1:2], None, ALU.is_equal, None, [lgB, m8B], [ohB])
                    k.tt("dve", ee[:, :nst, :], ee[:, :nst, :], mk[:, :nst, :], ALU.mult, [eeB, mkB], [eeB])
                    k.op("dve", lambda h: h.reduce_sum(out=dn[:, :nst], in_=ee[:, :nst, :], axis=mybir.AxisListType.X),
                         [eeB], [dnB])
                    k.op("dve", lambda h: h.reciprocal(out=dn[:, :nst], in_=dn[:, :nst]), [dnB], [dnB])
                    for ts_ in range(nst):
                        k.ts("dve", ww[:, ts_, :], ee[:, ts_, :], dn[:, ts_:ts_ + 1], None, ALU.mult, None,
                             [eeB, dnB], [wwB])
                        k.op("pe", lambda h: h.transpose(pwt[0:NEXP, ts_ * 128:(ts_ + 1) * 128], ww[:, ts_, :], self.id_f[:]),
                             [wwB, self.idfB], [pwtB])
                    k.copy("dve", wT[:, :W], pwt[0:NEXP, :W], [pwtB], [wTB])
                    k.dma(dr["wexp"][s, :, t0:t0 + W], wT[:, :W], reads=[wTB])
                    if moe is not None:
                        cum, cumB = moe["cum"]
                        for ts_ in range(nst):
                            k.mm(prk[:, 0:8], moe["tris"][0][:], mk[:, ts_, :], True, True, [moe["tris"][1], mkB], [prkB])
                            k.mm(ptot[:, 0:8], self.ones_f[:], mk[:, ts_, :], True, True, [self.onesfB, mkB], [ptotB])
                            k.tt("dve", rk[:, ts_, :], prk[:, 0:8], cum[:], ALU.add, [prkB, cumB], [rkB])
                            k.tt("dve", cum[:], cum[:], ptot[:, 0:8], ALU.add, [cumB, ptotB], [cumB])
                        for (src_, so, do) in ((rk, 0, 16), (rk, 8, 17), (ww, 0, 18), (ww, 8, 19)):
                            k.tt("dve", t8[:, :nst, :], oh[:, :nst, so:so + 8], src_[:, :nst, :], ALU.mult, [ohB, rkB, wwB], [t8B])
                            k.op("dve", lambda h: h.reduce_sum(out=oh[:, :nst, do], in_=t8[:, :nst, :], axis=mybir.AxisListType.X),
                                 [t8B], [ohB])
                        k.dma(dr["rinfo"][s, t0:t0 + W, :].rearrange("(a p) c -> p a c", p=128), oh[:, :nst, :], reads=[ohB])
                        for ts_ in range(nst):
                            for kc in range(KC):
                                k.op("pe", lambda h: h.transpose(ptk[:, :].bitcast(BF16)[:, kc * 128:(kc + 1) * 128],
                                                                 hb[:, kc, ts_ * 128:(ts_ + 1) * 128], self.id_bf[:]),
                                     [hbB, self.idbB], [ptkB])
                            if ts_ % 2 == 0:
                                k.copy("act", htk[:, ts_, :], ptk[:, :].bitcast(BF16)[:, 0:D], [ptkB], [htkB])
                            else:
                                k.copy("dve", htk[:, ts_, :], ptk[:, :].bitcast(BF16)[:, 0:D], [ptkB], [htkB])
                        k.dma(dr["htok"][s, t0:t0 + W, :].rearrange("(a p) d -> p a d", p=128), htk[:, :nst, :], reads=[htkB])

    def ffn_pass(self, l, w1, w3, w2, F, include_ctx=True, expert=None):
        k, dr = self.k, self.dr
        xs, hs = dr["xs"], dr["hs"]
        FC = F // 128
        tl = tiles_for(include_ctx)
        with Pool_(k) as P:
            W1, W1B = P.sb([128, KC, F], BF16, "W1")
            W3, W3B = P.sb([128, KC, F], BF16, "W3")
            W2, W2B = P.sb([128, FC, D], BF16, "W2")
            k.dma(W1[:], w1.rearrange("(kc p) f -> p kc f", p=128), writes=[W1B], q="pool")
            k.dma(W3[:], w3.rearrange("(kc p) f -> p kc f", p=128), writes=[W3B], q="pool")
            k.dma(W2[:], w2.rearrange("(fc p) d -> p fc d", p=128), writes=[W2B], q="pool")
            ht = [P.sb([128, KC, TT], BF16, f"ht{i}") for i in range(2)]
            wb = [P.sb([128, TT], F32, f"wbc{i}") for i in range(2)] if expert is not None else None
            g, gB = P.sb([128, FC, TT], BF16, "g")
            gBs = [Buf(f"g{f}") for f in range(FC)]
            sa = [P.sb([128, TT], F32, f"sa{i}") for i in range(2)]
            yt = [P.sb([128, TT], F32, f"yt{i}") for i in range(2)]
            pa = [P.ps(f"pa{i}") for i in range(2)]
            pb = [P.ps(f"pb{i}") for i in range(2)]
            py = [P.ps(f"py{i}") for i in range(2)]

            def load(i):
                s, t0, W, c = tl[i]
                t, B = ht[i % 2]
                k.dma(t[:, :, :W], hs[s].rearrange("(kc p) t -> p kc t", p=128)[:, :, t0:t0 + W], writes=[B])
                if expert is not None:
                    wt_, wB_ = wb[i % 2]
                    k.dma(wt_[:, :W], dr["wexp"][s, expert:expert + 1, t0:t0 + W].partition_broadcast(128), writes=[wB_])

            load(0)
            cnt = 0
            ycnt = 0
            for i, (s, t0, W, c) in enumerate(tl):
                if i + 1 < len(tl):
                    load(i + 1)
                h, hB = ht[i % 2]
                j = self.jidx(s, c)
                for f in range(FC):
                    a_, aB = pa[cnt % 2]
                    b_, bB = pb[cnt % 2]
                    s_, sB = sa[cnt % 2]
                    cnt += 1
                    for kc in range(KC):
                        k.mm(a_[:, :W], W1[:, kc, f * 128:(f + 1) * 128], h[:, kc, :W], kc == 0, kc == KC - 1, [W1B, hB], [aB])
                    for kc in range(KC):
                        k.mm(b_[:, :W], W3[:, kc, f * 128:(f + 1) * 128], h[:, kc, :W], kc == 0, kc == KC - 1, [W3B, hB], [bB])
                    k.act(s_[:, :W], a_[:, :W], AF.Silu, [aB], [sB])
                    if expert is not None:
                        wt_, wB_ = wb[i % 2]
                        k.tt("pool", s_[:, :W], s_[:, :W], wt_[:, :W], ALU.mult, [sB, wB_], [sB])
                    k.tt("dve", g[:, f, :W], b_[:, :W], s_[:, :W], ALU.mult, [bB, sB], [gBs[f]])
                o = 24 + 16 + 0
                for oc in range(KC):
                    y_, yB = py[ycnt % 2]
                    yo, yoB = yt[ycnt % 2]
                    ycnt += 1
                    for f in range(FC):
                        k.mm(y_[:, :W], W2[:, f, oc * 128:(oc + 1) * 128], g[:, f, :W], f == 0, f == FC - 1, [W2B, gBs[f]], [yB])
                    k.ts("dve", yo[:, :W], y_[:, :W], self.mod[:, l, 40 + oc, j:j + 1], None, ALU.mult, None,
                         [yB, self.modB], [yoB])
                    k.dma(xs[s, oc * 128:(oc + 1) * 128, t0:t0 + W], yo[:, :W], reads=[yoB], q="pool", accum_op=ALU.add)

    def outproj_pass(self, l, src, w, include_ctx, gate_off, extra_scale=None):
        k, dr = self.k, self.dr
        xs = dr["xs"]
        tl = tiles_for(include_ctx)
        with Pool_(k) as P:
            Wo, WoB = P.sb([128, KC, D], BF16, "Wo")
            k.dma(Wo[:], w.rearrange("(kc p) d -> p kc d", p=128), writes=[WoB], q="pool")
            ot = [P.sb([128, KC, TT], BF16, f"ot{i}") for i in range(2)]
            yt = [P.sb([128, TT], F32, f"yt{i}") for i in range(2)]
            py = [P.ps(f"py{i}") for i in range(2)]

            def load(i):
                s, t0, W, c = tl[i]
                t, B = ot[i % 2]
                k.dma(t[:, :, :W], src[s].rearrange("(kc p) t -> p kc t", p=128)[:, :, t0:t0 + W], writes=[B])

            load(0)
            ycnt = 0
            for i, (s, t0, W, c) in enumerate(tl):
                if i + 1 < len(tl):
                    load(i + 1)
                o_, oB = ot[i % 2]
                j = self.jidx(s, c)
                for oc in range(KC):
                    y_, yB = py[ycnt % 2]
                    yo, yoB = yt[ycnt % 2]
                    ycnt += 1
                    for kc in range(KC):
                        k.mm(y_[:, :W], Wo[:, kc, oc * 128:(oc + 1) * 128], o_[:, kc, :W], kc == 0, kc == KC - 1, [WoB, oB], [yB])
                    if extra_scale is None:
                        k.ts("dve", yo[:, :W], y_[:, :W], self.mod[:, l, gate_off + oc, j:j + 1], None, ALU.mult, None,
                             [yB, self.modB], [yoB])
                    else:
                        est, esB = extra_scale
                        k.ts("dve", yo[:, :W], y_[:, :W], self.mod[:, l, gate_off + oc, j:j + 1], est[:, oc:oc + 1],
                             ALU.mult, ALU.mult, [yB, self.modB, esB], [yoB])
                    k.dma(xs[s, oc * 128:(oc + 1) * 128, t0:t0 + W], yo[:, :W], reads=[yoB], q="pool", accum_op=ALU.add)

    def na_qkv_pass(self, wqkv):
        k, dr = self.k, self.dr
        hs, qs, ks, vs = dr["hs"], dr["qs"], dr["ks"], dr["vs"]
        tl = tiles_for(True)
        with Pool_(k) as P:
            Wq, WqB = P.sb([128, KC, 3 * D], BF16, "Wqkv")
            src = wqkv.rearrange("(kc p) n -> p kc n", p=128)
            for i in range(3):
                k.dma(Wq[:, :, i * D:(i + 1) * D], src[:, :, i * D:(i + 1) * D], writes=[WqB], q="pool")
            ht = [P.sb([128, KC, TT], BF16, f"ht{i}") for i in range(2)]
            qk = [P.sb([128, 2 * KC, TT], BF16, f"qk{i}") for i in range(2)]
            vt = [P.sb([128, 4, D], BF16, f"vt{i}") for i in range(2)]
            pp = [P.ps(f"pp{i}") for i in range(4)]

            def load(i):
                s, t0, W, c = tl[i]
                t, B = ht[i % 2]
                k.dma(t[:, :, :W], hs[s].rearrange("(kc p) t -> p kc t", p=128)[:, :, t0:t0 + W], writes=[B])

            load(0)
            cnt = 0
            for i, (s, t0, W, c) in enumerate(tl):
                if i + 1 < len(tl):
                    load(i + 1)
                h, hB = ht[i % 2]
                q_, qB = qk[i % 2]
                v_, vB = vt[i % 2]
                for oc in range(2 * KC):
                    p_, pB = pp[cnt % 4]
                    for kc in range(KC):
                        k.mm(p_[:, :W], Wq[:, kc, oc * 128:(oc + 1) * 128], h[:, kc, :W], kc == 0, kc == KC - 1, [WqB, hB], [pB])
                    if cnt % 2 == 0:
                        k.act(q_[:, oc, :W], p_[:, :W], AF.Copy, [pB], [qB], scale=(0.125 if oc < KC else 1.0))
                    else:
                        k.ts("dve", q_[:, oc, :W], p_[:, :W], (0.125 if oc < KC else 1.0), None, ALU.mult, None, [pB], [qB])
                    cnt += 1
                for ts_ in range(W // 128):
                    for hf_ in range(2):
                        p_, pB = pp[cnt % 4]
                        for kc in range(KC):
                            k.mm(p_[:, :], h[:, kc, ts_ * 128:(ts_ + 1) * 128], Wq[:, kc, 2 * D + hf_ * 512:2 * D + (hf_ + 1) * 512],
                                 kc == 0, kc == KC - 1, [WqB, hB], [pB])
                        if cnt % 2 == 0:
                            k.act(v_[:, ts_, hf_ * 512:(hf_ + 1) * 512], p_[:, :], AF.Copy, [pB], [vB])
                        else:
                            k.copy("dve", v_[:, ts_, hf_ * 512:(hf_ + 1) * 512], p_[:, :], [pB], [vB])
                        cnt += 1
                k.dma(qs[s].rearrange("(kc p) t -> p kc t", p=128)[:, :, t0:t0 + W], q_[:, 0:KC, :W], reads=[qB])
                k.dma(ks[s].rearrange("(kc p) t -> p kc t", p=128)[:, :, t0:t0 + W], q_[:, KC:2 * KC, :W], reads=[qB])
                k.dma(vs[s, t0:t0 + W, :].rearrange("(a p) f -> p a f", p=128), v_[:, :W // 128, :], reads=[vB])

    def na_attn_pass(self, tab, need_ctx):
        k, dr = self.k, self.dr
        qs, ks, vs, os_ = dr["qs"], dr["ks"], dr["vs"], dr["os"]
        NCH = TOK // 128
        with Pool_(k) as P:
            qT, qTB = P.sb([128, TOK], BF16, "qT")
            kT, kTB = P.sb([128, TOK], BF16, "kT")
            vv, vvB = P.sb([128, NCH, 128], BF16, "vv")
            tb, tbB = P.sb([128, 2, 2, 22, 64], F32, "tb")
            negt, negB = P.sb([128, 256], F32, "negt")
            k.memset("pool", negt[:], NEG, [negB])
            sbm = [P.sb([128, TT], F32, f"sbm{i}") for i in range(2)]
            pt = [P.sb([128, TT], BF16, f"pt{i}") for i in range(3)]
            rd, rdB = P.sb([128, TT], F32, "rd")
            ot = [P.sb([128, TT], BF16, f"ot{i}") for i in range(2)]
            pS = [P.ps(f"pS{i}") for i in range(3)]
            pO = [P.ps(f"pO{i}") for i in range(2)]
            pD = [P.ps(f"pD{i}") for i in range(2)]
            scnt = 0
            tcnt = 0
            for s in range(NS):
                for hp in range(8):
                    k.dma(qT[:], qs[s, hp * 128:(hp + 1) * 128, :], writes=[qTB])
                    k.dma(kT[:], ks[s, hp * 128:(hp + 1) * 128, :], writes=[kTB])
                    k.dma(vv[:], vs[s, :, hp * 128:(hp + 1) * 128].rearrange("(a p) f -> p a f", p=128), writes=[vvB])
                    if s == 0 or True:
                        k.dma(tb[:], tab[hp], writes=[tbB])
                    qtiles = ([(0, NCTX, -1)] if need_ctx else []) + [(NCTX + ti * TT, TT, ti) for ti in range(8)]
                    for (t0, W, ti) in qtiles:
                        O_, OB = pO[tcnt % 2]
                        D_, DB = pD[tcnt % 2]
                        o_, oB = ot[tcnt % 2]
                        tcnt += 1
                        if ti < 0:
                            chunks = [(0, None), (1, None)]
                        else:
                            lo, hi = max(0, 4 * ti - 2), min(31, 4 * ti + 5)
                            chunks = [(2 + c, c) for c in range(lo, hi + 1)] + [(0, None), (1, None)]
                        steps = [(hd, ci, kch, c) for hd in range(2) for ci, (kch, c) in enumerate(chunks)]
                        slots = {}

                        def emitS(st):
                            nonlocal scnt
                            hd, ci, kch, c = st
                            pb_ = 64 * hd
                            S_, SB = pS[scnt % 3]
                            p_, pB = pt[scnt % 3]
                            m_, mB = sbm[scnt % 2]
                            scnt += 1
                            slots[st] = (p_, pB)
                            k.mm(S_[:, :W], kT[pb_:pb_ + 64, kch * 128:(kch + 1) * 128], qT[pb_:pb_ + 64, t0:t0 + W],
                                 True, True, [kTB, qTB], [SB])
                            if c is None:
                                k.act(p_[:, :W], S_[:, :W], AF.Exp, [SB], [pB])
                                return
                            R = 8 * ti
                            J0 = 10 - 2 * c + R
                            if ti == 0:
                                segs = [(0, 4, "U" if c <= 3 else "N"), (4, 8, "T")]
                            elif ti == 7:
                                segs = [(0, 5, "T"), (5, 8, "U" if c >= 28 else "N")]
                            else:
                                segs = [(0, 8, "T")]
                            for (b0, b1, kind) in segs:
                                c0, c1 = b0 * 64, b1 * 64
                                if kind == "N":
                                    in1 = negt[:, 0:c1 - c0]
                                    rb = [negB]
                                else:
                                    tix = 0 if kind == "T" else 1
                                    in1 = tb[:, tix, hd, J0 + b0:J0 + b1, :].rearrange("p a b -> p (a b)")
                                    rb = [tbB]
                                k.tt("dve", m_[:, c0:c1], S_[:, c0:c1], in1, ALU.add, [SB] + rb, [mB])
                            k.act(p_[:, :W], m_[:, :W], AF.Exp, [mB], [pB])

                        def emitO(st):
                            hd, ci, kch, c = st
                            pb_ = 64 * hd
                            p_, pB = slots.pop(st)
                            first, last = ci == 0, ci == len(chunks) - 1
                            k.mm(O_[pb_:pb_ + 64, :W], vv[:, kch, pb_:pb_ + 64], p_[:, :W], first, last, [vvB, pB], [OB])
                            k.mm(D_[pb_:pb_ + 64, :W], self.ones_bf[:, 0:64], p_[:, :W], first, last, [self.onesB, pB], [DB])

                        LA = 2
                        for i_ in range(len(steps) + LA):
                            if i_ < len(steps):
                                emitS(steps[i_])
                            if i_ - LA >= 0:
                                emitO(steps[i_ - LA])
                        k.op("dve", lambda h: h.reciprocal(out=rd[:, :W], in_=D_[:, :W]), [DB], [rdB])
                        k.tt("dve", o_[:, :W], O_[:, :W], rd[:, :W], ALU.mult, [OB, rdB], [oB])
                        k.dma(os_[s, hp * 128:(hp + 1) * 128, t0:t0 + W], o_[:, :W], reads=[oB])

    def pool_pass(self, l, pw, include_ctx=True):
        k, dr = self.k, self.dr
        xs, hs = dr["xs"], dr["hs"]
        tl = tiles_for(include_ctx)
        PADW = 80
        with Pool_(k) as P:
            Wp, WpB = P.sb([128, 4, 2, 256], BF16, "Wp")
            k.dma(Wp[:], pw.rearrange("g (kc p) f -> p g kc f", p=128), writes=[WpB], q="pool")
            ls, lsB = P.sb([128, KC], F32, "ls")
            k.dma(ls[:], dr["pool_scale"][:, :], writes=[lsB])
            ic, icB = P.sb([128, 4, 2, 256], F32, "ic")
            k.dma(ic[:], dr["pool_ic"][:, :, :, :], writes=[icB])
            ht = [P.sb([128, KC, TT], BF16, f"ht{i}") for i in range(2)]
            lv = [P.sb([128, 8 * PADW], F32, f"lv{i}") for i in range(3)]
            lvc = [P.sb([128, 272], F32, f"lvc{i}") for i in range(3)]
            pl, plB = P.sb([128, KC, TT], BF16, "pl")
            plBs = [Buf(f"pl{i}") for i in range(KC)]
            mn, mnB = P.sb([128, TT], F32, "mn")
            yt = [P.sb([128, TT], F32, f"yt{i}") for i in range(2)]
            py = [P.ps(f"py{i}") for i in range(2)]
            for t_, B_ in lv + lvc:
                k.memset("pool", t_[:], 0.0, [B_])

            def load(i):
                s, t0, W, c = tl[i]
                t, B = ht[i % 2]
                k.dma(t[:, :, :W], hs[s].rearrange("(kc p) t -> p kc t", p=128)[:, :, t0:t0 + W], writes=[B])

            load(0)
            ycnt = 0
            for i, (s, t0, W, c) in enumerate(tl):
                if i + 1 < len(tl):
                    load(i + 1)
                h, hB = ht[i % 2]
                j = self.jidx(s, c)
                rows, rl, pw_, ri = (1, 256, 272, 1) if c else (8, 64, PADW, 0)

                def view(t, off, n=rl):
                    return t[:, 0:rows * pw_].rearrange("p (r w) -> p r w", w=pw_)[:, :, 8 + off:8 + off + n]

                for kc in range(KC):
                    gi = kc // 2
                    win = (2, 4, 8, 16)[gi]
                    a_, aB = (lvc if c else lv)[0]
                    b_, bB = (lvc if c else lv)[1]
                    c_, cB = (lvc if c else lv)[2]
                    hv = h[:, kc, :W].rearrange("p (r w) -> p r w", w=rl)
                    k.copy("pool", view(a_, 0), hv, [hB], [aB])
                    n2 = rl + 14
                    k.tt("pool", view(b_, -7, n2), view(a_, -8, n2), view(a_, -7, n2), ALU.add, [aB], [bB])
                    cur, curB = b_, bB
                    if win >= 4:
                        n4 = rl + 12
                        k.tt("pool", view(c_, -6, n4), view(b_, -7, n4), view(b_, -5, n4), ALU.add, [bB], [cB])
                        cur, curB = c_, cB
                    if win >= 8:
                        n8 = rl + 8
                        k.tt("dve", view(b_, -4, n8), view(c_, -6, n8), view(c_, -2, n8), ALU.add, [cB], [bB])
                        cur, curB = b_, bB
                    if win >= 16:
                        k.tt("dve", view(c_, 0), view(b_, -4), view(b_, 4), ALU.add, [bB], [cB])
                        cur, curB = c_, cB
                    mv = mn[:, :W].rearrange("p (r w) -> p r w", w=rl)
                    icv = ic[:, gi, ri, 0:rl]
                    for r in range(rows):
                        k.tt("dve", mv[:, r, :], view(cur, 0)[:, r, :], icv, ALU.mult, [curB, icB], [mnB])
                    k.tt("dve", pl[:, kc, :W], mn[:, :W], h[:, kc, :W], ALU.subtract, [mnB, hB], [plBs[kc]])
                for oc in range(KC):
                    gi = oc // 2
                    y_, yB = py[ycnt % 2]
                    yo, yoB = yt[ycnt % 2]
                    ycnt += 1
                    for kk in range(2):
                        k.mm(y_[:, :W], Wp[:, gi, kk, (oc % 2) * 128:(oc % 2 + 1) * 128], pl[:, 2 * gi + kk, :W], kk == 0, kk == 1,
                             [WpB, plBs[2 * gi + kk]], [yB])
                    k.ts("dve", yo[:, :W], y_[:, :W], self.mod[:, l, 16 + oc, j:j + 1], ls[:, oc:oc + 1], ALU.mult, ALU.mult,
                         [yB, self.modB, lsB], [yoB])
                    k.dma(xs[s, oc * 128:(oc + 1) * 128, t0:t0 + W], yo[:, :W], reads=[yoB], q="pool", accum_op=ALU.add)


BLK = 512
GF = 896
NG = DFE // GF
I32 = mybir.dt.int32


def moe_layer(pg, l, f, include_ctx):
    k, dr = pg.k, pg.dr
    tl = tiles_for(include_ctx)
    subt = [(s, t0 + a * 128, c) for (s, t0, W, c) in tl for a in range(W // 128)]
    NSUB = len(subt)
    NB = NSUB * 2 * 128 // BLK + NEXP
    w1f = dr[f"moe_w1_{f}"].rearrange("e k (g c) -> (e k g) c", c=GF)
    w3f = dr[f"moe_w3_{f}"].rearrange("e k (g c) -> (e k g) c", c=GF)
    w2f = dr[f"moe_w2_{f}"].rearrange("e f d -> (e f) d")
    hbuf, obuf = dr["hbuf"], dr["obuf"]
    with Pool_(k) as PM:
        cum = PM.sb([128, NEXP], F32, "cum")
        tris = PM.sb([128, 128], F32, "tris")
        starts = PM.sb([128, NEXP], F32, "starts")
        posall = PM.sb([128, NSUB, 2], I32, "posall")
        pwall = PM.sb([128, NSUB, 2], F32, "pwall")
        widx1 = PM.sb([128, NB, NG, KC], I32, "widx1")
        widx2 = PM.sb([128, NB, 28], I32, "widx2")
        k.memset("pool", cum[0][:], 0.0, [cum[1]])
        k.dma(tris[0][:], dr["tris"][:, :], writes=[tris[1]])
        pg.norm_pass(l, 1, include_ctx=include_ctx, router=dr["moe_router"][f], moe={"cum": cum, "tris": tris})
        with Pool_(k) as P:
            thr, thrB = P.sb([128, 48], F32, "thr"); k.dma(thr[:], dr["thr48"][:, :], writes=[thrB])
            io, ioB = P.sb([128, 48], F32, "io"); k.dma(io[:], dr["iota48"][:, :], writes=[ioB])
            pk1, pk1B = P.sb([128, KC], F32, "pk1"); k.dma(pk1[:], dr["pk1"][:, :], writes=[pk1B])
            pk2, pk2B = P.sb([128, 28], F32, "pk2"); k.dma(pk2[:], dr["pk2"][:, :], writes=[pk2B])
            tmp, tmpB = P.sb([128, 48], F32, "tmp")
            nblk, nblkB = P.sb([128, NEXP], F32, "nblk")
            sblk, sblkB = P.sb([128, NEXP], F32, "sblk")
            eb, ebB = P.sb([128, 48], F32, "eb")
            w1f_, w1fB = P.sb([128, NB, NG, KC], F32, "w1f")
            w2f_, w2fB = P.sb([128, NB, 28], F32, "w2f")
            for e in range(NEXP):
                k.ts("dve", tmp[:], thr[:], cum[0][:, e:e + 1], None, ALU.is_lt, None, [thrB, cum[1]], [tmpB])
                k.op("dve", lambda h: h.reduce_sum(out=nblk[:, e:e + 1], in_=tmp[:], axis=mybir.AxisListType.X), [tmpB], [nblkB])
            k.memset("pool", sblk[:], 0.0, [sblkB])
            for e in range(1, NEXP):
                k.tt("dve", sblk[:, e:e + 1], sblk[:, e - 1:e], nblk[:, e - 1:e], ALU.add, [sblkB, nblkB], [sblkB])
            k.ts("dve", starts[0][:], sblk[:], float(BLK), None, ALU.mult, None, [sblkB], [starts[1]])
            k.memset("pool", eb[:], -1.0, [ebB])
            for e in range(NEXP):
                k.ts("dve", tmp[:], io[:], sblk[:, e:e + 1], None, ALU.is_ge, None, [ioB, sblkB], [tmpB])
                k.tt("dve", eb[:], eb[:], tmp[:], ALU.add, [ebB, tmpB], [ebB])
            e4, e4B = P.sb([128, 48], F32, "e4")
            e35, e35B = P.sb([128, 48], F32, "e35")
            k.ts("dve", e4[:], eb[:], 4096.0, None, ALU.mult, None, [ebB], [e4B])
            k.ts("dve", e35[:], eb[:], float(DFE), None, ALU.mult, None, [ebB], [e35B])
            for g in range(NG):
                for kc in range(KC):
                    k.ts("dve", w1f_[:, :, g, kc], e4[:, :NB], pk1[:, kc:kc + 1], None, ALU.add, None, [e4B, pk1B], [w1fB])
            for g in range(1, NG):
                k.ts("dve", w1f_[:, :, g, :], w1f_[:, :, g, :], float(g), None, ALU.add, None, [w1fB], [w1fB])
            for fc in range(28):
                k.ts("dve", w2f_[:, :, fc], e35[:, :NB], pk2[:, fc:fc + 1], None, ALU.add, None, [e35B, pk2B], [w2fB])
            k.copy("dve", widx1[0][:], w1f_[:], [w1fB], [widx1[1]])
            k.copy("dve", widx2[0][:], w2f_[:], [w2fB], [widx2[1]])
            if "widx_dbg" in dr:
                k.dma(dr["widx_dbg"][:, 0:NB * NG * KC], widx1[0][:].rearrange("p a b c -> p (a b c)"), reads=[widx1[1]])
        with Pool_(k) as P:
            z, zB = P.sb([128, 4, D], BF16, "z")
            k.memset("pool", z[:], 0.0, [zB])
            for b in range(NB):
                k.dma(hbuf[b * BLK:(b + 1) * BLK, :].rearrange("(a p) d -> p a d", p=128), z[:], reads=[zB])
            k.barrier()
            ht = [P.sb([128, D], BF16, f"sht{i}") for i in range(3)]
            inf = [P.sb([128, 24], F32, f"inf{i}") for i in range(3)]
            t8, t8B = P.sb([128, 2, NEXP], F32, "t8")
            pf, pfB = P.sb([128, 2], F32, "pf")

            def loadi(i):
                s, tk, c = subt[i]
                k.dma(inf[i % 3][0][:], dr["rinfo"][s, tk:tk + 128, :], writes=[inf[i % 3][1]])

            def loadh(i):
                s, tk, c = subt[i]
                k.dma(ht[i % 3][0][:], dr["htok"][s, tk:tk + 128, :], writes=[ht[i % 3][1]])

            loadi(0)
            for i in range(NSUB):
                if i + 1 < NSUB:
                    loadi(i + 1)
                n_, nB_ = inf[i % 3]
                k.tt("dve", t8[:, 0, :], n_[:, 0:8], starts[0][:], ALU.mult, [nB_, starts[1]], [t8B])
                k.tt("dve", t8[:, 1, :], n_[:, 8:16], starts[0][:], ALU.mult, [nB_, starts[1]], [t8B])
                k.op("dve", lambda h: h.reduce_sum(out=pf[:, 0:2], in_=t8[:, :, :], axis=mybir.AxisListType.X), [t8B], [pfB])
                k.tt("dve", pf[:], pf[:], n_[:, 16:18], ALU.add, [pfB, nB_], [pfB])
                k.copy("dve", posall[0][:, i, :], pf[:], [pfB], [posall[1]])
                k.copy("dve", pwall[0][:, i, :], n_[:, 18:20], [nB_], [pwall[1]])
            k.barrier()
            loadh(0)
            for i in range(NSUB):
                if i + 1 < NSUB:
                    loadh(i + 1)
                h_, hB_ = ht[i % 3]
                for j in range(2):
                    k.idma(hbuf[:, :], h_[:], posall[0][:, i, j:j + 1], gather=False, reads=[hB_, posall[1]])
        with Pool_(k) as P:
            Wg = []
            for i in range(2):
                Wg.append(((P.sb([128, KC, GF], BF16, f"W1g{i}")[0], [Buf() for _ in range(KC)]),
                           (P.sb([128, KC, GF], BF16, f"W3g{i}")[0], [Buf() for _ in range(KC)]),
                           (P.sb([128, 7, D], BF16, f"W2g{i}")[0], [Buf() for _ in range(7)])))
            hblk = [P.sb([128, 4, D], BF16, f"hblk{i}") for i in range(1)] * 2
            hT = [P.sb([128, KC, BLK], BF16, f"hT{i}") for i in range(2)]
            gT = [P.sb([128, 7, BLK], BF16, f"gT{i}") for i in range(2)]
            yacc = [P.sb([128, 4, D], F32, f"yacc{i}") for i in range(1)] * 2
            sa = [P.sb([128, BLK], F32, f"sa{i}") for i in range(2)]
            pa = [P.ps(f"pa{i}") for i in range(2)]
            pb = [P.ps(f"pb{i}") for i in range(2)]
            py = [P.ps(f"py{i}") for i in range(2)]
            pth, pthB = P.ps("pth")
            groups = [(b, g) for b in range(NB) for g in range(NG)]

            def wload(gi):
                b, g = groups[gi]
                (W1, W1B), (W3, W3B), (W2, W2B) = Wg[gi % 2]
                for kc in range(KC):
                    k.idma(W1[:, kc, :], w1f[:, :], widx1[0][:, b, g, kc:kc + 1], gather=True, reads=[widx1[1]], writes=[W1B[kc]])
                    k.idma(W3[:, kc, :], w3f[:, :], widx1[0][:, b, g, kc:kc + 1], gather=True, reads=[widx1[1]], writes=[W3B[kc]])
                for j in range(7):
                    k.idma(W2[:, j, :], w2f[:, :], widx2[0][:, b, g * 7 + j:g * 7 + j + 1], gather=True, reads=[widx2[1]], writes=[W2B[j]])

            def hload(b):
                k.dma(hblk[b % 2][0][:], hbuf[b * BLK:(b + 1) * BLK, :].rearrange("(a p) d -> p a d", p=128), writes=[hblk[b % 2][1]])

            wload(0)
            hload(0)
            cnt = 0
            ycnt = 0
            for gi, (b, g) in enumerate(groups):
                if gi + 1 < len(groups):
                    wload(gi + 1)
                (W1, W1B), (W3, W3B), (W2, W2B) = Wg[gi % 2]
                h_, hB_ = hT[b % 2]
                ya, yaB = yacc[b % 2]
                if g == 0:
                    hb_, hbB_ = hblk[b % 2]
                    for kc in range(KC):
                        for a in range(4):
                            k.op("pe", lambda h: h.transpose(pth[:, 0:256].bitcast(BF16)[:, a * 128:(a + 1) * 128],
                                                             hb_[:, a, kc * 128:(kc + 1) * 128], pg.id_bf[:]), [hbB_, pg.idbB], [pthB])
                        if kc % 2 == 0:
                            k.copy("act", h_[:, kc, :], pth[:, 0:256].bitcast(BF16)[:, 0:BLK], [pthB], [hB_])
                        else:
                            k.copy("dve", h_[:, kc, :], pth[:, 0:256].bitcast(BF16)[:, 0:BLK], [pthB], [hB_])
                    if b + 1 < NB:
                        hload(b + 1)
                g_, gB_ = gT[gi % 2]
                for j in range(7):
                    a_, aB = pa[cnt % 2]; b_, bB = pb[cnt % 2]; s_, sB = sa[cnt % 2]
                    cnt += 1
                    for kc in range(KC):
                        k.mm(a_[:, :], W1[:, kc, j * 128:(j + 1) * 128], h_[:, kc, :], kc == 0, kc == KC - 1, [W1B[kc], hB_], [aB])
                    for kc in range(KC):
                        k.mm(b_[:, :], W3[:, kc, j * 128:(j + 1) * 128], h_[:, kc, :], kc == 0, kc == KC - 1, [W3B[kc], hB_], [bB])
                    k.act(s_[:], a_[:, :], AF.Silu, [aB], [sB])
                    k.tt("dve", g_[:, j, :], b_[:, :], s_[:], ALU.mult, [bB, sB], [gB_])
                for a in range(4):
                    for hf_ in range(2):
                        y_, yB = py[ycnt % 2]
                        ycnt += 1
                        for j in range(7):
                            k.mm(y_[:, :], g_[:, j, a * 128:(a + 1) * 128], W2[:, j, hf_ * 512:(hf_ + 1) * 512], j == 0, j == 6, [gB_, W2B[j]], [yB])
                        dst = ya[:, a, hf_ * 512:(hf_ + 1) * 512]
                        if g == 0:
                            k.copy("act", dst, y_[:, :], [yB], [yaB])
                        else:
                            k.tt("dve", dst, dst, y_[:, :], ALU.add, [yaB, yB], [yaB])
                if g == NG - 1:
                    k.dma(obuf[b * BLK:(b + 1) * BLK, :].rearrange("(a p) d -> p a d", p=128), ya[:], reads=[yaB])
        with Pool_(k) as P:
            r1 = [P.sb([128, D], F32, f"r1_{i}") for i in range(2)]
            r2 = [P.sb([128, D], F32, f"r2_{i}") for i in range(2)]
            yo = [P.sb([128, KC, 128], F32, f"yo{i}") for i in range(2)]
            pt = [P.ps(f"pt{i}") for i in range(2)]

            def gload(i):
                k.idma(r1[i % 2][0][:], obuf[:, :], posall[0][:, i, 0:1], gather=True, reads=[posall[1]], writes=[r1[i % 2][1]])
                k.idma(r2[i % 2][0][:], obuf[:, :], posall[0][:, i, 1:2], gather=True, reads=[posall[1]], writes=[r2[i % 2][1]])

            gload(0)
            for i, (s, tk, c) in enumerate(subt):
                if i + 1 < NSUB:
                    gload(i + 1)
                a_, aB = r1[i % 2]; b_, bB = r2[i % 2]; o_, oB = yo[i % 2]
                j = pg.jidx(s, c)
                k.ts("dve", a_[:], a_[:], pwall[0][:, i, 0:1], None, ALU.mult, None, [aB, pwall[1]], [aB])
                k.stt(a_[:], b_[:], pwall[0][:, i, 1:2], a_[:], ALU.mult, ALU.add, [bB, pwall[1], aB], [aB])
                for hf_ in range(2):
                    p_, pB = pt[hf_]
                    for q in range(4):
                        oc = hf_ * 4 + q
                        k.op("pe", lambda h: h.transpose(p_[:, q * 128:(q + 1) * 128], a_[:, oc * 128:(oc + 1) * 128], pg.id_f[:]),
                             [aB, pg.idfB], [pB])
                    for q in range(4):
                        oc = hf_ * 4 + q
                        if q % 2 == 0:
                            k.act(o_[:, oc, :], p_[:, q * 128:(q + 1) * 128], AF.Identity, [pB, pg.modB], [oB], scale=pg.mod[:, l, 40 + oc, j:j + 1])
                        else:
                            k.ts("dve", o_[:, oc, :], p_[:, q * 128:(q + 1) * 128], pg.mod[:, l, 40 + oc, j:j + 1], None, ALU.mult, None,
                                 [pB, pg.modB], [oB])
                k.dma(dr["xs"][s].rearrange("(kc p) t -> p kc t", p=128)[:, :, tk:tk + 128], o_[:], reads=[oB], q="pool", accum_op=ALU.add)


def build(stop_after=99, debug=False):
    nc = bass.Bass("TRN2", target_bir_lowering=False)
    dr = {}

    def inp(name, shape, dt=F32):
        dr[name] = nc.dram_tensor(name, list(shape), dt, kind="ExternalInput").ap()

    def scr(name, shape, dt, out=False):
        dr[name] = nc.dram_tensor(name, list(shape), dt, kind="ExternalOutput" if out else "Internal").ap()

    inp("xin", [NS, D, TOK]); inp("cvec", [128, KC, 3]); inp("ada_w", [DEPTH, D, 6 * D]); inp("ada_b", [128, DEPTH, 48])
    inp("norm_g", [128, DEPTH, 2, KC]); inp("final_g", [128, KC])
    inp("ones_bf", [128, 128], BF16); inp("id_bf", [128, 128], BF16); inp("id_f", [128, 128]); inp("ones_f", [128, 128])
    inp("na_w_qkv", [2, D, 3 * D]); inp("na_w_o", [2, D, D]); inp("na_tab", [2, 8, 128, 2, 2, 22, 64])
    inp("ffn_w1", [2, D, DFF]); inp("ffn_w3", [2, D, DFF]); inp("ffn_w2", [2, DFF, D])
    inp("moe_router", [2, 128, KC, NEXP])
    for f_ in range(2):
        inp(f"moe_w1_{f_}", [NEXP, D, DFE]); inp(f"moe_w3_{f_}", [NEXP, D, DFE]); inp(f"moe_w2_{f_}", [NEXP, DFE, D])
    inp("tris", [128, 128]); inp("thr48", [128, 48]); inp("iota48", [128, 48]); inp("pk1", [128, KC]); inp("pk2", [128, 28])
    inp("pool_w", [4, 256, 256]); inp("pool_scale", [128, KC]); inp("pool_ic", [128, 4, 2, 256])
    gdn_inputs(inp)
    scr("xs", [NS, D, TOK], F32, out=debug)
    scr("hs", [NS, D, TOK], BF16)
    scr("qs", [NS, D, TOK], BF16); scr("ks", [NS, D, TOK], BF16); scr("vs", [NS, TOK, D], BF16); scr("os", [NS, D, TOK], BF16)
    scr("wexp", [NS, NEXP, TOK], F32)
    scr("htok", [NS, TOK, D], BF16, out=debug); scr("rinfo", [NS, TOK, 24], F32, out=debug)
    scr("hbuf", [42 * BLK, D], BF16, out=debug); scr("obuf", [42 * BLK, D], F32, out=debug)

    gdn_scratch(scr)
    scr("out", [NS, D, NLAT], F32, out=True)

    with ExitStack() as es:
        k = K(nc, es)
        pg = Prog(nc, k, dr, stop_after)
        with Pool_(k) as PC:
            pg.setup_consts(PC)
            for s in range(NS):
                for kc in range(KC):
                    k.dma(dr["xs"][s, kc * 128:(kc + 1) * 128, :], dr["xin"][s, kc * 128:(kc + 1) * 128, :])
            pg.adaln()
            k.barrier()
            step = 0
            for l in range(DEPTH):
                last = l == DEPTH - 1
                if step >= stop_after:
                    break
                pg.norm_pass(l, 0, include_ctx=True)
                kind = l % 3
                jx = l // 3
                if kind == 0:
                    pg.na_qkv_pass(dr["na_w_qkv"][jx])
                    pg.na_attn_pass(dr["na_tab"][jx], need_ctx=not last)
                    pg.outproj_pass(l, dr["os"], dr["na_w_o"][jx], include_ctx=not last, gate_off=16)
                elif kind == 1:
                    gdn_mixer(pg, l, need_ctx=not last)
                else:
                    pg.pool_pass(l, dr["pool_w"], include_ctx=not last)
                step += 1
                if step >= stop_after:
                    break
                f = l // 2
                if l % 2 == 0:
                    pg.norm_pass(l, 1, include_ctx=not last)
                    H = DFF // 2
                    for hh in range(2):
                        pg.ffn_pass(l, dr["ffn_w1"][f][:, hh * H:(hh + 1) * H], dr["ffn_w3"][f][:, hh * H:(hh + 1) * H],
                                    dr["ffn_w2"][f][hh * H:(hh + 1) * H, :], H, include_ctx=not last)
                else:
                    moe_layer(pg, l, f, include_ctx=not last)
                step += 1
            pg.norm_pass(0, 0, include_ctx=False, final=True)
            k.barrier()
    return nc


GDN_IN = 4128
NCHK = TOK // 128


def gdn_inputs(inp):
    inp("gdn_w_in", [D, GDN_IN]); inp("gdn_w_o", [D, D]); inp("gdn_cw", [128, 24, 4])
    inp("gdn_dtb", [128, 16]); inp("gdn_alog", [128, 16]); inp("gdn_ng", [128, 1])
    inp("gdn_tri", [128, 2, 128]); inp("gdn_gm", [128, 2, 3, 128]); inp("gdn_ms", [128, 7, 256], BF16)


def gdn_scratch(scr):
    scr("pj", [NS, 4 * D, TOK], BF16)
    scr("gq", [NS, D, TOK], BF16); scr("gk", [NS, D, TOK], BF16); scr("gv", [NS, D, TOK], BF16)
    scr("gt", [NS, TOK, 48], F32)
    scr("gcs", [NS, 16, TOK], F32)
    scr("gbs", [NS, 16, TOK], F32)


def gdn_proj_pass(pg):
    k, dr = pg.k, pg.dr
    hs, pj = dr["hs"], dr["pj"]
    tl = tiles_for(True)
    with Pool_(k) as P:
        Wi, WiB = P.sb([128, KC, GDN_IN], BF16, "Wi")
        src = dr["gdn_w_in"].rearrange("(kc p) n -> p kc n", p=128)
        for i in range(4):
            k.dma(Wi[:, :, i * 1032:(i + 1) * 1032], src[:, :, i * 1032:(i + 1) * 1032], writes=[WiB], q="pool")
        dtb, dtbB = P.sb([128, 16], F32, "dtb"); k.dma(dtb[:], dr["gdn_dtb"][:, :], writes=[dtbB])
        nA, nAB = P.sb([128, 16], F32, "nA"); k.dma(nA[:], dr["gdn_alog"][:, :], writes=[nAB])
        k.act(nA[:], nA[:], AF.Exp, [nAB], [nAB])
        k.ts("dve", nA[:], nA[:], -1.0, None, ALU.mult, None, [nAB], [nAB])
        tri, triB = P.sb([128, 2, 128], F32, "tri"); k.dma(tri[:], dr["gdn_tri"][:, :, :], writes=[triB])
        ht = [P.sb([128, KC, TT], BF16, f"ht{i}") for i in range(2)]
        pt = [P.sb([128, 32, TT], BF16, f"pjt{i}") for i in range(1)]
        gtt, gttB = P.sb([128, 4, 48], F32, "gtt")
        tmpa, tmpaB = P.sb([128, 16], F32, "tmpa")
        gcT, gcTB = P.sb([16, TT], F32, "gcT")
        bT, bTB = P.sb([16, TT], F32, "bT")
        pp = [P.ps(f"pp{i}") for i in range(4)]
        pab, pabB = P.ps("pab")
        pgc, pgcB = P.ps("pgc")
        ptr, ptrB = P.ps("ptr")
        pbf, pbfB = P.ps("pbf")

        def load(i):
            s, t0, W, c = tl[i]
            t, B = ht[i % 2]
            k.dma(t[:, :, :W], hs[s].rearrange("(kc p) t -> p kc t", p=128)[:, :, t0:t0 + W], writes=[B])

        load(0)
        cnt = 0
        for i, (s, t0, W, c) in enumerate(tl):
            if i + 1 < len(tl):
                load(i + 1)
            h, hB = ht[i % 2]
            p_, pjB = pt[0]
            for oc in range(32):
                q_, qB = pp[cnt % 4]
                for kc in range(KC):
                    k.mm(q_[:, :W], Wi[:, kc, oc * 128:(oc + 1) * 128], h[:, kc, :W], kc == 0, kc == KC - 1, [WiB, hB], [qB])
                if cnt % 2 == 0:
                    k.act(p_[:, oc, :W], q_[:, :W], AF.Copy, [qB], [pjB])
                else:
                    k.copy("dve", p_[:, oc, :W], q_[:, :W], [qB], [pjB])
                cnt += 1
            k.dma(pj[s].rearrange("(oc p) t -> p oc t", p=128)[:, :, t0:t0 + W], p_[:, :, :W], reads=[pjB])
            for kc in range(KC):
                k.mm(pbf[0:16, :W], Wi[:, kc, 4096 + 16:4096 + 32], h[:, kc, :W], kc == 0, kc == KC - 1, [WiB, hB], [pbfB])
            k.act(bT[:, :W], pbf[0:16, :W], AF.Sigmoid, [pbfB], [bTB])
            k.dma(dr["gbs"][s, :, t0:t0 + W], bT[:, :W], reads=[bTB])
            nst = W // 128
            for ts_ in range(nst):
                for kc in range(KC):
                    k.mm(pab[:, ts_ * 32:(ts_ + 1) * 32], h[:, kc, ts_ * 128:(ts_ + 1) * 128], Wi[:, kc, 4096:4128],
                         kc == 0, kc == KC - 1, [WiB, hB], [pabB])
            for ts_ in range(nst):
                k.tt("dve", tmpa[:], pab[:, ts_ * 32:ts_ * 32 + 16], dtb[:], ALU.add, [pabB, dtbB], [tmpaB])
                k.act(tmpa[:], tmpa[:], AF.Exp, [tmpaB], [tmpaB])
                k.act(tmpa[:], tmpa[:], AF.Ln, [tmpaB], [tmpaB], bias=1.0)
                k.tt("dve", gtt[:, ts_, 0:16], tmpa[:], nA[:], ALU.mult, [tmpaB, nAB], [gttB])
                k.act(gtt[:, ts_, 16:32], pab[:, ts_ * 32 + 16:ts_ * 32 + 32], AF.Sigmoid, [pabB], [gttB])
                for d in range(2):
                    k.mm(pgc[:, ts_ * 16 + d * 8:ts_ * 16 + d * 8 + 8], tri[:, d, :], gtt[:, ts_, d * 8:d * 8 + 8], True, True,
                         [triB, gttB], [pgcB])
                k.copy("dve", gtt[:, ts_, 32:48], pgc[:, ts_ * 16:ts_ * 16 + 16], [pgcB], [gttB])
                k.op("pe", lambda hh: hh.transpose(ptr[0:16, ts_ * 128:(ts_ + 1) * 128], gtt[:, ts_, 32:48], pg.id_f[:]),
                     [gttB, pg.idfB], [ptrB])
            k.copy("dve", gcT[:, :W], ptr[0:16, :W], [ptrB], [gcTB])
            k.dma(dr["gcs"][s, :, t0:t0 + W], gcT[:, :W], reads=[gcTB])
            k.dma(dr["gt"][s, t0:t0 + W, :].rearrange("(a p) c -> p a c", p=128), gtt[:, :nst, :], reads=[gttB])


def gdn_conv_pass(pg):
    k, dr = pg.k, pg.dr
    pj = dr["pj"]
    tl = tiles_for(True)
    with Pool_(k) as P:
        cw, cwB = P.sb([128, 24, 4], F32, "cw"); k.dma(cw[:], dr["gdn_cw"][:, :, :], writes=[cwB])
        e1, e1B = P.sb([128, 1], F32, "e1"); k.memset("pool", e1[:], EPS, [e1B])
        e2, e2B = P.sb([128, 1], F32, "e2"); k.memset("pool", e2[:], EPS * 128.0, [e2B])
        pin = [P.sb([128, 24, TT + 4], BF16, f"pin{i}") for i in range(2)]
        ot, otB = P.sb([128, 24, TT], BF16, "cot")
        acc = [P.sb([128, TT], F32, f"acc{i}") for i in range(2)]
        sl = [P.sb([128, TT], F32, f"sl{i}") for i in range(2)]
        sq = [P.sb([128, TT], BF16, f"sq{i}") for i in range(2)]
        rn = [P.sb([128, TT], F32, f"rn{i}") for i in range(2)]
        pss = [P.ps(f"pss{i}") for i in range(2)]

        def load(i):
            s, t0, W, c = tl[i]
            t, B = pin[i % 2]
            seq0, seq1 = (0, NCTX) if c else (NCTX, TOK)
            k.memset("pool", t[:], 0.0, [B])
            lo, hi = max(seq0, t0 - 2), min(seq1, t0 + W + 1)
            k.dma(t[:, :, lo - (t0 - 2):hi - (t0 - 2)], pj[s].rearrange("(oc p) t -> p oc t", p=128)[:, 0:24, lo:hi], writes=[B])

        load(0)
        cnt = 0
        for i, (s, t0, W, c) in enumerate(tl):
            if i + 1 < len(tl):
                load(i + 1)
            x, xB = pin[i % 2]
            for oc in range(24):
                a_, aB = acc[cnt % 2]; s_, sB = sl[cnt % 2]; q_, qB = sq[cnt % 2]; r_, rB = rn[cnt % 2]; p_, pB = pss[cnt % 2]
                cnt += 1
                k.ts("dve", a_[:, :W], x[:, oc, 0:W], cw[:, oc, 0:1], None, ALU.mult, None, [xB, cwB], [aB])
                for j in range(1, 4):
                    k.stt(a_[:, :W], x[:, oc, j:j + W], cw[:, oc, j:j + 1], a_[:, :W], ALU.mult, ALU.add, [xB, cwB, aB], [aB])
                if oc >= 16:
                    k.act(ot[:, oc, :W], a_[:, :W], AF.Silu, [aB], [otB])
                    continue
                k.act(s_[:, :W], a_[:, :W], AF.Silu, [aB], [sB])
                k.act(q_[:, :W], s_[:, :W], AF.Square, [sB], [qB])
                k.mm(p_[:, :W], pg.ones_bf[:], q_[:, :W], True, True, [pg.onesB, qB], [pB])
                if oc < 8:
                    k.act(r_[:, :W], p_[:, :W], AF.Sqrt, [pB, e2B], [rB], scale=128.0, bias=e2[:, 0:1])
                else:
                    k.act(r_[:, :W], p_[:, :W], AF.Sqrt, [pB, e1B], [rB], bias=e1[:, 0:1])
                k.op("dve", lambda hh: hh.reciprocal(out=r_[:, :W], in_=r_[:, :W]), [rB], [rB])
                k.tt("pool", ot[:, oc, :W], s_[:, :W], r_[:, :W], ALU.mult, [sB, rB], [otB])
            for gi, nm in enumerate(("gq", "gk", "gv")):
                k.dma(dr[nm][s].rearrange("(oc p) t -> p oc t", p=128)[:, :, t0:t0 + W], ot[:, gi * 8:(gi + 1) * 8, :W], reads=[otB])


def _rr(gens):
    gens = list(gens)
    while gens:
        nxt = []
        for g in gens:
            try:
                next(g)
                nxt.append(g)
            except StopIteration:
                pass
        gens = nxt


def gdn_core_pass(pg):
    k, dr = pg.k, pg.dr
    with Pool_(k) as P:
        gm, gmB = P.sb([128, 2, 3, 128], F32, "gm"); k.dma(gm[:], dr["gdn_gm"][:, :, :, :], writes=[gmB])
        ms, msB = P.sb([128, 7, 256], BF16, "ms"); k.dma(ms[:], dr["gdn_ms"][:, :, :], writes=[msB])
        gng, gngB = P.sb([128, 1], F32, "gng"); k.dma(gng[:], dr["gdn_ng"][:, :], writes=[gngB])
        e1, e1B = P.sb([128, 1], F32, "e1"); k.memset("pool", e1[:], EPS, [e1B])
        qT, qTB = P.sb([128, TOK], BF16, "qT"); kT, kTB = P.sb([128, TOK], BF16, "kT")
        ktok, ktokB = P.sb([128, NCHK, 128], BF16, "ktok"); vtok, vtokB = P.sb([128, NCHK, 128], BF16, "vtok")
        gt, gtB = P.sb([128, NCHK, 48], F32, "gt")
        gcb, gcbB = P.sb([128, TOK], F32, "gcb"); bb, bbB = P.sb([128, TOK], F32, "bb")
        U, UB_ = P.sb([128, NCHK, 128], F32, "U"); UB = [Buf(f"U{n}") for n in range(NCHK)]
        WT, _ = P.sb([128, NCHK, 128], BF16, "WT"); WTB = [Buf(f"WT{n}") for n in range(NCHK)]
        AQ, _ = P.sb([128, NCHK, 128], BF16, "AQ"); AQB = [Buf(f"AQ{n}") for n in range(NCHK)]
        KTL, _ = P.sb([128, NCHK, 128], BF16, "KTL"); KTLB = [Buf(f"KTL{n}") for n in range(NCHK)]
        qd, qdB = P.sb([128, TOK], BF16, "qd")
        vT, vTB = qd, qdB
        zT, zTB = kT, kTB
        oacc, oaccB = P.sb([128, TOK], F32, "oacc"); oB = [Buf(f"o{n}") for n in range(NCHK)]
        egc, egcB = P.sb([128, NCHK], F32, "egc"); ett, ettB = P.sb([128, NCHK], F32, "ett"); egl, eglB = P.sb([128, NCHK], F32, "egl")
        S, SB = P.sb([128, 128], F32, "S"); Sb, SbB = P.sb([128, 128], BF16, "Sb")
        vn = [P.sb([128, 128], BF16, f"vn{i}") for i in range(2)]
        tmpx, tmpxB = P.sb([128, 512], F32, "tmpx")
        NI = 4
        wk = []
        for ii in range(NI):
            w = {}
            for nm in ("xa", "xb", "xc", "t1"):
                w[nm] = P.sb([128, 256], F32, f"{nm}{ii}")
            w["ea"], w["eb"], w["ec"] = w["xa"], w["xb"], w["xc"]
            for nm in ("L", "M", "X0", "X1", "Y0", "Y1", "Wm", "Wn", "vb", "kbg"):
                w[nm] = P.sb([128, 256], BF16, f"{nm}{ii}")
            w["pA"] = P.ps(f"pA{ii}"); w["pB"] = P.ps(f"pB{ii}"); w["pC"] = w["pB"]
            wk.append(w)
        pX, pXB = wk[0]["pA"]; pY, pYB = wk[1]["pA"]

        for s in range(NS):
            k.dma(gt[:], dr["gt"][s].rearrange("(n p) c -> p n c", p=128), writes=[gtB])
            for h in range(8):
                rows = slice(h * 128, (h + 1) * 128)
                k.dma(qT[:], dr["gq"][s, rows, :], writes=[qTB]); k.dma(kT[:], dr["gk"][s, rows, :], writes=[kTB])
                k.dma(vT[:], dr["gv"][s, rows, :], writes=[vTB])
                ktBs = [Buf() for _ in range(NCHK)]
                vtBs = [Buf() for _ in range(NCHK)]
                for n in range(NCHK):
                    cs = slice(n * 128, (n + 1) * 128)
                    ka, kaB = wk[(2 * n) % NI]["pA"]
                    va, vaB = wk[(2 * n + 1) % NI]["pA"]
                    k.op("pe", lambda hh: hh.transpose(ka[:, 0:128].bitcast(BF16)[:, 0:128], kT[:, cs], pg.id_bf[:]), [kTB, pg.idbB], [kaB])
                    k.copy("act", ktok[:, n, :], ka[:, 0:128].bitcast(BF16)[:, 0:128], [kaB], [ktBs[n]] + ([ktokB] if n == 0 else []))
                    k.op("pe", lambda hh: hh.transpose(va[:, 0:128].bitcast(BF16)[:, 0:128], vT[:, cs], pg.id_bf[:]), [vTB, pg.idbB], [vaB])
                    k.copy("dve", vtok[:, n, :], va[:, 0:128].bitcast(BF16)[:, 0:128], [vaB], [vtBs[n]] + ([vtokB] if n == 0 else []))
                k.op("act", lambda hh: hh.activation(out=egc[:, 0:1], in_=egc[:, 0:1], func=AF.Copy), ktBs, [ktokB, egcB])
                k.op("dve", lambda hh: hh.tensor_copy(out=ett[:, 0:1], in_=ett[:, 0:1]), vtBs, [vtokB, ettB])
                for d in range(2):
                    col = d * 8 + h
                    k.dma(gcb[:], dr["gcs"][s, col:col + 1, :].partition_broadcast(128), writes=[gcbB])
                    k.dma(bb[:], dr["gbs"][s, col:col + 1, :].partition_broadcast(128), writes=[bbB])
                    gcv = gt[:, :, 32 + col]
                    btv = gt[:, :, 16 + col]
                    lastoff = 127 if d == 0 else 0
                    glv = gcb[:, lastoff:TOK:128]
                    k.act(egc[:], gcv, AF.Exp, [gtB], [egcB])
                    k.tt("dve", ett[:], glv, gcv, ALU.subtract, [gcbB, gtB], [ettB])
                    k.act(ett[:], ett[:], AF.Exp, [ettB], [ettB])
                    k.act(egl[:], glv, AF.Exp, [gcbB], [eglB])
                    for c0 in range(0, TOK, 512):
                        wd = min(512, TOK - c0)
                        k.act(tmpx[:, :wd], gcb[:, c0:c0 + wd], AF.Exp, [gcbB], [tmpxB])
                        k.tt("dve", qd[:, c0:c0 + wd], qT[:, c0:c0 + wd], tmpx[:, :wd], ALU.mult, [qTB, tmpxB], [qdB])

                    def inst(n0_, w):
                        (xa, xaB), (xb, xbB), (xc, xcB) = w["xa"], w["xb"], w["xc"]
                        (t1, t1B), (L, LB), (M, MB) = w["t1"], w["L"], w["M"]
                        (pA, pAB), (pB_, pBB) = w["pA"], w["pB"]
                        vb, vbB = w["vb"]; kbg, kbgB = w["kbg"]
                        for u in range(2):
                            n = n0_ + u
                            cs = slice(n * 128, (n + 1) * 128); us = slice(u * 128, (u + 1) * 128)
                            gcn = gt[:, n, 32 + col:33 + col]
                            k.stt(xa[:, us], gcb[:, cs], gcn, gm[:, d, 0, :], ALU.subtract, ALU.add, [gcbB, gtB, gmB], [xaB])
                            k.stt(xb[:, us], gcb[:, cs], gcn, gm[:, d, 1, :], ALU.subtract, ALU.add, [gcbB, gtB, gmB], [xbB])
                            k.stt(xc[:, us], gcb[:, cs], gcn, gm[:, d, 2, :], ALU.subtract, ALU.add, [gcbB, gtB, gmB], [xcB])
                            k.mm(pA[:, us], kT[:, cs], kT[:, cs], True, True, [kTB], [pAB])
                            k.mm(pB_[:, us], kT[:, cs], qT[:, cs], True, True, [kTB, qTB], [pBB])
                        yield
                        k.act(xa[:], xa[:], AF.Exp, [xaB], [xaB])
                        k.act(xb[:], xb[:], AF.Exp, [xbB], [xbB])
                        k.act(xc[:], xc[:], AF.Exp, [xcB], [xcB], scale=-1.0)
                        yield
                        for u in range(2):
                            n = n0_ + u
                            cs = slice(n * 128, (n + 1) * 128); us = slice(u * 128, (u + 1) * 128)
                            btn = gt[:, n, 16 + col:17 + col]
                            k.stt(L[:, us], pA[:, us], btn, xc[:, us], ALU.mult, ALU.mult, [pAB, gtB, xcB], [LB])
                            k.ts("pool", vb[:, us], vtok[:, n, :], btn, None, ALU.mult, None, [vtokB, gtB], [vbB])
                            k.ts("pool", kbg[:, us], ktok[:, n, :], btn, egc[:, n:n + 1], ALU.mult, ALU.mult, [ktokB, gtB, egcB], [kbgB])
                            k.ts("pool", KTL[:, n, :], ktok[:, n, :], ett[:, n:n + 1], None, ALU.mult, None, [ktokB, ettB], [KTLB[n]])
                        k.tt("dve", t1[:], pA[:, 0:256], xb[:], ALU.mult, [pAB, xbB], [t1B])
                        k.tt("pool", M[:], t1[:], bb[:, n0_ * 128:(n0_ + 2) * 128], ALU.mult, [t1B, bbB], [MB])
                        k.tt("dve", AQ[:, n0_:n0_ + 2, :].rearrange("p a b -> p (a b)"), pB_[:, 0:256], xa[:], ALU.mult, [pBB, xaB],
                             [AQB[n0_], AQB[n0_ + 1]])
                        yield
                        X, XB = w["X0"]; Y, YB = w["Y0"]; Xn, XnB = w["X1"]; Yn, YnB = w["Y1"]
                        Wm, WmB = w["Wm"]; Wn, WnB = w["Wn"]
                        k.tt("pool", Wm[:], L[:], ms[:, 0, :], ALU.mult, [LB, msB], [WmB])
                        for u in range(2):
                            us = slice(u * 128, (u + 1) * 128)
                            k.tt("pool", X[:, us], pg.id_bf[:], Wm[:, us], ALU.subtract, [pg.idbB, WmB], [XB])
                        k.tt("pool", Wn[:], M[:], ms[:, 0, :], ALU.mult, [MB, msB], [WnB])
                        for u in range(2):
                            us = slice(u * 128, (u + 1) * 128)
                            k.tt("pool", Y[:, us], pg.id_bf[:], Wn[:, us], ALU.subtract, [pg.idbB, WnB], [YB])
                        yield
                        for lv in range(1, 7):
                            lastlv = lv == 6
                            for u in range(2):
                                us = slice(u * 128, (u + 1) * 128)
                                if not lastlv:
                                    k.mm(pA[:, us], M[:, us], X[:, us], True, True, [MB, XB], [pAB])
                                k.mm(pB_[:, us], L[:, us], Y[:, us], True, True, [LB, YB], [pBB])
                            yield
                            if not lastlv:
                                k.tt("dve", Wm[:], pA[:, 0:256], ms[:, lv, :], ALU.mult, [pAB, msB], [WmB])
                            k.tt("dve", Wn[:], pB_[:, 0:256], ms[:, lv, :], ALU.mult, [pBB, msB], [WnB])
                            yield
                            for u in range(2):
                                us = slice(u * 128, (u + 1) * 128)
                                if not lastlv:
                                    k.mm(pA[:, us], Y[:, us], Wm[:, us], True, True, [YB, WmB], [pAB])
                                k.mm(pB_[:, us], X[:, us], Wn[:, us], True, True, [XB, WnB], [pBB])
                            yield
                            if not lastlv:
                                k.tt("dve", Xn[:], X[:], pA[:, 0:256], ALU.subtract, [XB, pAB], [XnB])
                            k.tt("dve", Yn[:], Y[:], pB_[:, 0:256], ALU.subtract, [YB, pBB], [YnB])
                            X, XB, Xn, XnB = Xn, XnB, X, XB
                            Y, YB, Yn, YnB = Yn, YnB, Y, YB
                            yield
                        for u in range(2):
                            us = slice(u * 128, (u + 1) * 128)
                            k.mm(pA[:, us], Y[:, us], vb[:, us], True, True, [YB, vbB], [pAB])
                            k.mm(pB_[:, us], kbg[:, us], Y[:, us], True, True, [kbgB, YB], [pBB])
                        yield
                        k.copy("act", U[:, n0_:n0_ + 2, :].rearrange("p a b -> p (a b)"), pA[:, 0:256], [pAB], [UB[n0_], UB[n0_ + 1]])
                        k.copy("act", WT[:, n0_:n0_ + 2, :].rearrange("p a b -> p (a b)"), pB_[:, 0:256], [pBB], [WTB[n0_], WTB[n0_ + 1]])
                        yield

                    import contextlib
                    sc_ = (lambda nm: pg.nc.named_scope(nm)) if (s == 0 and h == 1) else (lambda nm: contextlib.nullcontext())
                    with sc_(f"gi_inst{d}"):
                        for n0 in range(0, NCHK, 2 * NI):
                            _rr([inst(n0 + 2 * ii, wk[ii]) for ii in range(NI) if n0 + 2 * ii < NCHK])
                    k.memset("pool", S[:], 0.0, [SB]); k.memset("pool", Sb[:], 0.0, [SbB])
                    order = [0, 1] + list(range(2, NCHK)) if d == 0 else [1, 0] + list(range(NCHK - 1, 1, -1))
                    for it, n in enumerate(order):
                        cs = slice(n * 128, (n + 1) * 128)
                        v_, vB_ = vn[it % 2]
                        k.mm(pX[:, 0:128], WT[:, n, :], Sb[:], True, True, [WTB[n], SbB], [pXB])
                        k.tt("dve", v_[:], U[:, n, :], pX[:, 0:128], ALU.subtract, [UB[n], pXB], [vB_])
                        k.mm(pY[:, 0:128], Sb[:], qd[:, cs], True, False, [SbB, qdB], [pYB])
                        k.mm(pY[:, 0:128], v_[:], AQ[:, n, :], False, True, [vB_, AQB[n]], [pYB])
                        if d == 0:
                            k.copy("act", oacc[:, cs], pY[:, 0:128], [pYB], [oB[n]])
                        else:
                            k.tt("dve", oacc[:, cs], oacc[:, cs], pY[:, 0:128], ALU.add, [oB[n], pYB], [oB[n]])
                        k.mm(pX[:, 0:128], KTL[:, n, :], v_[:], True, True, [KTLB[n], vB_], [pXB])
                        k.stt(Sb[:], S[:], egl[:, n:n + 1], pX[:, 0:128], ALU.mult, ALU.add, [SB, eglB, pXB], [SbB])
                        k.stt(S[:], S[:], egl[:, n:n + 1], pX[:, 0:128], ALU.mult, ALU.add, [SB, eglB, pXB], [SB])
                k.dma(zT[:], dr["pj"][s, 3 * D + h * 128:3 * D + (h + 1) * 128, :], writes=[zTB])
                for c0 in range(0, TOK, 512):
                    wd = min(512, TOK - c0)
                    obs = oB[c0 // 128:(c0 + wd) // 128]
                    xa, xaB = wk[0]["xa"]; xb, xbB = wk[0]["xb"]
                    sqt, sqB = P_sq = (qd, qdB)
                    k.act(sqt[:, c0:c0 + wd], oacc[:, c0:c0 + wd], AF.Square, obs, [qdB])
                    k.mm(pX[:, :wd], pg.ones_bf[:], sqt[:, c0:c0 + wd], True, True, [pg.onesB, qdB], [pXB])
                    k.act(tmpx[:, :wd], pX[:, :wd], AF.Sqrt, [pXB, e1B], [tmpxB], scale=1.0 / 128.0, bias=e1[:, 0:1])
                    k.op("dve", lambda hh: hh.reciprocal(out=tmpx[:, :wd], in_=tmpx[:, :wd]), [tmpxB], [tmpxB])
                    k.tt("dve", oacc[:, c0:c0 + wd], oacc[:, c0:c0 + wd], tmpx[:, :wd], ALU.mult, obs + [tmpxB], obs)
                    k.act(tmpx[:, :wd], zT[:, c0:c0 + wd], AF.Silu, [zTB], [tmpxB])
                    k.stt(qd[:, c0:c0 + wd], oacc[:, c0:c0 + wd], gng[:, 0:1], tmpx[:, :wd], ALU.mult, ALU.mult, obs + [gngB, tmpxB], [qdB])
                k.dma(dr["os"][s, rows, :], qd[:], reads=[qdB])


def gdn_mixer(pg, l, need_ctx):
    with pg.nc.named_scope("g_proj"):
        gdn_proj_pass(pg)
    with pg.nc.named_scope("g_conv"):
        gdn_conv_pass(pg)
    with pg.nc.named_scope("g_core"):
        gdn_core_pass(pg)
    with pg.nc.named_scope("g_out"):
        pg.outproj_pass(l, pg.dr["os"], pg.dr["gdn_w_o"], include_ctx=need_ctx, gate_off=16)


def _fm(v):
    v = np.asarray(v, np.float32)
    lead = v.shape[:-1]
    return np.ascontiguousarray(np.moveaxis(v.reshape(lead + (KC, 128)), -1, 0))


def _na_tables(rpb):
    col = np.arange(64)
    c0 = np.clip(col - 8, 0, 48)
    in_win = (col[None, :] >= c0[:, None]) & (col[None, :] < c0[:, None] + 16)
    dc = np.clip(col[None, :] - col[:, None], -15, 15) + 15
    out = np.full((8, 128, 2, 2, 22, 64), NEG, np.float32)
    for jj in range(22):
        j = 17 - jj
        for a in range(2):
            drr = j + a
            if drr < 0 or drr > 14:
                continue
            base = np.where(in_win[None], rpb[:, drr][:, dc], NEG).astype(np.float32)
            base = np.transpose(base, (0, 2, 1))
            for h in range(16):
                out[h // 2, a * 64:(a + 1) * 64, 1, h % 2, jj, :] = base[h]
                if 3 <= drr <= 10:
                    out[h // 2, a * 64:(a + 1) * 64, 0, h % 2, jj, :] = base[h]
    return out


def _pool_ic():
    out = np.ones((4, 2, 256), np.float32)
    for gi, win in enumerate((2, 4, 8, 16)):
        for ri, T in enumerate((64, 256)):
            t = np.arange(T)
            lo = np.clip(t - win // 2, 0, T)
            hi = np.clip(t + win // 2, 0, T)
            out[gi, ri, :T] = 1.0 / (hi - lo).astype(np.float32)
    return np.ascontiguousarray(np.broadcast_to(out[None], (128, 4, 2, 256)))


def make_in_maps(inputs, n_cores=8):
    import ml_dtypes
    x, c, ctx, c_ctx = inputs["x"], inputs["c"], inputs["ctx"], inputs["c_ctx"]
    shared = {
        "ada_w": np.ascontiguousarray(inputs["ada_w"], np.float32),
        "ada_b": np.ascontiguousarray(np.transpose(np.asarray(inputs["ada_b"], np.float32).reshape(DEPTH, 48, 128), (2, 0, 1))),
        "norm_g": _fm(inputs["norm_g"]),
        "final_g": _fm(inputs["final_g"]),
        "ones_bf": np.ones((128, 128), ml_dtypes.bfloat16),
        "id_bf": np.eye(128, dtype=np.float32).astype(ml_dtypes.bfloat16),
        "id_f": np.eye(128, dtype=np.float32),
        "ones_f": np.ones((128, 128), np.float32),
        "na_w_qkv": np.ascontiguousarray(inputs["na_w_qkv"], np.float32),
        "na_w_o": np.ascontiguousarray(inputs["na_w_o"], np.float32),
        "na_tab": np.stack([_na_tables(np.asarray(inputs["na_rpb"][j], np.float32)) for j in range(2)]),
        "ffn_w1": np.ascontiguousarray(inputs["ffn_w1"], np.float32),
        "ffn_w3": np.ascontiguousarray(inputs["ffn_w3"], np.float32),
        "ffn_w2": np.ascontiguousarray(inputs["ffn_w2"], np.float32),
        "moe_router": np.ascontiguousarray(np.transpose(np.asarray(inputs["moe_router"], np.float32).reshape(2, KC, 128, NEXP), (0, 2, 1, 3))),
        "moe_w1_0": np.ascontiguousarray(inputs["moe_w1"][0], np.float32), "moe_w1_1": np.ascontiguousarray(inputs["moe_w1"][1], np.float32),
        "moe_w3_0": np.ascontiguousarray(inputs["moe_w3"][0], np.float32), "moe_w3_1": np.ascontiguousarray(inputs["moe_w3"][1], np.float32),
        "moe_w2_0": np.ascontiguousarray(inputs["moe_w2"][0], np.float32), "moe_w2_1": np.ascontiguousarray(inputs["moe_w2"][1], np.float32),
        "tris": np.ascontiguousarray((np.arange(128)[:, None] < np.arange(128)[None, :]).astype(np.float32)),
        "thr48": np.ascontiguousarray(np.broadcast_to((np.arange(48) * 512.0).astype(np.float32)[None], (128, 48))),
        "iota48": np.ascontiguousarray(np.broadcast_to(np.arange(48).astype(np.float32)[None], (128, 48))),
        "pk1": np.ascontiguousarray(((np.arange(KC)[None, :] * 128 + np.arange(128)[:, None]) * 4).astype(np.float32)),
        "pk2": np.ascontiguousarray((np.arange(28)[None, :] * 128 + np.arange(128)[:, None]).astype(np.float32)),
        "pool_w": np.ascontiguousarray(inputs["pool_w"][0], np.float32),
        "pool_scale": _fm(inputs["pool_scale"][0]),
        "pool_ic": _pool_ic(),
    }
    shared.update(gdn_host(inputs))
    maps = []
    for ci in range(n_cores):
        b0 = ci * NS
        xin = np.empty((NS, D, TOK), np.float32)
        for s in range(NS):
            xin[s, :, :NCTX] = np.asarray(ctx[b0 + s], np.float32).T
            xin[s, :, NCTX:] = np.asarray(x[b0 + s], np.float32).T
        cv = np.stack([c[b0], c[b0 + 1], c_ctx], axis=0).astype(np.float32)
        m = dict(shared)
        m["xin"] = xin
        m["cvec"] = np.ascontiguousarray(np.transpose(cv.reshape(3, KC, 128), (2, 1, 0)))
        maps.append(m)
    return maps


def gdn_host(inputs):
    import ml_dtypes
    cw = np.asarray(inputs["gdn_conv"][0], np.float32)
    cwl = np.ascontiguousarray(np.transpose(cw.reshape(4, 24, 128), (2, 1, 0)))
    dtb = np.asarray(inputs["gdn_dt_bias"][0], np.float32).reshape(16)
    alog = np.asarray(inputs["gdn_a_log"][0], np.float32).reshape(16)
    p = np.arange(128)
    tri = np.zeros((128, 2, 128), np.float32)
    tri[:, 0, :] = (p[:, None] <= p[None, :])
    tri[:, 1, :] = (p[:, None] >= p[None, :])
    gm = np.zeros((128, 2, 3, 128), np.float32)
    P_, F_ = p[:, None], p[None, :]
    gm[:, 0, 0, :] = np.where(F_ >= P_, 0.0, NEG); gm[:, 0, 1, :] = np.where(F_ > P_, 0.0, NEG); gm[:, 0, 2, :] = np.where(F_ < P_, 0.0, -NEG)
    gm[:, 1, 0, :] = np.where(F_ <= P_, 0.0, NEG); gm[:, 1, 1, :] = np.where(F_ < P_, 0.0, NEG); gm[:, 1, 2, :] = np.where(F_ > P_, 0.0, -NEG)
    ms = np.zeros((128, 7, 128), np.float32)
    for kk in range(7):
        ms[:, kk, :] = ((P_ >> kk) != (F_ >> kk)) & ((P_ >> (kk + 1)) == (F_ >> (kk + 1)))
    return {
        "gdn_w_in": np.ascontiguousarray(inputs["gdn_w_in"][0], np.float32),
        "gdn_w_o": np.ascontiguousarray(inputs["gdn_w_o"][0], np.float32),
        "gdn_cw": cwl,
        "gdn_dtb": np.ascontiguousarray(np.broadcast_to(dtb[None], (128, 16))),
        "gdn_alog": np.ascontiguousarray(np.broadcast_to(alog[None], (128, 16))),
        "gdn_ng": np.ascontiguousarray(np.asarray(inputs["gdn_norm_g"][0], np.float32).reshape(128, 1)),
        "gdn_tri": tri, "gdn_gm": gm, "gdn_ms": np.concatenate([ms, ms], axis=2).astype(ml_dtypes.bfloat16),
    }


def kernel(**inputs):
    nc = build()
    maps = make_in_maps(inputs)
    res = run_bass_kernel_spmd(nc, maps, core_ids=list(range(8)))
    B = inputs["x"].shape[0]
    out = np.empty((B, NLAT, D), np.float32)
    for ci in range(8):
        o = res.results[ci]["out"]
        for s in range(NS):
            out[ci * NS + s] = o[s].T
    return out
```

```python
import numpy as np
from contextlib import ExitStack
import concourse.bass as bass
import concourse.mybir as mybir
from concourse.bass_utils import run_bass_kernel_spmd

F32 = mybir.dt.float32
BF16 = mybir.dt.bfloat16
U32 = mybir.dt.uint32
AF = mybir.ActivationFunctionType
ALU = mybir.AluOpType

D = 1024
KC = 8
NLAT = 4096
NCTX = 256
TT = 512
NS = 2
DEPTH = 4
EPS = 1e-6
NEG = -1e30
DFF = 2816
DFE = 3584
NEXP = 8


class _Sem:
    def __init__(self, h, name):
        self.h = h
        self.val = 0
        self.name = name


class _Eng:
    def __init__(self, name, h, sem):
        self.name = name
        self.h = h
        self.sem = sem
        self.seen = {}


class Buf:
    __slots__ = ("name", "w", "r")

    def __init__(self, name=""):
        self.name = name
        self.w = None
        self.r = {}


class K:
    def __init__(self, nc, es, ndma=24):
        self.nc = nc
        self.es = es
        self.E = {}
        for name, h in (("pe", nc.tensor), ("act", nc.scalar), ("dve", nc.vector),
                        ("pool", nc.gpsimd), ("sp", nc.sync)):
            s = _Sem(es.enter_context(nc.semaphore("sem_" + name)), name)
            self.E[name] = _Eng(name, h, s)
        self.dsems = [_Sem(es.enter_context(nc.semaphore(f"dsem{i}")), f"d{i}") for i in range(ndma)]
        self.dnext = 0
        self.allsems = [e.sem for e in self.E.values()] + self.dsems

    def _need(self, e, ev):
        if ev is None:
            return
        sem, val = ev
        if sem is e.sem and e.name == "pe":
            return
        if e.seen.get(sem.name, 0) >= val:
            return
        e.h.wait_ge(sem.h, val)
        e.seen[sem.name] = val

    def _deps(self, e, reads, writes):
        for b in reads:
            self._need(e, b.w)
        for b in writes:
            self._need(e, b.w)
            for sname, ev in list(b.r.items()):
                self._need(e, ev)

    def _mark(self, ev, reads, writes):
        for b in reads:
            b.r[ev[0].name] = ev
        for b in writes:
            b.w = ev
            b.r = {}

    def op(self, ename, fn, reads=(), writes=()):
        e = self.E[ename]
        self._deps(e, reads, writes)
        ins = fn(e.h)
        e.sem.val += 1
        ins.then_inc(e.sem.h, 1)
        self._mark((e.sem, e.sem.val), reads, writes)

    def dma(self, out, in_, reads=(), writes=(), q="sp", **kw):
        e = self.E[q]
        ds = self.dsems[self.dnext]
        self.dnext = (self.dnext + 1) % len(self.dsems)
        self._need(e, (ds, ds.val))
        self._deps(e, reads, writes)
        ins = e.h.dma_start(out=out, in_=in_, **kw)
        ds.val += 16
        ins.then_inc(ds.h, 16)
        self._mark((ds, ds.val), reads, writes)

    def idma(self, out, in_, idx_ap, gather, reads=(), writes=(), bounds=None):
        e = self.E["pool"]
        ds = self.dsems[self.dnext]
        self.dnext = (self.dnext + 1) % len(self.dsems)
        self._need(e, (ds, ds.val))
        self._deps(e, reads, writes)
        off = bass.IndirectOffsetOnAxis(ap=idx_ap, axis=0)
        if gather:
            ins = e.h.indirect_dma_start(out=out, out_offset=None, in_=in_, in_offset=off)
        else:
            ins = e.h.indirect_dma_start(out=out, out_offset=off, in_=in_, in_offset=None)
        ds.val += 16
        ins.then_inc(ds.h, 16)
        self._mark((ds, ds.val), reads, writes)

    def barrier(self):
        for e in self.E.values():
            for s in self.allsems:
                if s.val > 0:
                    self._need(e, (s, s.val))

    def mm(self, out, lhsT, rhs, start, stop, reads, writes):
        self.op("pe", lambda h: h.matmul(out, lhsT, rhs, start=start, stop=stop), reads, writes)

    def act(self, out, in_, func, reads, writes, **kw):
        self.op("act", lambda h: h.activation(out=out, in_=in_, func=func, **kw), reads, writes)

    def tt(self, eng, out, in0, in1, op, reads, writes):
        self.op(eng, lambda h: h.tensor_tensor(out=out, in0=in0, in1=in1, op=op), reads, writes)

    def ts(self, eng, out, in0, s1, s2, op0, op1, reads, writes):
        if op1 is None:
            self.op(eng, lambda h: h.tensor_scalar(out=out, in0=in0, scalar1=s1, scalar2=None, op0=op0),
                    reads, writes)
        else:
            self.op(eng, lambda h: h.tensor_scalar(out=out, in0=in0, scalar1=s1, scalar2=s2, op0=op0, op1=op1),
                    reads, writes)

    def stt(self, out, in0, scalar, in1, op0, op1, reads, writes):
        self.op("dve", lambda h: h.scalar_tensor_tensor(out=out, in0=in0, scalar=scalar, in1=in1, op0=op0, op1=op1),
                reads, writes)

    def copy(self, eng, out, in_, reads, writes):
        if eng == "act":
            self.op("act", lambda h: h.activation(out=out, in_=in_, func=AF.Copy), reads, writes)
        else:
            self.op(eng, lambda h: h.tensor_copy(out=out, in_=in_), reads, writes)

    def memset(self, eng, ap, val, writes):
        self.op(eng, lambda h: h.memset(ap, val), (), writes)


_UID = [0]


class Pool_:
    def __init__(self, k):
        self.k = k
        self.es = ExitStack()
        self.n = 0

    def __enter__(self):
        self.es.__enter__()
        return self

    def __exit__(self, *a):
        self.k.barrier()
        return self.es.__exit__(*a)

    def sb(self, shape, dt, name=None):
        _UID[0] += 1
        t = self.es.enter_context(self.k.nc.sbuf_tensor(f"{name or 't'}_{_UID[0]}", list(shape), dt))
        return t, Buf(name or "sb")

    def ps(self, name=None, shape=(128, 512), dt=F32):
        _UID[0] += 1
        t = self.es.enter_context(self.k.nc.psum_tensor(f"{name or 'p'}_{_UID[0]}", list(shape), dt))
        return t, Buf(name or "ps")


TOK = NCTX + NLAT


def tiles_for(include_ctx=True):
    out = []
    for s in range(NS):
        if include_ctx:
            out.append((s, 0, NCTX, True))
        for i in range(NLAT // TT):
            out.append((s, NCTX + i * TT, TT, False))
    return out


class Prog:
    def __init__(self, nc, k, dr, stop_after=99):
        self.nc = nc
        self.k = k
        self.dr = dr
        self.stop_after = stop_after

    def setup_consts(self, P):
        k, dr = self.k, self.dr
        self.ones_bf, self.onesB = P.sb([128, 128], BF16, "ones")
        self.id_bf, self.idbB = P.sb([128, 128], BF16, "idb")
        self.id_f, self.idfB = P.sb([128, 128], F32, "idf")
        self.ones_f, self.onesfB = P.sb([128, 128], F32, "onesf")
        self.mod, self.modB = P.sb([128, DEPTH, 48, 3], F32, "mod")
        self.ng, self.ngB = P.sb([128, DEPTH, 2, KC], F32, "ng")
        self.fg, self.fgB = P.sb([128, KC], F32, "fg")
        self.epsb, self.epsB = P.sb([128, 1], F32, "eps")
        k.dma(self.ones_bf[:], dr["ones_bf"][:, :], writes=[self.onesB])
        k.dma(self.id_bf[:], dr["id_bf"][:, :], writes=[self.idbB])
        k.dma(self.id_f[:], dr["id_f"][:, :], writes=[self.idfB])
        k.dma(self.ones_f[:], dr["ones_f"][:, :], writes=[self.onesfB])
        k.dma(self.ng[:], dr["norm_g"][:, :, :, :], writes=[self.ngB])
        k.dma(self.fg[:], dr["final_g"][:, :], writes=[self.fgB])
        k.memset("pool", self.epsb[:], EPS, [self.epsB])

    def adaln(self):
        k, dr = self.k, self.dr
        with Pool_(k) as P:
            cv, cvB = P.sb([128, KC, 3], F32, "cv")
            sc, scB = P.sb([128, KC, 3], F32, "sc")
            ab, abB = P.sb([128, DEPTH, 48], F32, "ab")
            wa = [P.sb([128, KC, 768], F32, f"wa{i}") for i in range(2)]
            ps = [P.ps(f"adaps{i}") for i in range(2)]
            k.dma(cv[:], dr["cvec"][:, :, :], writes=[cvB])
            k.dma(ab[:], dr["ada_b"][:, :, :], writes=[abB])
            k.act(sc[:], cv[:], AF.Silu, [cvB], [scB])
            gi = 0
            for l in range(DEPTH):
                src = dr["ada_w"][l].rearrange("(kc p) n -> p kc n", p=128)
                for g in range(8):
                    wt, wB = wa[gi % 2]
                    pt, pB = ps[gi % 2]
                    gi += 1
                    k.dma(wt[:], src[:, :, g * 768:(g + 1) * 768], writes=[wB])
                    for o in range(6):
                        for kc in range(KC):
                            k.mm(pt[:, o * 3:(o + 1) * 3], wt[:, kc, o * 128:(o + 1) * 128], sc[:, kc, :],
                                 kc == 0, kc == KC - 1, [wB, scB], [pB])
                    pv = pt[:, 0:18].rearrange("p (o j) -> p o j", j=3)
                    for j in range(3):
                        k.tt("dve", self.mod[:, l, g * 6:(g + 1) * 6, j], pv[:, :, j], ab[:, l, g * 6:(g + 1) * 6],
                             ALU.add, [pB, abB], [self.modB])

    def mod_vecs(self, P, l, sub):
        k = self.k
        A, AB = P.sb([128, KC, 3], F32, "modA")
        o = sub * 24
        for j in range(3):
            k.stt(A[:, :, j], self.mod[:, l, o + 8:o + 16, j], 1.0, self.ng[:, l, sub, :], ALU.add, ALU.mult,
                  [self.modB, self.ngB], [AB])
        return A, AB

    def jidx(self, s, is_ctx):
        return 2 if is_ctx else s

    def norm_pass(self, l, sub, include_ctx=True, router=None, final=False, moe=None):
        k, dr = self.k, self.dr
        xs, hs = dr["xs"], dr["hs"]
        tl = tiles_for(include_ctx)
        with Pool_(k) as P:
            if not final:
                A, AB = self.mod_vecs(P, l, sub)
            xt = [P.sb([128, KC, TT], F32, f"xt{i}") for i in range(2)]
            sq, sqB = P.sb([128, KC, TT], BF16, "sq")
            rs, rsB = P.sb([128, TT], F32, "rs")
            hf, hfB = P.sb([128, KC, TT], F32, "hf")
            hb, hbB = P.sb([128, KC, TT], BF16, "hb")
            pss, pssB = P.ps("pss")
            if router is not None:
                wr, wrB = P.sb([128, KC, NEXP], F32, "wr")
                k.dma(wr[:], router, writes=[wrB])
                plg, plgB = P.ps("plg")
                pwt, pwtB = P.ps("pwt")
                lg, lgB = P.sb([128, 4, NEXP], F32, "lg")
                m8, m8B = P.sb([128, 4, 8], F32, "m8")
                nm1, nm1B = P.sb([128, 4], F32, "nm1")
                ee, eeB = P.sb([128, 4, NEXP], F32, "ee")
                mk, mkB = P.sb([128, 4, NEXP], F32, "mk")
                dn, dnB = P.sb([128, 4], F32, "dn")
                ww, wwB = P.sb([128, 4, NEXP], F32, "ww")
                wT, wTB = P.sb([NEXP, TT], F32, "wT")
                if moe is not None:
                    oh, ohB = P.sb([128, 4, 24], F32, "oh")
                    rk, rkB = P.sb([128, 4, NEXP], F32, "rk")
                    t8, t8B = P.sb([128, 4, NEXP], F32, "t8")
                    htk, htkB = P.sb([128, 4, D], BF16, "htk")
                    prk, prkB = P.ps("prk")
                    ptot, ptotB = P.ps("ptot")
                    ptk, ptkB = P.ps("ptk")
                    I32_ = mybir.dt.int32
                    sub_i = 0

            def load(i):
                s, t0, W, c = tl[i]
                t, B = xt[i % 2]
                k.dma(t[:, :, :W], xs[s].rearrange("(kc p) t -> p kc t", p=128)[:, :, t0:t0 + W], writes=[B])

            load(0)
            for i, (s, t0, W, c) in enumerate(tl):
                if i + 1 < len(tl):
                    load(i + 1)
                x, xB = xt[i % 2]
                j = self.jidx(s, c)
                k.act(sq[:, :, :W], x[:, :, :W], AF.Square, [xB], [sqB])
                for kc in range(KC):
                    k.mm(pss[:, :W], self.ones_bf[:], sq[:, kc, :W], kc == 0, kc == KC - 1, [self.onesB, sqB], [pssB])
                k.act(rs[:, :W], pss[:, :W], AF.Sqrt, [pssB, self.epsB], [rsB], scale=1.0 / D, bias=self.epsb[:, 0:1])
                k.op("dve", lambda h: h.reciprocal(out=rs[:, :W], in_=rs[:, :W]), [rsB], [rsB])
                for kc in range(KC):
                    k.tt("dve", hf[:, kc, :W], x[:, kc, :W], rs[:, :W], ALU.mult, [xB, rsB], [hfB])
                if final:
                    for kc in range(KC):
                        k.act(hf[:, kc, :W], hf[:, kc, :W], AF.Identity, [hfB, self.fgB], [hfB], scale=self.fg[:, kc:kc + 1])
                    k.dma(dr["out"][s].rearrange("(kc p) t -> p kc t", p=128)[:, :, t0 - NCTX:t0 - NCTX + W],
                          hf[:, :, :W], reads=[hfB])
                    continue
                o = sub * 24
                for kc in range(KC):
                    k.act(hf[:, kc, :W], hf[:, kc, :W], AF.Identity, [hfB, AB, self.modB], [hfB],
                          scale=A[:, kc, j:j + 1], bias=self.mod[:, l, o + kc, j:j + 1])
                k.copy("pool", hb[:, :, :W], hf[:, :, :W], [hfB], [hbB])
                k.dma(hs[s].rearrange("(kc p) t -> p kc t", p=128)[:, :, t0:t0 + W], hb[:, :, :W], reads=[hbB])
                if router is not None:
                    nst = W // 128
                    for ts_ in range(nst):
                        for kc in range(KC):
                            k.mm(plg[:, ts_ * 8:(ts_ + 1) * 8], hf[:, kc, ts_ * 128:(ts_ + 1) * 128], wr[:, kc, :],
                                 kc == 0, kc == KC - 1, [hfB, wrB], [plgB])
                    k.copy("dve", lg[:, :nst, :], plg[:, 0:nst * 8].rearrange("p (a e) -> p a e", e=8), [plgB], [lgB])
                    for ts_ in range(nst):
                        k.op("dve", lambda h: h.max(out=m8[:, ts_, :], in_=lg[:, ts_, :]), [lgB], [m8B])
                    k.ts("dve", nm1[:, :nst], m8[:, :nst, 0], -1.0, None, ALU.mult, None, [m8B], [nm1B])
                    for ts_ in range(nst):
                        k.act(ee[:, ts_, :], lg[:, ts_, :], AF.Exp, [lgB, nm1B], [eeB], bias=nm1[:, ts_:ts_ + 1])
                        k.ts("dve", mk[:, ts_, :], lg[:, ts_, :], m8[:, ts_, 1:2], None, ALU.is_ge, None,
                             [lgB, m8B], [mkB])
                        if moe is not None:
                            k.ts("dve", oh[:, ts_, 0:8], lg[:, ts_, :], m8[:, ts_, 0:1], None, ALU.is_equal, None, [lgB, m8B], [ohB])
                            k.ts("dve", oh[:, ts_, 8:16], lg[:, ts_, :], m8[:, ts_, 1:2], None, ALU.is_equal, None, [lgB, m8B], [ohB])
                    k.tt("dve", ee[:, :nst, :], ee[:, :nst, :], mk[:, :nst, :], ALU.mult, [eeB, mkB], [eeB])
                    k.op("dve", lambda h: h.reduce_sum(out=dn[:, :nst], in_=ee[:, :nst, :], axis=mybir.AxisListType.X),
                         [eeB], [dnB])
                    k.op("dve", lambda h: h.reciprocal(out=dn[:, :nst], in_=dn[:, :nst]), [dnB], [dnB])
                    for ts_ in range(nst):
                        k.ts("dve", ww[:, ts_, :], ee[:, ts_, :], dn[:, ts_:ts_ + 1], None, ALU.mult, None,
                             [eeB, dnB], [wwB])
                        k.op("pe", lambda h: h.transpose(pwt[0:NEXP, ts_ * 128:(ts_ + 1) * 128], ww[:, ts_, :], self.id_f[:]),
                             [wwB, self.idfB], [pwtB])
                    k.copy("dve", wT[:, :W], pwt[0:NEXP, :W], [pwtB], [wTB])
                    k.dma(dr["wexp"][s, :, t0:t0 + W], wT[:, :W], reads=[wTB])
                    if moe is not None:
                        cum, cumB = moe["cum"]
                        for ts_ in range(nst):
                            k.mm(prk[:, 0:8], moe["tris"][0][:], mk[:, ts_, :], True, True, [moe["tris"][1], mkB], [prkB])
                            k.mm(ptot[:, 0:8], self.ones_f[:], mk[:, ts_, :], True, True, [self.onesfB, mkB], [ptotB])
                            k.tt("dve", rk[:, ts_, :], prk[:, 0:8], cum[:], ALU.add, [prkB, cumB], [rkB])
                            k.tt("dve", cum[:], cum[:], ptot[:, 0:8], ALU.add, [cumB, ptotB], [cumB])
                        for (src_, so, do) in ((rk, 0, 16), (rk, 8, 17), (ww, 0, 18), (ww, 8, 19)):
                            k.tt("dve", t8[:, :nst, :], oh[:, :nst, so:so + 8], src_[:, :nst, :], ALU.mult, [ohB, rkB, wwB], [t8B])
                            k.op("dve", lambda h: h.reduce_sum(out=oh[:, :nst, do], in_=t8[:, :nst, :], axis=mybir.AxisListType.X),
                                 [t8B], [ohB])
                        k.dma(dr["rinfo"][s, t0:t0 + W, :].rearrange("(a p) c -> p a c", p=128), oh[:, :nst, :], reads=[ohB])
                        for ts_ in range(nst):
                            for kc in range(KC):
                                k.op("pe", lambda h: h.transpose(ptk[:, :].bitcast(BF16)[:, kc * 128:(kc + 1) * 128],
                                                                 hb[:, kc, ts_ * 128:(ts_ + 1) * 128], self.id_bf[:]),
                                     [hbB, self.idbB], [ptkB])
                            if ts_ % 2 == 0:
                                k.copy("act", htk[:, ts_, :], ptk[:, :].bitcast(BF16)[:, 0:D], [ptkB], [htkB])
                            else:
                                k.copy("dve", htk[:, ts_, :], ptk[:, :].bitcast(BF16)[:, 0:D], [ptkB], [htkB])
                        k.dma(dr["htok"][s, t0:t0 + W, :].rearrange("(a p) d -> p a d", p=128), htk[:, :nst, :], reads=[htkB])

    def ffn_pass(self, l, w1, w3, w2, F, include_ctx=True, expert=None):
        k, dr = self.k, self.dr
        xs, hs = dr["xs"], dr["hs"]
        FC = F // 128
        tl = tiles_for(include_ctx)
        with Pool_(k) as P:
            W1, W1B = P.sb([128, KC, F], BF16, "W1")
            W3, W3B = P.sb([128, KC, F], BF16, "W3")
            W2, W2B = P.sb([128, FC, D], BF16, "W2")
            k.dma(W1[:], w1.rearrange("(kc p) f -> p kc f", p=128), writes=[W1B], q="pool")
            k.dma(W3[:], w3.rearrange("(kc p) f -> p kc f", p=128), writes=[W3B], q="pool")
            k.dma(W2[:], w2.rearrange("(fc p) d -> p fc d", p=128), writes=[W2B], q="pool")
            ht = [P.sb([128, KC, TT], BF16, f"ht{i}") for i in range(2)]
            wb = [P.sb([128, TT], F32, f"wbc{i}") for i in range(2)] if expert is not None else None
            g, gB = P.sb([128, FC, TT], BF16, "g")
            gBs = [Buf(f"g{f}") for f in range(FC)]
            sa = [P.sb([128, TT], F32, f"sa{i}") for i in range(2)]
            yt = [P.sb([128, TT], F32, f"yt{i}") for i in range(2)]
            pa = [P.ps(f"pa{i}") for i in range(2)]
            pb = [P.ps(f"pb{i}") for i in range(2)]
            py = [P.ps(f"py{i}") for i in range(2)]

            def load(i):
                s, t0, W, c = tl[i]
                t, B = ht[i % 2]
                k.dma(t[:, :, :W], hs[s].rearrange("(kc p) t -> p kc t", p=128)[:, :, t0:t0 + W], writes=[B])
                if expert is not None:
                    wt_, wB_ = wb[i % 2]
                    k.dma(wt_[:, :W], dr["wexp"][s, expert:expert + 1, t0:t0 + W].partition_broadcast(128), writes=[wB_])

            load(0)
            cnt = 0
            ycnt = 0
            for i, (s, t0, W, c) in enumerate(tl):
                if i + 1 < len(tl):
                    load(i + 1)
                h, hB = ht[i % 2]
                j = self.jidx(s, c)
                for f in range(FC):
                    a_, aB = pa[cnt % 2]
                    b_, bB = pb[cnt % 2]
                    s_, sB = sa[cnt % 2]
                    cnt += 1
                    for kc in range(KC):
                        k.mm(a_[:, :W], W1[:, kc, f * 128:(f + 1) * 128], h[:, kc, :W], kc == 0, kc == KC - 1, [W1B, hB], [aB])
                    for kc in range(KC):
                        k.mm(b_[:, :W], W3[:, kc, f * 128:(f + 1) * 128], h[:, kc, :W], kc == 0, kc == KC - 1, [W3B, hB], [bB])
                    k.act(s_[:, :W], a_[:, :W], AF.Silu, [aB], [sB])
                    if expert is not None:
                        wt_, wB_ = wb[i % 2]
                        k.tt("pool", s_[:, :W], s_[:, :W], wt_[:, :W], ALU.mult, [sB, wB_], [sB])
                    k.tt("dve", g[:, f, :W], b_[:, :W], s_[:, :W], ALU.mult, [bB, sB], [gBs[f]])
                o = 24 + 16 + 0
                for oc in range(KC):
                    y_, yB = py[ycnt % 2]
                    yo, yoB = yt[ycnt % 2]
                    ycnt += 1
                    for f in range(FC):
                        k.mm(y_[:, :W], W2[:, f, oc * 128:(oc + 1) * 128], g[:, f, :W], f == 0, f == FC - 1, [W2B, gBs[f]], [yB])
                    k.ts("dve", yo[:, :W], y_[:, :W], self.mod[:, l, 40 + oc, j:j + 1], None, ALU.mult, None,
                         [yB, self.modB], [yoB])
                    k.dma(xs[s, oc * 128:(oc + 1) * 128, t0:t0 + W], yo[:, :W], reads=[yoB], q="pool", accum_op=ALU.add)

    def outproj_pass(self, l, src, w, include_ctx, gate_off, extra_scale=None):
        k, dr = self.k, self.dr
        xs = dr["xs"]
        tl = tiles_for(include_ctx)
        with Pool_(k) as P:
            Wo, WoB = P.sb([128, KC, D], BF16, "Wo")
            k.dma(Wo[:], w.rearrange("(kc p) d -> p kc d", p=128), writes=[WoB], q="pool")
            ot = [P.sb([128, KC, TT], BF16, f"ot{i}") for i in range(2)]
            yt = [P.sb([128, TT], F32, f"yt{i}") for i in range(2)]
            py = [P.ps(f"py{i}") for i in range(2)]

            def load(i):
                s, t0, W, c = tl[i]
                t, B = ot[i % 2]
                k.dma(t[:, :, :W], src[s].rearrange("(kc p) t -> p kc t", p=128)[:, :, t0:t0 + W], writes=[B])

            load(0)
            ycnt = 0
            for i, (s, t0, W, c) in enumerate(tl):
                if i + 1 < len(tl):
                    load(i + 1)
                o_, oB = ot[i % 2]
                j = self.jidx(s, c)
                for oc in range(KC):
                    y_, yB = py[ycnt % 2]
                    yo, yoB = yt[ycnt % 2]
                    ycnt += 1
                    for kc in range(KC):
                        k.mm(y_[:, :W], Wo[:, kc, oc * 128:(oc + 1) * 128], o_[:, kc, :W], kc == 0, kc == KC - 1, [WoB, oB], [yB])
                    if extra_scale is None:
                        k.ts("dve", yo[:, :W], y_[:, :W], self.mod[:, l, gate_off + oc, j:j + 1], None, ALU.mult, None,
                             [yB, self.modB], [yoB])
                    else:
                        est, esB = extra_scale
                        k.ts("dve", yo[:, :W], y_[:, :W], self.mod[:, l, gate_off + oc, j:j + 1], est[:, oc:oc + 1],
                             ALU.mult, ALU.mult, [yB, self.modB, esB], [yoB])
                    k.dma(xs[s, oc * 128:(oc + 1) * 128, t0:t0 + W], yo[:, :W], reads=[yoB], q="pool", accum_op=ALU.add)

    def na_qkv_pass(self, wqkv):
        k, dr = self.k, self.dr
        hs, qs, ks, vs = dr["hs"], dr["qs"], dr["ks"], dr["vs"]
        tl = tiles_for(True)
        with Pool_(k) as P:
            Wq, WqB = P.sb([128, KC, 3 * D], BF16, "Wqkv")
            src = wqkv.rearrange("(kc p) n -> p kc n", p=128)
            for i in range(3):
                k.dma(Wq[:, :, i * D:(i + 1) * D], src[:, :, i * D:(i + 1) * D], writes=[WqB], q="pool")
            ht = [P.sb([128, KC, TT], BF16, f"ht{i}") for i in range(2)]
            qk = [P.sb([128, 2 * KC, TT], BF16, f"qk{i}") for i in range(2)]
            vt = [P.sb([128, 4, D], BF16, f"vt{i}") for i in range(2)]
            pp = [P.ps(f"pp{i}") for i in range(4)]

            def load(i):
                s, t0, W, c = tl[i]
                t, B = ht[i % 2]
                k.dma(t[:, :, :W], hs[s].rearrange("(kc p) t -> p kc t", p=128)[:, :, t0:t0 + W], writes=[B])

            load(0)
            cnt = 0
            for i, (s, t0, W, c) in enumerate(tl):
                if i + 1 < len(tl):
                    load(i + 1)
                h, hB = ht[i % 2]
                q_, qB = qk[i % 2]
                v_, vB = vt[i % 2]
                for oc in range(2 * KC):
                    p_, pB = pp[cnt % 4]
                    for kc in range(KC):
                        k.mm(p_[:, :W], Wq[:, kc, oc * 128:(oc + 1) * 128], h[:, kc, :W], kc == 0, kc == KC - 1, [WqB, hB], [pB])
                    if cnt % 2 == 0:
                        k.act(q_[:, oc, :W], p_[:, :W], AF.Copy, [pB], [qB], scale=(0.125 if oc < KC else 1.0))
                    else:
                        k.ts("dve", q_[:, oc, :W], p_[:, :W], (0.125 if oc < KC else 1.0), None, ALU.mult, None, [pB], [qB])
                    cnt += 1
                for ts_ in range(W // 128):
                    for hf_ in range(2):
                        p_, pB = pp[cnt % 4]
                        for kc in range(KC):
                            k.mm(p_[:, :], h[:, kc, ts_ * 128:(ts_ + 1) * 128], Wq[:, kc, 2 * D + hf_ * 512:2 * D + (hf_ + 1) * 512],
                                 kc == 0, kc == KC - 1, [WqB, hB], [pB])
                        if cnt % 2 == 0:
                            k.act(v_[:, ts_, hf_ * 512:(hf_ + 1) * 512], p_[:, :], AF.Copy, [pB], [vB])
                        else:
                            k.copy("dve", v_[:, ts_, hf_ * 512:(hf_ + 1) * 512], p_[:, :], [pB], [vB])
                        cnt += 1
                k.dma(qs[s].rearrange("(kc p) t -> p kc t", p=128)[:, :, t0:t0 + W], q_[:, 0:KC, :W], reads=[qB])
                k.dma(ks[s].rearrange("(kc p) t -> p kc t", p=128)[:, :, t0:t0 + W], q_[:, KC:2 * KC, :W], reads=[qB])
                k.dma(vs[s, t0:t0 + W, :].rearrange("(a p) f -> p a f", p=128), v_[:, :W // 128, :], reads=[vB])

    def na_attn_pass(self, tab, need_ctx):
        k, dr = self.k, self.dr
        qs, ks, vs, os_ = dr["qs"], dr["ks"], dr["vs"], dr["os"]
        NCH = TOK // 128
        with Pool_(k) as P:
            sets = []
            for i_ in range(2):
                sets.append((P.sb([128, TOK], BF16, f"qT{i_}"), P.sb([128, TOK], BF16, f"kT{i_}"),
                             P.sb([128, NCH, 128], BF16, f"vv{i_}"), P.sb([128, 2, 2, 22, 64], F32, f"tb{i_}")))

            def ld(ix):
                s_, hp_ = ix // 8, ix % 8
                (a0, a0B), (a1, a1B), (a2, a2B), (a3, a3B) = sets[ix % 2]
                k.dma(a0[:], qs[s_, hp_ * 128:(hp_ + 1) * 128, :], writes=[a0B])
                k.dma(a1[:], ks[s_, hp_ * 128:(hp_ + 1) * 128, :], writes=[a1B])
                k.dma(a2[:], vs[s_, :, hp_ * 128:(hp_ + 1) * 128].rearrange("(a p) f -> p a f", p=128), writes=[a2B])
                k.dma(a3[:], tab[hp_], writes=[a3B])
            negt, negB = P.sb([128, 256], F32, "negt")
            k.memset("pool", negt[:], NEG, [negB])
            sbm = [P.sb([128, TT], F32, f"sbm{i}") for i in range(2)]
            pt = [P.sb([128, TT], BF16, f"pt{i}") for i in range(3)]
            rd, rdB = P.sb([128, TT], F32, "rd")
            ot = [P.sb([128, TT], BF16, f"ot{i}") for i in range(2)]
            pS = [P.ps(f"pS{i}") for i in range(3)]
            pO = [P.ps(f"pO{i}") for i in range(2)]
            pD = [P.ps(f"pD{i}") for i in range(2)]
            scnt = 0
            tcnt = 0
            ld(0)
            for s in range(NS):
                for hp in range(8):
                    ix_ = s * 8 + hp
                    if ix_ + 1 < NS * 8:
                        ld(ix_ + 1)
                    (qT, qTB), (kT, kTB), (vv, vvB), (tb, tbB) = sets[ix_ % 2]
                    qtiles = ([(0, NCTX, -1)] if need_ctx else []) + [(NCTX + ti * TT, TT, ti) for ti in range(8)]
                    for (t0, W, ti) in qtiles:
                        O_, OB = pO[tcnt % 2]
                        D_, DB = pD[tcnt % 2]
                        o_, oB = ot[tcnt % 2]
                        tcnt += 1
                        if ti < 0:
                            chunks = [(0, None), (1, None)]
                        else:
                            lo, hi = max(0, 4 * ti - 2), min(31, 4 * ti + 5)
                            chunks = [(2 + c, c) for c in range(lo, hi + 1)] + [(0, None), (1, None)]
                        steps = [(hd, ci, kch, c) for hd in range(2) for ci, (kch, c) in enumerate(chunks)]
                        slots = {}

                        def emitS(st):
                            nonlocal scnt
                            hd, ci, kch, c = st
                            pb_ = 64 * hd
                            S_, SB = pS[scnt % 3]
                            p_, pB = pt[scnt % 3]
                            m_, mB = sbm[scnt % 2]
                            scnt += 1
                            slots[st] = (p_, pB)
                            k.mm(S_[:, :W], kT[pb_:pb_ + 64, kch * 128:(kch + 1) * 128], qT[pb_:pb_ + 64, t0:t0 + W],
                                 True, True, [kTB, qTB], [SB])
                            if c is None:
                                k.act(p_[:, :W], S_[:, :W], AF.Exp, [SB], [pB])
                                return
                            R = 8 * ti
                            J0 = 10 - 2 * c + R
                            if ti == 0:
                                segs = [(0, 4, "U" if c <= 3 else "N"), (4, 8, "T")]
                            elif ti == 7:
                                segs = [(0, 5, "T"), (5, 8, "U" if c >= 28 else "N")]
                            else:
                                segs = [(0, 8, "T")]
                            for (b0, b1, kind) in segs:
                                c0, c1 = b0 * 64, b1 * 64
                                if kind == "N":
                                    in1 = negt[:, 0:c1 - c0]
                                    rb = [negB]
                                else:
                                    tix = 0 if kind == "T" else 1
                                    in1 = tb[:, tix, hd, J0 + b0:J0 + b1, :].rearrange("p a b -> p (a b)")
                                    rb = [tbB]
                                k.tt("dve", m_[:, c0:c1], S_[:, c0:c1], in1, ALU.add, [SB] + rb, [mB])
                            k.act(p_[:, :W], m_[:, :W], AF.Exp, [mB], [pB])

                        def emitO(st):
                            hd, ci, kch, c = st
                            pb_ = 64 * hd
                            p_, pB = slots.pop(st)
                            first, last = ci == 0, ci == len(chunks) - 1
                            k.mm(O_[pb_:pb_ + 64, :W], vv[:, kch, pb_:pb_ + 64], p_[:, :W], first, last, [vvB, pB], [OB])
                            k.mm(D_[pb_:pb_ + 64, :W], self.ones_bf[:, 0:64], p_[:, :W], first, last, [self.onesB, pB], [DB])

                        LA = 2
                        for i_ in range(len(steps) + LA):
                            if i_ < len(steps):
                                emitS(steps[i_])
                            if i_ - LA >= 0:
                                emitO(steps[i_ - LA])
                        k.op("dve", lambda h: h.reciprocal(out=rd[:, :W], in_=D_[:, :W]), [DB], [rdB])
                        k.tt("dve", o_[:, :W], O_[:, :W], rd[:, :W], ALU.mult, [OB, rdB], [oB])
                        k.dma(os_[s, hp * 128:(hp + 1) * 128, t0:t0 + W], o_[:, :W], reads=[oB])

    def pool_pass(self, l, pw, include_ctx=True):
        k, dr = self.k, self.dr
        xs, hs = dr["xs"], dr["hs"]
        tl = tiles_for(include_ctx)
        PADW = 80
        with Pool_(k) as P:
            Wp, WpB = P.sb([128, 4, 2, 256], BF16, "Wp")
            k.dma(Wp[:], pw.rearrange("g (kc p) f -> p g kc f", p=128), writes=[WpB], q="pool")
            ls, lsB = P.sb([128, KC], F32, "ls")
            k.dma(ls[:], dr["pool_scale"][:, :], writes=[lsB])
            ic, icB = P.sb([128, 4, 2, 256], F32, "ic")
            k.dma(ic[:], dr["pool_ic"][:, :, :, :], writes=[icB])
            ht = [P.sb([128, KC, TT], BF16, f"ht{i}") for i in range(2)]
            lv = [P.sb([128, 8 * PADW], F32, f"lv{i}") for i in range(3)]
            lvc = [P.sb([128, 272], F32, f"lvc{i}") for i in range(3)]
            pl, plB = P.sb([128, KC, TT], BF16, "pl")
            plBs = [Buf(f"pl{i}") for i in range(KC)]
            mn, mnB = P.sb([128, TT], F32, "mn")
            yt = [P.sb([128, TT], F32, f"yt{i}") for i in range(2)]
            py = [P.ps(f"py{i}") for i in range(2)]
            for t_, B_ in lv + lvc:
                k.memset("pool", t_[:], 0.0, [B_])

            def load(i):
                s, t0, W, c = tl[i]
                t, B = ht[i % 2]
                k.dma(t[:, :, :W], hs[s].rearrange("(kc p) t -> p kc t", p=128)[:, :, t0:t0 + W], writes=[B])

            load(0)
            ycnt = 0
            for i, (s, t0, W, c) in enumerate(tl):
                if i + 1 < len(tl):
                    load(i + 1)
                h, hB = ht[i % 2]
                j = self.jidx(s, c)
                rows, rl, pw_, ri = (1, 256, 272, 1) if c else (8, 64, PADW, 0)

                def view(t, off, n=rl):
                    return t[:, 0:rows * pw_].rearrange("p (r w) -> p r w", w=pw_)[:, :, 8 + off:8 + off + n]

                for kc in range(KC):
                    gi = kc // 2
                    win = (2, 4, 8, 16)[gi]
                    a_, aB = (lvc if c else lv)[0]
                    b_, bB = (lvc if c else lv)[1]
                    c_, cB = (lvc if c else lv)[2]
                    hv = h[:, kc, :W].rearrange("p (r w) -> p r w", w=rl)
                    k.copy("pool", view(a_, 0), hv, [hB], [aB])
                    n2 = rl + 14
                    k.tt("pool", view(b_, -7, n2), view(a_, -8, n2), view(a_, -7, n2), ALU.add, [aB], [bB])
                    cur, curB = b_, bB
                    if win >= 4:
                        n4 = rl + 12
                        k.tt("pool", view(c_, -6, n4), view(b_, -7, n4), view(b_, -5, n4), ALU.add, [bB], [cB])
                        cur, curB = c_, cB
                    if win >= 8:
                        n8 = rl + 8
                        k.tt("dve", view(b_, -4, n8), view(c_, -6, n8), view(c_, -2, n8), ALU.add, [cB], [bB])
                        cur, curB = b_, bB
                    if win >= 16:
                        k.tt("dve", view(c_, 0), view(b_, -4), view(b_, 4), ALU.add, [bB], [cB])
                        cur, curB = c_, cB
                    mv = mn[:, :W].rearrange("p (r w) -> p r w", w=rl)
                    icv = ic[:, gi, ri, 0:rl]
                    for r in range(rows):
                        k.tt("dve", mv[:, r, :], view(cur, 0)[:, r, :], icv, ALU.mult, [curB, icB], [mnB])
                    k.tt("dve", pl[:, kc, :W], mn[:, :W], h[:, kc, :W], ALU.subtract, [mnB, hB], [plBs[kc]])
                for oc in range(KC):
                    gi = oc // 2
                    y_, yB = py[ycnt % 2]
                    yo, yoB = yt[ycnt % 2]
                    ycnt += 1
                    for kk in range(2):
                        k.mm(y_[:, :W], Wp[:, gi, kk, (oc % 2) * 128:(oc % 2 + 1) * 128], pl[:, 2 * gi + kk, :W], kk == 0, kk == 1,
                             [WpB, plBs[2 * gi + kk]], [yB])
                    k.ts("dve", yo[:, :W], y_[:, :W], self.mod[:, l, 16 + oc, j:j + 1], ls[:, oc:oc + 1], ALU.mult, ALU.mult,
                         [yB, self.modB, lsB], [yoB])
                    k.dma(xs[s, oc * 128:(oc + 1) * 128, t0:t0 + W], yo[:, :W], reads=[yoB], q="pool", accum_op=ALU.add)


BLK = 512
GF = 896
NG = DFE // GF
I32 = mybir.dt.int32


def moe_layer(pg, l, f, include_ctx):
    k, dr = pg.k, pg.dr
    tl = tiles_for(include_ctx)
    subt = [(s, t0 + a * 128, c) for (s, t0, W, c) in tl for a in range(W // 128)]
    NSUB = len(subt)
    NB = NSUB * 2 * 128 // BLK + NEXP
    w1f = dr[f"moe_w1_{f}"].rearrange("e k (g c) -> (e k g) c", c=GF)
    w3f = dr[f"moe_w3_{f}"].rearrange("e k (g c) -> (e k g) c", c=GF)
    w2f = dr[f"moe_w2_{f}"].rearrange("e f d -> (e f) d")
    hbuf, obuf = dr["hbuf"], dr["obuf"]
    with Pool_(k) as PM:
        cum = PM.sb([128, NEXP], F32, "cum")
        tris = PM.sb([128, 128], F32, "tris")
        starts = PM.sb([128, NEXP], F32, "starts")
        posall = PM.sb([128, NSUB, 2], I32, "posall")
        pwall = PM.sb([128, NSUB, 2], F32, "pwall")
        widx1 = PM.sb([128, NB, NG, KC], I32, "widx1")
        widx2 = PM.sb([128, NB, 28], I32, "widx2")
        k.memset("pool", cum[0][:], 0.0, [cum[1]])
        k.dma(tris[0][:], dr["tris"][:, :], writes=[tris[1]])
        pg.norm_pass(l, 1, include_ctx=include_ctx, router=dr["moe_router"][f], moe={"cum": cum, "tris": tris})
        with Pool_(k) as P:
            thr, thrB = P.sb([128, 48], F32, "thr"); k.dma(thr[:], dr["thr48"][:, :], writes=[thrB])
            io, ioB = P.sb([128, 48], F32, "io"); k.dma(io[:], dr["iota48"][:, :], writes=[ioB])
            pk1, pk1B = P.sb([128, KC], F32, "pk1"); k.dma(pk1[:], dr["pk1"][:, :], writes=[pk1B])
            pk2, pk2B = P.sb([128, 28], F32, "pk2"); k.dma(pk2[:], dr["pk2"][:, :], writes=[pk2B])
            tmp, tmpB = P.sb([128, 48], F32, "tmp")
            nblk, nblkB = P.sb([128, NEXP], F32, "nblk")
            sblk, sblkB = P.sb([128, NEXP], F32, "sblk")
            eb, ebB = P.sb([128, 48], F32, "eb")
            w1f_, w1fB = P.sb([128, NB, NG, KC], F32, "w1f")
            w2f_, w2fB = P.sb([128, NB, 28], F32, "w2f")
            for e in range(NEXP):
                k.ts("dve", tmp[:], thr[:], cum[0][:, e:e + 1], None, ALU.is_lt, None, [thrB, cum[1]], [tmpB])
                k.op("dve", lambda h: h.reduce_sum(out=nblk[:, e:e + 1], in_=tmp[:], axis=mybir.AxisListType.X), [tmpB], [nblkB])
            k.memset("pool", sblk[:], 0.0, [sblkB])
            for e in range(1, NEXP):
                k.tt("dve", sblk[:, e:e + 1], sblk[:, e - 1:e], nblk[:, e - 1:e], ALU.add, [sblkB, nblkB], [sblkB])
            k.ts("dve", starts[0][:], sblk[:], float(BLK), None, ALU.mult, None, [sblkB], [starts[1]])
            k.memset("pool", eb[:], -1.0, [ebB])
            for e in range(NEXP):
                k.ts("dve", tmp[:], io[:], sblk[:, e:e + 1], None, ALU.is_ge, None, [ioB, sblkB], [tmpB])
                k.tt("dve", eb[:], eb[:], tmp[:], ALU.add, [ebB, tmpB], [ebB])
            e4, e4B = P.sb([128, 48], F32, "e4")
            e35, e35B = P.sb([128, 48], F32, "e35")
            k.ts("dve", e4[:], eb[:], 4096.0, None, ALU.mult, None, [ebB], [e4B])
            k.ts("dve", e35[:], eb[:], float(DFE), None, ALU.mult, None, [ebB], [e35B])
            for g in range(NG):
                for kc in range(KC):
                    k.ts("dve", w1f_[:, :, g, kc], e4[:, :NB], pk1[:, kc:kc + 1], None, ALU.add, None, [e4B, pk1B], [w1fB])
            for g in range(1, NG):
                k.ts("dve", w1f_[:, :, g, :], w1f_[:, :, g, :], float(g), None, ALU.add, None, [w1fB], [w1fB])
            for fc in range(28):
                k.ts("dve", w2f_[:, :, fc], e35[:, :NB], pk2[:, fc:fc + 1], None, ALU.add, None, [e35B, pk2B], [w2fB])
            k.copy("dve", widx1[0][:], w1f_[:], [w1fB], [widx1[1]])
            k.copy("dve", widx2[0][:], w2f_[:], [w2fB], [widx2[1]])
            if "widx_dbg" in dr:
                k.dma(dr["widx_dbg"][:, 0:NB * NG * KC], widx1[0][:].rearrange("p a b c -> p (a b c)"), reads=[widx1[1]])
        with Pool_(k) as P:
            z, zB = P.sb([128, 4, D], BF16, "z")
            k.memset("pool", z[:], 0.0, [zB])
            for b in range(NB):
                k.dma(hbuf[b * BLK:(b + 1) * BLK, :].rearrange("(a p) d -> p a d", p=128), z[:], reads=[zB])
            k.barrier()
            ht = [P.sb([128, D], BF16, f"sht{i}") for i in range(3)]
            inf = [P.sb([128, 24], F32, f"inf{i}") for i in range(3)]
            t8, t8B = P.sb([128, 2, NEXP], F32, "t8")
            pf, pfB = P.sb([128, 2], F32, "pf")

            def loadi(i):
                s, tk, c = subt[i]
                k.dma(inf[i % 3][0][:], dr["rinfo"][s, tk:tk + 128, :], writes=[inf[i % 3][1]])

            def loadh(i):
                s, tk, c = subt[i]
                k.dma(ht[i % 3][0][:], dr["htok"][s, tk:tk + 128, :], writes=[ht[i % 3][1]])

            loadi(0)
            for i in range(NSUB):
                if i + 1 < NSUB:
                    loadi(i + 1)
                n_, nB_ = inf[i % 3]
                k.tt("dve", t8[:, 0, :], n_[:, 0:8], starts[0][:], ALU.mult, [nB_, starts[1]], [t8B])
                k.tt("dve", t8[:, 1, :], n_[:, 8:16], starts[0][:], ALU.mult, [nB_, starts[1]], [t8B])
                k.op("dve", lambda h: h.reduce_sum(out=pf[:, 0:2], in_=t8[:, :, :], axis=mybir.AxisListType.X), [t8B], [pfB])
                k.tt("dve", pf[:], pf[:], n_[:, 16:18], ALU.add, [pfB, nB_], [pfB])
                k.copy("dve", posall[0][:, i, :], pf[:], [pfB], [posall[1]])
                k.copy("dve", pwall[0][:, i, :], n_[:, 18:20], [nB_], [pwall[1]])
            k.barrier()
            loadh(0)
            for i in range(NSUB):
                if i + 1 < NSUB:
                    loadh(i + 1)
                h_, hB_ = ht[i % 3]
                for j in range(2):
                    k.idma(hbuf[:, :], h_[:], posall[0][:, i, j:j + 1], gather=False, reads=[hB_, posall[1]])
        with Pool_(k) as P:
            Wg = []
            for i in range(2):
                Wg.append(((P.sb([128, KC, GF], BF16, f"W1g{i}")[0], [Buf() for _ in range(KC)]),
                           (P.sb([128, KC, GF], BF16, f"W3g{i}")[0], [Buf() for _ in range(KC)]),
                           (P.sb([128, 7, D], BF16, f"W2g{i}")[0], [Buf() for _ in range(7)])))
            hblk = [P.sb([128, 4, D], BF16, f"hblk{i}") for i in range(1)] * 2
            hT = [P.sb([128, KC, BLK], BF16, f"hT{i}") for i in range(2)]
            gT = [P.sb([128, 7, BLK], BF16, f"gT{i}") for i in range(2)]
            yacc = [P.sb([128, 4, D], F32, f"yacc{i}") for i in range(1)] * 2
            sa = [P.sb([128, BLK], F32, f"sa{i}") for i in range(2)]
            pa = [P.ps(f"pa{i}") for i in range(2)]
            pb = [P.ps(f"pb{i}") for i in range(2)]
            py = [P.ps(f"py{i}") for i in range(2)]
            pth, pthB = P.ps("pth")
            groups = [(b, g) for b in range(NB) for g in range(NG)]

            def wload(gi):
                b, g = groups[gi]
                (W1, W1B), (W3, W3B), (W2, W2B) = Wg[gi % 2]
                for kc in range(KC):
                    k.idma(W1[:, kc, :], w1f[:, :], widx1[0][:, b, g, kc:kc + 1], gather=True, reads=[widx1[1]], writes=[W1B[kc]])
                    k.idma(W3[:, kc, :], w3f[:, :], widx1[0][:, b, g, kc:kc + 1], gather=True, reads=[widx1[1]], writes=[W3B[kc]])
                for j in range(7):
                    k.idma(W2[:, j, :], w2f[:, :], widx2[0][:, b, g * 7 + j:g * 7 + j + 1], gather=True, reads=[widx2[1]], writes=[W2B[j]])

            def hload(b):
                k.dma(hblk[b % 2][0][:], hbuf[b * BLK:(b + 1) * BLK, :].rearrange("(a p) d -> p a d", p=128), writes=[hblk[b % 2][1]])

            wload(0)
            hload(0)
            cnt = 0
            ycnt = 0
            for gi, (b, g) in enumerate(groups):
                if gi + 1 < len(groups):
                    wload(gi + 1)
                (W1, W1B), (W3, W3B), (W2, W2B) = Wg[gi % 2]
                h_, hB_ = hT[b % 2]
                ya, yaB = yacc[b % 2]
                if g == 0:
                    hb_, hbB_ = hblk[b % 2]
                    for kc in range(KC):
                        for a in range(4):
                            k.op("pe", lambda h: h.transpose(pth[:, 0:256].bitcast(BF16)[:, a * 128:(a + 1) * 128],
                                                             hb_[:, a, kc * 128:(kc + 1) * 128], pg.id_bf[:]), [hbB_, pg.idbB], [pthB])
                        if kc % 2 == 0:
                            k.copy("act", h_[:, kc, :], pth[:, 0:256].bitcast(BF16)[:, 0:BLK], [pthB], [hB_])
                        else:
                            k.copy("dve", h_[:, kc, :], pth[:, 0:256].bitcast(BF16)[:, 0:BLK], [pthB], [hB_])
                    if b + 1 < NB:
                        hload(b + 1)
                g_, gB_ = gT[gi % 2]
                for j in range(7):
                    a_, aB = pa[cnt % 2]; b_, bB = pb[cnt % 2]; s_, sB = sa[cnt % 2]
                    cnt += 1
                    for kc in range(KC):
                        k.mm(a_[:, :], W1[:, kc, j * 128:(j + 1) * 128], h_[:, kc, :], kc == 0, kc == KC - 1, [W1B[kc], hB_], [aB])
                    for kc in range(KC):
                        k.mm(b_[:, :], W3[:, kc, j * 128:(j + 1) * 128], h_[:, kc, :], kc == 0, kc == KC - 1, [W3B[kc], hB_], [bB])
                    k.act(s_[:], a_[:, :], AF.Silu, [aB], [sB])
                    k.tt("dve", g_[:, j, :], b_[:, :], s_[:], ALU.mult, [bB, sB], [gB_])
                for a in range(4):
                    for hf_ in range(2):
                        y_, yB = py[ycnt % 2]
                        ycnt += 1
                        for j in range(7):
                            k.mm(y_[:, :], g_[:, j, a * 128:(a + 1) * 128], W2[:, j, hf_ * 512:(hf_ + 1) * 512], j == 0, j == 6, [gB_, W2B[j]], [yB])
                        dst = ya[:, a, hf_ * 512:(hf_ + 1) * 512]
                        if g == 0:
                            k.copy("act", dst, y_[:, :], [yB], [yaB])
                        else:
                            k.tt("dve", dst, dst, y_[:, :], ALU.add, [yaB, yB], [yaB])
                if g == NG - 1:
                    k.dma(obuf[b * BLK:(b + 1) * BLK, :].rearrange("(a p) d -> p a d", p=128), ya[:], reads=[yaB])
        with Pool_(k) as P:
            r1 = [P.sb([128, D], F32, f"r1_{i}") for i in range(2)]
            r2 = [P.sb([128, D], F32, f"r2_{i}") for i in range(2)]
            yo = [P.sb([128, KC, 128], F32, f"yo{i}") for i in range(2)]
            pt = [P.ps(f"pt{i}") for i in range(2)]

            def gload(i):
                k.idma(r1[i % 2][0][:], obuf[:, :], posall[0][:, i, 0:1], gather=True, reads=[posall[1]], writes=[r1[i % 2][1]])
                k.idma(r2[i % 2][0][:], obuf[:, :], posall[0][:, i, 1:2], gather=True, reads=[posall[1]], writes=[r2[i % 2][1]])

            gload(0)
            for i, (s, tk, c) in enumerate(subt):
                if i + 1 < NSUB:
                    gload(i + 1)
                a_, aB = r1[i % 2]; b_, bB = r2[i % 2]; o_, oB = yo[i % 2]
                j = pg.jidx(s, c)
                k.ts("dve", a_[:], a_[:], pwall[0][:, i, 0:1], None, ALU.mult, None, [aB, pwall[1]], [aB])
                k.stt(a_[:], b_[:], pwall[0][:, i, 1:2], a_[:], ALU.mult, ALU.add, [bB, pwall[1], aB], [aB])
                for hf_ in range(2):
                    p_, pB = pt[hf_]
                    for q in range(4):
                        oc = hf_ * 4 + q
                        k.op("pe", lambda h: h.transpose(p_[:, q * 128:(q + 1) * 128], a_[:, oc * 128:(oc + 1) * 128], pg.id_f[:]),
                             [aB, pg.idfB], [pB])
                    for q in range(4):
                        oc = hf_ * 4 + q
                        if q % 2 == 0:
                            k.act(o_[:, oc, :], p_[:, q * 128:(q + 1) * 128], AF.Identity, [pB, pg.modB], [oB], scale=pg.mod[:, l, 40 + oc, j:j + 1])
                        else:
                            k.ts("dve", o_[:, oc, :], p_[:, q * 128:(q + 1) * 128], pg.mod[:, l, 40 + oc, j:j + 1], None, ALU.mult, None,
                                 [pB, pg.modB], [oB])
                k.dma(dr["xs"][s].rearrange("(kc p) t -> p kc t", p=128)[:, :, tk:tk + 128], o_[:], reads=[oB], q="pool", accum_op=ALU.add)


def build(stop_after=99, debug=False):
    nc = bass.Bass("TRN2", target_bir_lowering=False)
    dr = {}

    def inp(name, shape, dt=F32):
        dr[name] = nc.dram_tensor(name, list(shape), dt, kind="ExternalInput").ap()

    def scr(name, shape, dt, out=False):
        dr[name] = nc.dram_tensor(name, list(shape), dt, kind="ExternalOutput" if out else "Internal").ap()

    inp("xin", [NS, D, TOK]); inp("cvec", [128, KC, 3]); inp("ada_w", [DEPTH, D, 6 * D]); inp("ada_b", [128, DEPTH, 48])
    inp("norm_g", [128, DEPTH, 2, KC]); inp("final_g", [128, KC])
    inp("ones_bf", [128, 128], BF16); inp("id_bf", [128, 128], BF16); inp("id_f", [128, 128]); inp("ones_f", [128, 128])
    inp("na_w_qkv", [2, D, 3 * D]); inp("na_w_o", [2, D, D]); inp("na_tab", [2, 8, 128, 2, 2, 22, 64])
    inp("ffn_w1", [2, D, DFF]); inp("ffn_w3", [2, D, DFF]); inp("ffn_w2", [2, DFF, D])
    inp("moe_router", [2, 128, KC, NEXP])
    for f_ in range(2):
        inp(f"moe_w1_{f_}", [NEXP, D, DFE]); inp(f"moe_w3_{f_}", [NEXP, D, DFE]); inp(f"moe_w2_{f_}", [NEXP, DFE, D])
    inp("tris", [128, 128]); inp("thr48", [128, 48]); inp("iota48", [128, 48]); inp("pk1", [128, KC]); inp("pk2", [128, 28])
    inp("pool_w", [4, 256, 256]); inp("pool_scale", [128, KC]); inp("pool_ic", [128, 4, 2, 256])
    gdn_inputs(inp)
    scr("xs", [NS, D, TOK], F32, out=debug)
    scr("hs", [NS, D, TOK], BF16)
    scr("qs", [NS, D, TOK], BF16); scr("ks", [NS, D, TOK], BF16); scr("vs", [NS, TOK, D], BF16); scr("os", [NS, D, TOK], BF16)
    scr("wexp", [NS, NEXP, TOK], F32)
    scr("htok", [NS, TOK, D], BF16, out=debug); scr("rinfo", [NS, TOK, 24], F32, out=debug)
    scr("hbuf", [42 * BLK, D], BF16, out=debug); scr("obuf", [42 * BLK, D], F32, out=debug)

    gdn_scratch(scr)
    scr("out", [NS, D, NLAT], F32, out=True)

    with ExitStack() as es:
        k = K(nc, es)
        pg = Prog(nc, k, dr, stop_after)
        with Pool_(k) as PC:
            pg.setup_consts(PC)
            for s in range(NS):
                for kc in range(KC):
                    k.dma(dr["xs"][s, kc * 128:(kc + 1) * 128, :], dr["xin"][s, kc * 128:(kc + 1) * 128, :])
            pg.adaln()
            k.barrier()
            step = 0
            for l in range(DEPTH):
                last = l == DEPTH - 1
                if step >= stop_after:
                    break
                pg.norm_pass(l, 0, include_ctx=True)
                kind = l % 3
                jx = l // 3
                if kind == 0:
                    pg.na_qkv_pass(dr["na_w_qkv"][jx])
                    pg.na_attn_pass(dr["na_tab"][jx], need_ctx=not last)
                    pg.outproj_pass(l, dr["os"], dr["na_w_o"][jx], include_ctx=not last, gate_off=16)
                elif kind == 1:
                    gdn_mixer(pg, l, need_ctx=not last)
                else:
                    pg.pool_pass(l, dr["pool_w"], include_ctx=not last)
                step += 1
                if step >= stop_after:
                    break
                f = l // 2
                if l % 2 == 0:
                    pg.norm_pass(l, 1, include_ctx=not last)
                    H = DFF // 2
                    for hh in range(2):
                        pg.ffn_pass(l, dr["ffn_w1"][f][:, hh * H:(hh + 1) * H], dr["ffn_w3"][f][:, hh * H:(hh + 1) * H],
                                    dr["ffn_w2"][f][hh * H:(hh + 1) * H, :], H, include_ctx=not last)
                else:
                    moe_layer(pg, l, f, include_ctx=not last)
                step += 1
            pg.norm_pass(0, 0, include_ctx=False, final=True)
            k.barrier()
    return nc


GDN_IN = 4128
NCHK = TOK // 128


def gdn_inputs(inp):
    inp("gdn_w_in", [D, GDN_IN]); inp("gdn_w_o", [D, D]); inp("gdn_cw", [128, 24, 4])
    inp("gdn_dtb", [128, 16]); inp("gdn_alog", [128, 16]); inp("gdn_ng", [128, 1])
    inp("gdn_tri", [128, 2, 128]); inp("gdn_gm", [128, 2, 3, 128]); inp("gdn_ms", [128, 7, 256], BF16)


def gdn_scratch(scr):
    scr("pj", [NS, 4 * D, TOK], BF16)
    scr("gq", [NS, D, TOK], BF16); scr("gk", [NS, D, TOK], BF16); scr("gv", [NS, D, TOK], BF16)
    scr("gt", [NS, TOK, 48], F32)
    scr("gcs", [NS, 16, TOK], F32)
    scr("gbs", [NS, 16, TOK], F32)


def gdn_proj_pass(pg):
    k, dr = pg.k, pg.dr
    hs, pj = dr["hs"], dr["pj"]
    tl = tiles_for(True)
    with Pool_(k) as P:
        Wi, WiB = P.sb([128, KC, GDN_IN], BF16, "Wi")
        src = dr["gdn_w_in"].rearrange("(kc p) n -> p kc n", p=128)
        for i in range(4):
            k.dma(Wi[:, :, i * 1032:(i + 1) * 1032], src[:, :, i * 1032:(i + 1) * 1032], writes=[WiB], q="pool")
        dtb, dtbB = P.sb([128, 16], F32, "dtb"); k.dma(dtb[:], dr["gdn_dtb"][:, :], writes=[dtbB])
        nA, nAB = P.sb([128, 16], F32, "nA"); k.dma(nA[:], dr["gdn_alog"][:, :], writes=[nAB])
        k.act(nA[:], nA[:], AF.Exp, [nAB], [nAB])
        k.ts("dve", nA[:], nA[:], -1.0, None, ALU.mult, None, [nAB], [nAB])
        tri, triB = P.sb([128, 2, 128], F32, "tri"); k.dma(tri[:], dr["gdn_tri"][:, :, :], writes=[triB])
        ht = [P.sb([128, KC, TT], BF16, f"ht{i}") for i in range(2)]
        pt = [P.sb([128, 32, TT], BF16, f"pjt{i}") for i in range(1)]
        gtt, gttB = P.sb([128, 4, 48], F32, "gtt")
        tmpa, tmpaB = P.sb([128, 16], F32, "tmpa")
        gcT, gcTB = P.sb([16, TT], F32, "gcT")
        bT, bTB = P.sb([16, TT], F32, "bT")
        pp = [P.ps(f"pp{i}") for i in range(4)]
        pab, pabB = P.ps("pab")
        pgc, pgcB = P.ps("pgc")
        ptr, ptrB = P.ps("ptr")
        pbf, pbfB = P.ps("pbf")

        def load(i):
            s, t0, W, c = tl[i]
            t, B = ht[i % 2]
            k.dma(t[:, :, :W], hs[s].rearrange("(kc p) t -> p kc t", p=128)[:, :, t0:t0 + W], writes=[B])

        load(0)
        cnt = 0
        for i, (s, t0, W, c) in enumerate(tl):
            if i + 1 < len(tl):
                load(i + 1)
            h, hB = ht[i % 2]
            p_, pjB = pt[0]
            for oc in range(32):
                q_, qB = pp[cnt % 4]
                for kc in range(KC):
                    k.mm(q_[:, :W], Wi[:, kc, oc * 128:(oc + 1) * 128], h[:, kc, :W], kc == 0, kc == KC - 1, [WiB, hB], [qB])
                if cnt % 2 == 0:
                    k.act(p_[:, oc, :W], q_[:, :W], AF.Copy, [qB], [pjB])
                else:
                    k.copy("dve", p_[:, oc, :W], q_[:, :W], [qB], [pjB])
                cnt += 1
            k.dma(pj[s].rearrange("(oc p) t -> p oc t", p=128)[:, :, t0:t0 + W], p_[:, :, :W], reads=[pjB])
            for kc in range(KC):
                k.mm(pbf[0:16, :W], Wi[:, kc, 4096 + 16:4096 + 32], h[:, kc, :W], kc == 0, kc == KC - 1, [WiB, hB], [pbfB])
            k.act(bT[:, :W], pbf[0:16, :W], AF.Sigmoid, [pbfB], [bTB])
            k.dma(dr["gbs"][s, :, t0:t0 + W], bT[:, :W], reads=[bTB])
            nst = W // 128
            for ts_ in range(nst):
                for kc in range(KC):
                    k.mm(pab[:, ts_ * 32:(ts_ + 1) * 32], h[:, kc, ts_ * 128:(ts_ + 1) * 128], Wi[:, kc, 4096:4128],
                         kc == 0, kc == KC - 1, [WiB, hB], [pabB])
            for ts_ in range(nst):
                k.tt("dve", tmpa[:], pab[:, ts_ * 32:ts_ * 32 + 16], dtb[:], ALU.add, [pabB, dtbB], [tmpaB])
                k.act(tmpa[:], tmpa[:], AF.Exp, [tmpaB], [tmpaB])
                k.act(tmpa[:], tmpa[:], AF.Ln, [tmpaB], [tmpaB], bias=1.0)
                k.tt("dve", gtt[:, ts_, 0:16], tmpa[:], nA[:], ALU.mult, [tmpaB, nAB], [gttB])
                k.act(gtt[:, ts_, 16:32], pab[:, ts_ * 32 + 16:ts_ * 32 + 32], AF.Sigmoid, [pabB], [gttB])
                for d in range(2):
                    k.mm(pgc[:, ts_ * 16 + d * 8:ts_ * 16 + d * 8 + 8], tri[:, d, :], gtt[:, ts_, d * 8:d * 8 + 8], True, True,
                         [triB, gttB], [pgcB])
                k.copy("dve", gtt[:, ts_, 32:48], pgc[:, ts_ * 16:ts_ * 16 + 16], [pgcB], [gttB])
                k.op("pe", lambda hh: hh.transpose(ptr[0:16, ts_ * 128:(ts_ + 1) * 128], gtt[:, ts_, 32:48], pg.id_f[:]),
                     [gttB, pg.idfB], [ptrB])
            k.copy("dve", gcT[:, :W], ptr[0:16, :W], [ptrB], [gcTB])
            k.dma(dr["gcs"][s, :, t0:t0 + W], gcT[:, :W], reads=[gcTB])
            k.dma(dr["gt"][s, t0:t0 + W, :].rearrange("(a p) c -> p a c", p=128), gtt[:, :nst, :], reads=[gttB])


def gdn_conv_pass(pg):
    k, dr = pg.k, pg.dr
    pj = dr["pj"]
    tl = tiles_for(True)
    with Pool_(k) as P:
        cw, cwB = P.sb([128, 24, 4], F32, "cw"); k.dma(cw[:], dr["gdn_cw"][:, :, :], writes=[cwB])
        e1, e1B = P.sb([128, 1], F32, "e1"); k.memset("pool", e1[:], EPS, [e1B])
        e2, e2B = P.sb([128, 1], F32, "e2"); k.memset("pool", e2[:], EPS * 128.0, [e2B])
        pin = [P.sb([128, 24, TT + 4], BF16, f"pin{i}") for i in range(2)]
        ot, otB = P.sb([128, 24, TT], BF16, "cot")
        acc = [P.sb([128, TT], F32, f"acc{i}") for i in range(2)]
        sl = [P.sb([128, TT], F32, f"sl{i}") for i in range(2)]
        sq = [P.sb([128, TT], BF16, f"sq{i}") for i in range(2)]
        rn = [P.sb([128, TT], F32, f"rn{i}") for i in range(2)]
        pss = [P.ps(f"pss{i}") for i in range(2)]

        def load(i):
            s, t0, W, c = tl[i]
            t, B = pin[i % 2]
            seq0, seq1 = (0, NCTX) if c else (NCTX, TOK)
            k.memset("pool", t[:], 0.0, [B])
            lo, hi = max(seq0, t0 - 2), min(seq1, t0 + W + 1)
            k.dma(t[:, :, lo - (t0 - 2):hi - (t0 - 2)], pj[s].rearrange("(oc p) t -> p oc t", p=128)[:, 0:24, lo:hi], writes=[B])

        load(0)
        cnt = 0
        for i, (s, t0, W, c) in enumerate(tl):
            if i + 1 < len(tl):
                load(i + 1)
            x, xB = pin[i % 2]
            for oc in range(24):
                a_, aB = acc[cnt % 2]; s_, sB = sl[cnt % 2]; q_, qB = sq[cnt % 2]; r_, rB = rn[cnt % 2]; p_, pB = pss[cnt % 2]
                cnt += 1
                k.ts("dve", a_[:, :W], x[:, oc, 0:W], cw[:, oc, 0:1], None, ALU.mult, None, [xB, cwB], [aB])
                for j in range(1, 4):
                    k.stt(a_[:, :W], x[:, oc, j:j + W], cw[:, oc, j:j + 1], a_[:, :W], ALU.mult, ALU.add, [xB, cwB, aB], [aB])
                if oc >= 16:
                    k.act(ot[:, oc, :W], a_[:, :W], AF.Silu, [aB], [otB])
                    continue
                k.act(s_[:, :W], a_[:, :W], AF.Silu, [aB], [sB])
                k.act(q_[:, :W], s_[:, :W], AF.Square, [sB], [qB])
                k.mm(p_[:, :W], pg.ones_bf[:], q_[:, :W], True, True, [pg.onesB, qB], [pB])
                if oc < 8:
                    k.act(r_[:, :W], p_[:, :W], AF.Sqrt, [pB, e2B], [rB], scale=128.0, bias=e2[:, 0:1])
                else:
                    k.act(r_[:, :W], p_[:, :W], AF.Sqrt, [pB, e1B], [rB], bias=e1[:, 0:1])
                k.op("dve", lambda hh: hh.reciprocal(out=r_[:, :W], in_=r_[:, :W]), [rB], [rB])
                k.tt("pool", ot[:, oc, :W], s_[:, :W], r_[:, :W], ALU.mult, [sB, rB], [otB])
            for gi, nm in enumerate(("gq", "gk", "gv")):
                k.dma(dr[nm][s].rearrange("(oc p) t -> p oc t", p=128)[:, :, t0:t0 + W], ot[:, gi * 8:(gi + 1) * 8, :W], reads=[otB])


def _rr(gens):
    gens = list(gens)
    while gens:
        nxt = []
        for g in gens:
            try:
                next(g)
                nxt.append(g)
            except StopIteration:
                pass
        gens = nxt


def gdn_core_pass(pg):
    k, dr = pg.k, pg.dr
    with Pool_(k) as P:
        gm, gmB = P.sb([128, 2, 3, 128], F32, "gm"); k.dma(gm[:], dr["gdn_gm"][:, :, :, :], writes=[gmB])
        ms, msB = P.sb([128, 7, 256], BF16, "ms"); k.dma(ms[:], dr["gdn_ms"][:, :, :], writes=[msB])
        gng, gngB = P.sb([128, 1], F32, "gng"); k.dma(gng[:], dr["gdn_ng"][:, :], writes=[gngB])
        e1, e1B = P.sb([128, 1], F32, "e1"); k.memset("pool", e1[:], EPS, [e1B])
        qT, qTB = P.sb([128, TOK], BF16, "qT"); kT, kTB = P.sb([128, TOK], BF16, "kT")
        ktok, ktokB = P.sb([128, NCHK, 128], BF16, "ktok"); vtok, vtokB = P.sb([128, NCHK, 128], BF16, "vtok")
        gt, gtB = P.sb([128, NCHK, 48], F32, "gt")
        gcb, gcbB = P.sb([128, TOK], F32, "gcb"); bb, bbB = P.sb([128, TOK], F32, "bb")
        U, UB_ = P.sb([128, NCHK, 128], F32, "U"); UB = [Buf(f"U{n}") for n in range(NCHK)]
        WT, _ = P.sb([128, NCHK, 128], BF16, "WT"); WTB = [Buf(f"WT{n}") for n in range(NCHK)]
        AQ, _ = P.sb([128, NCHK, 128], BF16, "AQ"); AQB = [Buf(f"AQ{n}") for n in range(NCHK)]
        KTL, _ = P.sb([128, NCHK, 128], BF16, "KTL"); KTLB = [Buf(f"KTL{n}") for n in range(NCHK)]
        qd, qdB = P.sb([128, TOK], BF16, "qd")
        vT, vTB = qd, qdB
        zT, zTB = kT, kTB
        oacc, oaccB = P.sb([128, TOK], F32, "oacc"); oB = [Buf(f"o{n}") for n in range(NCHK)]
        egc, egcB = P.sb([128, NCHK], F32, "egc"); ett, ettB = P.sb([128, NCHK], F32, "ett"); egl, eglB = P.sb([128, NCHK], F32, "egl")
        S, SB = P.sb([128, 128], F32, "S"); Sb, SbB = P.sb([128, 128], BF16, "Sb")
        vn = [P.sb([128, 128], BF16, f"vn{i}") for i in range(2)]
        tmpx, tmpxB = P.sb([128, 512], F32, "tmpx")
        NI = 4
        wk = []
        for ii in range(NI):
            w = {}
            for nm in ("xa", "xb", "xc", "t1"):
                w[nm] = P.sb([128, 256], F32, f"{nm}{ii}")
            w["ea"], w["eb"], w["ec"] = w["xa"], w["xb"], w["xc"]
            for nm in ("L", "M", "X0", "X1", "Y0", "Y1", "Wm", "Wn", "vb", "kbg"):
                w[nm] = P.sb([128, 256], BF16, f"{nm}{ii}")
            w["pA"] = P.ps(f"pA{ii}"); w["pB"] = P.ps(f"pB{ii}"); w["pC"] = w["pB"]
            wk.append(w)
        pX, pXB = wk[0]["pA"]; pY, pYB = wk[1]["pA"]

        for s in range(NS):
            k.dma(gt[:], dr["gt"][s].rearrange("(n p) c -> p n c", p=128), writes=[gtB])
            for h in range(8):
                rows = slice(h * 128, (h + 1) * 128)
                k.dma(qT[:], dr["gq"][s, rows, :], writes=[qTB]); k.dma(kT[:], dr["gk"][s, rows, :], writes=[kTB])
                k.dma(vT[:], dr["gv"][s, rows, :], writes=[vTB])
                ktBs = [Buf() for _ in range(NCHK)]
                vtBs = [Buf() for _ in range(NCHK)]
                for n in range(NCHK):
                    cs = slice(n * 128, (n + 1) * 128)
                    ka, kaB = wk[(2 * n) % NI]["pA"]
                    va, vaB = wk[(2 * n + 1) % NI]["pA"]
                    k.op("pe", lambda hh: hh.transpose(ka[:, 0:128].bitcast(BF16)[:, 0:128], kT[:, cs], pg.id_bf[:]), [kTB, pg.idbB], [kaB])
                    k.copy("act", ktok[:, n, :], ka[:, 0:128].bitcast(BF16)[:, 0:128], [kaB], [ktBs[n]] + ([ktokB] if n == 0 else []))
                    k.op("pe", lambda hh: hh.transpose(va[:, 0:128].bitcast(BF16)[:, 0:128], vT[:, cs], pg.id_bf[:]), [vTB, pg.idbB], [vaB])
                    k.copy("dve", vtok[:, n, :], va[:, 0:128].bitcast(BF16)[:, 0:128], [vaB], [vtBs[n]] + ([vtokB] if n == 0 else []))
                k.op("act", lambda hh: hh.activation(out=egc[:, 0:1], in_=egc[:, 0:1], func=AF.Copy), ktBs, [ktokB, egcB])
                k.op("dve", lambda hh: hh.tensor_copy(out=ett[:, 0:1], in_=ett[:, 0:1]), vtBs, [vtokB, ettB])
                for d in range(2):
                    col = d * 8 + h
                    k.dma(gcb[:], dr["gcs"][s, col:col + 1, :].partition_broadcast(128), writes=[gcbB])
                    k.dma(bb[:], dr["gbs"][s, col:col + 1, :].partition_broadcast(128), writes=[bbB])
                    gcv = gt[:, :, 32 + col]
                    btv = gt[:, :, 16 + col]
                    lastoff = 127 if d == 0 else 0
                    glv = gcb[:, lastoff:TOK:128]
                    k.act(egc[:], gcv, AF.Exp, [gtB], [egcB])
                    k.tt("dve", ett[:], glv, gcv, ALU.subtract, [gcbB, gtB], [ettB])
                    k.act(ett[:], ett[:], AF.Exp, [ettB], [ettB])
                    k.act(egl[:], glv, AF.Exp, [gcbB], [eglB])
                    for c0 in range(0, TOK, 512):
                        wd = min(512, TOK - c0)
                        k.act(tmpx[:, :wd], gcb[:, c0:c0 + wd], AF.Exp, [gcbB], [tmpxB])
                        k.tt("dve", qd[:, c0:c0 + wd], qT[:, c0:c0 + wd], tmpx[:, :wd], ALU.mult, [qTB, tmpxB], [qdB])

                    def inst(n0_, w):
                        (xa, xaB), (xb, xbB), (xc, xcB) = w["xa"], w["xb"], w["xc"]
                        (t1, t1B), (L, LB), (M, MB) = w["t1"], w["L"], w["M"]
                        (pA, pAB), (pB_, pBB) = w["pA"], w["pB"]
                        vb, vbB = w["vb"]; kbg, kbgB = w["kbg"]
                        for u in range(2):
                            n = n0_ + u
                            cs = slice(n * 128, (n + 1) * 128); us = slice(u * 128, (u + 1) * 128)
                            gcn = gt[:, n, 32 + col:33 + col]
                            k.stt(xa[:, us], gcb[:, cs], gcn, gm[:, d, 0, :], ALU.subtract, ALU.add, [gcbB, gtB, gmB], [xaB])
                            k.stt(xb[:, us], gcb[:, cs], gcn, gm[:, d, 1, :], ALU.subtract, ALU.add, [gcbB, gtB, gmB], [xbB])
                            k.stt(xc[:, us], gcb[:, cs], gcn, gm[:, d, 2, :], ALU.subtract, ALU.add, [gcbB, gtB, gmB], [xcB])
                            k.mm(pA[:, us], kT[:, cs], kT[:, cs], True, True, [kTB], [pAB])
                            k.mm(pB_[:, us], kT[:, cs], qT[:, cs], True, True, [kTB, qTB], [pBB])
                        yield
                        k.act(xa[:], xa[:], AF.Exp, [xaB], [xaB])
                        k.act(xb[:], xb[:], AF.Exp, [xbB], [xbB])
                        k.act(xc[:], xc[:], AF.Exp, [xcB], [xcB], scale=-1.0)
                        yield
                        for u in range(2):
                            n = n0_ + u
                            cs = slice(n * 128, (n + 1) * 128); us = slice(u * 128, (u + 1) * 128)
                            btn = gt[:, n, 16 + col:17 + col]
                            k.stt(L[:, us], pA[:, us], btn, xc[:, us], ALU.mult, ALU.mult, [pAB, gtB, xcB], [LB])
                            k.ts("pool", vb[:, us], vtok[:, n, :], btn, None, ALU.mult, None, [vtokB, gtB], [vbB])
                            k.ts("pool", kbg[:, us], ktok[:, n, :], btn, egc[:, n:n + 1], ALU.mult, ALU.mult, [ktokB, gtB, egcB], [kbgB])
                            k.ts("pool", KTL[:, n, :], ktok[:, n, :], ett[:, n:n + 1], None, ALU.mult, None, [ktokB, ettB], [KTLB[n]])
                        k.tt("dve", t1[:], pA[:, 0:256], xb[:], ALU.mult, [pAB, xbB], [t1B])
                        k.tt("pool", M[:], t1[:], bb[:, n0_ * 128:(n0_ + 2) * 128], ALU.mult, [t1B, bbB], [MB])
                        k.tt("dve", AQ[:, n0_:n0_ + 2, :].rearrange("p a b -> p (a b)"), pB_[:, 0:256], xa[:], ALU.mult, [pBB, xaB],
                             [AQB[n0_], AQB[n0_ + 1]])
                        yield
                        X, XB = w["X0"]; Y, YB = w["Y0"]; Xn, XnB = w["X1"]; Yn, YnB = w["Y1"]
                        Wm, WmB = w["Wm"]; Wn, WnB = w["Wn"]
                        k.tt("pool", Wm[:], L[:], ms[:, 0, :], ALU.mult, [LB, msB], [WmB])
                        for u in range(2):
                            us = slice(u * 128, (u + 1) * 128)
                            k.tt("pool", X[:, us], pg.id_bf[:], Wm[:, us], ALU.subtract, [pg.idbB, WmB], [XB])
                        k.tt("pool", Wn[:], M[:], ms[:, 0, :], ALU.mult, [MB, msB], [WnB])
                        for u in range(2):
                            us = slice(u * 128, (u + 1) * 128)
                            k.tt("pool", Y[:, us], pg.id_bf[:], Wn[:, us], ALU.subtract, [pg.idbB, WnB], [YB])
                        yield
                        for lv in range(1, 7):
                            lastlv = lv == 6
                            for u in range(2):
                                us = slice(u * 128, (u + 1) * 128)
                                if not lastlv:
                                    k.mm(pA[:, us], M[:, us], X[:, us], True, True, [MB, XB], [pAB])
                                k.mm(pB_[:, us], L[:, us], Y[:, us], True, True, [LB, YB], [pBB])
                            yield
                            if not lastlv:
                                k.tt("dve", Wm[:], pA[:, 0:256], ms[:, lv, :], ALU.mult, [pAB, msB], [WmB])
                            k.tt("dve", Wn[:], pB_[:, 0:256], ms[:, lv, :], ALU.mult, [pBB, msB], [WnB])
                            yield
                            for u in range(2):
                                us = slice(u * 128, (u + 1) * 128)
                                if not lastlv:
                                    k.mm(pA[:, us], Y[:, us], Wm[:, us], True, True, [YB, WmB], [pAB])
                                k.mm(pB_[:, us], X[:, us], Wn[:, us], True, True, [XB, WnB], [pBB])
                            yield
                            if not lastlv:
                                k.tt("dve", Xn[:], X[:], pA[:, 0:256], ALU.subtract, [XB, pAB], [XnB])
                            k.tt("dve", Yn[:], Y[:], pB_[:, 0:256], ALU.subtract, [YB, pBB], [YnB])
                            X, XB, Xn, XnB = Xn, XnB, X, XB
                            Y, YB, Yn, YnB = Yn, YnB, Y, YB
                            yield
                        for u in range(2):
                            us = slice(u * 128, (u + 1) * 128)
                            k.mm(pA[:, us], Y[:, us], vb[:, us], True, True, [YB, vbB], [pAB])
                            k.mm(pB_[:, us], kbg[:, us], Y[:, us], True, True, [kbgB, YB], [pBB])
                        yield
                        k.copy("act", U[:, n0_:n0_ + 2, :].rearrange("p a b -> p (a b)"), pA[:, 0:256], [pAB], [UB[n0_], UB[n0_ + 1]])
                        k.copy("act", WT[:, n0_:n0_ + 2, :].rearrange("p a b -> p (a b)"), pB_[:, 0:256], [pBB], [WTB[n0_], WTB[n0_ + 1]])
                        yield

                    import contextlib
                    sc_ = (lambda nm: pg.nc.named_scope(nm)) if (s == 0 and h == 1) else (lambda nm: contextlib.nullcontext())
                    with sc_(f"gi_inst{d}"):
                        for n0 in range(0, NCHK, 2 * NI):
                            _rr([inst(n0 + 2 * ii, wk[ii]) for ii in range(NI) if n0 + 2 * ii < NCHK])
                    k.memset("pool", S[:], 0.0, [SB]); k.memset("pool", Sb[:], 0.0, [SbB])
                    order = [0, 1] + list(range(2, NCHK)) if d == 0 else [1, 0] + list(range(NCHK - 1, 1, -1))
                    for it, n in enumerate(order):
                        cs = slice(n * 128, (n + 1) * 128)
                        v_, vB_ = vn[it % 2]
                        k.mm(pX[:, 0:128], WT[:, n, :], Sb[:], True, True, [WTB[n], SbB], [pXB])
                        k.tt("dve", v_[:], U[:, n, :], pX[:, 0:128], ALU.subtract, [UB[n], pXB], [vB_])
                        k.mm(pY[:, 0:128], Sb[:], qd[:, cs], True, False, [SbB, qdB], [pYB])
                        k.mm(pY[:, 0:128], v_[:], AQ[:, n, :], False, True, [vB_, AQB[n]], [pYB])
                        if d == 0:
                            k.copy("act", oacc[:, cs], pY[:, 0:128], [pYB], [oB[n]])
                        else:
                            k.tt("dve", oacc[:, cs], oacc[:, cs], pY[:, 0:128], ALU.add, [oB[n], pYB], [oB[n]])
                        k.mm(pX[:, 0:128], KTL[:, n, :], v_[:], True, True, [KTLB[n], vB_], [pXB])
                        k.stt(Sb[:], S[:], egl[:, n:n + 1], pX[:, 0:128], ALU.mult, ALU.add, [SB, eglB, pXB], [SbB])
                        k.stt(S[:], S[:], egl[:, n:n + 1], pX[:, 0:128], ALU.mult, ALU.add, [SB, eglB, pXB], [SB])
                k.dma(zT[:], dr["pj"][s, 3 * D + h * 128:3 * D + (h + 1) * 128, :], writes=[zTB])
                for c0 in range(0, TOK, 512):
                    wd = min(512, TOK - c0)
                    obs = oB[c0 // 128:(c0 + wd) // 128]
                    xa, xaB = wk[0]["xa"]; xb, xbB = wk[0]["xb"]
                    sqt, sqB = P_sq = (qd, qdB)
                    k.act(sqt[:, c0:c0 + wd], oacc[:, c0:c0 + wd], AF.Square, obs, [qdB])
                    k.mm(pX[:, :wd], pg.ones_bf[:], sqt[:, c0:c0 + wd], True, True, [pg.onesB, qdB], [pXB])
                    k.act(tmpx[:, :wd], pX[:, :wd], AF.Sqrt, [pXB, e1B], [tmpxB], scale=1.0 / 128.0, bias=e1[:, 0:1])
                    k.op("dve", lambda hh: hh.reciprocal(out=tmpx[:, :wd], in_=tmpx[:, :wd]), [tmpxB], [tmpxB])
                    k.tt("dve", oacc[:, c0:c0 + wd], oacc[:, c0:c0 + wd], tmpx[:, :wd], ALU.mult, obs + [tmpxB], obs)
                    k.act(tmpx[:, :wd], zT[:, c0:c0 + wd], AF.Silu, [zTB], [tmpxB])
                    k.stt(qd[:, c0:c0 + wd], oacc[:, c0:c0 + wd], gng[:, 0:1], tmpx[:, :wd], ALU.mult, ALU.mult, obs + [gngB, tmpxB], [qdB])
                k.dma(dr["os"][s, rows, :], qd[:], reads=[qdB])


def gdn_mixer(pg, l, need_ctx):
    with pg.nc.named_scope("g_proj"):
        gdn_proj_pass(pg)
    with pg.nc.named_scope("g_conv"):
        gdn_conv_pass(pg)
    with pg.nc.named_scope("g_core"):
        gdn_core_pass(pg)
    with pg.nc.named_scope("g_out"):
        pg.outproj_pass(l, pg.dr["os"], pg.dr["gdn_w_o"], include_ctx=need_ctx, gate_off=16)


def _fm(v):
    v = np.asarray(v, np.float32)
    lead = v.shape[:-1]
    return np.ascontiguousarray(np.moveaxis(v.reshape(lead + (KC, 128)), -1, 0))


def _na_tables(rpb):
    col = np.arange(64)
    c0 = np.clip(col - 8, 0, 48)
    in_win = (col[None, :] >= c0[:, None]) & (col[None, :] < c0[:, None] + 16)
    dc = np.clip(col[None, :] - col[:, None], -15, 15) + 15
    out = np.full((8, 128, 2, 2, 22, 64), NEG, np.float32)
    for jj in range(22):
        j = 17 - jj
        for a in range(2):
            drr = j + a
            if drr < 0 or drr > 14:
                continue
            base = np.where(in_win[None], rpb[:, drr][:, dc], NEG).astype(np.float32)
            base = np.transpose(base, (0, 2, 1))
            for h in range(16):
                out[h // 2, a * 64:(a + 1) * 64, 1, h % 2, jj, :] = base[h]
                if 3 <= drr <= 10:
                    out[h // 2, a * 64:(a + 1) * 64, 0, h % 2, jj, :] = base[h]
    return out


def _pool_ic():
    out = np.ones((4, 2, 256), np.float32)
    for gi, win in enumerate((2, 4, 8, 16)):
        for ri, T in enumerate((64, 256)):
            t = np.arange(T)
            lo = np.clip(t - win // 2, 0, T)
            hi = np.clip(t + win // 2, 0, T)
            out[gi, ri, :T] = 1.0 / (hi - lo).astype(np.float32)
    return np.ascontiguousarray(np.broadcast_to(out[None], (128, 4, 2, 256)))


def make_in_maps(inputs, n_cores=8):
    import ml_dtypes
    x, c, ctx, c_ctx = inputs["x"], inputs["c"], inputs["ctx"], inputs["c_ctx"]
    shared = {
        "ada_w": np.ascontiguousarray(inputs["ada_w"], np.float32),
        "ada_b": np.ascontiguousarray(np.transpose(np.asarray(inputs["ada_b"], np.float32).reshape(DEPTH, 48, 128), (2, 0, 1))),
        "norm_g": _fm(inputs["norm_g"]),
        "final_g": _fm(inputs["final_g"]),
        "ones_bf": np.ones((128, 128), ml_dtypes.bfloat16),
        "id_bf": np.eye(128, dtype=np.float32).astype(ml_dtypes.bfloat16),
        "id_f": np.eye(128, dtype=np.float32),
        "ones_f": np.ones((128, 128), np.float32),
        "na_w_qkv": np.ascontiguousarray(inputs["na_w_qkv"], np.float32),
        "na_w_o": np.ascontiguousarray(inputs["na_w_o"], np.float32),
        "na_tab": np.stack([_na_tables(np.asarray(inputs["na_rpb"][j], np.float32)) for j in range(2)]),
        "ffn_w1": np.ascontiguousarray(inputs["ffn_w1"], np.float32),
        "ffn_w3": np.ascontiguousarray(inputs["ffn_w3"], np.float32),
        "ffn_w2": np.ascontiguousarray(inputs["ffn_w2"], np.float32),
        "moe_router": np.ascontiguousarray(np.transpose(np.asarray(inputs["moe_router"], np.float32).reshape(2, KC, 128, NEXP), (0, 2, 1, 3))),
        "moe_w1_0": np.ascontiguousarray(inputs["moe_w1"][0], np.float32), "moe_w1_1": np.ascontiguousarray(inputs["moe_w1"][1], np.float32),
        "moe_w3_0": np.ascontiguousarray(inputs["moe_w3"][0], np.float32), "moe_w3_1": np.ascontiguousarray(inputs["moe_w3"][1], np.float32),
        "moe_w2_0": np.ascontiguousarray(inputs["moe_w2"][0], np.float32), "moe_w2_1": np.ascontiguousarray(inputs["moe_w2"][1], np.float32),
        "tris": np.ascontiguousarray((np.arange(128)[:, None] < np.arange(128)[None, :]).astype(np.float32)),
        "thr48": np.ascontiguousarray(np.broadcast_to((np.arange(48) * 512.0).astype(np.float32)[None], (128, 48))),
        "iota48": np.ascontiguousarray(np.broadcast_to(np.arange(48).astype(np.float32)[None], (128, 48))),
        "pk1": np.ascontiguousarray(((np.arange(KC)[None, :] * 128 + np.arange(128)[:, None]) * 4).astype(np.float32)),
        "pk2": np.ascontiguousarray((np.arange(28)[None, :] * 128 + np.arange(128)[:, None]).astype(np.float32)),
        "pool_w": np.ascontiguousarray(inputs["pool_w"][0], np.float32),
        "pool_scale": _fm(inputs["pool_scale"][0]),
        "pool_ic": _pool_ic(),
    }
    shared.update(gdn_host(inputs))
    maps = []
    for ci in range(n_cores):
        b0 = ci * NS
        xin = np.empty((NS, D, TOK), np.float32)
        for s in range(NS):
            xin[s, :, :NCTX] = np.asarray(ctx[b0 + s], np.float32).T
            xin[s, :, NCTX:] = np.asarray(x[b0 + s], np.float32).T
        cv = np.stack([c[b0], c[b0 + 1], c_ctx], axis=0).astype(np.float32)
        m = dict(shared)
        m["xin"] = xin
        m["cvec"] = np.ascontiguousarray(np.transpose(cv.reshape(3, KC, 128), (2, 1, 0)))
        maps.append(m)
    return maps


def gdn_host(inputs):
    import ml_dtypes
    cw = np.asarray(inputs["gdn_conv"][0], np.float32)
    cwl = np.ascontiguousarray(np.transpose(cw.reshape(4, 24, 128), (2, 1, 0)))
    dtb = np.asarray(inputs["gdn_dt_bias"][0], np.float32).reshape(16)
    alog = np.asarray(inputs["gdn_a_log"][0], np.float32).reshape(16)
    p = np.arange(128)
    tri = np.zeros((128, 2, 128), np.float32)
    tri[:, 0, :] = (p[:, None] <= p[None, :])
    tri[:, 1, :] = (p[:, None] >= p[None, :])
    gm = np.zeros((128, 2, 3, 128), np.float32)
    P_, F_ = p[:, None], p[None, :]
    gm[:, 0, 0, :] = np.where(F_ >= P_, 0.0, NEG); gm[:, 0, 1, :] = np.where(F_ > P_, 0.0, NEG); gm[:, 0, 2, :] = np.where(F_ < P_, 0.0, -NEG)
    gm[:, 1, 0, :] = np.where(F_ <= P_, 0.0, NEG); gm[:, 1, 1, :] = np.where(F_ < P_, 0.0, NEG); gm[:, 1, 2, :] = np.where(F_ > P_, 0.0, -NEG)
    ms = np.zeros((128, 7, 128), np.float32)
    for kk in range(7):
        ms[:, kk, :] = ((P_ >> kk) != (F_ >> kk)) & ((P_ >> (kk + 1)) == (F_ >> (kk + 1)))
    return {
        "gdn_w_in": np.ascontiguousarray(inputs["gdn_w_in"][0], np.float32),
        "gdn_w_o": np.ascontiguousarray(inputs["gdn_w_o"][0], np.float32),
        "gdn_cw": cwl,
        "gdn_dtb": np.ascontiguousarray(np.broadcast_to(dtb[None], (128, 16))),
        "gdn_alog": np.ascontiguousarray(np.broadcast_to(alog[None], (128, 16))),
        "gdn_ng": np.ascontiguousarray(np.asarray(inputs["gdn_norm_g"][0], np.float32).reshape(128, 1)),
        "gdn_tri": tri, "gdn_gm": gm, "gdn_ms": np.concatenate([ms, ms], axis=2).astype(ml_dtypes.bfloat16),
    }


def kernel(**inputs):
    nc = build()
    maps = make_in_maps(inputs)
    res = run_bass_kernel_spmd(nc, maps, core_ids=list(range(8)))
    B = inputs["x"].shape[0]
    out = np.empty((B, NLAT, D), np.float32)
    for ci in range(8):
        o = res.results[ci]["out"]
        for s in range(NS):
            out[ci * NS + s] = o[s].T
    return out
```
